# Optimizing a Trainium2 kernel written in Bass

```python
import jax
import jax.numpy as jnp
from jax import lax
import numpy as np

D_MODEL = 1024
BATCH = 2
SEQ = 16384
DEPTH = 2

EPS = 1e-6
CONV_WIDTH = 4
LRU_WIDTH = D_MODEL // 2
LRU_BLOCKS = 8
LRU_BLOCK = LRU_WIDTH // LRU_BLOCKS
LRU_C = 8.0
ATT_HEAD_DIM = 64
ATT_Q_HEADS = (D_MODEL // 2) // ATT_HEAD_DIM
ATT_KV_HEADS = 2
ATT_GROUPS = ATT_Q_HEADS // ATT_KV_HEADS
ATT_Q_WIDTH = ATT_Q_HEADS * ATT_HEAD_DIM
ATT_KV_WIDTH = ATT_KV_HEADS * ATT_HEAD_DIM
WINDOW = 128
ROPE_THETA = 10000.0
EVEN_IN_COLS = 2 * LRU_WIDTH + ATT_Q_WIDTH + 2 * ATT_KV_WIDTH
EVEN_MIX_WIDTH = LRU_WIDTH + ATT_Q_WIDTH
D_FF_DENSE = 2816
GDN_HEADS = 8
GDN_HEAD_DIM = 128
GDN_WIDTH = GDN_HEADS * GDN_HEAD_DIM
GDN_CHUNK = 64
ODD_IN_COLS = 4 * GDN_WIDTH + 2 * GDN_HEADS
N_EXPERTS = 8
TOP_K = 2
D_FF_EXPERT = 3584
N_EVEN = (DEPTH + 1) // 2
N_ODD = DEPTH // 2

kernel_name = "hybrid_rglru_swa_gdn_moe"


def rms_norm(x, g):
    xf = x.astype(jnp.float32)
    y = xf * lax.rsqrt(jnp.mean(xf * xf, axis=-1, keepdims=True) + EPS)
    return (y * g.astype(jnp.float32)).astype(x.dtype)


def l2_norm(x):
    return x * lax.rsqrt(jnp.sum(x * x, axis=-1, keepdims=True) + EPS)


def causal_depthwise_conv(x, w):
    seq = x.shape[1]
    xp = jnp.pad(x, ((0, 0), (CONV_WIDTH - 1, 0), (0, 0)))
    return sum(w[j] * xp[:, j:j + seq] for j in range(CONV_WIDTH))


def rope(x, pos):
    half = x.shape[-1] // 2
    inv_freq = ROPE_THETA ** (-jnp.arange(half, dtype=jnp.float32) / half)
    ang = pos.astype(jnp.float32)[:, None] * inv_freq[None, :]
    cos = jnp.cos(ang)[None, :, None, :]
    sin = jnp.sin(ang)[None, :, None, :]
    xf = x.astype(jnp.float32)
    x1, x2 = xf[..., :half], xf[..., half:]
    return jnp.concatenate([x1 * cos - x2 * sin, x2 * cos + x1 * sin], axis=-1)


def swiglu(h, w_gate, w_up, w_down):
    return (jax.nn.silu(h @ w_gate) * (h @ w_up)) @ w_down


def rg_lru(xb, conv_w, conv_b, w_a, b_a, w_i, b_i, lam):
    bsz, seq, _ = xb.shape
    xc = causal_depthwise_conv(xb, conv_w) + conv_b
    xh = xc.reshape(bsz, seq, LRU_BLOCKS, LRU_BLOCK)
    r = jax.nn.sigmoid(jnp.einsum('bshi,hij->bshj', xh, w_a).reshape(bsz, seq, LRU_WIDTH) + b_a)
    gi = jax.nn.sigmoid(jnp.einsum('bshi,hij->bshj', xh, w_i).reshape(bsz, seq, LRU_WIDTH) + b_i)
    log_a = -LRU_C * r * jax.nn.softplus(-lam)
    a = jnp.exp(log_a)
    u = jnp.sqrt(-jnp.expm1(2.0 * log_a)) * (gi * xc)

    def combine(c1, c2):
        a1, b1 = c1
        a2, b2 = c2
        return a1 * a2, a2 * b1 + b2

    _, h = lax.associative_scan(combine, (a, u), axis=1)
    return h


def sliding_window_attention(q, k, v, sinks):
    bsz, seq = q.shape[:2]
    nb = seq // WINDOW
    qb = q.astype(jnp.float32).reshape(bsz, nb, WINDOW, ATT_KV_HEADS, ATT_GROUPS, ATT_HEAD_DIM)

    def with_prev(t):
        tb = t.astype(jnp.float32).reshape(bsz, nb, WINDOW, ATT_KV_HEADS, ATT_HEAD_DIM)
        prev = jnp.pad(tb, ((0, 0), (1, 0), (0, 0), (0, 0), (0, 0)))[:, :-1]
        return jnp.concatenate([prev, tb], axis=2)

    kb, vb = with_prev(k), with_prev(v)
    s = jnp.einsum('bnqhgd,bnkhd->bnhgqk', qb, kb) * (ATT_HEAD_DIM ** -0.5)
    qi = jnp.arange(WINDOW)[:, None] + WINDOW
    kj = jnp.arange(2 * WINDOW)[None, :]
    rel = qi - kj
    band = (rel >= 0) & (rel < WINDOW)
    valid = band[None] & ((jnp.arange(nb)[:, None, None] > 0) | (kj >= WINDOW)[None])
    s = jnp.where(valid[None, :, None, None], s, -1e30)
    sink = sinks.astype(jnp.float32).reshape(ATT_KV_HEADS, ATT_GROUPS)[None, None, :, :, None, None]
    m = jnp.maximum(jnp.max(s, axis=-1, keepdims=True), sink)
    p = jnp.exp(s - m)
    denom = jnp.sum(p, axis=-1, keepdims=True) + jnp.exp(sink - m)
    o = jnp.einsum('bnhgqk,bnkhd->bnqhgd', p / denom, vb)
    return o.reshape(bsz, seq, ATT_Q_WIDTH)


def even_mixer(h, pos, w_in, conv_w, conv_b, w_a, b_a, w_i, b_i, lam, q_norm, k_norm, sinks, w_out):
    bsz, seq, _ = h.shape
    proj = h @ w_in
    cuts = np.cumsum([LRU_WIDTH, LRU_WIDTH, ATT_Q_WIDTH, ATT_KV_WIDTH]).tolist()
    xb, gate, q, k, v = jnp.split(proj, cuts, axis=-1)
    y_lru = rg_lru(xb.astype(jnp.float32), conv_w, conv_b, w_a, b_a, w_i, b_i, lam) \
        * jax.nn.gelu(gate.astype(jnp.float32))
    q = rope(rms_norm(q.reshape(bsz, seq, ATT_Q_HEADS, ATT_HEAD_DIM), q_norm), pos)
    k = rope(rms_norm(k.reshape(bsz, seq, ATT_KV_HEADS, ATT_HEAD_DIM), k_norm), pos)
    v = v.reshape(bsz, seq, ATT_KV_HEADS, ATT_HEAD_DIM)
    y_att = sliding_window_attention(q, k, v, sinks)
    y = jnp.concatenate([y_lru.astype(h.dtype), y_att.astype(h.dtype)], axis=-1)
    return y @ w_out


def gated_deltanet(h, w_in, conv_w, a_log, dt_bias, out_norm, w_out):
    bsz, seq, _ = h.shape
    nc = seq // GDN_CHUNK
    C = GDN_CHUNK
    proj = (h @ w_in).astype(jnp.float32)
    cuts = [3 * GDN_WIDTH, 4 * GDN_WIDTH, 4 * GDN_WIDTH + GDN_HEADS]
    qkv, gate, a_in, b_in = jnp.split(proj, cuts, axis=-1)
    qkv = jax.nn.silu(causal_depthwise_conv(qkv, conv_w.astype(jnp.float32)))
    q, k, v = jnp.split(qkv, 3, axis=-1)
    q = l2_norm(q.reshape(bsz, seq, GDN_HEADS, GDN_HEAD_DIM)) * (GDN_HEAD_DIM ** -0.5)
    k = l2_norm(k.reshape(bsz, seq, GDN_HEADS, GDN_HEAD_DIM))
    v = v.reshape(bsz, seq, GDN_HEADS, GDN_HEAD_DIM)
    beta = jax.nn.sigmoid(b_in)
    g = -jnp.exp(a_log.astype(jnp.float32)) * jax.nn.softplus(a_in + dt_bias.astype(jnp.float32))

    def to_chunks(t):
        return t.reshape(bsz, nc, C, GDN_HEADS, -1).transpose(0, 3, 1, 2, 4)

    qc, kc, vc = to_chunks(q), to_chunks(k), to_chunks(v)
    gc = g.reshape(bsz, nc, C, GDN_HEADS).transpose(0, 3, 1, 2)
    bc = beta.reshape(bsz, nc, C, GDN_HEADS).transpose(0, 3, 1, 2)
    G = jnp.cumsum(gc, axis=-1)
    tri = jnp.tril(jnp.ones((C, C), dtype=bool))
    strict = jnp.tril(jnp.ones((C, C), dtype=bool), -1)
    diff = G[..., :, None] - G[..., None, :]
    decay = jnp.where(tri, jnp.exp(jnp.where(tri, diff, 0.0)), 0.0)
    kk = jnp.einsum('bhnid,bhnjd->bhnij', kc, kc)
    A = jnp.where(strict, bc[..., :, None] * kk * decay, 0.0) + jnp.eye(C, dtype=jnp.float32)
    u = lax.linalg.triangular_solve(A, bc[..., None] * vc, left_side=True, lower=True, unit_diagonal=True)
    w = lax.linalg.triangular_solve(A, bc[..., None] * jnp.exp(G)[..., None] * kc,
                                    left_side=True, lower=True, unit_diagonal=True)
    a_qk = jnp.where(tri, jnp.einsum('bhnid,bhnjd->bhnij', qc, kc) * decay, 0.0)
    q_dec = qc * jnp.exp(G)[..., None]
    k_dec = kc * jnp.exp(G[..., -1:] - G)[..., None]
    g_last = jnp.exp(G[..., -1])
    xs = (jnp.moveaxis(u, 2, 0), jnp.moveaxis(w, 2, 0), jnp.moveaxis(a_qk, 2, 0),
          jnp.moveaxis(q_dec, 2, 0), jnp.moveaxis(k_dec, 2, 0), jnp.moveaxis(g_last, 2, 0))

    def step(state, inp):
        u_c, w_c, a_c, qd_c, kd_c, gl_c = inp
        v_new = u_c - jnp.einsum('bhcd,bhde->bhce', w_c, state)
        o_c = jnp.einsum('bhcd,bhde->bhce', qd_c, state) + jnp.einsum('bhcj,bhje->bhce', a_c, v_new)
        state = state * gl_c[..., None, None] + jnp.einsum('bhcd,bhce->bhde', kd_c, v_new)
        return state, o_c

    s0 = jnp.zeros((bsz, GDN_HEADS, GDN_HEAD_DIM, GDN_HEAD_DIM), jnp.float32)
    _, o = lax.scan(step, s0, xs)
    o = o.transpose(1, 0, 3, 2, 4).reshape(bsz, seq, GDN_HEADS, GDN_HEAD_DIM)
    o = rms_norm(o, out_norm) * jax.nn.silu(gate.reshape(bsz, seq, GDN_HEADS, GDN_HEAD_DIM))
    return o.reshape(bsz, seq, GDN_WIDTH).astype(h.dtype) @ w_out


def moe_swiglu(h, router, w_gate, w_up, w_down):
    bsz, seq, d = h.shape
    t = h.reshape(-1, d)
    logits = (t @ router).astype(jnp.float32)
    top_v, top_i = lax.top_k(logits, TOP_K)
    top_w = jax.nn.softmax(top_v, axis=-1)
    gates = jnp.sum(jax.nn.one_hot(top_i, N_EXPERTS, dtype=jnp.float32) * top_w[..., None], axis=1)
    out = jnp.zeros(t.shape, jnp.float32)
    for e in range(N_EXPERTS):
        y_e = swiglu(t, w_gate[e], w_up[e], w_down[e]).astype(jnp.float32)
        out = out + gates[:, e:e + 1] * y_e
    return out.astype(h.dtype).reshape(bsz, seq, d)


def setup_inputs(seed: int = 0) -> dict:
    key = jax.random.key(seed)
    ks = jax.random.split(key, 32)
    f32 = jnp.float32
    D = D_MODEL

    def nrm(k, shape, scale):
        return scale * jax.random.normal(k, shape, f32)

    lru_u = jax.random.uniform(ks[10], (N_EVEN, LRU_WIDTH), f32, minval=0.9, maxval=0.999)
    lru_s = lru_u ** (1.0 / LRU_C)
    dt = jnp.exp(jax.random.uniform(ks[21], (N_ODD, GDN_HEADS), f32,
                                    minval=float(np.log(0.001)), maxval=float(np.log(0.1))))
    return {
        "x": nrm(ks[0], (BATCH, SEQ, D), 1.0),
        "ln_mix": 1.0 + nrm(ks[1], (DEPTH, D), 0.02),
        "ln_ffn": 1.0 + nrm(ks[2], (DEPTH, D), 0.02),
        "e_w_in": nrm(ks[3], (N_EVEN, D, EVEN_IN_COLS), D ** -0.5),
        "e_lru_conv_w": nrm(ks[4], (N_EVEN, CONV_WIDTH, LRU_WIDTH), CONV_WIDTH ** -0.5),
        "e_lru_conv_b": nrm(ks[5], (N_EVEN, LRU_WIDTH), 0.02),
        "e_lru_w_a": nrm(ks[6], (N_EVEN, LRU_BLOCKS, LRU_BLOCK, LRU_BLOCK), LRU_BLOCK ** -0.5),
        "e_lru_b_a": nrm(ks[7], (N_EVEN, LRU_WIDTH), 0.1),
        "e_lru_w_i": nrm(ks[8], (N_EVEN, LRU_BLOCKS, LRU_BLOCK, LRU_BLOCK), LRU_BLOCK ** -0.5),
        "e_lru_b_i": nrm(ks[9], (N_EVEN, LRU_WIDTH), 0.1),
        "e_lru_lambda": jnp.log(lru_s) - jnp.log1p(-lru_s),
        "e_q_norm": 1.0 + nrm(ks[11], (N_EVEN, ATT_HEAD_DIM), 0.02),
        "e_k_norm": 1.0 + nrm(ks[12], (N_EVEN, ATT_HEAD_DIM), 0.02),
        "e_sinks": nrm(ks[13], (N_EVEN, ATT_Q_HEADS), 1.0),
        "e_w_out": nrm(ks[14], (N_EVEN, EVEN_MIX_WIDTH, D), EVEN_MIX_WIDTH ** -0.5),
        "e_ffn_w_gate": nrm(ks[15], (N_EVEN, D, D_FF_DENSE), D ** -0.5),
        "e_ffn_w_up": nrm(ks[16], (N_EVEN, D, D_FF_DENSE), D ** -0.5),
        "e_ffn_w_down": nrm(ks[17], (N_EVEN, D_FF_DENSE, D), D_FF_DENSE ** -0.5),
        "o_w_in": nrm(ks[18], (N_ODD, D, ODD_IN_COLS), D ** -0.5),
        "o_conv_w": nrm(ks[19], (N_ODD, CONV_WIDTH, 3 * GDN_WIDTH), CONV_WIDTH ** -0.5),
        "o_a_log": jnp.log(jax.random.uniform(ks[20], (N_ODD, GDN_HEADS), f32, minval=1.0, maxval=16.0)),
        "o_dt_bias": dt + jnp.log(-jnp.expm1(-dt)),
        "o_out_norm": 1.0 + nrm(ks[22], (N_ODD, GDN_HEAD_DIM), 0.02),
        "o_w_out": nrm(ks[23], (N_ODD, GDN_WIDTH, D), GDN_WIDTH ** -0.5),
        "o_router": nrm(ks[24], (N_ODD, D, N_EXPERTS), D ** -0.5),
        "o_moe_w_gate": nrm(ks[25], (N_ODD, N_EXPERTS, D, D_FF_EXPERT), D ** -0.5),
        "o_moe_w_up": nrm(ks[26], (N_ODD, N_EXPERTS, D, D_FF_EXPERT), D ** -0.5),
        "o_moe_w_down": nrm(ks[27], (N_ODD, N_EXPERTS, D_FF_EXPERT, D), D_FF_EXPERT ** -0.5),
    }


def reference(x, ln_mix, ln_ffn,
              e_w_in, e_lru_conv_w, e_lru_conv_b, e_lru_w_a, e_lru_b_a, e_lru_w_i, e_lru_b_i,
              e_lru_lambda, e_q_norm, e_k_norm, e_sinks, e_w_out, e_ffn_w_gate, e_ffn_w_up, e_ffn_w_down,
              o_w_in, o_conv_w, o_a_log, o_dt_bias, o_out_norm, o_w_out, o_router,
              o_moe_w_gate, o_moe_w_up, o_moe_w_down):
    pos = jnp.arange(x.shape[1], dtype=jnp.int32)
    h = x
    for layer in range(DEPTH):
        j = layer // 2
        hn = rms_norm(h, ln_mix[layer])
        if layer % 2 == 0:
            h = h + even_mixer(hn, pos, e_w_in[j], e_lru_conv_w[j], e_lru_conv_b[j], e_lru_w_a[j],
                               e_lru_b_a[j], e_lru_w_i[j], e_lru_b_i[j], e_lru_lambda[j],
                               e_q_norm[j], e_k_norm[j], e_sinks[j], e_w_out[j]).astype(h.dtype)
            h = h + swiglu(rms_norm(h, ln_ffn[layer]), e_ffn_w_gate[j], e_ffn_w_up[j],
                           e_ffn_w_down[j]).astype(h.dtype)
        else:
            h = h + gated_deltanet(hn, o_w_in[j], o_conv_w[j], o_a_log[j], o_dt_bias[j],
                                   o_out_norm[j], o_w_out[j]).astype(h.dtype)
            h = h + moe_swiglu(rms_norm(h, ln_ffn[layer]), o_router[j], o_moe_w_gate[j],
                               o_moe_w_up[j], o_moe_w_down[j]).astype(h.dtype)
    return h
```

```python
import numpy as np
from contextlib import ExitStack
import concourse.bass as bass
import concourse.mybir as mybir
from concourse.bass_utils import run_bass_kernel_spmd

F32 = mybir.dt.float32
BF16 = mybir.dt.bfloat16
AF = mybir.ActivationFunctionType
ALU = mybir.AluOpType
AX = mybir.AxisListType
EPS = 1e-6
NDS = 20


class Buf:
    __slots__ = ("w", "r", "name", "excl")

    def __init__(self, name="", excl=False):
        self.w = None
        self.r = {}
        self.name = name
        self.excl = excl


class Sched:
    def __init__(self, nc):
        self.nc = nc
        self.E = {}
        for nm, eng in (("pe", nc.tensor), ("dve", nc.vector), ("act", nc.scalar),
                        ("pool", nc.gpsimd), ("sp", nc.sync)):
            self.E[nm] = dict(eng=eng, sem=nc.alloc_semaphore("s_" + nm), cnt=0, known={}, name=nm)
        self.dsem = {q: [[nc.alloc_semaphore(f"d_{q}{i}"), 0] for i in range(NDS)]
                     for q in ("sp", "act", "pool")}
        self.drr = {q: 0 for q in self.dsem}
        self.nins = 0
        self.ccsem = nc.alloc_semaphore("s_cc")
        self.ccval = 0
        self.extra = []

    def collective(self, kind, in_ap, out_ap, groups):
        self.ccval += 1
        self.nc.gpsimd.collective_compute(kind, ALU.bypass, replica_groups=groups, ins=[in_ap], outs=[out_ap]).then_inc(self.ccsem)
        self.extra.append((self.ccsem, self.ccval))
        self.nins += 1

    def _deps(self, reads, writes):
        deps = []
        for b in reads:
            if b.w is not None:
                deps.append(b.w)
            if b.excl:
                deps.extend(b.r.values())
        for b in writes:
            if b.w is not None:
                deps.append(b.w)
            deps.extend(b.r.values())
        return deps

    def _wait(self, E, deps):
        for (sem, val) in deps:
            k = id(sem)
            if E["known"].get(k, 0) >= val:
                continue
            E["eng"].wait_ge(sem, val)
            E["known"][k] = val

    def op(self, en, fn, reads=(), writes=(), inc=True):
        E = self.E[en]
        deps = self._deps(reads, writes)
        if en == "pe":
            deps = [d for d in deps if d[0] is not E["sem"]]
        self._wait(E, deps)
        ins = fn(E["eng"])
        self.nins += 1
        if inc:
            E["cnt"] += 1
            ins.then_inc(E["sem"], 1)
            tok = (E["sem"], E["cnt"])
        else:
            tok = (E["sem"], E["cnt"] + 1)
        k = id(E["sem"])
        for b in reads:
            b.r[k] = tok
        for b in writes:
            b.w = tok
            b.r = {}
        return tok

    def dma(self, q, out, in_, reads=(), writes=(), **kw):
        E = self.E[q]
        deps = self._deps(reads, writes)
        slot = self.dsem[q][self.drr[q]]
        self.drr[q] = (self.drr[q] + 1) % NDS
        sem, val = slot
        if val > 0:
            deps.append((sem, val))
        self._wait(E, deps)
        slot[1] = val + 16
        E["eng"].dma_start(out=out, in_=in_, **kw).then_inc(sem, 16)
        self.nins += 1
        tok = (sem, val + 16)
        k = id(sem)
        for b in reads:
            b.r[k] = tok
        for b in writes:
            b.w = tok
            b.r = {}
        return tok

    def finish(self):
        for q, slots in self.dsem.items():
            E = self.E[q]
            self._wait(E, [(s, v) for s, v in slots if v > 0])


class BGCast:
    def __init__(self, pairs, CH=1024, NBUF=2):
        self.work = []
        for src, dst in pairs:
            M = src.shape[1]
            for c0 in range(0, M, CH):
                self.work.append((src, dst, c0, min(CH, M - c0)))
        self.CH, self.NBUF = CH, NBUF
        self.i = 0
        self.loaded = []
        self.kb = None

    def attach(self, kb):
        self.kb = kb
        self.st32 = [kb.sb([128, self.CH], F32, "bg32") for _ in range(self.NBUF)]
        self.st16 = [kb.sb([128, self.CH], BF16, "bg16") for _ in range(self.NBUF)]
        self._prefetch()

    def _prefetch(self):
        s = self.kb.s
        for si in range(self.NBUF):
            if self.i >= len(self.work):
                break
            src, dst, c0, w = self.work[self.i]
            a = self.st32[si]
            s.dma("sp", a[:, 0:w], src[:, c0:c0 + w], writes=[a.b])
            self.loaded.append((self.i, si))
            self.i += 1

    def _process(self):
        s = self.kb.s
        for wi, si in self.loaded:
            src, dst, c0, w = self.work[wi]
            a, b = self.st32[si], self.st16[si]
            s.op("pool", lambda e: e.tensor_copy(b[:, 0:w], a[:, 0:w]), reads=[a.b], writes=[b.b])
            s.dma("pool", dst[:, c0:c0 + w], b[:, 0:w], reads=[b.b])
        self.loaded = []

    def step(self):
        if self.kb is None:
            return
        self._process()
        self._prefetch()

    def detach(self):
        if self.kb is None:
            return
        self._process()
        self.kb = None

    def remaining(self):
        return self.work[self.i:]


class T:
    def __init__(self, t, nbuf=1, name=""):
        self.t = t
        self.b = Buf(name)

    def __getitem__(self, idx):
        return self.t[idx]


class KB:
    def __init__(self, nc):
        self.nc = nc
        self.s = Sched(nc)
        self.n = 0
        self.bg = None
        self.stack = ExitStack()
        self.banks = [self.psum(f"bank{i}") for i in range(8)]

    def sb(self, shape, dt, name=None):
        self.n += 1
        name = (name or "t") + f"_{self.n}"
        return T(self.stack.enter_context(self.nc.sbuf_tensor(name, list(shape), dt)), name=name)

    def sbp(self, shape, dt, name):
        self.n += 1
        return T(self.nc.alloc_sbuf_tensor(name + f"_{self.n}", list(shape), dt), name=name)

    def phase_end(self):
        barrier(self)
        self.stack.close()
        self.stack = ExitStack()

    def psum(self, name):
        t = T(self.nc.alloc_psum_tensor(name, [128, 512], F32), name=name)
        t.b.excl = True
        return t

    def dram(self, name, shape, dt, kind=None):
        if kind:
            return self.nc.dram_tensor(name, list(shape), dt, kind=kind).ap()
        return self.nc.dram_tensor(name, list(shape), dt).ap()


def bc(ap, shape):
    return ap.to_broadcast(list(shape))


def barrier(kb):
    s = kb.s
    toks = [(E["sem"], E["cnt"]) for E in s.E.values() if E["cnt"] > 0]
    for q, slots in s.dsem.items():
        toks += [(sm, v) for sm, v in slots if v > 0]
    toks += s.extra
    for E in s.E.values():
        s._wait(E, [t for t in toks if t[0] is not E["sem"]])


def mm_group(kb, bank, out_ap, pairs, reads, transpose=False):
    n = len(pairs)
    for i, (l, r) in enumerate(pairs):
        if transpose:
            fn = (lambda e, l=l, r=r, o=out_ap[i]: e.transpose(o, l, r))
        else:
            fn = (lambda e, l=l, r=r, i=i: e.matmul(out_ap, l, r, start=(i == 0), stop=(i == n - 1)))
        kb.s.op("pe", fn, reads=reads, writes=[bank.b], inc=(i == n - 1))


def cast_dram(kb, pairs, CH=4096):
    s = kb.s
    NB_ = 4
    st32 = [kb.sb([128, CH], F32) for _ in range(NB_)]
    st16 = [kb.sb([128, CH], BF16) for _ in range(NB_)]
    work = []
    for src, dst in pairs:
        M = src.shape[1]
        for c0 in range(0, M, CH):
            work.append((src, dst, c0, min(CH, M - c0)))
    engs = ["pool", "act", "dve"]

    def load(i):
        src, dst, c0, w = work[i]
        a = st32[i % NB_]
        s.dma("sp", a[:, 0:w], src[:, c0:c0 + w], writes=[a.b])

    for i in range(min(2, len(work))):
        load(i)
    for i in range(len(work)):
        if i + 2 < len(work):
            load(i + 2)
        src, dst, c0, w = work[i]
        a = st32[i % NB_]
        b = st16[i % NB_]
        en = engs[i % 3]
        if en == "act":
            s.op("act", lambda e, a=a, b=b, w=w: e.copy(b[:, 0:w], a[:, 0:w]), reads=[a.b], writes=[b.b])
        else:
            s.op(en, lambda e, a=a, b=b, w=w: e.tensor_copy(b[:, 0:w], a[:, 0:w]), reads=[a.b], writes=[b.b])
        s.dma("sp", dst[:, c0:c0 + w], b[:, 0:w], reads=[b.b])


def rms_stats(kb, x_ap, xbuf, junk, ss, rt, rstd):
    s = kb.s
    s.op("act", lambda e: e.activation(junk[:], x_ap, AF.Square, accum_out=ss[:]), reads=[xbuf], writes=[junk.b, ss.b])
    s.op("act", lambda e: e.activation(rt[:], ss[:], AF.Sqrt, bias=kb.eps_t[:], scale=1.0 / 1024.0), reads=[ss.b], writes=[rt.b])
    s.op("dve", lambda e: e.reciprocal(rstd[:], rt[:]), reads=[rt.b], writes=[rstd.b])


class FFNRes:
    pass


def ffn_alloc(kb, TS, NC, nrot=3):
    r = FFNRes()
    r.TS = TS
    r.nrot = nrot
    r.wg = [kb.sb([128, 8, 256], BF16, "wg") for _ in range(nrot)]
    r.wu = [kb.sb([128, 8, 256], BF16, "wu") for _ in range(nrot)]
    r.wd = kb.sb([128, NC, 1024], BF16, "wd")
    r.hT = kb.sb([128, NC, TS], BF16, "hT")
    r.sg = [kb.sb([128, TS], F32, "sg") for _ in range(2)]
    r.blk = 0
    r.ch = 0
    r.dn = 0
    return r


def ffn_expert(kb, r, hnT, wg_d, wu_d, wd_d, NC, evac, loader=None, hook=None):
    s = kb.s
    TS = r.TS
    NB = NC // 2
    for g0 in range(0, NC, 7):
        g1 = min(NC, g0 + 7)
        if loader is not None:
            loader("d", g0 // 7, r.wd[:, g0:g1, :].rearrange("p c n -> p (c n)"))
            continue
        s.dma("act", r.wd[:, g0:g1, :], wd_d[:, g0 * 1024:g1 * 1024].rearrange("p (c n) -> p c n", n=1024),
              writes=[r.wd.b])
    for b in range(NB):
        wg = r.wg[r.blk % r.nrot]
        wu = r.wu[r.blk % r.nrot]
        r.blk += 1
        if hook is not None:
            hook()
        if loader is not None:
            loader("g", b, wg)
            loader("u", b, wu)
        else:
            s.dma("sp", wg[:], wg_d[:, b * 2048:(b + 1) * 2048].rearrange("p (k n) -> p k n", n=256), writes=[wg.b])
            s.dma("sp", wu[:], wu_d[:, b * 2048:(b + 1) * 2048].rearrange("p (k n) -> p k n", n=256), writes=[wu.b])
        for c in range(2):
            ch = b * 2 + c
            bg = kb.banks[(2 * r.ch) % 4]
            bu = kb.banks[(2 * r.ch + 1) % 4]
            sg = r.sg[r.ch % 2]
            r.ch += 1
            mm_group(kb, bg, bg[:, 0:TS], [(wg[:, k, c * 128:(c + 1) * 128], hnT[:, k, :]) for k in range(8)],
                     reads=[wg.b, hnT.b])
            mm_group(kb, bu, bu[:, 0:TS], [(wu[:, k, c * 128:(c + 1) * 128], hnT[:, k, :]) for k in range(8)],
                     reads=[wu.b, hnT.b])
            s.op("act", lambda e, sg=sg, bg=bg: e.activation(sg[:], bg[:, 0:TS], AF.Silu), reads=[bg.b], writes=[sg.b])
            s.op("dve", lambda e, sg=sg, bu=bu, ch=ch: e.tensor_tensor(r.hT[:, ch, :], sg[:], bu[:, 0:TS], ALU.mult),
                 reads=[sg.b, bu.b], writes=[r.hT.b])
    for t in range(TS // 128):
        for half in range(2):
            bk = kb.banks[4 + (r.dn % 4)]
            r.dn += 1
            mm_group(kb, bk, bk[:, :], [(r.hT[:, ch, t * 128:(t + 1) * 128], r.wd[:, ch, half * 512:(half + 1) * 512])
                                        for ch in range(NC)], reads=[r.hT.b, r.wd.b])
            evac(t, half, bk)


def phase4(kb, C, D):
    s = kb.s
    NT, NE, NCm = C["NTOK"], C["NEXP"], C["NC_MOE"]
    TS = 512
    ident = kb.sb([128, 128], F32, "ident")
    s.dma("sp", ident[:], D["ident"], writes=[ident.b])
    kb.eps_t = kb.sb([128, 1], F32, "eps")
    s.op("pool", lambda e: e.memset(kb.eps_t[:], EPS), writes=[kb.eps_t.b])
    g = kb.sb([128, 8], F32, "gffn")
    s.dma("sp", g[:], D["gffn1"], writes=[g.b])
    wo = kb.sb([128, 8, 1024], BF16, "wo")
    s.dma("sp", wo[:], D["wo1"].rearrange("p (k n) -> p k n", n=1024), writes=[wo.b])
    rtr = kb.sb([128, 8, 8], F32, "rtr")
    s.dma("sp", rtr[:], D["router"].rearrange("p (k n) -> p k n", n=8), writes=[rtr.b])
    r = ffn_alloc(kb, TS, NCm)
    hnT = [kb.sb([128, 8, TS], BF16, "hnT") for _ in range(2)]
    acc = kb.sb([128, TS // 128, 1024], F32, "acc")
    accb = [Buf() for _ in range(TS // 128)]
    gw = kb.sb([128, TS // 128, 8], F32, "gw")
    og = [kb.sb([128, 8, 128], BF16, "og") for _ in range(2)]
    h1 = [kb.sb([128, 1024], F32, "h1") for _ in range(2)]
    xs = kb.sb([128, 1024], F32, "xs")
    junk = kb.sb([128, 1024], BF16, "junk")
    hn32 = kb.sb([128, 8, 128], F32, "hn32")
    sm = {k: kb.sb([128, 8], F32, k) for k in ("lg", "mx", "g1", "g2")}
    sc = {k: kb.sb([128, 1], F32, k) for k in ("ss", "rt", "rstd", "dd", "ex", "den", "w1", "w2")}
    ogt_v = None if C.get("fused") else D["ogt"].rearrange("(c p) n -> p c n", p=128)
    if C.get("fused"):
        ogc = [kb.sb([128, 4, 8, 128], BF16, "ogc") for _ in range(2)]
        sel4 = kb.sb([128, 4], F32, "sel4")
        s.dma("sp", sel4[:], D["sel4"], writes=[sel4.b])
    it = 0
    for st in range(NT // TS):
        hT_ = hnT[st % 2]
        for t in range(TS // 128):
            tok0 = st * TS + t * 128
            o_ = og[it % 2]
            h_ = h1[it % 2]
            it += 1
            if C.get("fused"):
                cd_ = ogc[it % 2]
                for r_ in range(4):
                    g_ = r_ * NT + tok0
                    b_, c0_ = g_ // 2048, g_ % 2048
                    s.dma("act", cd_[:, r_], D["ogt"][b_ * 1024:(b_ + 1) * 1024, :].rearrange("(c p) n -> p c n", p=128)[:, :, c0_:c0_ + 128],
                          writes=[cd_.b])
                s.op("dve", lambda e: e.tensor_scalar(o_[:], cd_[:, 0], sel4[:, 0:1], None, ALU.mult),
                     reads=[cd_.b, sel4.b], writes=[o_.b])
                for r_ in range(1, 4):
                    s.op("dve", lambda e, r_=r_: e.scalar_tensor_tensor(o_[:], cd_[:, r_], sel4[:, r_:r_ + 1], o_[:], ALU.mult, ALU.add),
                         reads=[cd_.b, sel4.b, o_.b], writes=[o_.b])
            else:
                s.dma("act", o_[:], ogt_v[:, :, tok0:tok0 + 128], writes=[o_.b])
            s.dma("act", h_[:], D["h1"][tok0:tok0 + 128, :], writes=[h_.b])
            for half in range(2):
                bk = kb.banks[4 + half]
                mm_group(kb, bk, bk[:, :], [(o_[:, c, :], wo[:, c, half * 512:(half + 1) * 512]) for c in range(8)],
                         reads=[o_.b, wo.b])
                s.op("dve", lambda e, bk=bk, h_=h_, t=t, half=half: e.tensor_tensor(
                    acc[:, t, half * 512:(half + 1) * 512], h_[:, half * 512:(half + 1) * 512], bk[:, :], ALU.add),
                    reads=[bk.b, h_.b], writes=[accb[t]])
            rms_stats(kb, acc[:, t, :], accb[t], junk, sc["ss"], sc["rt"], sc["rstd"])
            s.op("act", lambda e, t=t: e.activation(xs[:], acc[:, t, :], AF.Copy, scale=sc["rstd"][:]),
                 reads=[accb[t], sc["rstd"].b], writes=[xs.b])
            for hh in range(2):
                bk = kb.banks[6 + hh]
                mm_group(kb, bk, [bk[:, j * 128:(j + 1) * 128] for j in range(4)],
                         [(xs[:, (hh * 4 + j) * 128:(hh * 4 + j + 1) * 128], ident[:]) for j in range(4)],
                         reads=[xs.b, ident.b], transpose=True)
                s.op("dve", lambda e, bk=bk, hh=hh: e.tensor_tensor(
                    hn32[:, hh * 4:(hh + 1) * 4, :], bk[:, :].rearrange("p (c n) -> p c n", n=128),
                    bc(g[:, hh * 4:(hh + 1) * 4].unsqueeze(2), [128, 4, 128]), ALU.mult),
                    reads=[bk.b, g.b], writes=[hn32.b])
            s.op("pool", lambda e, t=t, hT_=hT_: e.tensor_copy(hT_[:, :, t * 128:(t + 1) * 128], hn32[:]),
                 reads=[hn32.b], writes=[hT_.b])
            bk = kb.banks[4]
            mm_group(kb, bk, bk[:, 0:8], [(hn32[:, c, :], rtr[:, c, :]) for c in range(8)], reads=[hn32.b, rtr.b])
            lg, mx, g1, g2 = sm["lg"], sm["mx"], sm["g1"], sm["g2"]
            s.op("dve", lambda e, bk=bk: e.tensor_copy(lg[:], bk[:, 0:8]), reads=[bk.b], writes=[lg.b])
            s.op("dve", lambda e: e.max(mx[:], lg[:]), reads=[lg.b], writes=[mx.b])
            s.op("dve", lambda e: e.tensor_tensor(sc["dd"][:], mx[:, 1:2], mx[:, 0:1], ALU.subtract),
                 reads=[mx.b], writes=[sc["dd"].b])
            s.op("act", lambda e: e.activation(sc["ex"][:], sc["dd"][:], AF.Exp), reads=[sc["dd"].b], writes=[sc["ex"].b])
            s.op("dve", lambda e: e.tensor_scalar(sc["den"][:], sc["ex"][:], 1.0, None, ALU.add),
                 reads=[sc["ex"].b], writes=[sc["den"].b])
            s.op("dve", lambda e: e.reciprocal(sc["w1"][:], sc["den"][:]), reads=[sc["den"].b], writes=[sc["w1"].b])
            s.op("dve", lambda e: e.tensor_tensor(sc["w2"][:], sc["ex"][:], sc["w1"][:], ALU.mult),
                 reads=[sc["ex"].b, sc["w1"].b], writes=[sc["w2"].b])
            s.op("dve", lambda e: e.tensor_scalar(g1[:], lg[:], mx[:, 0:1], sc["w1"][:], ALU.is_equal, ALU.mult),
                 reads=[lg.b, mx.b, sc["w1"].b], writes=[g1.b])
            s.op("dve", lambda e: e.tensor_scalar(g2[:], lg[:], mx[:, 1:2], sc["w2"][:], ALU.is_equal, ALU.mult),
                 reads=[lg.b, mx.b, sc["w2"].b], writes=[g2.b])
            s.op("dve", lambda e, t=t: e.tensor_tensor(gw[:, t, :], g1[:], g2[:], ALU.add),
                 reads=[g1.b, g2.b], writes=[gw.b])
        for ex in range(NE):
            def evac(t, half, bk, ex=ex):
                s.op("dve", lambda e: e.scalar_tensor_tensor(
                    acc[:, t, half * 512:(half + 1) * 512], bk[:, :], gw[:, t, ex:ex + 1],
                    acc[:, t, half * 512:(half + 1) * 512], ALU.mult, ALU.add),
                    reads=[bk.b, gw.b, accb[t]], writes=[accb[t]])
            ffn_expert(kb, r, hT_, D["mwg"][ex * 128:(ex + 1) * 128, :], D["mwu"][ex * 128:(ex + 1) * 128, :],
                       D["mwd"][ex * 128:(ex + 1) * 128, :], NCm, evac)
        for t in range(TS // 128):
            tok0 = st * TS + t * 128
            s.dma("act", D["out"][tok0:tok0 + 128, :], acc[:, t, :], reads=[accb[t]])


def nbank(kb):
    kb.bk = (getattr(kb, "bk", -1) + 1) % 8
    return kb.banks[kb.bk]


def norm_T(kb, W, x_ap, xbuf, gcol, dst, dcols, ncol=128):
    s = kb.s
    rms_stats(kb, x_ap, xbuf, W["junk"], W["ss"], W["rt"], W["rstd"])
    xs = W["xsb"]
    s.op("act", lambda e: e.activation(xs[:], x_ap, AF.Copy, scale=W["rstd"][:]), reads=[xbuf, W["rstd"].b], writes=[xs.b])
    bk = nbank(kb)
    bv = bk[:, :].bitcast(BF16)
    mm_group(kb, bk, [bv[:, j * 128:(j + 1) * 128] for j in range(8)],
             [(xs[:, j * 128:(j + 1) * 128], W["identb"][:]) for j in range(8)], reads=[xs.b, W["identb"].b], transpose=True)
    s.op("dve", lambda e: e.tensor_tensor(dst[:, :, dcols], bv.rearrange("p (c n) -> p c n", n=128),
                                           bc(gcol[:, 0:8].unsqueeze(2), [128, 8, 128]), ALU.mult),
         reads=[bk.b, gcol.b], writes=[dst.b])


def norm_scratch(kb):
    W = {}
    W["junk"] = kb.sb([128, 1024], BF16, "junk")
    W["xsb"] = kb.sb([128, 1024], BF16, "xsb")
    for k in ("ss", "rt", "rstd"):
        W[k] = kb.sb([128, 1], F32, k)
    ident = kb.sb([128, 128], F32, "ident")
    kb.s.dma("sp", ident[:], kb.D["ident"], writes=[ident.b])
    W["ident"] = ident
    W["identb"] = kb.sb([128, 128], BF16, "identb")
    kb.s.op("dve", lambda e: e.tensor_copy(W["identb"][:], ident[:]), reads=[ident.b], writes=[W["identb"].b])
    kb.eps_t = kb.sb([128, 1], F32, "eps")
    kb.s.op("pool", lambda e: e.memset(kb.eps_t[:], EPS), writes=[kb.eps_t.b])
    return W


def phase1(kb, C, D):
    s = kb.s
    NT = C["NTOK"]
    TS = 512
    kb.D = D
    W = norm_scratch(kb)
    if kb.bg:
        kb.bg.attach(kb)

    def ld(name, shape, dt=F32, src=None, q="sp"):
        t = kb.sb(shape, dt, name)
        s.dma(q, t[:], D[name] if src is None else src, writes=[t.b])
        return t

    gmix = ld("gmix0", [128, 8])
    win = kb.sb([128, 8, 1792], BF16, "win")
    s.dma("sp", win[:], D["win"].rearrange("p (k n) -> p k n", n=1792), writes=[win.b])
    lruc = ld("lruc", [128, 4, 8])
    wab32 = ld("wa_bd", [128, 4, 128])
    wib32 = ld("wi_bd", [128, 4, 128])
    wab = kb.sb([128, 4, 128], BF16, "wab")
    wib = kb.sb([128, 4, 128], BF16, "wib")
    s.op("dve", lambda e: e.tensor_copy(wab[:], wab32[:]), reads=[wab32.b], writes=[wab.b])
    s.op("dve", lambda e: e.tensor_copy(wib[:], wib32[:]), reads=[wib32.b], writes=[wib.b])
    qkg = ld("qkg", [64, 2])
    esink = ld("sinks_b", [128, 8])
    s.op("act", lambda e: e.activation(esink[:], esink[:], AF.Exp), reads=[esink.b], writes=[esink.b])
    prot32 = ld("prot", [64, 64])
    prot = kb.sb([64, 64], BF16, "protb")
    s.op("dve", lambda e: e.tensor_copy(prot[:], prot32[:]), reads=[prot32.b], writes=[prot.b])
    ones64 = kb.sb([64, 64], F32, "ones64")
    s.op("pool", lambda e: e.memset(ones64[:], 1.0), writes=[ones64.b])
    mcur = ld("mask_cur", [128, 128])
    mprev = ld("mask_prev", [128, 128])
    mprev0 = ld("mask_prev0", [128, 128])
    cl = kb.sb([128, 4], F32, "cl")
    cl2 = kb.sb([128, 4], F32, "cl2")
    s.op("act", lambda e: e.activation(cl[:], lruc[:, :, 7], AF.Exp, scale=-1.0), reads=[lruc.b], writes=[cl.b])
    s.op("act", lambda e: e.activation(cl[:], cl[:], AF.Ln, bias=1.0), reads=[cl.b], writes=[cl.b])
    s.op("dve", lambda e: e.tensor_scalar(cl2[:], cl[:], -16.0, None, ALU.mult), reads=[cl.b], writes=[cl2.b])
    s.op("dve", lambda e: e.tensor_scalar(cl[:], cl[:], -8.0, None, ALU.mult), reads=[cl.b, cl2.b], writes=[cl.b])
    zeros = kb.sb([128, TS], F32, "zeros")
    s.op("pool", lambda e: e.memset(zeros[:], 0.0), writes=[zeros.b])
    eps64 = kb.eps_t

    hnT = [kb.sb([128, 8, TS], BF16, "hnT") for _ in range(2)]
    xt = [kb.sb([128, 1024], F32, "xt") for _ in range(3)]
    xb = [[kb.sb([128, 3 + TS], F32, "xb") for _ in range(2)] for _ in range(4)]
    hcar = [kb.sb([128, 1], F32, "hcar") for _ in range(4)]
    pcar = [kb.sb([128, 1], F32, "pcar") for _ in range(4)]
    for c in range(4):
        s.op("pool", lambda e, c=c: e.memset(hcar[c][:], 0.0), writes=[hcar[c].b])
        s.op("pool", lambda e, c=c: e.memset(pcar[c][:], 1.0), writes=[pcar[c].b])
    L = {k: kb.sb([128, TS], F32, k) for k in ("xc", "r", "gi", "a", "a2", "u", "h", "P", "gg")}
    xcb = kb.sb([128, TS], BF16, "xcb")
    yo = [kb.sb([128, TS], BF16, "yo") for _ in range(4)]
    QR = kb.sb([64, 8, TS], BF16, "QR")
    KR = kb.sb([64, 2, 128 + TS], BF16, "KR")
    Vg = [kb.sb([128, 2, 65], BF16, "Vg") for _ in range(6)]
    for v in Vg:
        s.op("pool", lambda e, v=v: e.memset(v[:], 1.0), writes=[v.b])
    A = {k: kb.sb([64, TS], F32, k) for k in ("sq", "ln", "qn32", "t1", "t2")}
    qnb = kb.sb([64, TS], BF16, "qnb")
    cs = [kb.sb([64, 2, TS], F32, "cs") for _ in range(2)]
    E_ = [kb.sb([128, 512], F32, "E") for _ in range(2)]
    Pt = [kb.sb([128, 512], BF16, "Pt") for _ in range(4)]
    Y = kb.sb([128, 8, 64], BF16, "Y")
    yT = [kb.sb([128, 4, 128], BF16, "yT") for _ in range(2)]
    den = kb.sb([128, 4], F32, "den")
    cnt = {"x": 0, "v": 0, "e": 0, "p": 0, "y": 0, "yo": 0}

    def load_norm(src_ap, dst, dcols):
        x_ = xt[cnt["x"] % 3]
        cnt["x"] += 1
        s.dma("act", x_[:], src_ap, writes=[x_.b])
        norm_T(kb, W, x_[:], x_.b, gmix, dst, dcols)

    def proj(h_, cols0, ncols, ntok, tcols):
        bk = nbank(kb)
        mm_group(kb, bk, bk[0:ncols, 0:ntok], [(win[:, k, cols0:cols0 + ncols], h_[:, k, tcols]) for k in range(8)],
                 reads=[win.b, h_.b])
        return bk

    def qk_norm_rope(bk, ntok, gidx, cst, ccols, dst_ap, dst_buf):
        n = ntok
        s.op("act", lambda e: e.activation(A["sq"][:, 0:n], bk[0:64, 0:n], AF.Square), reads=[bk.b], writes=[A["sq"].b])
        b2 = nbank(kb)
        mm_group(kb, b2, b2[0:64, 0:n], [(ones64[:], A["sq"][:, 0:n])], reads=[ones64.b, A["sq"].b])
        s.op("act", lambda e: e.activation(A["ln"][:, 0:n], b2[0:64, 0:n], AF.Ln, bias=eps64[0:64, :], scale=1.0 / 64),
             reads=[b2.b], writes=[A["ln"].b])
        s.op("act", lambda e: e.activation(A["ln"][:, 0:n], A["ln"][:, 0:n], AF.Exp, scale=-0.5),
             reads=[A["ln"].b], writes=[A["ln"].b])
        s.op("dve", lambda e: e.scalar_tensor_tensor(A["qn32"][:, 0:n], bk[0:64, 0:n], qkg[:, gidx:gidx + 1],
                                                     A["ln"][:, 0:n], ALU.mult, ALU.mult),
             reads=[bk.b, qkg.b, A["ln"].b], writes=[A["qn32"].b])
        s.op("pool", lambda e: e.tensor_copy(qnb[:, 0:n], A["qn32"][:, 0:n]), reads=[A["qn32"].b], writes=[qnb.b])
        b3 = nbank(kb)
        mm_group(kb, b3, b3[0:64, 0:n], [(prot[:], qnb[:, 0:n])], reads=[prot.b, qnb.b])
        s.op("pool", lambda e: e.tensor_tensor(A["t1"][:, 0:n], A["qn32"][:, 0:n], cst[:, 0, ccols], ALU.mult),
             reads=[A["qn32"].b, cst.b], writes=[A["t1"].b])
        s.op("dve", lambda e: e.tensor_tensor(A["t2"][:, 0:n], b3[0:64, 0:n], cst[:, 1, ccols], ALU.mult),
             reads=[b3.b, cst.b], writes=[A["t2"].b])
        s.op("pool", lambda e: e.tensor_tensor(dst_ap, A["t1"][:, 0:n], A["t2"][:, 0:n], ALU.add),
             reads=[A["t1"].b, A["t2"].b], writes=[dst_buf])

    def v_tile(h_, tcols):
        v = Vg[cnt["v"] % 6]
        cnt["v"] += 1
        bk = nbank(kb)
        mm_group(kb, bk, bk[:, 0:128], [(h_[:, k, tcols], win[:, k, 1664:1792]) for k in range(8)], reads=[win.b, h_.b])
        s.op("act", lambda e: e.copy(v[:, :, 0:64], bk[:, 0:128].rearrange("p (h d) -> p h d", d=64)),
             reads=[bk.b], writes=[v.b])
        return v

    hh_ = hnT[1]
    load_norm(D["xhalo"], hh_, slice(0, 128))
    csh = cs[1]
    s.dma("sp", csh[:, :, 0:128], D["cossin"][:, :, 0:128], writes=[csh.b])
    for c in range(4):
        bk = proj(hh_, c * 128, 128, 128, slice(0, 128))
        s.op("act", lambda e, c=c, bk=bk: e.copy(xb[c][0][:, 0:3], bk[:, 125:128]), reads=[bk.b], writes=[xb[c][0].b])
    for h in range(2):
        bk = proj(hh_, 1536 + h * 64, 64, 128, slice(0, 128))
        qk_norm_rope(bk, 128, 1, csh, slice(0, 128), KR[:, h, 0:128], KR.b)
    vprev = v_tile(hh_, slice(0, 128))

    for st in range(NT // TS):
        h_ = hnT[st % 2]
        cst = cs[st % 2]
        s.dma("sp", cst[:], D["cossin"][:, :, 128 + st * TS:128 + (st + 1) * TS], writes=[cst.b])
        for t in range(4):
            tok0 = st * TS + t * 128
            load_norm(D["x"][tok0:tok0 + 128, :], h_, slice(t * 128, (t + 1) * 128))
        allc = slice(0, TS)
        for c in range(4):
            xb_ = xb[c][st % 2]
            xbn = xb[c][(st + 1) % 2]
            bk = proj(h_, c * 128, 128, TS, allc)
            s.op("act", lambda e, bk=bk, xb_=xb_: e.copy(xb_[:, 3:3 + TS], bk[:, 0:TS]), reads=[bk.b], writes=[xb_.b])
            s.op("pool", lambda e, xb_=xb_, xbn=xbn: e.tensor_copy(xbn[:, 0:3], xb_[:, TS:TS + 3]), reads=[xb_.b], writes=[xbn.b])
            bkg = proj(h_, 512 + c * 128, 128, TS, allc)
            s.op("act", lambda e, bkg=bkg: e.activation(L["gg"][:], bkg[:, 0:TS], AF.Gelu_apprx_tanh), reads=[bkg.b], writes=[L["gg"].b])
            xc = L["xc"]
            s.op("dve", lambda e, xb_=xb_, c=c: e.tensor_scalar(xc[:], xb_[:, 0:TS], lruc[:, c, 0:1], lruc[:, c, 4:5], ALU.mult, ALU.add),
                 reads=[xb_.b, lruc.b], writes=[xc.b])
            for j in range(1, 4):
                s.op("dve", lambda e, xb_=xb_, c=c, j=j: e.scalar_tensor_tensor(xc[:], xb_[:, j:j + TS], lruc[:, c, j:j + 1], xc[:], ALU.mult, ALU.add),
                     reads=[xb_.b, lruc.b, xc.b], writes=[xc.b])
            s.op("pool", lambda e: e.tensor_copy(xcb[:], xc[:]), reads=[xc.b], writes=[xcb.b])
            b1 = nbank(kb)
            mm_group(kb, b1, b1[:, 0:TS], [(wab[:, c, :], xcb[:])], reads=[wab.b, xcb.b])
            b2 = nbank(kb)
            mm_group(kb, b2, b2[:, 0:TS], [(wib[:, c, :], xcb[:])], reads=[wib.b, xcb.b])
            s.op("act", lambda e, b1=b1, c=c: e.activation(L["r"][:], b1[:, 0:TS], AF.Sigmoid, bias=lruc[:, c, 5:6]),
                 reads=[b1.b, lruc.b], writes=[L["r"].b])
            s.op("act", lambda e, b2=b2, c=c: e.activation(L["gi"][:], b2[:, 0:TS], AF.Sigmoid, bias=lruc[:, c, 6:7]),
                 reads=[b2.b, lruc.b], writes=[L["gi"].b])
            s.op("act", lambda e, c=c: e.activation(L["a"][:], L["r"][:], AF.Exp, scale=cl[:, c:c + 1]),
                 reads=[L["r"].b, cl.b], writes=[L["a"].b])
            s.op("act", lambda e, c=c: e.activation(L["a2"][:], L["r"][:], AF.Exp, scale=cl2[:, c:c + 1]),
                 reads=[L["r"].b, cl2.b], writes=[L["a2"].b])
            s.op("dve", lambda e: e.tensor_scalar(L["a2"][:], L["a2"][:], -1.0, 1.0, ALU.mult, ALU.add),
                 reads=[L["a2"].b], writes=[L["a2"].b])
            s.op("act", lambda e: e.activation(L["a2"][:], L["a2"][:], AF.Sqrt), reads=[L["a2"].b], writes=[L["a2"].b])
            s.op("pool", lambda e: e.tensor_tensor(L["u"][:], L["gi"][:], xc[:], ALU.mult), reads=[L["gi"].b, xc.b], writes=[L["u"].b])
            s.op("dve", lambda e: e.tensor_tensor(L["u"][:], L["u"][:], L["a2"][:], ALU.mult), reads=[L["u"].b, L["a2"].b], writes=[L["u"].b])
            s.op("dve", lambda e, c=c: e.tensor_tensor_scan(L["h"][:], L["a"][:], L["u"][:], hcar[c][:], ALU.mult, ALU.add),
                 reads=[L["a"].b, L["u"].b, hcar[c].b], writes=[L["h"].b])
            s.op("dve", lambda e, c=c: e.tensor_tensor_scan(L["P"][:], L["a"][:], zeros[:], pcar[c][:], ALU.mult, ALU.add),
                 reads=[L["a"].b, zeros.b, pcar[c].b], writes=[L["P"].b])
            s.op("act", lambda e, c=c: e.copy(hcar[c][:], L["h"][:, TS - 1:TS]), reads=[L["h"].b], writes=[hcar[c].b])
            s.op("act", lambda e, c=c: e.copy(pcar[c][:], L["P"][:, TS - 1:TS]), reads=[L["P"].b], writes=[pcar[c].b])
            y1 = yo[cnt["yo"] % 4]
            y2 = yo[(cnt["yo"] + 1) % 4]
            cnt["yo"] += 2
            s.op("pool", lambda e, y1=y1: e.tensor_tensor(y1[:], L["h"][:], L["gg"][:], ALU.mult), reads=[L["h"].b, L["gg"].b], writes=[y1.b])
            s.op("pool", lambda e, y2=y2: e.tensor_tensor(y2[:], L["P"][:], L["gg"][:], ALU.mult), reads=[L["P"].b, L["gg"].b], writes=[y2.b])
            s.dma("sp", D["yloc"][c * 128:(c + 1) * 128, st * TS:(st + 1) * TS], y1[:], reads=[y1.b])
            s.dma("sp", D["pg"][c * 128:(c + 1) * 128, st * TS:(st + 1) * TS], y2[:], reads=[y2.b])
            if kb.bg:
                for _ in range(3):
                    kb.bg.step()
        for hq in range(8):
            bk = proj(h_, 1024 + hq * 64, 64, TS, allc)
            qk_norm_rope(bk, TS, 0, cst, allc, QR[:, hq, :], QR.b)
        for h in range(2):
            bk = proj(h_, 1536 + h * 64, 64, TS, allc)
            qk_norm_rope(bk, TS, 1, cst, allc, KR[:, h, 128:128 + TS], KR.b)
        vt = [vprev] + [v_tile(h_, slice(t * 128, (t + 1) * 128)) for t in range(4)]
        for b in range(4):
            mp = mprev0 if (st == 0 and b == 0) else mprev
            for h in range(2):
                pts = []
                for w_, (kc0, mk) in enumerate(((b * 128, mp), (128 + b * 128, mcur))):
                    bk = nbank(kb)
                    mm_group(kb, bk, bk[:, :].rearrange("p (g n) -> p g n", n=128),
                             [(KR[:, h, kc0:kc0 + 128], QR[:, 4 * h:4 * h + 4, b * 128:(b + 1) * 128])], reads=[KR.b, QR.b])
                    e_ = E_[cnt["e"] % 2]
                    cnt["e"] += 1
                    s.op("act", lambda e, e_=e_, bk=bk: e.activation(e_[:], bk[:, :], AF.Exp, scale=0.125), reads=[bk.b], writes=[e_.b])
                    p_ = Pt[cnt["p"] % 4]
                    cnt["p"] += 1
                    s.op("dve" if w_ == 0 else "pool", lambda e, p_=p_, e_=e_, mk=mk: e.tensor_tensor(
                        p_[:].rearrange("p (g n) -> p g n", n=128), e_[:].rearrange("p (g n) -> p g n", n=128),
                        bc(mk[:].unsqueeze(1), [128, 4, 128]), ALU.mult), reads=[e_.b, mk.b], writes=[p_.b])
                    pts.append(p_)
                bo = nbank(kb)
                for g in range(4):
                    mm_group(kb, bo, bo[:, g * 65:(g + 1) * 65],
                             [(pts[0][:, g * 128:(g + 1) * 128], vt[b][:, h, :]), (pts[1][:, g * 128:(g + 1) * 128], vt[b + 1][:, h, :])],
                             reads=[pts[0].b, pts[1].b, vt[b].b, vt[b + 1].b])
                bov = bo[:, 0:260].rearrange("p (g n) -> p g n", n=65)
                s.op("dve", lambda e, bov=bov, h=h: e.tensor_tensor(den[:], bov[:, :, 64], esink[:, 4 * h:4 * h + 4], ALU.add),
                     reads=[bo.b, esink.b], writes=[den.b])
                s.op("dve", lambda e: e.reciprocal(den[:], den[:]), reads=[den.b], writes=[den.b])
                s.op("dve", lambda e, bov=bov, h=h: e.tensor_tensor(Y[:, 4 * h:4 * h + 4, :], bov[:, :, 0:64],
                                                                    bc(den[:].unsqueeze(2), [128, 4, 64]), ALU.mult),
                     reads=[bo.b, den.b], writes=[Y.b])
            bt = nbank(kb)
            btv = bt[:, :].bitcast(BF16)
            Yf = Y[:].rearrange("p h d -> p (h d)")
            mm_group(kb, bt, [btv[:, j * 128:(j + 1) * 128] for j in range(4)],
                     [(Yf[:, j * 128:(j + 1) * 128], W["identb"][:]) for j in range(4)], reads=[Y.b, W["identb"].b], transpose=True)
            yt_ = yT[cnt["y"] % 2]
            cnt["y"] += 1
            s.op("act", lambda e, yt_=yt_, btv=btv: e.copy(yt_[:], btv[:, 0:512].rearrange("p (c n) -> p c n", n=128)),
                 reads=[bt.b], writes=[yt_.b])
            tok0 = st * TS + b * 128
            s.dma("sp", D["yatt"].rearrange("(c p) n -> p c n", p=128)[:, :, tok0:tok0 + 128], yt_[:], reads=[yt_.b])
        vprev = vt[4]
        s.op("pool", lambda e: e.tensor_copy(KR[:, :, 0:128], KR[:, :, TS:TS + 128]), reads=[KR.b], writes=[KR.b])
    seg = kb.sb([128, 8], F32, "seg")
    for c in range(4):
        s.op("act", lambda e, c=c: e.copy(seg[:, c:c + 1], pcar[c][:]), reads=[pcar[c].b], writes=[seg.b])
        s.op("act", lambda e, c=c: e.copy(seg[:, 4 + c:5 + c], hcar[c][:]), reads=[hcar[c].b], writes=[seg.b])
    s.dma("sp", D["seg"], seg[:], reads=[seg.b])
    if kb.bg:
        kb.bg.detach()


def phase2(kb, C, D):
    s = kb.s
    NT = C["NTOK"]
    TS = 512
    NCf = 22
    kb.D = D
    W = norm_scratch(kb)
    gffn = kb.sb([128, 8], F32, "gffn0")
    s.dma("sp", gffn[:], D["gffn0"], writes=[gffn.b])
    gmix = kb.sb([128, 8], F32, "gmix1")
    s.dma("sp", gmix[:], D["gmix1"], writes=[gmix.b])
    wo = kb.sb([128, 8, 1024], BF16, "wo0")
    s.dma("sp", wo[:], D["wo0"].rearrange("p (k n) -> p k n", n=1024), writes=[wo.b])
    sega = kb.sb([128, 4, 8], F32, "sega")
    s.dma("sp", sega[:], D["segall"].rearrange("(r p) n -> p r n", p=128), writes=[sega.b])
    sel = kb.sb([128, 4], F32, "sel")
    s.dma("sp", sel[:], D["sel"], writes=[sel.b])
    hin = kb.sb([128, 4], F32, "hin")
    tmp = kb.sb([128, 4], F32, "tmpc")
    s.op("pool", lambda e: e.memset(hin[:], 0.0), writes=[hin.b])
    for i in range(3):
        s.op("dve", lambda e, i=i: e.tensor_tensor(tmp[:], sega[:, i, 0:4], hin[:], ALU.mult), reads=[sega.b, hin.b], writes=[tmp.b])
        s.op("dve", lambda e, i=i: e.tensor_tensor(tmp[:], tmp[:], sega[:, i, 4:8], ALU.add), reads=[sega.b, tmp.b], writes=[tmp.b])
        s.op("dve", lambda e: e.tensor_tensor(tmp[:], tmp[:], hin[:], ALU.subtract), reads=[tmp.b, hin.b], writes=[tmp.b])
        s.op("dve", lambda e, i=i: e.scalar_tensor_tensor(hin[:], tmp[:], sel[:, i:i + 1], hin[:], ALU.mult, ALU.add),
             reads=[tmp.b, sel.b, hin.b], writes=[hin.b])
    r = ffn_alloc(kb, TS, NCf, nrot=2 if kb.bg else 3)
    hnT = [kb.sb([128, 8, TS], BF16, "hnT") for _ in range(2)]
    acc = kb.sb([128, TS // 128, 1024], F32, "acc")
    accb = [Buf() for _ in range(TS // 128)]
    xt = [kb.sb([128, 1024], F32, "xt") for _ in range(2)]
    yl = kb.sb([128, 4, TS], BF16, "yl")
    pg = kb.sb([128, 4, TS], BF16, "pgt")
    ya = kb.sb([128, 4, TS], BF16, "ya")
    ym = yl
    if kb.bg:
        kb.bg.attach(kb)
    hn1 = [kb.sb([128, 8, 128], BF16, "hn1") for _ in range(2)]
    it = 0
    for st in range(NT // TS):
        cols = slice(st * TS, (st + 1) * TS)
        s.dma("act", yl[:], D["yloc"].rearrange("(c p) n -> p c n", p=128)[:, :, cols], writes=[yl.b])
        s.dma("act", pg[:], D["pg"].rearrange("(c p) n -> p c n", p=128)[:, :, cols], writes=[pg.b])
        s.dma("act", ya[:], D["yatt"].rearrange("(c p) n -> p c n", p=128)[:, :, cols], writes=[ya.b])
        for c in range(4):
            s.op("dve", lambda e, c=c: e.scalar_tensor_tensor(ym[:, c, :], pg[:, c, :], hin[:, c:c + 1], yl[:, c, :], ALU.mult, ALU.add),
                 reads=[pg.b, hin.b, yl.b], writes=[ym.b])
        h_ = hnT[st % 2]
        for t in range(TS // 128):
            tok0 = st * TS + t * 128
            tc_ = slice(t * 128, (t + 1) * 128)
            x_ = xt[it % 2]
            it += 1
            s.dma("act", x_[:], D["x"][tok0:tok0 + 128, :], writes=[x_.b])
            for half in range(2):
                bk = nbank(kb)
                mm_group(kb, bk, bk[:, :], [((ym[:, c, tc_] if c < 4 else ya[:, c - 4, tc_]), wo[:, c, half * 512:(half + 1) * 512])
                                            for c in range(8)], reads=[ym.b, ya.b, wo.b])
                s.op("dve", lambda e, bk=bk, x_=x_, t=t, half=half: e.tensor_tensor(
                    acc[:, t, half * 512:(half + 1) * 512], x_[:, half * 512:(half + 1) * 512], bk[:, :], ALU.add),
                    reads=[bk.b, x_.b], writes=[accb[t]])
            norm_T(kb, W, acc[:, t, :], accb[t], gffn, h_, tc_)

        def evac(t, half, bk):
            s.op("dve", lambda e: e.tensor_tensor(acc[:, t, half * 512:(half + 1) * 512], acc[:, t, half * 512:(half + 1) * 512],
                                                  bk[:, :], ALU.add), reads=[bk.b, accb[t]], writes=[accb[t]])
        def bghook():
            if kb.bg:
                kb.bg.step()
                kb.bg.step()
        ffn_expert(kb, r, h_, D["fwg"], D["fwu"], D["fwd"], NCf, evac, hook=bghook)
        for t in range(TS // 128):
            tok0 = st * TS + t * 128
            s.dma("act", D["h1"][tok0:tok0 + 128, :], acc[:, t, :], reads=[accb[t]])
            o_ = hn1[t % 2]
            norm_T(kb, W, acc[:, t, :], accb[t], gmix, o_, slice(0, 128))
            s.dma("act", D["hn1T"].rearrange("(c p) n -> p c n", p=128)[:, :, tok0:tok0 + 128], o_[:], reads=[o_.b])
    if kb.bg:
        kb.bg.detach()


def phase3(kb, C, D):
    s = kb.s
    NTF, NTK = C["NTF"], C["NTK"]
    TS = 512
    NST = NTF // TS
    kb.D = D
    W = norm_scratch(kb)
    ident, identb = W["ident"], W["identb"]

    def ld(name, shape, dt=F32, src=None):
        t = kb.sb(shape, dt, name)
        s.dma("sp", t[:], D[name] if src is None else src, writes=[t.b])
        return t

    wqkv = kb.sb([128, 8, 768], BF16, "wqkv")
    s.dma("sp", wqkv[:], D["wqkv"].rearrange("p (k n) -> p k n", n=768), writes=[wqkv.b])
    wgate = kb.sb([128, 8, 260], BF16, "wgab")
    s.dma("sp", wgate[:, :, 0:256], D["wgate"].rearrange("p (k n) -> p k n", n=256), writes=[wgate.b])
    s.dma("sp", wgate[:, :, 256:260], D["wab"].rearrange("p (k n) -> p k n", n=4), writes=[wgate.b])
    abS = kb.sb([128, 4, 4], F32, "abS")
    convw = ld("convw", [128, 6, 4])
    hc = ld("hconst", [128, 4])
    onw = ld("onw_b", [128, 128])
    U = ld("maskU", [128, 128])
    Lo = ld("maskL", [128, 128])
    Bs = ld("maskB", [128, 128])
    C0 = ld("maskC0", [128, 128])
    C1 = ld("maskC1", [128, 128])
    nalog = kb.sb([128, 2], F32, "nalog")
    s.op("act", lambda e: e.activation(nalog[:], hc[:, 0:2], AF.Exp), reads=[hc.b], writes=[nalog.b])
    s.op("dve", lambda e: e.tensor_scalar(nalog[:], nalog[:], -1.0, None, ALU.mult), reads=[nalog.b], writes=[nalog.b])
    onesb = kb.sb([128, 128], BF16, "onesb")
    s.op("pool", lambda e: e.memset(onesb[:], 1.0), writes=[onesb.b])

    hnT = [kb.sb([128, 8, TS], BF16, "hnT") for _ in range(2)]
    cb = [[kb.sb([128, 3 + TS], F32, "cb") for _ in range(2)] for _ in range(6)]
    for b in range(6):
        s.op("pool", lambda e, b=b: e.memset(cb[b][0][:, 0:3], 0.0), writes=[cb[b][0].b])
    cacc = [kb.sb([128, TS], F32, "cacc") for _ in range(2)]
    csl = [kb.sb([128, TS], F32, "csl") for _ in range(2)]
    sqb = [kb.sb([128, TS], BF16, "sqb") for _ in range(2)]
    lnr = [kb.sb([128, TS], F32, "lnr") for _ in range(2)]
    FTa = [kb.sb([128, 6, TS], BF16, "FTa") for _ in range(2)]
    sgate = [[kb.sb([128, 256], F32, "sgate") for _ in range(4)] for _ in range(2)]
    sc = {k: kb.sb([128, 4, 2], F32, k) for k in ("xa", "gtm", "beta")}
    cs8 = [{k: kb.sb([128, 8], F32, k) for k in ("eG", "eGlG", "egl0", "egl1", "bEG", "dGl")} for _ in range(2)]
    S32 = [kb.sb([128, 128], F32, "S32") for _ in range(2)]
    Sbf = [kb.sb([128, 128], BF16, "Sbf") for _ in range(2)]
    for h in range(2):
        s.op("pool", lambda e, h=h: e.memset(S32[h][:], 0.0), writes=[S32[h].b])
        s.op("pool", lambda e, h=h: e.memset(Sbf[h][:], 0.0), writes=[Sbf[h].b])

    def mkset(bf_names, f_names, small=()):
        d = {}
        for k in bf_names:
            d[k] = kb.sb([128, 256] if k == "VK" else [128, 128], BF16, k)
        for k in f_names:
            d[k] = kb.sb([128, 128], F32, k)
        for k in small:
            d[k] = kb.sb([128, 1], F32, k)
        return d
    HO = [[[mkset(("wT", "kdec", "aqkT"), ("u",)) for _ in range(2)] for _ in range(4)] for _ in range(2)]
    PT = [[mkset(("VK", "Lb", "Nb", "P", "Q", "XL", "XN", "P2", "Q2", "XL2", "XN2", "wtok"), ("gL", "E", "ET", "t1", "t2"))
           for _ in range(2)] for _ in range(4)]
    STp = [mkset(("vn", "ogb"), ("av", "O", "on"), ("ss", "rt", "rstd")) for _ in range(2)]
    ogs = [kb.sb([128, 2, TS], BF16, "ogs") for _ in range(2)]

    def evc(en, out, src, reads, wbuf, scale=None):
        if en == "act":
            if scale is None:
                s.op("act", lambda e: e.copy(out, src), reads=reads, writes=[wbuf])
            else:
                s.op("act", lambda e: e.activation(out, src, AF.Copy, scale=scale), reads=reads, writes=[wbuf])
        else:
            if scale is None:
                s.op(en, lambda e: e.tensor_copy(out, src), reads=reads, writes=[wbuf])
            else:
                s.op(en, lambda e: e.tensor_scalar(out, src, scale, None, ALU.mult), reads=reads, writes=[wbuf])

    def pre_gen(st):
        par = st % 2
        h_ = hnT[par]
        F_ = FTa[par]
        c8 = cs8[par]
        tok0 = st * TS
        rk, col0 = tok0 // NTK, tok0 % NTK
        if C.get("chunked"):
            s.dma("act", h_[:], D["hng"].rearrange("(c r p) n -> p c r n", r=4, p=128)[:, :, rk, col0:col0 + TS], writes=[h_.b])
        else:
            s.dma("act", h_[:], D["hng"][rk * 1024:(rk + 1) * 1024, :].rearrange("(c p) n -> p c n", p=128)[:, :, col0:col0 + TS],
                  writes=[h_.b])
        for b in range(6):
            c_ = cb[b][par]
            cn = cb[b][(st + 1) % 2]
            ca, cl_, sq_, ln_ = cacc[b % 2], csl[b % 2], sqb[b % 2], lnr[b % 2]
            bk = nbank(kb)
            mm_group(kb, bk, bk[:, 0:TS], [(wqkv[:, k, b * 128:(b + 1) * 128], h_[:, k, :]) for k in range(8)], reads=[wqkv.b, h_.b])
            s.op("act", lambda e: e.copy(c_[:, 3:3 + TS], bk[:, 0:TS]), reads=[bk.b], writes=[c_.b])
            s.op("pool", lambda e: e.tensor_copy(cn[:, 0:3], c_[:, TS:TS + 3]), reads=[c_.b], writes=[cn.b])
            s.op("dve", lambda e: e.tensor_scalar(ca[:], c_[:, 0:TS], convw[:, b, 0:1], None, ALU.mult),
                 reads=[c_.b, convw.b], writes=[ca.b])
            for j in range(1, 4):
                s.op("dve", lambda e: e.scalar_tensor_tensor(ca[:], c_[:, j:j + TS], convw[:, b, j:j + 1], ca[:], ALU.mult, ALU.add),
                     reads=[c_.b, convw.b, ca.b], writes=[ca.b])
            yield
            if b >= 4:
                s.op("act", lambda e: e.activation(F_[:, b, :], ca[:], AF.Silu), reads=[ca.b], writes=[F_.b])
                continue
            s.op("act", lambda e: e.activation(cl_[:], ca[:], AF.Silu), reads=[ca.b], writes=[cl_.b])
            s.op("act", lambda e: e.activation(sq_[:], cl_[:], AF.Square), reads=[cl_.b], writes=[sq_.b])
            b2 = nbank(kb)
            mm_group(kb, b2, b2[:, 0:TS], [(onesb[:], sq_[:])], reads=[onesb.b, sq_.b])
            s.op("act", lambda e: e.activation(ln_[:], b2[:, 0:TS], AF.Ln, bias=kb.eps_t[:]), reads=[b2.b], writes=[ln_.b])
            s.op("act", lambda e: e.activation(ln_[:], ln_[:], AF.Exp, scale=-0.5), reads=[ln_.b], writes=[ln_.b])
            s.op("dve", lambda e: e.scalar_tensor_tensor(F_[:, b, :], cl_[:], (128.0 ** -0.5) if b in (0, 2) else 1.0, ln_[:], ALU.mult, ALU.mult),
                 reads=[cl_.b, ln_.b], writes=[F_.b])
            yield
        for t in range(4):
            tc_ = slice(t * 128, (t + 1) * 128)
            bk = nbank(kb)
            sg_ = sgate[par][t]
            mm_group(kb, bk, bk[:, 0:260], [(h_[:, k, tc_], wgate[:, k, :]) for k in range(8)], reads=[wgate.b, h_.b])
            s.op("act", lambda e: e.activation(sg_[:], bk[:, 0:256], AF.Silu), reads=[bk.b], writes=[sg_.b])
            s.op("act", lambda e: e.copy(abS[:, t, :], bk[:, 256:260]), reads=[bk.b], writes=[abS.b])
            yield
        abv = abS
        s.op("dve", lambda e: e.tensor_tensor(sc["xa"][:], abv[:, :, 0:2], bc(hc[:, 2:4].unsqueeze(1), [128, 4, 2]), ALU.add),
             reads=[abS.b, hc.b], writes=[sc["xa"].b])
        s.op("act", lambda e: e.activation(sc["beta"][:], abv[:, :, 2:4], AF.Sigmoid), reads=[abS.b], writes=[sc["beta"].b])
        s.op("act", lambda e: e.activation(sc["xa"][:], sc["xa"][:], AF.Exp), reads=[sc["xa"].b], writes=[sc["xa"].b])
        s.op("act", lambda e: e.activation(sc["xa"][:], sc["xa"][:], AF.Ln, bias=1.0), reads=[sc["xa"].b], writes=[sc["xa"].b])
        s.op("dve", lambda e: e.tensor_tensor(sc["gtm"][:], sc["xa"][:], bc(nalog[:].unsqueeze(1), [128, 4, 2]), ALU.mult),
             reads=[sc["xa"].b, nalog.b], writes=[sc["gtm"].b])
        g8 = sc["gtm"][:].rearrange("p t n -> p (t n)")
        b8 = sc["beta"][:].rearrange("p t n -> p (t n)")
        bcs = nbank(kb)
        for i, m in enumerate((U, Bs, C0, C1)):
            mm_group(kb, bcs, bcs[:, i * 8:(i + 1) * 8], [(m[:], g8)], reads=[m.b, sc["gtm"].b])
        s.op("act", lambda e: e.activation(c8["eG"][:], bcs[:, 0:8], AF.Exp), reads=[bcs.b], writes=[c8["eG"].b])
        s.op("act", lambda e: e.copy(c8["dGl"][:], bcs[:, 8:16]), reads=[bcs.b], writes=[c8["dGl"].b])
        s.op("act", lambda e: e.activation(c8["egl0"][:], bcs[:, 16:24], AF.Exp), reads=[bcs.b], writes=[c8["egl0"].b])
        s.op("act", lambda e: e.activation(c8["egl1"][:], bcs[:, 24:32], AF.Exp), reads=[bcs.b], writes=[c8["egl1"].b])
        s.op("dve", lambda e: e.tensor_tensor(c8["dGl"][:], c8["dGl"][:], bcs[:, 0:8], ALU.subtract), reads=[bcs.b, c8["dGl"].b], writes=[c8["dGl"].b])
        s.op("act", lambda e: e.activation(c8["eGlG"][:], c8["dGl"][:], AF.Exp), reads=[c8["dGl"].b], writes=[c8["eGlG"].b])
        s.op("dve", lambda e: e.tensor_tensor(c8["bEG"][:], c8["eG"][:], b8, ALU.mult), reads=[c8["eG"].b, sc["beta"].b], writes=[c8["bEG"].b])
        yield
        chains = [(t, h) for t in range(4) for h in range(2)]
        for (t, h) in chains:
            tc_ = slice(t * 128, (t + 1) * 128)
            col = slice(t * 2 + h, t * 2 + h + 1)
            d = PT[t][h]
            o = HO[par][t][h]
            bk = nbank(kb)
            bv = bk[:, :].bitcast(BF16)
            mm_group(kb, bk, [bv[:, 0:128], bv[:, 128:256]], [(F_[:, 2 * h + 1, tc_], identb[:]), (F_[:, 4 + h, tc_], identb[:])],
                     reads=[F_.b, identb.b], transpose=True)
            evc("act", d["VK"][:, 0:128], bv[:, 128:256], [bk.b, sc["beta"].b], d["VK"].b, scale=b8[:, col])
            evc("act", d["VK"][:, 128:256], bv[:, 0:128], [bk.b, c8["bEG"].b], d["VK"].b, scale=c8["bEG"][:, col])
            evc("act", o["kdec"][:], bv[:, 0:128], [bk.b, c8["eGlG"].b], o["kdec"].b, scale=c8["eGlG"][:, col])
            s.op("pool", lambda e: e.tensor_scalar(d["gL"][:], Lo[:], g8[:, col], None, ALU.mult),
                 reads=[Lo.b, sc["gtm"].b], writes=[d["gL"].b])
            bD = nbank(kb)
            mm_group(kb, bD, bD[:, 0:128], [(U[:], d["gL"][:])], reads=[U.b, d["gL"].b])
            mm_group(kb, bD, bD[:, 128:256], [(d["gL"][:], U[:])], reads=[U.b, d["gL"].b])
            s.op("act", lambda e: e.activation(d["E"][:], bD[:, 0:128], AF.Exp), reads=[bD.b], writes=[d["E"].b])
            s.op("act", lambda e: e.activation(d["ET"][:], bD[:, 128:256], AF.Exp), reads=[bD.b], writes=[d["ET"].b])
            bK = nbank(kb)
            mm_group(kb, bK, bK[:, 0:256].rearrange("p (a n) -> p a n", n=128), [(F_[:, 2 * h + 1, tc_], F_[:, 2 * h:2 * h + 2, tc_])], reads=[F_.b])
            s.op("dve", lambda e: e.scalar_tensor_tensor(d["t1"][:], bK[:, 128:256], b8[:, col], d["E"][:], ALU.mult, ALU.mult),
                 reads=[bK.b, sc["beta"].b, d["E"].b], writes=[d["t1"].b])
            s.op("dve", lambda e: e.tensor_tensor(d["t2"][:], bK[:, 0:128], d["ET"][:], ALU.mult),
                 reads=[bK.b, d["ET"].b], writes=[d["t2"].b])
            s.op("pool", lambda e: e.tensor_tensor(d["Lb"][:], d["t1"][:], Lo[:], ALU.mult), reads=[d["t1"].b, Lo.b], writes=[d["Lb"].b])
            s.op("pool", lambda e: e.tensor_tensor(o["aqkT"][:], d["t2"][:], U[:], ALU.mult), reads=[d["t2"].b, U.b], writes=[o["aqkT"].b])
            yield
        for (t, h) in chains:
            d = PT[t][h]
            bN = nbank(kb)
            bNv = bN[:, :].bitcast(BF16)
            mm_group(kb, bN, [bNv[:, 0:128]], [(d["Lb"][:], identb[:])], reads=[d["Lb"].b, identb.b], transpose=True)
            evc("act", d["Nb"][:], bNv[:, 0:128], [bN.b], d["Nb"].b)
            s.op("pool", lambda e: e.tensor_tensor(d["P"][:], identb[:], d["Nb"][:], ALU.subtract), reads=[identb.b, d["Nb"].b], writes=[d["P"].b])
        yield
        cur = {ch: dict(XL=PT[ch[0]][ch[1]]["Lb"], XN=PT[ch[0]][ch[1]]["Nb"], P=PT[ch[0]][ch[1]]["P"]) for ch in chains}
        for lvl in range(5):
            alt = (lvl % 2 == 0)
            last = (lvl == 4)
            for i_, ch in enumerate(chains):
                d = PT[ch[0]][ch[1]]
                c_ = cur[ch]
                nXL, nXN = (d["XL"], d["XN"]) if alt else (d["XL2"], d["XN2"])
                bq = nbank(kb)
                mm_group(kb, bq, bq[:, 0:128], [(c_["XN"][:], c_["XL"][:])], reads=[c_["XN"].b, c_["XL"].b])
                if not last:
                    mm_group(kb, bq, bq[:, 128:256], [(c_["XL"][:], c_["XN"][:])], reads=[c_["XN"].b, c_["XL"].b])
                en = "act" if i_ % 2 == 0 else "dve"
                evc(en, nXL[:], bq[:, 0:128], [bq.b], nXL.b)
                if not last:
                    evc(en, nXN[:], bq[:, 128:256], [bq.b], nXN.b)
                c_["XL"], c_["XN"] = nXL, nXN
                if i_ % 2 == 1:
                    yield
            for i_, ch in enumerate(chains):
                d = PT[ch[0]][ch[1]]
                c_ = cur[ch]
                nP = d["P2"] if alt else d["P"]
                Po = c_["P"]
                bp = nbank(kb)
                mm_group(kb, bp, bp[:, 0:128], [(c_["XL"][:], Po[:])], reads=[Po.b, c_["XL"].b])
                s.op("dve", lambda e: e.tensor_tensor(nP[:], Po[:], bp[:, 0:128], ALU.add), reads=[Po.b, bp.b], writes=[nP.b])
                c_["P"] = nP
                if i_ % 2 == 1:
                    yield
        for ch in chains:
            d = PT[ch[0]][ch[1]]
            o = HO[par][ch[0]][ch[1]]
            TT = cur[ch]["P"]
            bu = nbank(kb)
            mm_group(kb, bu, bu[:, 0:256], [(TT[:], d["VK"][:])], reads=[TT.b, d["VK"].b])
            evc("act", o["u"][:], bu[:, 0:128], [bu.b], o["u"].b)
            evc("act", d["wtok"][:], bu[:, 128:256], [bu.b], d["wtok"].b)
            bw = nbank(kb)
            bwv = bw[:, :].bitcast(BF16)
            mm_group(kb, bw, [bwv[:, 0:128]], [(d["wtok"][:], identb[:])], reads=[d["wtok"].b, identb.b], transpose=True)
            evc("act", o["wT"][:], bwv[:, 0:128], [bw.b], o["wT"].b)
            yield

    def scan_gen(st):
        par = st % 2
        F_ = FTa[par]
        c8 = cs8[par]
        og_ = ogs[par]
        tok0 = st * TS
        for t in range(4):
            tc_ = slice(t * 128, (t + 1) * 128)
            for ci in range(2):
                pr = slice(64 * ci, 64 * ci + 64)
                cc = slice(t * 128 + 64 * ci, t * 128 + 64 * ci + 64)
                egl = c8["egl0"] if ci == 0 else c8["egl1"]
                for h in range(2):
                    col = slice(t * 2 + h, t * 2 + h + 1)
                    o = HO[par][t][h]
                    d = STp[h]
                    bs = nbank(kb)
                    mm_group(kb, bs, bs[pr, 0:128], [(o["wT"][:, pr], Sbf[h][:])], reads=[o["wT"].b, Sbf[h].b])
                    mm_group(kb, bs, bs[pr, 128:256], [(F_[:, 2 * h, cc], Sbf[h][:])], reads=[F_.b, Sbf[h].b])
                    s.op("dve", lambda e: e.tensor_tensor(d["vn"][pr, :], o["u"][pr, :], bs[pr, 0:128], ALU.subtract),
                         reads=[o["u"].b, bs.b], writes=[d["vn"].b])
                    b2 = nbank(kb)
                    mm_group(kb, b2, b2[:, 0:128], [(o["kdec"][pr, :], d["vn"][pr, :])], reads=[o["kdec"].b, d["vn"].b])
                    mm_group(kb, b2, b2[pr, 128:256], [(o["aqkT"][pr, pr], d["vn"][pr, :])], reads=[o["aqkT"].b, d["vn"].b])
                    s.op("dve", lambda e: e.scalar_tensor_tensor(S32[h][:], S32[h][:], egl[:, col], b2[:, 0:128], ALU.mult, ALU.add),
                         reads=[S32[h].b, egl.b, b2.b], writes=[S32[h].b])
                    s.op("dve", lambda e: e.tensor_copy(Sbf[h][:], S32[h][:]), reads=[S32[h].b], writes=[Sbf[h].b])
                    s.op("dve", lambda e: e.tensor_copy(d["av"][pr, :], b2[pr, 128:256]), reads=[b2.b], writes=[d["av"].b])
                    s.op("dve", lambda e: e.scalar_tensor_tensor(d["O"][pr, :], bs[pr, 128:256], c8["eG"][pr, col], d["av"][pr, :], ALU.mult, ALU.add),
                         reads=[bs.b, c8["eG"].b, d["av"].b], writes=[d["O"].b])
                    yield
            for h in range(2):
                d = STp[h]
                sg_ = sgate[par][t]
                s.op("act", lambda e: e.activation(d["on"][:], d["O"][:], AF.Square, accum_out=d["ss"][:]), reads=[d["O"].b], writes=[d["on"].b, d["ss"].b])
                s.op("act", lambda e: e.activation(d["rt"][:], d["ss"][:], AF.Sqrt, bias=kb.eps_t[:], scale=1.0 / 128.0), reads=[d["ss"].b], writes=[d["rt"].b])
                s.op("dve", lambda e: e.reciprocal(d["rstd"][:], d["rt"][:]), reads=[d["rt"].b], writes=[d["rstd"].b])
                s.op("dve", lambda e: e.scalar_tensor_tensor(d["on"][:], d["O"][:], d["rstd"][:], onw[:], ALU.mult, ALU.mult),
                     reads=[d["O"].b, d["rstd"].b, onw.b], writes=[d["on"].b])
                s.op("pool", lambda e: e.tensor_tensor(d["ogb"][:], d["on"][:], sg_[:, h * 128:(h + 1) * 128], ALU.mult),
                     reads=[d["on"].b, sg_.b], writes=[d["ogb"].b])
                bo = nbank(kb)
                bov = bo[:, :].bitcast(BF16)
                mm_group(kb, bo, [bov[:, 0:128]], [(d["ogb"][:], identb[:])], reads=[d["ogb"].b, identb.b], transpose=True)
                evc("act", og_[:, h, tc_], bov[:, 0:128], [bo.b], og_.b)
                yield
        if C.get("chunked"):
            b_, c0_ = tok0 // 2048, tok0 % 2048
            s.dma("sp", D["ogt"][b_ * 256:(b_ + 1) * 256, :].rearrange("(h p) n -> p h n", p=128)[:, :, c0_:c0_ + TS], og_[:], reads=[og_.b])
        else:
            s.dma("sp", D["ogt"].rearrange("(h p) n -> p h n", p=128)[:, :, tok0:tok0 + TS], og_[:], reads=[og_.b])

    usebg = bool(kb.bg) and bool(C.get("bg"))
    if usebg:
        kb.bg.attach(kb)
    for _ in pre_gen(0):
        pass
    RATIO = C.get("ratio", 3)
    bgk = 0
    for st in range(NST):
        gs = scan_gen(st)
        gp = pre_gen(st + 1) if st + 1 < NST else iter(())
        alive_s = alive_p = True
        while alive_s or alive_p:
            if alive_s:
                try:
                    next(gs)
                except StopIteration:
                    alive_s = False
            for _ in range(RATIO):
                if alive_p:
                    try:
                        next(gp)
                    except StopIteration:
                        alive_p = False
            bgk += 1
            if usebg and bgk % 2 == 0:
                kb.bg.step()
    if usebg:
        kb.bg.detach()


def kmajor(W):
    K, N = W.shape
    return np.ascontiguousarray(W.reshape(K // 128, 128, N).transpose(1, 0, 2).reshape(128, -1))

def blocks(W, FB=256):
    K, F = W.shape
    NB = F // FB
    return np.ascontiguousarray(W.reshape(8, 128, NB, FB).transpose(1, 2, 0, 3).reshape(128, -1))

def pcol(v):
    return np.ascontiguousarray(v.reshape(-1, 128).T)

def prep_p1(inp, NT, nseg):
    x = inp["x"]
    B, S, _ = x.shape
    f32 = np.float32
    win = kmajor(inp["e_w_in"][0])
    lruc = np.zeros((128, 4, 8), f32)
    cw = inp["e_lru_conv_w"][0]
    for c in range(4):
        sl = slice(c * 128, (c + 1) * 128)
        for j in range(4):
            lruc[:, c, j] = cw[j, sl]
        lruc[:, c, 4] = inp["e_lru_conv_b"][0][sl]
        lruc[:, c, 5] = inp["e_lru_b_a"][0][sl]
        lruc[:, c, 6] = inp["e_lru_b_i"][0][sl]
        lruc[:, c, 7] = inp["e_lru_lambda"][0][sl]
    def bd(w):
        o = np.zeros((128, 4, 128), f32)
        for c in range(4):
            o[0:64, c, 0:64] = w[2 * c]
            o[64:128, c, 64:128] = w[2 * c + 1]
        return o
    prot = np.zeros((64, 64), f32)
    for m in range(32):
        prot[m + 32, m] = -1.0
        prot[m, m + 32] = 1.0
    k_ = np.arange(128)[:, None]; q_ = np.arange(128)[None, :]
    mcur = (k_ <= q_).astype(f32); mprev = (k_ > q_).astype(f32)
    half = 32
    inv_freq = (10000.0 ** (-np.arange(half, dtype=np.float32) / half)).astype(f32)
    common = dict(win32=win, lruc=lruc, wa_bd=bd(inp["e_lru_w_a"][0]), wi_bd=bd(inp["e_lru_w_i"][0]),
                  qkg=np.stack([inp["e_q_norm"][0], inp["e_k_norm"][0]], 1).astype(f32),
                  sinks_b=np.broadcast_to(inp["e_sinks"][0][None, :], (128, 8)).astype(f32).copy(),
                  prot=prot, mask_cur=mcur, mask_prev=mprev, gmix0=pcol(inp["ln_mix"][0]),
                  ident=np.eye(128, dtype=f32))
    outs = []
    for b in range(B):
        for j in range(nseg):
            t0 = j * NT
            d = dict(common)
            d["x"] = np.ascontiguousarray(x[b, t0:t0 + NT])
            d["xhalo"] = np.ascontiguousarray(x[b, t0 - 128:t0]) if j > 0 else np.zeros((128, 1024), f32)
            d["mask_prev0"] = mprev if j > 0 else np.zeros((128, 128), f32)
            pos = np.arange(t0 - 128, t0 + NT).astype(f32)
            ang = pos[None, :] * inv_freq[:, None]
            cs = np.zeros((64, 2, NT + 128), f32)
            cs[0:32, 0] = np.cos(ang); cs[32:64, 0] = np.cos(ang)
            cs[0:32, 1] = np.sin(ang); cs[32:64, 1] = np.sin(ang)
            d["cossin"] = cs
            outs.append(d)
    return outs


def gdn_masks():
    f32 = np.float32
    k = np.arange(128)[:, None]; i = np.arange(128)[None, :]
    same = (k // 64) == (i // 64)
    return dict(maskU=((k <= i) & same).astype(f32), maskL=((k > i) & same).astype(f32), maskB=same.astype(f32),
                maskC0=np.broadcast_to(k < 64, (128, 128)).astype(f32).copy(),
                maskC1=np.broadcast_to(k >= 64, (128, 128)).astype(f32).copy())


def prep_p3(inp, hp):
    f32 = np.float32
    w = inp["o_w_in"][0]
    hs = [2 * hp, 2 * hp + 1]
    cols = [off + h * 128 + np.arange(128) for (off, h) in ((0, hs[0]), (1024, hs[0]), (0, hs[1]), (1024, hs[1]), (2048, hs[0]), (2048, hs[1]))]
    wqkv = w[:, np.concatenate(cols)]
    wgate = w[:, np.concatenate([3072 + h * 128 + np.arange(128) for h in hs])]
    wab = w[:, [4096 + hs[0], 4096 + hs[1], 4104 + hs[0], 4104 + hs[1]]]
    cw = inp["o_conv_w"][0]
    convw = np.zeros((128, 6, 4), f32)
    for b, c in enumerate(cols):
        convw[:, b, :] = cw[:, c].T
    hconst = np.zeros((128, 4), f32)
    hconst[:, 0:2] = inp["o_a_log"][0][hs][None, :]
    hconst[:, 2:4] = inp["o_dt_bias"][0][hs][None, :]
    d = dict(wqkv32=kmajor(wqkv), wgate32=kmajor(wgate), wab32=kmajor(np.ascontiguousarray(wab)), convw=convw, hconst=hconst,
             onw_b=np.broadcast_to(inp["o_out_norm"][0][None, :], (128, 128)).astype(f32).copy(), ident=np.eye(128, dtype=f32))
    d.update(gdn_masks())
    return d


NT_CORE = 4096
NSEG = 4


def _mk_dram(kb, h, outs, scratch):
    D = {}
    for k, v in h.items():
        D[k] = kb.dram(k, list(v.shape), F32 if v.dtype == np.float32 else BF16, "ExternalInput")
    for k, (shape, dt) in scratch.items():
        D[k] = kb.dram(k, shape, dt)
    for k, (shape, dt) in outs.items():
        D[k] = kb.dram(k, shape, dt, "ExternalOutput")
    return D


def _build(h, outs, scratch, casts, phase, C):
    nc = bass.Bass("TRN2", target_bir_lowering=False)
    kb = KB(nc)
    D = _mk_dram(kb, h, outs, scratch)
    pairs = []
    for src, dst, rows in casts:
        for r0 in range(0, rows, 128):
            pairs.append((D[src][r0:r0 + 128, :], D[dst][r0:r0 + 128, :]))
    cast_dram(kb, pairs)
    kb.phase_end()
    phase(kb, C, D)
    kb.phase_end()
    kb.s.finish()
    return nc


def _run(nc, maps):
    res = run_bass_kernel_spmd(nc, maps, core_ids=list(range(len(maps))))
    return res.results


def kernel_unfused(**inputs):
    inp = {k: np.asarray(v) for k, v in inputs.items()}
    f32 = np.float32
    x = inp["x"]
    B, S, _ = x.shape
    NT = NT_CORE
    ncore = B * NSEG
    eye = np.eye(128, dtype=f32)
    hA = prep_p1(inp, NT, NSEG)
    ncA = _build(hA[0], dict(yloc=([512, NT], BF16), pg=([512, NT], BF16), yatt=([512, NT], BF16), seg=([128, 8], F32)),
                 dict(win=([128, 8 * 1792], BF16)), [("win32", "win", 128)], phase1, dict(NTOK=NT))
    rA = _run(ncA, hA)
    hB = []
    cB = dict(ident=eye, gffn0=pcol(inp["ln_ffn"][0]), gmix1=pcol(inp["ln_mix"][1]), wo032=kmajor(inp["e_w_out"][0]),
              fwg32=blocks(inp["e_ffn_w_gate"][0]), fwu32=blocks(inp["e_ffn_w_up"][0]), fwd32=kmajor(inp["e_ffn_w_down"][0]))
    for b in range(B):
        segall = np.concatenate([rA[b * NSEG + j]["seg"] for j in range(NSEG)], 0)
        for j in range(NSEG):
            c = b * NSEG + j
            sel = np.zeros((128, 4), f32)
            sel[:, :j] = 1.0
            d = dict(cB)
            d.update(x=hA[c]["x"], yloc=rA[c]["yloc"], pg=rA[c]["pg"], yatt=rA[c]["yatt"], segall=segall, sel=sel)
            hB.append(d)
    ncB = _build(hB[0], dict(h1=([NT, 1024], F32), hn1T=([1024, NT], BF16)),
                 dict(wo0=([128, 8192], BF16), fwg=([128, 22 * 1024], BF16), fwu=([128, 22 * 1024], BF16), fwd=([128, 22 * 1024], BF16)),
                 [("wo032", "wo0", 128), ("fwg32", "fwg", 128), ("fwu32", "fwu", 128), ("fwd32", "fwd", 128)], phase2, dict(NTOK=NT))
    rB = _run(ncB, hB)
    hC = []
    for b in range(B):
        hng = np.concatenate([rB[b * NSEG + j]["hn1T"] for j in range(NSEG)], 0)
        for hp_ in range(NSEG):
            d = prep_p3(inp, hp_)
            d["hng"] = hng
            hC.append(d)
    ncC = _build(hC[0], dict(ogt=([256, S], BF16)),
                 dict(wqkv=([128, 8 * 768], BF16), wgate=([128, 8 * 256], BF16), wab=([128, 32], BF16)),
                 [("wqkv32", "wqkv", 128), ("wgate32", "wgate", 128), ("wab32", "wab", 128)], phase3, dict(NTF=S, NTK=NT))
    rC = _run(ncC, hC)
    NE = inp["o_moe_w_gate"].shape[1]
    NCm = inp["o_moe_w_gate"].shape[3] // 128
    cD = dict(ident=eye, gffn1=pcol(inp["ln_ffn"][1]), router=kmajor(inp["o_router"][0]), wo1_32=kmajor(inp["o_w_out"][0]),
              mwg_32=np.concatenate([blocks(inp["o_moe_w_gate"][0][e]) for e in range(NE)], 0),
              mwu_32=np.concatenate([blocks(inp["o_moe_w_up"][0][e]) for e in range(NE)], 0),
              mwd_32=np.concatenate([kmajor(inp["o_moe_w_down"][0][e]) for e in range(NE)], 0))
    hD = []
    for b in range(B):
        ogt_full = np.concatenate([rC[b * NSEG + hp_]["ogt"] for hp_ in range(NSEG)], 0)
        for j in range(NSEG):
            c = b * NSEG + j
            d = dict(cD)
            d.update(ogt=np.ascontiguousarray(ogt_full[:, j * NT:(j + 1) * NT]), h1=rB[c]["h1"])
            hD.append(d)
    ncD = _build(hD[0], dict(out=([NT, 1024], F32)),
                 dict(wo1=([128, 8192], BF16), mwg=([NE * 128, NCm * 1024], BF16), mwu=([NE * 128, NCm * 1024], BF16),
                      mwd=([NE * 128, NCm * 1024], BF16)),
                 [("wo1_32", "wo1", 128), ("mwg_32", "mwg", NE * 128), ("mwu_32", "mwu", NE * 128), ("mwd_32", "mwd", NE * 128)],
                 phase4, dict(NTOK=NT, NEXP=NE, NC_MOE=NCm))
    rD = _run(ncD, hD)
    out = np.stack([np.concatenate([rD[b * NSEG + j]["out"] for j in range(NSEG)], 0) for b in range(B)], 0)
    return out.astype(f32)


def p4_consts(NSL):
    c = np.zeros((128, 64), np.float32)
    c[:, 0:8] = 512.0 * np.arange(8)[None, :]
    c[:, 8] = np.arange(128)
    c[:, 16:16 + NSL] = 512.0 * np.arange(NSL)[None, :]
    k = np.arange(128)[:, None]
    i = np.arange(128)[None, :]
    return c, (k < i).astype(np.float32)


def sparse_dram(kb, D, NT, NE, NC):
    nc = kb.nc
    NB = NC // 2
    NSL = 2 * NT // 512 + NE
    D["mwg_b"] = [nc.dram_tensor(f"mwg_b{b}", [NE * 128, 2048], BF16).ap() for b in range(NB)]
    D["mwu_b"] = [nc.dram_tensor(f"mwu_b{b}", [NE * 128, 2048], BF16).ap() for b in range(NB)]
    D["mwd_g"] = [nc.dram_tensor(f"mwd_g{b}", [NE * 128, 7 * 1024], BF16).ap() for b in range(NC // 7)]
    D["h2"] = nc.dram_tensor("h2", [NT, 1024], F32).ap()
    D["hnd"] = nc.dram_tensor("hnd", [NT, 1024], BF16).ap()
    D["xsl"] = nc.dram_tensor("xsl", [NSL * 512, 1028], BF16).ap()
    D["ysl"] = nc.dram_tensor("ysl", [NSL * 512, 1024], F32).ap()
    pairs = []
    for e in range(NE):
        rs = slice(e * 128, (e + 1) * 128)
        for b in range(NB):
            pairs.append((D["mwg_32"][rs, b * 2048:(b + 1) * 2048], D["mwg_b"][b][rs, :]))
            pairs.append((D["mwu_32"][rs, b * 2048:(b + 1) * 2048], D["mwu_b"][b][rs, :]))
        for b in range(NC // 7):
            pairs.append((D["mwd_32"][rs, b * 7168:(b + 1) * 7168], D["mwd_g"][b][rs, :]))
    return pairs


def kernel(**inputs):
    inp = {k: np.asarray(v) for k, v in inputs.items()}
    f32 = np.float32
    x = inp["x"]
    B, S, _ = x.shape
    NT = NT_CORE
    NE = inp["o_moe_w_gate"].shape[1]
    NCm = inp["o_moe_w_gate"].shape[3] // 128
    eye = np.eye(128, dtype=f32)
    hA = prep_p1(inp, NT, NSEG)
    common = dict(ident=eye, gffn0=pcol(inp["ln_ffn"][0]), gmix1=pcol(inp["ln_mix"][1]), wo032=kmajor(inp["e_w_out"][0]),
                  fwg32=blocks(inp["e_ffn_w_gate"][0]), fwu32=blocks(inp["e_ffn_w_up"][0]), fwd32=kmajor(inp["e_ffn_w_down"][0]),
                  gffn1=pcol(inp["ln_ffn"][1]), router=kmajor(inp["o_router"][0]), wo1_32=kmajor(inp["o_w_out"][0]),
                  mwg_32=np.concatenate([blocks(inp["o_moe_w_gate"][0][e]) for e in range(NE)], 0),
                  mwu_32=np.concatenate([blocks(inp["o_moe_w_up"][0][e]) for e in range(NE)], 0),
                  mwd_32=np.concatenate([kmajor(inp["o_moe_w_down"][0][e]) for e in range(NE)], 0))
    p3 = [prep_p3(inp, hp_) for hp_ in range(NSEG)]
    cst4, SU4 = p4_consts(2 * NT // 512 + NE)
    common.update(gffn1_b=np.broadcast_to(inp["ln_ffn"][1][None, :], (128, 1024)).astype(f32).copy(), p4cst=cst4, maskSU=SU4)
    maps = []
    for b in range(B):
        for j in range(NSEG):
            c = b * NSEG + j
            d = dict(common)
            d.update(hA[c])
            d.update(p3[j])
            sel = np.zeros((128, 4), f32)
            sel[:, :j] = 1.0
            sel4 = np.zeros((128, 4), f32)
            sel4[:, j] = 1.0
            d.update(sel=sel, sel4=sel4)
            maps.append(d)
    import os as _os
    KSTOP = int(_os.environ.get("KSTOP", "0"))
    if KSTOP:
        tiny = np.zeros((128, 8), f32)
        for d in maps:
            d.update(mwg_32=tiny, mwu_32=tiny, mwd_32=tiny)
    nc = bass.Bass("TRN2", target_bir_lowering=False)
    kb = KB(nc)
    bf = lambda n: ([128, n], BF16)
    scratch = dict(win=bf(8 * 1792), wo0=bf(8192), fwg=bf(22 * 1024), fwu=bf(22 * 1024), fwd=bf(22 * 1024),
                   wqkv=bf(8 * 768), wgate=bf(8 * 256), wab=bf(32), wo1=bf(8192),
                   yloc=([512, NT], BF16), pg=([512, NT], BF16), yatt=([512, NT], BF16), seg=([128, 8], F32),
                   h1=([NT, 1024], F32), hn1T=([1024, NT], BF16), ogt_own=([(S // 2048) * 256, 2048], BF16))
    D = _mk_dram(kb, maps[0], dict(out=([NT, 1024], F32)), scratch)
    for k, shape, dt in (("segall", [NSEG * 128, 8], F32), ("hng", [8 * NSEG * 128, NT], BF16), ("ogt_all", [(S // 2048) * NSEG * 256, 2048], BF16)):
        D[k] = nc.dram_tensor(k, shape, dt, addr_space="Local", kind="Internal").ap()
    groups = [list(range(b * NSEG, (b + 1) * NSEG)) for b in range(B)]
    casts = [("win32", "win", 128), ("wo032", "wo0", 128), ("fwg32", "fwg", 128), ("fwu32", "fwu", 128), ("fwd32", "fwd", 128),
             ("wqkv32", "wqkv", 128), ("wgate32", "wgate", 128), ("wab32", "wab", 128), ("wo1_32", "wo1", 128)]
    pairs = []
    for src_, dst_, rows in casts:
        for r0 in range(0, rows, 128):
            pairs.append((D[src_][r0:r0 + 128, :], D[dst_][r0:r0 + 128, :]))
    cast_dram(kb, pairs)
    kb.phase_end()
    if not KSTOP:
        kb.bg = BGCast(sparse_dram(kb, D, NT, NE, NCm))
    phase1(kb, dict(NTOK=NT), D)
    kb.phase_end()
    kb.s.collective("AllGather", D["seg"], D["segall"], groups)
    kb.phase_end()
    if KSTOP == 1:
        kb.s.finish()
        return _run(nc, maps)
    phase2(kb, dict(NTOK=NT), D)
    kb.phase_end()
    if KSTOP == 2:
        kb.s.finish()
        return _run(nc, maps)
    for c_ in range(8):
        kb.s.collective("AllGather", D["hn1T"][c_ * 128:(c_ + 1) * 128, :], D["hng"][c_ * 512:(c_ + 1) * 512, :], groups)
    kb.phase_end()
    if KSTOP == 3:
        kb.s.finish()
        return _run(nc, maps)
    D3 = dict(D)
    D3["ogt"] = D["ogt_own"]
    phase3(kb, dict(NTF=S, NTK=NT, chunked=True), D3)
    kb.phase_end()
    if KSTOP == 4:
        kb.s.finish()
        return _run(nc, maps)
    for b_ in range(S // 2048):
        kb.s.collective("AllGather", D["ogt_own"][b_ * 256:(b_ + 1) * 256, :], D["ogt_all"][b_ * 1024:(b_ + 1) * 1024, :], groups)
    kb.phase_end()
    if KSTOP == 5:
        kb.s.finish()
        return _run(nc, maps)
    D4 = dict(D)
    D4["ogt"] = D["ogt_all"]
    phase4s(kb, dict(NTOK=NT, NEXP=NE, NC_MOE=NCm, fused=True), D4)
    kb.phase_end()
    kb.s.finish()
    res = _run(nc, maps)
    out = np.stack([np.concatenate([res[b * NSEG + j]["out"] for j in range(NSEG)], 0) for b in range(B)], 0)
    return out.astype(f32)


I32 = mybir.dt.int32


def idma(kb, out, in_, idx_ap, scatter, reads=(), writes=()):
    s = kb.s
    E = s.E["pool"]
    deps = s._deps(reads, writes)
    slot = s.dsem["pool"][s.drr["pool"]]
    s.drr["pool"] = (s.drr["pool"] + 1) % NDS
    sem, val = slot
    if val > 0:
        deps.append((sem, val))
    s._wait(E, deps)
    slot[1] = val + 16
    off = bass.IndirectOffsetOnAxis(ap=idx_ap, axis=0)
    if scatter:
        kb.nc.gpsimd.indirect_dma_start(out=out, out_offset=off, in_=in_, in_offset=None).then_inc(sem, 16)
    else:
        kb.nc.gpsimd.indirect_dma_start(out=out, out_offset=None, in_=in_, in_offset=off).then_inc(sem, 16)
    s.nins += 1
    tok = (sem, val + 16)
    k = id(sem)
    for b in reads:
        b.r[k] = tok
    for b in writes:
        b.w = tok
        b.r = {}


def phase4s(kb, C, D):
    s = kb.s
    NT, NE, NCm = C["NTOK"], C["NEXP"], C["NC_MOE"]
    TS = 512
    NTT = NT // 128
    NSL = (2 * NT) // TS + NE
    NB = NCm // 2
    kb.D = D
    IDX1 = kb.sbp([128, NTT], I32, "IDX1")
    IDX2 = kb.sbp([128, NTT], I32, "IDX2")
    IDXW = kb.sbp([128, NSL], I32, "IDXW")
    W = norm_scratch(kb)
    ident, identb = W["ident"], W["identb"]
    g = kb.sb([128, 8], F32, "gffn")
    s.dma("sp", g[:], D["gffn1"], writes=[g.b])
    gtm = kb.sb([128, 1024], F32, "gtm")
    s.dma("sp", gtm[:], D["gffn1_b"], writes=[gtm.b])
    wo = kb.sb([128, 8, 1024], BF16, "wo")
    s.dma("sp", wo[:], D["wo1"].rearrange("p (k n) -> p k n", n=1024), writes=[wo.b])
    rtr = kb.sb([128, 8, 8], F32, "rtr")
    s.dma("sp", rtr[:], D["router"].rearrange("p (k n) -> p k n", n=8), writes=[rtr.b])
    SU32 = kb.sb([128, 128], F32, "SU32")
    s.dma("sp", SU32[:], D["maskSU"], writes=[SU32.b])
    SUb = kb.sb([128, 128], BF16, "SUb")
    s.op("dve", lambda e: e.tensor_copy(SUb[:], SU32[:]), reads=[SU32.b], writes=[SUb.b])
    onesb = kb.sb([128, 128], BF16, "onesb")
    s.op("pool", lambda e: e.memset(onesb[:], 1.0), writes=[onesb.b])
    cst = kb.sb([128, 64], F32, "p4cst")
    s.dma("sp", cst[:], D["p4cst"], writes=[cst.b])
    R1 = kb.sb([128, NTT, 8], F32, "R1")
    R2 = kb.sb([128, NTT, 8], F32, "R2")
    W12 = kb.sb([128, NTT, 2], F32, "W12")
    if C.get("fused"):
        ogc = [kb.sb([128, 4, 8, 128], BF16, "ogc") for _ in range(2)]
        sel4 = kb.sb([128, 4], F32, "sel4")
        s.dma("sp", sel4[:], D["sel4"], writes=[sel4.b])
    else:
        ogt_v = D["ogt"].rearrange("(c p) n -> p c n", p=128)
    og = [kb.sb([128, 8, 128], BF16, "og") for _ in range(2)]
    h1 = [kb.sb([128, 1024], F32, "h1") for _ in range(2)]
    acc = [kb.sb([128, 1024], F32, "acc") for _ in range(2)]
    xs2 = [kb.sb([128, 1024], F32, "xs") for _ in range(2)]
    junk2 = [kb.sb([128, 1024], BF16, "junk") for _ in range(2)]
    hn322 = [kb.sb([128, 8, 128], F32, "hn32") for _ in range(2)]
    HN = [kb.sb([128, 1028], BF16, "HN") for _ in range(2)]
    sm2 = [{k: kb.sb([128, 8], F32, k) for k in ("lg", "mx")} for _ in range(2)]
    sc2 = [{k: kb.sb([128, 1], F32, k) for k in ("ss", "rt", "rstd", "dd", "ex", "den")} for _ in range(2)]
    bh2, bhn, bxs, bys = Buf(), Buf(), Buf(), Buf()
    if kb.bg:
        kb.bg.attach(kb)
    for tt in range(NTT):
        if kb.bg:
            for _ in range(3):
                kb.bg.step()
        tok0 = tt * 128
        o_, h_, a_, hn_ = og[tt % 2], h1[tt % 2], acc[tt % 2], HN[tt % 2]
        xs, junk, hn32, sm, sc = xs2[tt % 2], junk2[tt % 2], hn322[tt % 2], sm2[tt % 2], sc2[tt % 2]
        if C.get("fused"):
            cd_ = ogc[tt % 2]
            for r_ in range(4):
                g_ = r_ * NT + tok0
                b_, c0_ = g_ // 2048, g_ % 2048
                s.dma("act", cd_[:, r_], D["ogt"][b_ * 1024:(b_ + 1) * 1024, :].rearrange("(c p) n -> p c n", p=128)[:, :, c0_:c0_ + 128],
                      writes=[cd_.b])
            s.op("dve", lambda e: e.tensor_scalar(o_[:], cd_[:, 0], sel4[:, 0:1], None, ALU.mult), reads=[cd_.b, sel4.b], writes=[o_.b])
            for r_ in range(1, 4):
                s.op("dve", lambda e: e.scalar_tensor_tensor(o_[:], cd_[:, r_], sel4[:, r_:r_ + 1], o_[:], ALU.mult, ALU.add),
                     reads=[cd_.b, sel4.b, o_.b], writes=[o_.b])
        else:
            s.dma("act", o_[:], ogt_v[:, :, tok0:tok0 + 128], writes=[o_.b])
        s.dma("act", h_[:], D["h1"][tok0:tok0 + 128, :], writes=[h_.b])
        for half in range(2):
            bk = nbank(kb)
            mm_group(kb, bk, bk[:, :], [(o_[:, c, :], wo[:, c, half * 512:(half + 1) * 512]) for c in range(8)], reads=[o_.b, wo.b])
            s.op("dve", lambda e: e.tensor_tensor(a_[:, half * 512:(half + 1) * 512], h_[:, half * 512:(half + 1) * 512], bk[:, :], ALU.add),
                 reads=[bk.b, h_.b], writes=[a_.b])
        s.dma("sp", D["h2"][tok0:tok0 + 128, :], a_[:], reads=[a_.b], writes=[bh2])
        rms_stats(kb, a_[:], a_.b, junk, sc["ss"], sc["rt"], sc["rstd"])
        s.op("act", lambda e: e.activation(xs[:], a_[:], AF.Copy, scale=sc["rstd"][:]), reads=[a_.b, sc["rstd"].b], writes=[xs.b])
        s.op("pool", lambda e: e.tensor_tensor(hn_[:, 0:1024], xs[:], gtm[:], ALU.mult), reads=[xs.b, gtm.b], writes=[hn_.b])
        s.dma("sp", D["hnd"][tok0:tok0 + 128, :], hn_[:, 0:1024], reads=[hn_.b], writes=[bhn])
        for hh in range(2):
            bk = nbank(kb)
            mm_group(kb, bk, [bk[:, j * 128:(j + 1) * 128] for j in range(4)],
                     [(xs[:, (hh * 4 + j) * 128:(hh * 4 + j + 1) * 128], ident[:]) for j in range(4)], reads=[xs.b, ident.b], transpose=True)
            s.op("dve", lambda e: e.tensor_tensor(hn32[:, hh * 4:(hh + 1) * 4, :], bk[:, :].rearrange("p (c n) -> p c n", n=128),
                                                   bc(g[:, hh * 4:(hh + 1) * 4].unsqueeze(2), [128, 4, 128]), ALU.mult),
                 reads=[bk.b, g.b], writes=[hn32.b])
        bk = nbank(kb)
        mm_group(kb, bk, bk[:, 0:8], [(hn32[:, c, :], rtr[:, c, :]) for c in range(8)], reads=[hn32.b, rtr.b])
        lg, mx = sm["lg"], sm["mx"]
        s.op("dve", lambda e: e.tensor_copy(lg[:], bk[:, 0:8]), reads=[bk.b], writes=[lg.b])
        s.op("dve", lambda e: e.max(mx[:], lg[:]), reads=[lg.b], writes=[mx.b])
        s.op("dve", lambda e: e.tensor_tensor(sc["dd"][:], mx[:, 1:2], mx[:, 0:1], ALU.subtract), reads=[mx.b], writes=[sc["dd"].b])
        s.op("act", lambda e: e.activation(sc["ex"][:], sc["dd"][:], AF.Exp), reads=[sc["dd"].b], writes=[sc["ex"].b])
        s.op("dve", lambda e: e.tensor_scalar(sc["den"][:], sc["ex"][:], 1.0, None, ALU.add), reads=[sc["ex"].b], writes=[sc["den"].b])
        s.op("dve", lambda e: e.reciprocal(W12[:, tt, 0:1], sc["den"][:]), reads=[sc["den"].b], writes=[W12.b])
        s.op("dve", lambda e: e.tensor_tensor(W12[:, tt, 1:2], sc["ex"][:], W12[:, tt, 0:1], ALU.mult), reads=[sc["ex"].b, W12.b], writes=[W12.b])
        s.op("dve", lambda e: e.tensor_scalar(R1[:, tt, :], lg[:], mx[:, 0:1], None, ALU.is_equal), reads=[lg.b, mx.b], writes=[R1.b])
        s.op("dve", lambda e: e.tensor_scalar(R2[:, tt, :], lg[:], mx[:, 1:2], None, ALU.is_equal), reads=[lg.b, mx.b], writes=[R2.b])
    if kb.bg:
        while kb.bg.loaded or kb.bg.remaining():
            kb.bg.step()
        kb.bg.detach()
    barrier(kb)
    NC8 = NTT * 8
    Rm = kb.sb([128, NC8], BF16, "Rm")
    s.op("dve", lambda e: e.tensor_tensor(Rm[:], R1[:].rearrange("p t e -> p (t e)"), R2[:].rearrange("p t e -> p (t e)"), ALU.add),
         reads=[R1.b, R2.b], writes=[Rm.b])
    bw_ = nbank(kb)
    mm_group(kb, bw_, bw_[:, 0:NC8], [(SUb[:], Rm[:])], reads=[SUb.b, Rm.b])
    bt_ = nbank(kb)
    mm_group(kb, bt_, bt_[:, 0:NC8], [(onesb[:], Rm[:])], reads=[onesb.b, Rm.b])
    totT = kb.sb([128, 8, NTT], F32, "totT")
    s.op("dve", lambda e: e.tensor_copy(totT[:], bt_[:, 0:NC8].rearrange("p (t e) -> p e t", e=8)), reads=[bt_.b], writes=[totT.b])
    ones32 = kb.sb([128, 64], F32, "ones32")
    s.op("pool", lambda e: e.memset(ones32[:], 1.0), writes=[ones32.b])
    cumI = kb.sb([128, 8, NTT], F32, "cumI")
    for e_ in range(8):
        s.op("dve", lambda e: e.tensor_tensor_scan(cumI[:, e_, :], ones32[:, 0:NTT], totT[:, e_, :], 0.0, ALU.mult, ALU.add),
             reads=[ones32.b, totT.b], writes=[cumI.b])
    toff = kb.sb([128, 8, NTT], F32, "toff")
    s.op("dve", lambda e: e.tensor_tensor(toff[:], cumI[:], totT[:], ALU.subtract), reads=[cumI.b, totT.b], writes=[toff.b])
    cnt = kb.sb([128, 8], F32, "cnt")
    s.op("dve", lambda e: e.tensor_copy(cnt[:], cumI[:, :, NTT - 1]), reads=[cumI.b], writes=[cnt.b])
    cmp = kb.sb([128, 8, 8], F32, "cmp")
    s.op("dve", lambda e: e.tensor_tensor(cmp[:], bc(cnt[:].unsqueeze(2), [128, 8, 8]), bc(cst[:, 0:8].unsqueeze(1), [128, 8, 8]), ALU.is_gt),
         reads=[cnt.b, cst.b], writes=[cmp.b])
    padded = kb.sb([128, 8], F32, "padded")
    s.op("dve", lambda e: e.tensor_reduce(padded[:], cmp[:], AX.X, ALU.add), reads=[cmp.b], writes=[padded.b])
    s.op("dve", lambda e: e.tensor_scalar(padded[:], padded[:], float(TS), None, ALU.mult), reads=[padded.b], writes=[padded.b])
    ends = kb.sb([128, 8], F32, "ends")
    s.op("dve", lambda e: e.tensor_tensor_scan(ends[:], ones32[:, 0:8], padded[:], 0.0, ALU.mult, ALU.add), reads=[ones32.b, padded.b], writes=[ends.b])
    base = kb.sb([128, 8], F32, "base")
    s.op("dve", lambda e: e.tensor_tensor(base[:], ends[:], padded[:], ALU.subtract), reads=[ends.b, padded.b], writes=[base.b])
    smat = kb.sb([128, NTT, 8], F32, "smat")
    s.op("dve", lambda e: e.tensor_tensor(smat[:], bw_[:, 0:NC8].rearrange("p (t e) -> p t e", e=8), toff[:].rearrange("p e t -> p t e"), ALU.add),
         reads=[bw_.b, toff.b], writes=[smat.b])
    s.op("dve", lambda e: e.tensor_tensor(smat[:], smat[:], bc(base[:].unsqueeze(1), [128, NTT, 8]), ALU.add), reads=[smat.b, base.b], writes=[smat.b])
    tmp3 = kb.sb([128, NTT, 8], F32, "tmp3")
    sl = kb.sb([128, NTT], F32, "sl")
    for Rx, IDX in ((R1, IDX1), (R2, IDX2)):
        s.op("dve", lambda e: e.tensor_tensor(tmp3[:], smat[:], Rx[:], ALU.mult), reads=[smat.b, Rx.b], writes=[tmp3.b])
        s.op("dve", lambda e: e.tensor_reduce(sl[:], tmp3[:], AX.X, ALU.add), reads=[tmp3.b], writes=[sl.b])
        s.op("dve", lambda e: e.tensor_copy(IDX[:], sl[:]), reads=[sl.b], writes=[IDX.b])
    cw = kb.sb([128, NSL, 8], F32, "cw")
    s.op("dve", lambda e: e.tensor_tensor(cw[:], bc(ends[:].unsqueeze(1), [128, NSL, 8]), bc(cst[:, 16:16 + NSL].unsqueeze(2), [128, NSL, 8]), ALU.is_le),
         reads=[ends.b, cst.b], writes=[cw.b])
    ew = kb.sb([128, NSL], F32, "ew")
    s.op("dve", lambda e: e.tensor_reduce(ew[:], cw[:], AX.X, ALU.add), reads=[cw.b], writes=[ew.b])
    s.op("dve", lambda e: e.tensor_scalar(ew[:], ew[:], float(NE - 1), 128.0, ALU.min, ALU.mult), reads=[ew.b], writes=[ew.b])
    s.op("dve", lambda e: e.tensor_scalar(ew[:], ew[:], cst[:, 8:9], None, ALU.add), reads=[ew.b, cst.b], writes=[ew.b])
    s.op("dve", lambda e: e.tensor_copy(IDXW[:], ew[:]), reads=[ew.b], writes=[IDXW.b])
    zt = kb.sb([128, 1028], BF16, "zt")
    s.op("pool", lambda e: e.memset(zt[:], 0.0), writes=[zt.b])
    for r0 in range(0, NSL * TS, 128):
        s.dma("sp" if (r0 // 128) % 2 == 0 else "act", D["xsl"][r0:r0 + 128, :], zt[:], reads=[zt.b], writes=[bxs])
    barrier(kb)
    for tt in range(NTT):
        tok0 = tt * 128
        hn_ = HN[tt % 2]
        s.dma("sp", hn_[:, 0:1024], D["hnd"][tok0:tok0 + 128, :], reads=[bhn], writes=[hn_.b])
        for k_, IDX in ((0, IDX1), (1, IDX2)):
            s.op("dve", lambda e: e.tensor_copy(hn_[:, 1024:1026].bitcast(F32), W12[:, tt, k_:k_ + 1]), reads=[W12.b], writes=[hn_.b])
            idma(kb, D["xsl"][:, :], hn_[:, :], IDX[:, tt:tt + 1], True, reads=[hn_.b, IDX.b], writes=[bxs])
    kb.phase_end()
    W = norm_scratch(kb)
    identb = W["identb"]
    r = ffn_alloc(kb, TS, NCm)
    hnT = [kb.sb([128, 8, TS], BF16, "hnT") for _ in range(2)]
    xt = [kb.sb([128, 1028], BF16, "xslt") for _ in range(3)]
    GW = [kb.sb([128, 4], F32, "GW") for _ in range(2)]
    YS = kb.sb([128, 4, 1024], F32, "YS")
    ix = 0
    for w in range(NSL):
        hT_ = hnT[w % 2]
        gw_ = GW[w % 2]
        for sub in range(4):
            x_ = xt[ix % 3]
            ix += 1
            r0 = w * TS + sub * 128
            s.dma("sp", x_[:], D["xsl"][r0:r0 + 128, :], reads=[bxs], writes=[x_.b])
            s.op("pool", lambda e: e.tensor_copy(gw_[:, sub:sub + 1], x_[:, 1024:1026].bitcast(F32)), reads=[x_.b], writes=[gw_.b])
            bk = nbank(kb)
            bv = bk[:, :].bitcast(BF16)
            mm_group(kb, bk, [bv[:, j * 128:(j + 1) * 128] for j in range(8)],
                     [(x_[:, j * 128:(j + 1) * 128], identb[:]) for j in range(8)], reads=[x_.b, identb.b], transpose=True)
            s.op("act", lambda e: e.copy(hT_[:, :, sub * 128:(sub + 1) * 128], bv.rearrange("p (c n) -> p c n", n=128)), reads=[bk.b], writes=[hT_.b])
        iw = IDXW[:, w:w + 1]

        def evac(t, half, bk):
            s.op("dve", lambda e: e.tensor_scalar(YS[:, t, half * 512:(half + 1) * 512], bk[:, :], gw_[:, t:t + 1], None, ALU.mult),
                 reads=[bk.b, gw_.b], writes=[YS.b])

        def lw(kind, b, tile):
            if kind == "d":
                idma(kb, tile, D["mwd_g"][b][:, :], iw, False, reads=[IDXW.b], writes=[r.wd.b])
            else:
                idma(kb, tile[:].rearrange("p k n -> p (k n)"), D["mwg_b" if kind == "g" else "mwu_b"][b][:, :], iw, False,
                     reads=[IDXW.b], writes=[tile.b])
        ffn_expert(kb, r, hT_, None, None, None, NCm, evac, loader=lw)
        for t in range(4):
            r0 = w * TS + t * 128
            s.dma("act", D["ysl"][r0:r0 + 128, :], YS[:, t, :], reads=[YS.b], writes=[bys])
    kb.phase_end()
    acc = [kb.sb([128, 1024], F32, "acc") for _ in range(2)]
    y1 = [kb.sb([128, 1024], F32, "y1") for _ in range(2)]
    y2 = [kb.sb([128, 1024], F32, "y2") for _ in range(2)]
    for tt in range(NTT):
        tok0 = tt * 128
        a_, y1_, y2_ = acc[tt % 2], y1[tt % 2], y2[tt % 2]
        s.dma("sp", a_[:], D["h2"][tok0:tok0 + 128, :], reads=[bh2], writes=[a_.b])
        idma(kb, y1_[:, :], D["ysl"][:, :], IDX1[:, tt:tt + 1], False, reads=[bys, IDX1.b], writes=[y1_.b])
        idma(kb, y2_[:, :], D["ysl"][:, :], IDX2[:, tt:tt + 1], False, reads=[bys, IDX2.b], writes=[y2_.b])
        s.op("dve", lambda e: e.tensor_tensor(a_[:], a_[:], y1_[:], ALU.add), reads=[a_.b, y1_.b], writes=[a_.b])
        s.op("dve", lambda e: e.tensor_tensor(a_[:], a_[:], y2_[:], ALU.add), reads=[a_.b, y2_.b], writes=[a_.b])
        s.dma("act", D["out"][tok0:tok0 + 128, :], a_[:], reads=[a_.b])
```

```python
import numpy as np
from contextlib import ExitStack
import concourse.bass as bass
import concourse.mybir as mybir
from concourse.bass_utils import run_bass_kernel_spmd

F32 = mybir.dt.float32
BF16 = mybir.dt.bfloat16
AF = mybir.ActivationFunctionType
ALU = mybir.AluOpType
AX = mybir.AxisListType
EPS = 1e-6
NDS = 20


class Buf:
    __slots__ = ("w", "r", "name", "excl")

    def __init__(self, name="", excl=False):
        self.w = None
        self.r = {}
        self.name = name
        self.excl = excl


class Sched:
    def __init__(self, nc):
        self.nc = nc
        self.E = {}
        for nm, eng in (("pe", nc.tensor), ("dve", nc.vector), ("act", nc.scalar),
                        ("pool", nc.gpsimd), ("sp", nc.sync)):
            self.E[nm] = dict(eng=eng, sem=nc.alloc_semaphore("s_" + nm), cnt=0, known={}, name=nm)
        self.dsem = {q: [[nc.alloc_semaphore(f"d_{q}{i}"), 0] for i in range(NDS)]
                     for q in ("sp", "act", "pool")}
        self.drr = {q: 0 for q in self.dsem}
        self.nins = 0
        self.ccsem = nc.alloc_semaphore("s_cc")
        self.ccval = 0
        self.extra = []

    def collective(self, kind, in_ap, out_ap, groups):
        self.ccval += 1
        self.nc.gpsimd.collective_compute(kind, ALU.bypass, replica_groups=groups, ins=[in_ap], outs=[out_ap]).then_inc(self.ccsem)
        self.extra.append((self.ccsem, self.ccval))
        self.nins += 1

    def _deps(self, reads, writes):
        deps = []
        for b in reads:
            if b.w is not None:
                deps.append(b.w)
            if b.excl:
                deps.extend(b.r.values())
        for b in writes:
            if b.w is not None:
                deps.append(b.w)
            deps.extend(b.r.values())
        return deps

    def _wait(self, E, deps):
        for (sem, val) in deps:
            k = id(sem)
            if E["known"].get(k, 0) >= val:
                continue
            E["eng"].wait_ge(sem, val)
            E["known"][k] = val

    def op(self, en, fn, reads=(), writes=(), inc=True):
        E = self.E[en]
        deps = self._deps(reads, writes)
        if en == "pe":
            deps = [d for d in deps if d[0] is not E["sem"]]
        self._wait(E, deps)
        ins = fn(E["eng"])
        self.nins += 1
        if inc:
            E["cnt"] += 1
            ins.then_inc(E["sem"], 1)
            tok = (E["sem"], E["cnt"])
        else:
            tok = (E["sem"], E["cnt"] + 1)
        k = id(E["sem"])
        for b in reads:
            b.r[k] = tok
        for b in writes:
            b.w = tok
            b.r = {}
        return tok

    def dma(self, q, out, in_, reads=(), writes=(), **kw):
        E = self.E[q]
        deps = self._deps(reads, writes)
        slot = self.dsem[q][self.drr[q]]
        self.drr[q] = (self.drr[q] + 1) % NDS
        sem, val = slot
        if val > 0:
            deps.append((sem, val))
        self._wait(E, deps)
        slot[1] = val + 16
        E["eng"].dma_start(out=out, in_=in_, **kw).then_inc(sem, 16)
        self.nins += 1
        tok = (sem, val + 16)
        k = id(sem)
        for b in reads:
            b.r[k] = tok
        for b in writes:
            b.w = tok
            b.r = {}
        return tok

    def finish(self):
        for q, slots in self.dsem.items():
            E = self.E[q]
            self._wait(E, [(s, v) for s, v in slots if v > 0])


class BGCast:
    def __init__(self, pairs, CH=1024, NBUF=2):
        self.work = []
        for src, dst in pairs:
            M = src.shape[1]
            for c0 in range(0, M, CH):
                self.work.append((src, dst, c0, min(CH, M - c0)))
        self.CH, self.NBUF = CH, NBUF
        self.i = 0
        self.loaded = []
        self.kb = None

    def attach(self, kb):
        self.kb = kb
        self.st32 = [kb.sb([128, self.CH], F32, "bg32") for _ in range(self.NBUF)]
        self.st16 = [kb.sb([128, self.CH], BF16, "bg16") for _ in range(self.NBUF)]
        self._prefetch()

    def _prefetch(self):
        s = self.kb.s
        for si in range(self.NBUF):
            if self.i >= len(self.work):
                break
            src, dst, c0, w = self.work[self.i]
            a = self.st32[si]
            s.dma("sp", a[:, 0:w], src[:, c0:c0 + w], writes=[a.b])
            self.loaded.append((self.i, si))
            self.i += 1

    def _process(self):
        s = self.kb.s
        for wi, si in self.loaded:
            src, dst, c0, w = self.work[wi]
            a, b = self.st32[si], self.st16[si]
            s.op("pool", lambda e: e.tensor_copy(b[:, 0:w], a[:, 0:w]), reads=[a.b], writes=[b.b])
            s.dma("pool", dst[:, c0:c0 + w], b[:, 0:w], reads=[b.b])
        self.loaded = []

    def step(self):
        if self.kb is None:
            return
        self._process()
        self._prefetch()

    def detach(self):
        if self.kb is None:
            return
        self._process()
        self.kb = None

    def remaining(self):
        return self.work[self.i:]


class T:
    def __init__(self, t, nbuf=1, name=""):
        self.t = t
        self.b = Buf(name)

    def __getitem__(self, idx):
        return self.t[idx]


class KB:
    def __init__(self, nc):
        self.nc = nc
        self.s = Sched(nc)
        self.n = 0
        self.bg = None
        self.stack = ExitStack()
        self.banks = [self.psum(f"bank{i}") for i in range(8)]

    def sb(self, shape, dt, name=None):
        self.n += 1
        name = (name or "t") + f"_{self.n}"
        return T(self.stack.enter_context(self.nc.sbuf_tensor(name, list(shape), dt)), name=name)

    def sbp(self, shape, dt, name):
        self.n += 1
        return T(self.nc.alloc_sbuf_tensor(name + f"_{self.n}", list(shape), dt), name=name)

    def phase_end(self):
        barrier(self)
        self.stack.close()
        self.stack = ExitStack()

    def psum(self, name):
        t = T(self.nc.alloc_psum_tensor(name, [128, 512], F32), name=name)
        t.b.excl = True
        return t

    def dram(self, name, shape, dt, kind=None):
        if kind:
            return self.nc.dram_tensor(name, list(shape), dt, kind=kind).ap()
        return self.nc.dram_tensor(name, list(shape), dt).ap()


def bc(ap, shape):
    return ap.to_broadcast(list(shape))


def barrier(kb):
    s = kb.s
    toks = [(E["sem"], E["cnt"]) for E in s.E.values() if E["cnt"] > 0]
    for q, slots in s.dsem.items():
        toks += [(sm, v) for sm, v in slots if v > 0]
    toks += s.extra
    for E in s.E.values():
        s._wait(E, [t for t in toks if t[0] is not E["sem"]])


def mm_group(kb, bank, out_ap, pairs, reads, transpose=False):
    n = len(pairs)
    for i, (l, r) in enumerate(pairs):
        if transpose:
            fn = (lambda e, l=l, r=r, o=out_ap[i]: e.transpose(o, l, r))
        else:
            fn = (lambda e, l=l, r=r, i=i: e.matmul(out_ap, l, r, start=(i == 0), stop=(i == n - 1)))
        kb.s.op("pe", fn, reads=reads, writes=[bank.b], inc=(i == n - 1))


def cast_dram(kb, pairs, CH=4096):
    s = kb.s
    NB_ = 4
    st32 = [kb.sb([128, CH], F32) for _ in range(NB_)]
    st16 = [kb.sb([128, CH], BF16) for _ in range(NB_)]
    work = []
    for src, dst in pairs:
        M = src.shape[1]
        for c0 in range(0, M, CH):
            work.append((src, dst, c0, min(CH, M - c0)))
    engs = ["pool", "act", "dve"]

    def load(i):
        src, dst, c0, w = work[i]
        a = st32[i % NB_]
        s.dma("sp", a[:, 0:w], src[:, c0:c0 + w], writes=[a.b])

    for i in range(min(2, len(work))):
        load(i)
    for i in range(len(work)):
        if i + 2 < len(work):
            load(i + 2)
        src, dst, c0, w = work[i]
        a = st32[i % NB_]
        b = st16[i % NB_]
        en = engs[i % 3]
        if en == "act":
            s.op("act", lambda e, a=a, b=b, w=w: e.copy(b[:, 0:w], a[:, 0:w]), reads=[a.b], writes=[b.b])
        else:
            s.op(en, lambda e, a=a, b=b, w=w: e.tensor_copy(b[:, 0:w], a[:, 0:w]), reads=[a.b], writes=[b.b])
        s.dma("sp", dst[:, c0:c0 + w], b[:, 0:w], reads=[b.b])


def rms_stats(kb, x_ap, xbuf, junk, ss, rt, rstd):
    s = kb.s
    s.op("act", lambda e: e.activation(junk[:], x_ap, AF.Square, accum_out=ss[:]), reads=[xbuf], writes=[junk.b, ss.b])
    s.op("act", lambda e: e.activation(rt[:], ss[:], AF.Sqrt, bias=kb.eps_t[:], scale=1.0 / 1024.0), reads=[ss.b], writes=[rt.b])
    s.op("dve", lambda e: e.reciprocal(rstd[:], rt[:]), reads=[rt.b], writes=[rstd.b])


class FFNRes:
    pass


def ffn_alloc(kb, TS, NC, nrot=3):
    r = FFNRes()
    r.TS = TS
    r.nrot = nrot
    r.wg = [kb.sb([128, 8, 256], BF16, "wg") for _ in range(nrot)]
    r.wu = [kb.sb([128, 8, 256], BF16, "wu") for _ in range(nrot)]
    r.wd = kb.sb([128, NC, 1024], BF16, "wd")
    r.hT = kb.sb([128, NC, TS], BF16, "hT")
    r.sg = [kb.sb([128, TS], F32, "sg") for _ in range(2)]
    r.blk = 0
    r.ch = 0
    r.dn = 0
    return r


def ffn_expert(kb, r, hnT, wg_d, wu_d, wd_d, NC, evac, loader=None, hook=None):
    s = kb.s
    TS = r.TS
    NB = NC // 2
    for g0 in range(0, NC, 7):
        g1 = min(NC, g0 + 7)
        if loader is not None:
            loader("d", g0 // 7, r.wd[:, g0:g1, :].rearrange("p c n -> p (c n)"))
            continue
        s.dma("act", r.wd[:, g0:g1, :], wd_d[:, g0 * 1024:g1 * 1024].rearrange("p (c n) -> p c n", n=1024),
              writes=[r.wd.b])
    for b in range(NB):
        wg = r.wg[r.blk % r.nrot]
        wu = r.wu[r.blk % r.nrot]
        r.blk += 1
        if hook is not None:
            hook()
        if loader is not None:
            loader("g", b, wg)
            loader("u", b, wu)
        else:
            s.dma("sp", wg[:], wg_d[:, b * 2048:(b + 1) * 2048].rearrange("p (k n) -> p k n", n=256), writes=[wg.b])
            s.dma("sp", wu[:], wu_d[:, b * 2048:(b + 1) * 2048].rearrange("p (k n) -> p k n", n=256), writes=[wu.b])
        for c in range(2):
            ch = b * 2 + c
            bg = kb.banks[(2 * r.ch) % 4]
            bu = kb.banks[(2 * r.ch + 1) % 4]
            sg = r.sg[r.ch % 2]
            r.ch += 1
            mm_group(kb, bg, bg[:, 0:TS], [(wg[:, k, c * 128:(c + 1) * 128], hnT[:, k, :]) for k in range(8)],
                     reads=[wg.b, hnT.b])
            mm_group(kb, bu, bu[:, 0:TS], [(wu[:, k, c * 128:(c + 1) * 128], hnT[:, k, :]) for k in range(8)],
                     reads=[wu.b, hnT.b])
            s.op("act", lambda e, sg=sg, bg=bg: e.activation(sg[:], bg[:, 0:TS], AF.Silu), reads=[bg.b], writes=[sg.b])
            s.op("dve", lambda e, sg=sg, bu=bu, ch=ch: e.tensor_tensor(r.hT[:, ch, :], sg[:], bu[:, 0:TS], ALU.mult),
                 reads=[sg.b, bu.b], writes=[r.hT.b])
    for t in range(TS // 128):
        for half in range(2):
            bk = kb.banks[4 + (r.dn % 4)]
            r.dn += 1
            mm_group(kb, bk, bk[:, :], [(r.hT[:, ch, t * 128:(t + 1) * 128], r.wd[:, ch, half * 512:(half + 1) * 512])
                                        for ch in range(NC)], reads=[r.hT.b, r.wd.b])
            evac(t, half, bk)


def phase4(kb, C, D):
    s = kb.s
    NT, NE, NCm = C["NTOK"], C["NEXP"], C["NC_MOE"]
    TS = 512
    ident = kb.sb([128, 128], F32, "ident")
    s.dma("sp", ident[:], D["ident"], writes=[ident.b])
    kb.eps_t = kb.sb([128, 1], F32, "eps")
    s.op("pool", lambda e: e.memset(kb.eps_t[:], EPS), writes=[kb.eps_t.b])
    g = kb.sb([128, 8], F32, "gffn")
    s.dma("sp", g[:], D["gffn1"], writes=[g.b])
    wo = kb.sb([128, 8, 1024], BF16, "wo")
    s.dma("sp", wo[:], D["wo1"].rearrange("p (k n) -> p k n", n=1024), writes=[wo.b])
    rtr = kb.sb([128, 8, 8], F32, "rtr")
    s.dma("sp", rtr[:], D["router"].rearrange("p (k n) -> p k n", n=8), writes=[rtr.b])
    r = ffn_alloc(kb, TS, NCm)
    hnT = [kb.sb([128, 8, TS], BF16, "hnT") for _ in range(2)]
    acc = kb.sb([128, TS // 128, 1024], F32, "acc")
    accb = [Buf() for _ in range(TS // 128)]
    gw = kb.sb([128, TS // 128, 8], F32, "gw")
    og = [kb.sb([128, 8, 128], BF16, "og") for _ in range(2)]
    h1 = [kb.sb([128, 1024], F32, "h1") for _ in range(2)]
    xs = kb.sb([128, 1024], F32, "xs")
    junk = kb.sb([128, 1024], BF16, "junk")
    hn32 = kb.sb([128, 8, 128], F32, "hn32")
    sm = {k: kb.sb([128, 8], F32, k) for k in ("lg", "mx", "g1", "g2")}
    sc = {k: kb.sb([128, 1], F32, k) for k in ("ss", "rt", "rstd", "dd", "ex", "den", "w1", "w2")}
    ogt_v = None if C.get("fused") else D["ogt"].rearrange("(c p) n -> p c n", p=128)
    if C.get("fused"):
        ogc = [kb.sb([128, 4, 8, 128], BF16, "ogc") for _ in range(2)]
        sel4 = kb.sb([128, 4], F32, "sel4")
        s.dma("sp", sel4[:], D["sel4"], writes=[sel4.b])
    it = 0
    for st in range(NT // TS):
        hT_ = hnT[st % 2]
        for t in range(TS // 128):
            tok0 = st * TS + t * 128
            o_ = og[it % 2]
            h_ = h1[it % 2]
            it += 1
            if C.get("fused"):
                cd_ = ogc[it % 2]
                for r_ in range(4):
                    g_ = r_ * NT + tok0
                    b_, c0_ = g_ // 2048, g_ % 2048
                    s.dma("act", cd_[:, r_], D["ogt"][b_ * 1024:(b_ + 1) * 1024, :].rearrange("(c p) n -> p c n", p=128)[:, :, c0_:c0_ + 128],
                          writes=[cd_.b])
                s.op("dve", lambda e: e.tensor_scalar(o_[:], cd_[:, 0], sel4[:, 0:1], None, ALU.mult),
                     reads=[cd_.b, sel4.b], writes=[o_.b])
                for r_ in range(1, 4):
                    s.op("dve", lambda e, r_=r_: e.scalar_tensor_tensor(o_[:], cd_[:, r_], sel4[:, r_:r_ + 1], o_[:], ALU.mult, ALU.add),
                         reads=[cd_.b, sel4.b, o_.b], writes=[o_.b])
            else:
                s.dma("act", o_[:], ogt_v[:, :, tok0:tok0 + 128], writes=[o_.b])
            s.dma("act", h_[:], D["h1"][tok0:tok0 + 128, :], writes=[h_.b])
            for half in range(2):
                bk = kb.banks[4 + half]
                mm_group(kb, bk, bk[:, :], [(o_[:, c, :], wo[:, c, half * 512:(half + 1) * 512]) for c in range(8)],
                         reads=[o_.b, wo.b])
                s.op("dve", lambda e, bk=bk, h_=h_, t=t, half=half: e.tensor_tensor(
                    acc[:, t, half * 512:(half + 1) * 512], h_[:, half * 512:(half + 1) * 512], bk[:, :], ALU.add),
                    reads=[bk.b, h_.b], writes=[accb[t]])
            rms_stats(kb, acc[:, t, :], accb[t], junk, sc["ss"], sc["rt"], sc["rstd"])
            s.op("act", lambda e, t=t: e.activation(xs[:], acc[:, t, :], AF.Copy, scale=sc["rstd"][:]),
                 reads=[accb[t], sc["rstd"].b], writes=[xs.b])
            for hh in range(2):
                bk = kb.banks[6 + hh]
                mm_group(kb, bk, [bk[:, j * 128:(j + 1) * 128] for j in range(4)],
                         [(xs[:, (hh * 4 + j) * 128:(hh * 4 + j + 1) * 128], ident[:]) for j in range(4)],
                         reads=[xs.b, ident.b], transpose=True)
                s.op("dve", lambda e, bk=bk, hh=hh: e.tensor_tensor(
                    hn32[:, hh * 4:(hh + 1) * 4, :], bk[:, :].rearrange("p (c n) -> p c n", n=128),
                    bc(g[:, hh * 4:(hh + 1) * 4].unsqueeze(2), [128, 4, 128]), ALU.mult),
                    reads=[bk.b, g.b], writes=[hn32.b])
            s.op("pool", lambda e, t=t, hT_=hT_: e.tensor_copy(hT_[:, :, t * 128:(t + 1) * 128], hn32[:]),
                 reads=[hn32.b], writes=[hT_.b])
            bk = kb.banks[4]
            mm_group(kb, bk, bk[:, 0:8], [(hn32[:, c, :], rtr[:, c, :]) for c in range(8)], reads=[hn32.b, rtr.b])
            lg, mx, g1, g2 = sm["lg"], sm["mx"], sm["g1"], sm["g2"]
            s.op("dve", lambda e, bk=bk: e.tensor_copy(lg[:], bk[:, 0:8]), reads=[bk.b], writes=[lg.b])
            s.op("dve", lambda e: e.max(mx[:], lg[:]), reads=[lg.b], writes=[mx.b])
            s.op("dve", lambda e: e.tensor_tensor(sc["dd"][:], mx[:, 1:2], mx[:, 0:1], ALU.subtract),
                 reads=[mx.b], writes=[sc["dd"].b])
            s.op("act", lambda e: e.activation(sc["ex"][:], sc["dd"][:], AF.Exp), reads=[sc["dd"].b], writes=[sc["ex"].b])
            s.op("dve", lambda e: e.tensor_scalar(sc["den"][:], sc["ex"][:], 1.0, None, ALU.add),
                 reads=[sc["ex"].b], writes=[sc["den"].b])
            s.op("dve", lambda e: e.reciprocal(sc["w1"][:], sc["den"][:]), reads=[sc["den"].b], writes=[sc["w1"].b])
            s.op("dve", lambda e: e.tensor_tensor(sc["w2"][:], sc["ex"][:], sc["w1"][:], ALU.mult),
                 reads=[sc["ex"].b, sc["w1"].b], writes=[sc["w2"].b])
            s.op("dve", lambda e: e.tensor_scalar(g1[:], lg[:], mx[:, 0:1], sc["w1"][:], ALU.is_equal, ALU.mult),
                 reads=[lg.b, mx.b, sc["w1"].b], writes=[g1.b])
            s.op("dve", lambda e: e.tensor_scalar(g2[:], lg[:], mx[:, 1:2], sc["w2"][:], ALU.is_equal, ALU.mult),
                 reads=[lg.b, mx.b, sc["w2"].b], writes=[g2.b])
            s.op("dve", lambda e, t=t: e.tensor_tensor(gw[:, t, :], g1[:], g2[:], ALU.add),
                 reads=[g1.b, g2.b], writes=[gw.b])
        for ex in range(NE):
            def evac(t, half, bk, ex=ex):
                s.op("dve", lambda e: e.scalar_tensor_tensor(
                    acc[:, t, half * 512:(half + 1) * 512], bk[:, :], gw[:, t, ex:ex + 1],
                    acc[:, t, half * 512:(half + 1) * 512], ALU.mult, ALU.add),
                    reads=[bk.b, gw.b, accb[t]], writes=[accb[t]])
            ffn_expert(kb, r, hT_, D["mwg"][ex * 128:(ex + 1) * 128, :], D["mwu"][ex * 128:(ex + 1) * 128, :],
                       D["mwd"][ex * 128:(ex + 1) * 128, :], NCm, evac)
        for t in range(TS // 128):
            tok0 = st * TS + t * 128
            s.dma("act", D["out"][tok0:tok0 + 128, :], acc[:, t, :], reads=[accb[t]])


def nbank(kb):
    kb.bk = (getattr(kb, "bk", -1) + 1) % 8
    return kb.banks[kb.bk]


def norm_T(kb, W, x_ap, xbuf, gcol, dst, dcols, ncol=128):
    s = kb.s
    rms_stats(kb, x_ap, xbuf, W["junk"], W["ss"], W["rt"], W["rstd"])
    xs = W["xsb"]
    s.op("act", lambda e: e.activation(xs[:], x_ap, AF.Copy, scale=W["rstd"][:]), reads=[xbuf, W["rstd"].b], writes=[xs.b])
    bk = nbank(kb)
    bv = bk[:, :].bitcast(BF16)
    mm_group(kb, bk, [bv[:, j * 128:(j + 1) * 128] for j in range(8)],
             [(xs[:, j * 128:(j + 1) * 128], W["identb"][:]) for j in range(8)], reads=[xs.b, W["identb"].b], transpose=True)
    s.op("dve", lambda e: e.tensor_tensor(dst[:, :, dcols], bv.rearrange("p (c n) -> p c n", n=128),
                                           bc(gcol[:, 0:8].unsqueeze(2), [128, 8, 128]), ALU.mult),
         reads=[bk.b, gcol.b], writes=[dst.b])


def norm_scratch(kb):
    W = {}
    W["junk"] = kb.sb([128, 1024], BF16, "junk")
    W["xsb"] = kb.sb([128, 1024], BF16, "xsb")
    for k in ("ss", "rt", "rstd"):
        W[k] = kb.sb([128, 1], F32, k)
    ident = kb.sb([128, 128], F32, "ident")
    kb.s.dma("sp", ident[:], kb.D["ident"], writes=[ident.b])
    W["ident"] = ident
    W["identb"] = kb.sb([128, 128], BF16, "identb")
    kb.s.op("dve", lambda e: e.tensor_copy(W["identb"][:], ident[:]), reads=[ident.b], writes=[W["identb"].b])
    kb.eps_t = kb.sb([128, 1], F32, "eps")
    kb.s.op("pool", lambda e: e.memset(kb.eps_t[:], EPS), writes=[kb.eps_t.b])
    return W


def phase1(kb, C, D):
    s = kb.s
    NT = C["NTOK"]
    TS = 512
    kb.D = D
    W = norm_scratch(kb)
    if kb.bg:
        kb.bg.attach(kb)

    def ld(name, shape, dt=F32, src=None, q="sp"):
        t = kb.sb(shape, dt, name)
        s.dma(q, t[:], D[name] if src is None else src, writes=[t.b])
        return t

    gmix = ld("gmix0", [128, 8])
    win = kb.sb([128, 8, 1792], BF16, "win")
    s.dma("sp", win[:], D["win"].rearrange("p (k n) -> p k n", n=1792), writes=[win.b])
    lruc = ld("lruc", [128, 4, 8])
    wab32 = ld("wa_bd", [128, 4, 128])
    wib32 = ld("wi_bd", [128, 4, 128])
    wab = kb.sb([128, 4, 128], BF16, "wab")
    wib = kb.sb([128, 4, 128], BF16, "wib")
    s.op("dve", lambda e: e.tensor_copy(wab[:], wab32[:]), reads=[wab32.b], writes=[wab.b])
    s.op("dve", lambda e: e.tensor_copy(wib[:], wib32[:]), reads=[wib32.b], writes=[wib.b])
    qkg = ld("qkg", [64, 2])
    esink = ld("sinks_b", [128, 8])
    s.op("act", lambda e: e.activation(esink[:], esink[:], AF.Exp), reads=[esink.b], writes=[esink.b])
    prot32 = ld("prot", [64, 64])
    prot = kb.sb([64, 64], BF16, "protb")
    s.op("dve", lambda e: e.tensor_copy(prot[:], prot32[:]), reads=[prot32.b], writes=[prot.b])
    ones64 = kb.sb([64, 64], F32, "ones64")
    s.op("pool", lambda e: e.memset(ones64[:], 1.0), writes=[ones64.b])
    mcur = ld("mask_cur", [128, 128])
    mprev = ld("mask_prev", [128, 128])
    mprev0 = ld("mask_prev0", [128, 128])
    cl = kb.sb([128, 4], F32, "cl")
    cl2 = kb.sb([128, 4], F32, "cl2")
    s.op("act", lambda e: e.activation(cl[:], lruc[:, :, 7], AF.Exp, scale=-1.0), reads=[lruc.b], writes=[cl.b])
    s.op("act", lambda e: e.activation(cl[:], cl[:], AF.Ln, bias=1.0), reads=[cl.b], writes=[cl.b])
    s.op("dve", lambda e: e.tensor_scalar(cl2[:], cl[:], -16.0, None, ALU.mult), reads=[cl.b], writes=[cl2.b])
    s.op("dve", lambda e: e.tensor_scalar(cl[:], cl[:], -8.0, None, ALU.mult), reads=[cl.b, cl2.b], writes=[cl.b])
    zeros = kb.sb([128, TS], F32, "zeros")
    s.op("pool", lambda e: e.memset(zeros[:], 0.0), writes=[zeros.b])
    eps64 = kb.eps_t

    hnT = [kb.sb([128, 8, TS], BF16, "hnT") for _ in range(2)]
    xt = [kb.sb([128, 1024], F32, "xt") for _ in range(3)]
    xb = [[kb.sb([128, 3 + TS], F32, "xb") for _ in range(2)] for _ in range(4)]
    hcar = [kb.sb([128, 1], F32, "hcar") for _ in range(4)]
    pcar = [kb.sb([128, 1], F32, "pcar") for _ in range(4)]
    for c in range(4):
        s.op("pool", lambda e, c=c: e.memset(hcar[c][:], 0.0), writes=[hcar[c].b])
        s.op("pool", lambda e, c=c: e.memset(pcar[c][:], 1.0), writes=[pcar[c].b])
    L = {k: kb.sb([128, TS], F32, k) for k in ("xc", "r", "gi", "a", "a2", "u", "h", "P", "gg")}
    xcb = kb.sb([128, TS], BF16, "xcb")
    yo = [kb.sb([128, TS], BF16, "yo") for _ in range(4)]
    QR = kb.sb([64, 8, TS], BF16, "QR")
    KR = kb.sb([64, 2, 128 + TS], BF16, "KR")
    Vg = [kb.sb([128, 2, 65], BF16, "Vg") for _ in range(6)]
    for v in Vg:
        s.op("pool", lambda e, v=v: e.memset(v[:], 1.0), writes=[v.b])
    A = {k: kb.sb([64, TS], F32, k) for k in ("sq", "ln", "qn32", "t1", "t2")}
    qnb = kb.sb([64, TS], BF16, "qnb")
    cs = [kb.sb([64, 2, TS], F32, "cs") for _ in range(2)]
    E_ = [kb.sb([128, 512], F32, "E") for _ in range(2)]
    Pt = [kb.sb([128, 512], BF16, "Pt") for _ in range(4)]
    Y = kb.sb([128, 8, 64], BF16, "Y")
    yT = [kb.sb([128, 4, 128], BF16, "yT") for _ in range(2)]
    den = kb.sb([128, 4], F32, "den")
    cnt = {"x": 0, "v": 0, "e": 0, "p": 0, "y": 0, "yo": 0}

    def load_norm(src_ap, dst, dcols):
        x_ = xt[cnt["x"] % 3]
        cnt["x"] += 1
        s.dma("act", x_[:], src_ap, writes=[x_.b])
        norm_T(kb, W, x_[:], x_.b, gmix, dst, dcols)

    def proj(h_, cols0, ncols, ntok, tcols):
        bk = nbank(kb)
        mm_group(kb, bk, bk[0:ncols, 0:ntok], [(win[:, k, cols0:cols0 + ncols], h_[:, k, tcols]) for k in range(8)],
                 reads=[win.b, h_.b])
        return bk

    def qk_norm_rope(bk, ntok, gidx, cst, ccols, dst_ap, dst_buf):
        n = ntok
        s.op("act", lambda e: e.activation(A["sq"][:, 0:n], bk[0:64, 0:n], AF.Square), reads=[bk.b], writes=[A["sq"].b])
        b2 = nbank(kb)
        mm_group(kb, b2, b2[0:64, 0:n], [(ones64[:], A["sq"][:, 0:n])], reads=[ones64.b, A["sq"].b])
        s.op("act", lambda e: e.activation(A["ln"][:, 0:n], b2[0:64, 0:n], AF.Ln, bias=eps64[0:64, :], scale=1.0 / 64),
             reads=[b2.b], writes=[A["ln"].b])
        s.op("act", lambda e: e.activation(A["ln"][:, 0:n], A["ln"][:, 0:n], AF.Exp, scale=-0.5),
             reads=[A["ln"].b], writes=[A["ln"].b])
        s.op("dve", lambda e: e.scalar_tensor_tensor(A["qn32"][:, 0:n], bk[0:64, 0:n], qkg[:, gidx:gidx + 1],
                                                     A["ln"][:, 0:n], ALU.mult, ALU.mult),
             reads=[bk.b, qkg.b, A["ln"].b], writes=[A["qn32"].b])
        s.op("pool", lambda e: e.tensor_copy(qnb[:, 0:n], A["qn32"][:, 0:n]), reads=[A["qn32"].b], writes=[qnb.b])
        b3 = nbank(kb)
        mm_group(kb, b3, b3[0:64, 0:n], [(prot[:], qnb[:, 0:n])], reads=[prot.b, qnb.b])
        s.op("pool", lambda e: e.tensor_tensor(A["t1"][:, 0:n], A["qn32"][:, 0:n], cst[:, 0, ccols], ALU.mult),
             reads=[A["qn32"].b, cst.b], writes=[A["t1"].b])
        s.op("dve", lambda e: e.tensor_tensor(A["t2"][:, 0:n], b3[0:64, 0:n], cst[:, 1, ccols], ALU.mult),
             reads=[b3.b, cst.b], writes=[A["t2"].b])
        s.op("pool", lambda e: e.tensor_tensor(dst_ap, A["t1"][:, 0:n], A["t2"][:, 0:n], ALU.add),
             reads=[A["t1"].b, A["t2"].b], writes=[dst_buf])

    def v_tile(h_, tcols):
        v = Vg[cnt["v"] % 6]
        cnt["v"] += 1
        bk = nbank(kb)
        mm_group(kb, bk, bk[:, 0:128], [(h_[:, k, tcols], win[:, k, 1664:1792]) for k in range(8)], reads=[win.b, h_.b])
        s.op("act", lambda e: e.copy(v[:, :, 0:64], bk[:, 0:128].rearrange("p (h d) -> p h d", d=64)),
             reads=[bk.b], writes=[v.b])
        return v

    hh_ = hnT[1]
    load_norm(D["xhalo"], hh_, slice(0, 128))
    csh = cs[1]
    s.dma("sp", csh[:, :, 0:128], D["cossin"][:, :, 0:128], writes=[csh.b])
    for c in range(4):
        bk = proj(hh_, c * 128, 128, 128, slice(0, 128))
        s.op("act", lambda e, c=c, bk=bk: e.copy(xb[c][0][:, 0:3], bk[:, 125:128]), reads=[bk.b], writes=[xb[c][0].b])
    for h in range(2):
        bk = proj(hh_, 1536 + h * 64, 64, 128, slice(0, 128))
        qk_norm_rope(bk, 128, 1, csh, slice(0, 128), KR[:, h, 0:128], KR.b)
    vprev = v_tile(hh_, slice(0, 128))

    for st in range(NT // TS):
        h_ = hnT[st % 2]
        cst = cs[st % 2]
        s.dma("sp", cst[:], D["cossin"][:, :, 128 + st * TS:128 + (st + 1) * TS], writes=[cst.b])
        for t in range(4):
            tok0 = st * TS + t * 128
            load_norm(D["x"][tok0:tok0 + 128, :], h_, slice(t * 128, (t + 1) * 128))
        allc = slice(0, TS)
        for c in range(4):
            xb_ = xb[c][st % 2]
            xbn = xb[c][(st + 1) % 2]
            bk = proj(h_, c * 128, 128, TS, allc)
            s.op("act", lambda e, bk=bk, xb_=xb_: e.copy(xb_[:, 3:3 + TS], bk[:, 0:TS]), reads=[bk.b], writes=[xb_.b])
            s.op("pool", lambda e, xb_=xb_, xbn=xbn: e.tensor_copy(xbn[:, 0:3], xb_[:, TS:TS + 3]), reads=[xb_.b], writes=[xbn.b])
            bkg = proj(h_, 512 + c * 128, 128, TS, allc)
            s.op("act", lambda e, bkg=bkg: e.activation(L["gg"][:], bkg[:, 0:TS], AF.Gelu_apprx_tanh), reads=[bkg.b], writes=[L["gg"].b])
            xc = L["xc"]
            s.op("dve", lambda e, xb_=xb_, c=c: e.tensor_scalar(xc[:], xb_[:, 0:TS], lruc[:, c, 0:1], lruc[:, c, 4:5], ALU.mult, ALU.add),
                 reads=[xb_.b, lruc.b], writes=[xc.b])
            for j in range(1, 4):
                s.op("dve", lambda e, xb_=xb_, c=c, j=j: e.scalar_tensor_tensor(xc[:], xb_[:, j:j + TS], lruc[:, c, j:j + 1], xc[:], ALU.mult, ALU.add),
                     reads=[xb_.b, lruc.b, xc.b], writes=[xc.b])
            s.op("pool", lambda e: e.tensor_copy(xcb[:], xc[:]), reads=[xc.b], writes=[xcb.b])
            b1 = nbank(kb)
            mm_group(kb, b1, b1[:, 0:TS], [(wab[:, c, :], xcb[:])], reads=[wab.b, xcb.b])
            b2 = nbank(kb)
            mm_group(kb, b2, b2[:, 0:TS], [(wib[:, c, :], xcb[:])], reads=[wib.b, xcb.b])
            s.op("act", lambda e, b1=b1, c=c: e.activation(L["r"][:], b1[:, 0:TS], AF.Sigmoid, bias=lruc[:, c, 5:6]),
                 reads=[b1.b, lruc.b], writes=[L["r"].b])
            s.op("act", lambda e, b2=b2, c=c: e.activation(L["gi"][:], b2[:, 0:TS], AF.Sigmoid, bias=lruc[:, c, 6:7]),
                 reads=[b2.b, lruc.b], writes=[L["gi"].b])
            s.op("act", lambda e, c=c: e.activation(L["a"][:], L["r"][:], AF.Exp, scale=cl[:, c:c + 1]),
                 reads=[L["r"].b, cl.b], writes=[L["a"].b])
            s.op("act", lambda e, c=c: e.activation(L["a2"][:], L["r"][:], AF.Exp, scale=cl2[:, c:c + 1]),
                 reads=[L["r"].b, cl2.b], writes=[L["a2"].b])
            s.op("dve", lambda e: e.tensor_scalar(L["a2"][:], L["a2"][:], -1.0, 1.0, ALU.mult, ALU.add),
                 reads=[L["a2"].b], writes=[L["a2"].b])
            s.op("act", lambda e: e.activation(L["a2"][:], L["a2"][:], AF.Sqrt), reads=[L["a2"].b], writes=[L["a2"].b])
            s.op("pool", lambda e: e.tensor_tensor(L["u"][:], L["gi"][:], xc[:], ALU.mult), reads=[L["gi"].b, xc.b], writes=[L["u"].b])
            s.op("dve", lambda e: e.tensor_tensor(L["u"][:], L["u"][:], L["a2"][:], ALU.mult), reads=[L["u"].b, L["a2"].b], writes=[L["u"].b])
            s.op("dve", lambda e, c=c: e.tensor_tensor_scan(L["h"][:], L["a"][:], L["u"][:], hcar[c][:], ALU.mult, ALU.add),
                 reads=[L["a"].b, L["u"].b, hcar[c].b], writes=[L["h"].b])
            s.op("dve", lambda e, c=c: e.tensor_tensor_scan(L["P"][:], L["a"][:], zeros[:], pcar[c][:], ALU.mult, ALU.add),
                 reads=[L["a"].b, zeros.b, pcar[c].b], writes=[L["P"].b])
            s.op("act", lambda e, c=c: e.copy(hcar[c][:], L["h"][:, TS - 1:TS]), reads=[L["h"].b], writes=[hcar[c].b])
            s.op("act", lambda e, c=c: e.copy(pcar[c][:], L["P"][:, TS - 1:TS]), reads=[L["P"].b], writes=[pcar[c].b])
            y1 = yo[cnt["yo"] % 4]
            y2 = yo[(cnt["yo"] + 1) % 4]
            cnt["yo"] += 2
            s.op("pool", lambda e, y1=y1: e.tensor_tensor(y1[:], L["h"][:], L["gg"][:], ALU.mult), reads=[L["h"].b, L["gg"].b], writes=[y1.b])
            s.op("pool", lambda e, y2=y2: e.tensor_tensor(y2[:], L["P"][:], L["gg"][:], ALU.mult), reads=[L["P"].b, L["gg"].b], writes=[y2.b])
            s.dma("sp", D["yloc"][c * 128:(c + 1) * 128, st * TS:(st + 1) * TS], y1[:], reads=[y1.b])
            s.dma("sp", D["pg"][c * 128:(c + 1) * 128, st * TS:(st + 1) * TS], y2[:], reads=[y2.b])
            if kb.bg:
                for _ in range(3):
                    kb.bg.step()
        for hq in range(8):
            bk = proj(h_, 1024 + hq * 64, 64, TS, allc)
            qk_norm_rope(bk, TS, 0, cst, allc, QR[:, hq, :], QR.b)
        for h in range(2):
            bk = proj(h_, 1536 + h * 64, 64, TS, allc)
            qk_norm_rope(bk, TS, 1, cst, allc, KR[:, h, 128:128 + TS], KR.b)
        vt = [vprev] + [v_tile(h_, slice(t * 128, (t + 1) * 128)) for t in range(4)]
        for b in range(4):
            mp = mprev0 if (st == 0 and b == 0) else mprev
            for h in range(2):
                pts = []
                for w_, (kc0, mk) in enumerate(((b * 128, mp), (128 + b * 128, mcur))):
                    bk = nbank(kb)
                    mm_group(kb, bk, bk[:, :].rearrange("p (g n) -> p g n", n=128),
                             [(KR[:, h, kc0:kc0 + 128], QR[:, 4 * h:4 * h + 4, b * 128:(b + 1) * 128])], reads=[KR.b, QR.b])
                    e_ = E_[cnt["e"] % 2]
                    cnt["e"] += 1
                    s.op("act", lambda e, e_=e_, bk=bk: e.activation(e_[:], bk[:, :], AF.Exp, scale=0.125), reads=[bk.b], writes=[e_.b])
                    p_ = Pt[cnt["p"] % 4]
                    cnt["p"] += 1
                    s.op("dve" if w_ == 0 else "pool", lambda e, p_=p_, e_=e_, mk=mk: e.tensor_tensor(
                        p_[:].rearrange("p (g n) -> p g n", n=128), e_[:].rearrange("p (g n) -> p g n", n=128),
                        bc(mk[:].unsqueeze(1), [128, 4, 128]), ALU.mult), reads=[e_.b, mk.b], writes=[p_.b])
                    pts.append(p_)
                bo = nbank(kb)
                for g in range(4):
                    mm_group(kb, bo, bo[:, g * 65:(g + 1) * 65],
                             [(pts[0][:, g * 128:(g + 1) * 128], vt[b][:, h, :]), (pts[1][:, g * 128:(g + 1) * 128], vt[b + 1][:, h, :])],
                             reads=[pts[0].b, pts[1].b, vt[b].b, vt[b + 1].b])
                bov = bo[:, 0:260].rearrange("p (g n) -> p g n", n=65)
                s.op("dve", lambda e, bov=bov, h=h: e.tensor_tensor(den[:], bov[:, :, 64], esink[:, 4 * h:4 * h + 4], ALU.add),
                     reads=[bo.b, esink.b], writes=[den.b])
                s.op("dve", lambda e: e.reciprocal(den[:], den[:]), reads=[den.b], writes=[den.b])
                s.op("dve", lambda e, bov=bov, h=h: e.tensor_tensor(Y[:, 4 * h:4 * h + 4, :], bov[:, :, 0:64],
                                                                    bc(den[:].unsqueeze(2), [128, 4, 64]), ALU.mult),
                     reads=[bo.b, den.b], writes=[Y.b])
            bt = nbank(kb)
            btv = bt[:, :].bitcast(BF16)
            Yf = Y[:].rearrange("p h d -> p (h d)")
            mm_group(kb, bt, [btv[:, j * 128:(j + 1) * 128] for j in range(4)],
                     [(Yf[:, j * 128:(j + 1) * 128], W["identb"][:]) for j in range(4)], reads=[Y.b, W["identb"].b], transpose=True)
            yt_ = yT[cnt["y"] % 2]
            cnt["y"] += 1
            s.op("act", lambda e, yt_=yt_, btv=btv: e.copy(yt_[:], btv[:, 0:512].rearrange("p (c n) -> p c n", n=128)),
                 reads=[bt.b], writes=[yt_.b])
            tok0 = st * TS + b * 128
            s.dma("sp", D["yatt"].rearrange("(c p) n -> p c n", p=128)[:, :, tok0:tok0 + 128], yt_[:], reads=[yt_.b])
        vprev = vt[4]
        s.op("pool", lambda e: e.tensor_copy(KR[:, :, 0:128], KR[:, :, TS:TS + 128]), reads=[KR.b], writes=[KR.b])
    seg = kb.sb([128, 8], F32, "seg")
    for c in range(4):
        s.op("act", lambda e, c=c: e.copy(seg[:, c:c + 1], pcar[c][:]), reads=[pcar[c].b], writes=[seg.b])
        s.op("act", lambda e, c=c: e.copy(seg[:, 4 + c:5 + c], hcar[c][:]), reads=[hcar[c].b], writes=[seg.b])
    s.dma("sp", D["seg"], seg[:], reads=[seg.b])
    if kb.bg:
        kb.bg.detach()


def phase2(kb, C, D):
    s = kb.s
    NT = C["NTOK"]
    TS = 512
    NCf = 22
    kb.D = D
    W = norm_scratch(kb)
    gffn = kb.sb([128, 8], F32, "gffn0")
    s.dma("sp", gffn[:], D["gffn0"], writes=[gffn.b])
    gmix = kb.sb([128, 8], F32, "gmix1")
    s.dma("sp", gmix[:], D["gmix1"], writes=[gmix.b])
    wo = kb.sb([128, 8, 1024], BF16, "wo0")
    s.dma("sp", wo[:], D["wo0"].rearrange("p (k n) -> p k n", n=1024), writes=[wo.b])
    sega = kb.sb([128, 4, 8], F32, "sega")
    s.dma("sp", sega[:], D["segall"].rearrange("(r p) n -> p r n", p=128), writes=[sega.b])
    sel = kb.sb([128, 4], F32, "sel")
    s.dma("sp", sel[:], D["sel"], writes=[sel.b])
    hin = kb.sb([128, 4], F32, "hin")
    tmp = kb.sb([128, 4], F32, "tmpc")
    s.op("pool", lambda e: e.memset(hin[:], 0.0), writes=[hin.b])
    for i in range(3):
        s.op("dve", lambda e, i=i: e.tensor_tensor(tmp[:], sega[:, i, 0:4], hin[:], ALU.mult), reads=[sega.b, hin.b], writes=[tmp.b])
        s.op("dve", lambda e, i=i: e.tensor_tensor(tmp[:], tmp[:], sega[:, i, 4:8], ALU.add), reads=[sega.b, tmp.b], writes=[tmp.b])
        s.op("dve", lambda e: e.tensor_tensor(tmp[:], tmp[:], hin[:], ALU.subtract), reads=[tmp.b, hin.b], writes=[tmp.b])
        s.op("dve", lambda e, i=i: e.scalar_tensor_tensor(hin[:], tmp[:], sel[:, i:i + 1], hin[:], ALU.mult, ALU.add),
             reads=[tmp.b, sel.b, hin.b], writes=[hin.b])
    r = ffn_alloc(kb, TS, NCf, nrot=2 if kb.bg else 3)
    hnT = [kb.sb([128, 8, TS], BF16, "hnT") for _ in range(2)]
    acc = kb.sb([128, TS // 128, 1024], F32, "acc")
    accb = [Buf() for _ in range(TS // 128)]
    xt = [kb.sb([128, 1024], F32, "xt") for _ in range(2)]
    yl = kb.sb([128, 4, TS], BF16, "yl")
    pg = kb.sb([128, 4, TS], BF16, "pgt")
    ya = kb.sb([128, 4, TS], BF16, "ya")
    ym = yl
    if kb.bg:
        kb.bg.attach(kb)
    hn1 = [kb.sb([128, 8, 128], BF16, "hn1") for _ in range(2)]
    it = 0
    for st in range(NT // TS):
        cols = slice(st * TS, (st + 1) * TS)
        s.dma("act", yl[:], D["yloc"].rearrange("(c p) n -> p c n", p=128)[:, :, cols], writes=[yl.b])
        s.dma("act", pg[:], D["pg"].rearrange("(c p) n -> p c n", p=128)[:, :, cols], writes=[pg.b])
        s.dma("act", ya[:], D["yatt"].rearrange("(c p) n -> p c n", p=128)[:, :, cols], writes=[ya.b])
        for c in range(4):
            s.op("dve", lambda e, c=c: e.scalar_tensor_tensor(ym[:, c, :], pg[:, c, :], hin[:, c:c + 1], yl[:, c, :], ALU.mult, ALU.add),
                 reads=[pg.b, hin.b, yl.b], writes=[ym.b])
        h_ = hnT[st % 2]
        for t in range(TS // 128):
            tok0 = st * TS + t * 128
            tc_ = slice(t * 128, (t + 1) * 128)
            x_ = xt[it % 2]
            it += 1
            s.dma("act", x_[:], D["x"][tok0:tok0 + 128, :], writes=[x_.b])
            for half in range(2):
                bk = nbank(kb)
                mm_group(kb, bk, bk[:, :], [((ym[:, c, tc_] if c < 4 else ya[:, c - 4, tc_]), wo[:, c, half * 512:(half + 1) * 512])
                                            for c in range(8)], reads=[ym.b, ya.b, wo.b])
                s.op("dve", lambda e, bk=bk, x_=x_, t=t, half=half: e.tensor_tensor(
                    acc[:, t, half * 512:(half + 1) * 512], x_[:, half * 512:(half + 1) * 512], bk[:, :], ALU.add),
                    reads=[bk.b, x_.b], writes=[accb[t]])
            norm_T(kb, W, acc[:, t, :], accb[t], gffn, h_, tc_)

        def evac(t, half, bk):
            s.op("dve", lambda e: e.tensor_tensor(acc[:, t, half * 512:(half + 1) * 512], acc[:, t, half * 512:(half + 1) * 512],
                                                  bk[:, :], ALU.add), reads=[bk.b, accb[t]], writes=[accb[t]])
        def bghook():
            if kb.bg:
                kb.bg.step()
                kb.bg.step()
        ffn_expert(kb, r, h_, D["fwg"], D["fwu"], D["fwd"], NCf, evac, hook=bghook)
        for t in range(TS // 128):
            tok0 = st * TS + t * 128
            s.dma("act", D["h1"][tok0:tok0 + 128, :], acc[:, t, :], reads=[accb[t]])
            o_ = hn1[t % 2]
            norm_T(kb, W, acc[:, t, :], accb[t], gmix, o_, slice(0, 128))
            s.dma("act", D["hn1T"].rearrange("(c p) n -> p c n", p=128)[:, :, tok0:tok0 + 128], o_[:], reads=[o_.b])
    if kb.bg:
        kb.bg.detach()


def phase3(kb, C, D):
    s = kb.s
    NTF, NTK = C["NTF"], C["NTK"]
    TS = 512
    NST = NTF // TS
    kb.D = D
    W = norm_scratch(kb)
    ident, identb = W["ident"], W["identb"]

    def ld(name, shape, dt=F32, src=None):
        t = kb.sb(shape, dt, name)
        s.dma("sp", t[:], D[name] if src is None else src, writes=[t.b])
        return t

    wqkv = kb.sb([128, 8, 768], BF16, "wqkv")
    s.dma("sp", wqkv[:], D["wqkv"].rearrange("p (k n) -> p k n", n=768), writes=[wqkv.b])
    wgate = kb.sb([128, 8, 260], BF16, "wgab")
    s.dma("sp", wgate[:, :, 0:256], D["wgate"].rearrange("p (k n) -> p k n", n=256), writes=[wgate.b])
    s.dma("sp", wgate[:, :, 256:260], D["wab"].rearrange("p (k n) -> p k n", n=4), writes=[wgate.b])
    abS = kb.sb([128, 4, 4], F32, "abS")
    convw = ld("convw", [128, 6, 4])
    hc = ld("hconst", [128, 4])
    onw = ld("onw_b", [128, 128])
    U = ld("maskU", [128, 128])
    Lo = ld("maskL", [128, 128])
    Bs = ld("maskB", [128, 128])
    C0 = ld("maskC0", [128, 128])
    C1 = ld("maskC1", [128, 128])
    nalog = kb.sb([128, 2], F32, "nalog")
    s.op("act", lambda e: e.activation(nalog[:], hc[:, 0:2], AF.Exp), reads=[hc.b], writes=[nalog.b])
    s.op("dve", lambda e: e.tensor_scalar(nalog[:], nalog[:], -1.0, None, ALU.mult), reads=[nalog.b], writes=[nalog.b])
    onesb = kb.sb([128, 128], BF16, "onesb")
    s.op("pool", lambda e: e.memset(onesb[:], 1.0), writes=[onesb.b])

    hnT = [kb.sb([128, 8, TS], BF16, "hnT") for _ in range(2)]
    cb = [[kb.sb([128, 3 + TS], F32, "cb") for _ in range(2)] for _ in range(6)]
    for b in range(6):
        s.op("pool", lambda e, b=b: e.memset(cb[b][0][:, 0:3], 0.0), writes=[cb[b][0].b])
    cacc = [kb.sb([128, TS], F32, "cacc") for _ in range(2)]
    csl = [kb.sb([128, TS], F32, "csl") for _ in range(2)]
    sqb = [kb.sb([128, TS], BF16, "sqb") for _ in range(2)]
    lnr = [kb.sb([128, TS], F32, "lnr") for _ in range(2)]
    FTa = [kb.sb([128, 6, TS], BF16, "FTa") for _ in range(2)]
    sgate = [[kb.sb([128, 256], F32, "sgate") for _ in range(4)] for _ in range(2)]
    sc = {k: kb.sb([128, 4, 2], F32, k) for k in ("xa", "gtm", "beta")}
    cs8 = [{k: kb.sb([128, 8], F32, k) for k in ("eG", "eGlG", "egl0", "egl1", "bEG", "dGl")} for _ in range(2)]
    S32 = [kb.sb([128, 128], F32, "S32") for _ in range(2)]
    Sbf = [kb.sb([128, 128], BF16, "Sbf") for _ in range(2)]
    for h in range(2):
        s.op("pool", lambda e, h=h: e.memset(S32[h][:], 0.0), writes=[S32[h].b])
        s.op("pool", lambda e, h=h: e.memset(Sbf[h][:], 0.0), writes=[Sbf[h].b])

    def mkset(bf_names, f_names, small=()):
        d = {}
        for k in bf_names:
            d[k] = kb.sb([128, 256] if k == "VK" else [128, 128], BF16, k)
        for k in f_names:
            d[k] = kb.sb([128, 128], F32, k)
        for k in small:
            d[k] = kb.sb([128, 1], F32, k)
        return d
    HO = [[[mkset(("wT", "kdec", "aqkT"), ("u",)) for _ in range(2)] for _ in range(4)] for _ in range(2)]
    PT = [[mkset(("VK", "Lb", "Nb", "P", "Q", "XL", "XN", "P2", "Q2", "XL2", "XN2", "wtok"), ("gL", "E", "ET", "t1", "t2"))
           for _ in range(2)] for _ in range(4)]
    STp = [mkset(("vn", "ogb"), ("av", "O", "on"), ("ss", "rt", "rstd")) for _ in range(2)]
    ogs = [kb.sb([128, 2, TS], BF16, "ogs") for _ in range(2)]

    def evc(en, out, src, reads, wbuf, scale=None):
        if en == "act":
            if scale is None:
                s.op("act", lambda e: e.copy(out, src), reads=reads, writes=[wbuf])
            else:
                s.op("act", lambda e: e.activation(out, src, AF.Copy, scale=scale), reads=reads, writes=[wbuf])
        else:
            if scale is None:
                s.op(en, lambda e: e.tensor_copy(out, src), reads=reads, writes=[wbuf])
            else:
                s.op(en, lambda e: e.tensor_scalar(out, src, scale, None, ALU.mult), reads=reads, writes=[wbuf])

    def pre_gen(st):
        par = st % 2
        h_ = hnT[par]
        F_ = FTa[par]
        c8 = cs8[par]
        tok0 = st * TS
        rk, col0 = tok0 // NTK, tok0 % NTK
        if C.get("chunked"):
            s.dma("act", h_[:], D["hng"].rearrange("(c r p) n -> p c r n", r=4, p=128)[:, :, rk, col0:col0 + TS], writes=[h_.b])
        else:
            s.dma("act", h_[:], D["hng"][rk * 1024:(rk + 1) * 1024, :].rearrange("(c p) n -> p c n", p=128)[:, :, col0:col0 + TS],
                  writes=[h_.b])
        for b in range(6):
            c_ = cb[b][par]
            cn = cb[b][(st + 1) % 2]
            ca, cl_, sq_, ln_ = cacc[b % 2], csl[b % 2], sqb[b % 2], lnr[b % 2]
            bk = nbank(kb)
            mm_group(kb, bk, bk[:, 0:TS], [(wqkv[:, k, b * 128:(b + 1) * 128], h_[:, k, :]) for k in range(8)], reads=[wqkv.b, h_.b])
            s.op("act", lambda e: e.copy(c_[:, 3:3 + TS], bk[:, 0:TS]), reads=[bk.b], writes=[c_.b])
            s.op("pool", lambda e: e.tensor_copy(cn[:, 0:3], c_[:, TS:TS + 3]), reads=[c_.b], writes=[cn.b])
            s.op("dve", lambda e: e.tensor_scalar(ca[:], c_[:, 0:TS], convw[:, b, 0:1], None, ALU.mult),
                 reads=[c_.b, convw.b], writes=[ca.b])
            for j in range(1, 4):
                s.op("dve", lambda e: e.scalar_tensor_tensor(ca[:], c_[:, j:j + TS], convw[:, b, j:j + 1], ca[:], ALU.mult, ALU.add),
                     reads=[c_.b, convw.b, ca.b], writes=[ca.b])
            yield
            if b >= 4:
                s.op("act", lambda e: e.activation(F_[:, b, :], ca[:], AF.Silu), reads=[ca.b], writes=[F_.b])
                continue
            s.op("act", lambda e: e.activation(cl_[:], ca[:], AF.Silu), reads=[ca.b], writes=[cl_.b])
            s.op("act", lambda e: e.activation(sq_[:], cl_[:], AF.Square), reads=[cl_.b], writes=[sq_.b])
            b2 = nbank(kb)
            mm_group(kb, b2, b2[:, 0:TS], [(onesb[:], sq_[:])], reads=[onesb.b, sq_.b])
            s.op("act", lambda e: e.activation(ln_[:], b2[:, 0:TS], AF.Ln, bias=kb.eps_t[:]), reads=[b2.b], writes=[ln_.b])
            s.op("act", lambda e: e.activation(ln_[:], ln_[:], AF.Exp, scale=-0.5), reads=[ln_.b], writes=[ln_.b])
            s.op("dve", lambda e: e.scalar_tensor_tensor(F_[:, b, :], cl_[:], (128.0 ** -0.5) if b in (0, 2) else 1.0, ln_[:], ALU.mult, ALU.mult),
                 reads=[cl_.b, ln_.b], writes=[F_.b])
            yield
        for t in range(4):
            tc_ = slice(t * 128, (t + 1) * 128)
            bk = nbank(kb)
            sg_ = sgate[par][t]
            mm_group(kb, bk, bk[:, 0:260], [(h_[:, k, tc_], wgate[:, k, :]) for k in range(8)], reads=[wgate.b, h_.b])
            s.op("act", lambda e: e.activation(sg_[:], bk[:, 0:256], AF.Silu), reads=[bk.b], writes=[sg_.b])
            s.op("act", lambda e: e.copy(abS[:, t, :], bk[:, 256:260]), reads=[bk.b], writes=[abS.b])
            yield
        abv = abS
        s.op("dve", lambda e: e.tensor_tensor(sc["xa"][:], abv[:, :, 0:2], bc(hc[:, 2:4].unsqueeze(1), [128, 4, 2]), ALU.add),
             reads=[abS.b, hc.b], writes=[sc["xa"].b])
        s.op("act", lambda e: e.activation(sc["beta"][:], abv[:, :, 2:4], AF.Sigmoid), reads=[abS.b], writes=[sc["beta"].b])
        s.op("act", lambda e: e.activation(sc["xa"][:], sc["xa"][:], AF.Exp), reads=[sc["xa"].b], writes=[sc["xa"].b])
        s.op("act", lambda e: e.activation(sc["xa"][:], sc["xa"][:], AF.Ln, bias=1.0), reads=[sc["xa"].b], writes=[sc["xa"].b])
        s.op("dve", lambda e: e.tensor_tensor(sc["gtm"][:], sc["xa"][:], bc(nalog[:].unsqueeze(1), [128, 4, 2]), ALU.mult),
             reads=[sc["xa"].b, nalog.b], writes=[sc["gtm"].b])
        g8 = sc["gtm"][:].rearrange("p t n -> p (t n)")
        b8 = sc["beta"][:].rearrange("p t n -> p (t n)")
        bcs = nbank(kb)
        for i, m in enumerate((U, Bs, C0, C1)):
            mm_group(kb, bcs, bcs[:, i * 8:(i + 1) * 8], [(m[:], g8)], reads=[m.b, sc["gtm"].b])
        s.op("act", lambda e: e.activation(c8["eG"][:], bcs[:, 0:8], AF.Exp), reads=[bcs.b], writes=[c8["eG"].b])
        s.op("act", lambda e: e.copy(c8["dGl"][:], bcs[:, 8:16]), reads=[bcs.b], writes=[c8["dGl"].b])
        s.op("act", lambda e: e.activation(c8["egl0"][:], bcs[:, 16:24], AF.Exp), reads=[bcs.b], writes=[c8["egl0"].b])
        s.op("act", lambda e: e.activation(c8["egl1"][:], bcs[:, 24:32], AF.Exp), reads=[bcs.b], writes=[c8["egl1"].b])
        s.op("dve", lambda e: e.tensor_tensor(c8["dGl"][:], c8["dGl"][:], bcs[:, 0:8], ALU.subtract), reads=[bcs.b, c8["dGl"].b], writes=[c8["dGl"].b])
        s.op("act", lambda e: e.activation(c8["eGlG"][:], c8["dGl"][:], AF.Exp), reads=[c8["dGl"].b], writes=[c8["eGlG"].b])
        s.op("dve", lambda e: e.tensor_tensor(c8["bEG"][:], c8["eG"][:], b8, ALU.mult), reads=[c8["eG"].b, sc["beta"].b], writes=[c8["bEG"].b])
        yield
        chains = [(t, h) for t in range(4) for h in range(2)]
        for (t, h) in chains:
            tc_ = slice(t * 128, (t + 1) * 128)
            col = slice(t * 2 + h, t * 2 + h + 1)
            d = PT[t][h]
            o = HO[par][t][h]
            bk = nbank(kb)
            bv = bk[:, :].bitcast(BF16)
            mm_group(kb, bk, [bv[:, 0:128], bv[:, 128:256]], [(F_[:, 2 * h + 1, tc_], identb[:]), (F_[:, 4 + h, tc_], identb[:])],
                     reads=[F_.b, identb.b], transpose=True)
            evc("act", d["VK"][:, 0:128], bv[:, 128:256], [bk.b, sc["beta"].b], d["VK"].b, scale=b8[:, col])
            evc("act", d["VK"][:, 128:256], bv[:, 0:128], [bk.b, c8["bEG"].b], d["VK"].b, scale=c8["bEG"][:, col])
            evc("act", o["kdec"][:], bv[:, 0:128], [bk.b, c8["eGlG"].b], o["kdec"].b, scale=c8["eGlG"][:, col])
            s.op("pool", lambda e: e.tensor_scalar(d["gL"][:], Lo[:], g8[:, col], None, ALU.mult),
                 reads=[Lo.b, sc["gtm"].b], writes=[d["gL"].b])
            bD = nbank(kb)
            mm_group(kb, bD, bD[:, 0:128], [(U[:], d["gL"][:])], reads=[U.b, d["gL"].b])
            mm_group(kb, bD, bD[:, 128:256], [(d["gL"][:], U[:])], reads=[U.b, d["gL"].b])
            s.op("act", lambda e: e.activation(d["E"][:], bD[:, 0:128], AF.Exp), reads=[bD.b], writes=[d["E"].b])
            s.op("act", lambda e: e.activation(d["ET"][:], bD[:, 128:256], AF.Exp), reads=[bD.b], writes=[d["ET"].b])
            bK = nbank(kb)
            mm_group(kb, bK, bK[:, 0:256].rearrange("p (a n) -> p a n", n=128), [(F_[:, 2 * h + 1, tc_], F_[:, 2 * h:2 * h + 2, tc_])], reads=[F_.b])
            s.op("dve", lambda e: e.scalar_tensor_tensor(d["t1"][:], bK[:, 128:256], b8[:, col], d["E"][:], ALU.mult, ALU.mult),
                 reads=[bK.b, sc["beta"].b, d["E"].b], writes=[d["t1"].b])
            s.op("dve", lambda e: e.tensor_tensor(d["t2"][:], bK[:, 0:128], d["ET"][:], ALU.mult),
                 reads=[bK.b, d["ET"].b], writes=[d["t2"].b])
            s.op("pool", lambda e: e.tensor_tensor(d["Lb"][:], d["t1"][:], Lo[:], ALU.mult), reads=[d["t1"].b, Lo.b], writes=[d["Lb"].b])
            s.op("pool", lambda e: e.tensor_tensor(o["aqkT"][:], d["t2"][:], U[:], ALU.mult), reads=[d["t2"].b, U.b], writes=[o["aqkT"].b])
            yield
        for (t, h) in chains:
            d = PT[t][h]
            bN = nbank(kb)
            bNv = bN[:, :].bitcast(BF16)
            mm_group(kb, bN, [bNv[:, 0:128]], [(d["Lb"][:], identb[:])], reads=[d["Lb"].b, identb.b], transpose=True)
            evc("act", d["Nb"][:], bNv[:, 0:128], [bN.b], d["Nb"].b)
            s.op("pool", lambda e: e.tensor_tensor(d["P"][:], identb[:], d["Nb"][:], ALU.subtract), reads=[identb.b, d["Nb"].b], writes=[d["P"].b])
        yield
        cur = {ch: dict(XL=PT[ch[0]][ch[1]]["Lb"], XN=PT[ch[0]][ch[1]]["Nb"], P=PT[ch[0]][ch[1]]["P"]) for ch in chains}
        for lvl in range(5):
            alt = (lvl % 2 == 0)
            last = (lvl == 4)
            for i_, ch in enumerate(chains):
                d = PT[ch[0]][ch[1]]
                c_ = cur[ch]
                nXL, nXN = (d["XL"], d["XN"]) if alt else (d["XL2"], d["XN2"])
                bq = nbank(kb)
                mm_group(kb, bq, bq[:, 0:128], [(c_["XN"][:], c_["XL"][:])], reads=[c_["XN"].b, c_["XL"].b])
                if not last:
                    mm_group(kb, bq, bq[:, 128:256], [(c_["XL"][:], c_["XN"][:])], reads=[c_["XN"].b, c_["XL"].b])
                en = "act" if i_ % 2 == 0 else "dve"
                evc(en, nXL[:], bq[:, 0:128], [bq.b], nXL.b)
                if not last:
                    evc(en, nXN[:], bq[:, 128:256], [bq.b], nXN.b)
                c_["XL"], c_["XN"] = nXL, nXN
                if i_ % 2 == 1:
                    yield
            for i_, ch in enumerate(chains):
                d = PT[ch[0]][ch[1]]
                c_ = cur[ch]
                nP = d["P2"] if alt else d["P"]
                Po = c_["P"]
                bp = nbank(kb)
                mm_group(kb, bp, bp[:, 0:128], [(c_["XL"][:], Po[:])], reads=[Po.b, c_["XL"].b])
                s.op("dve", lambda e: e.tensor_tensor(nP[:], Po[:], bp[:, 0:128], ALU.add), reads=[Po.b, bp.b], writes=[nP.b])
                c_["P"] = nP
                if i_ % 2 == 1:
                    yield
        for ch in chains:
            d = PT[ch[0]][ch[1]]
            o = HO[par][ch[0]][ch[1]]
            TT = cur[ch]["P"]
            bu = nbank(kb)
            mm_group(kb, bu, bu[:, 0:256], [(TT[:], d["VK"][:])], reads=[TT.b, d["VK"].b])
            evc("act", o["u"][:], bu[:, 0:128], [bu.b], o["u"].b)
            evc("act", d["wtok"][:], bu[:, 128:256], [bu.b], d["wtok"].b)
            bw = nbank(kb)
            bwv = bw[:, :].bitcast(BF16)
            mm_group(kb, bw, [bwv[:, 0:128]], [(d["wtok"][:], identb[:])], reads=[d["wtok"].b, identb.b], transpose=True)
            evc("act", o["wT"][:], bwv[:, 0:128], [bw.b], o["wT"].b)
            yield

    def scan_gen(st):
        par = st % 2
        F_ = FTa[par]
        c8 = cs8[par]
        og_ = ogs[par]
        tok0 = st * TS
        for t in range(4):
            tc_ = slice(t * 128, (t + 1) * 128)
            for ci in range(2):
                pr = slice(64 * ci, 64 * ci + 64)
                cc = slice(t * 128 + 64 * ci, t * 128 + 64 * ci + 64)
                egl = c8["egl0"] if ci == 0 else c8["egl1"]
                for h in range(2):
                    col = slice(t * 2 + h, t * 2 + h + 1)
                    o = HO[par][t][h]
                    d = STp[h]
                    bs = nbank(kb)
                    mm_group(kb, bs, bs[pr, 0:128], [(o["wT"][:, pr], Sbf[h][:])], reads=[o["wT"].b, Sbf[h].b])
                    mm_group(kb, bs, bs[pr, 128:256], [(F_[:, 2 * h, cc], Sbf[h][:])], reads=[F_.b, Sbf[h].b])
                    s.op("dve", lambda e: e.tensor_tensor(d["vn"][pr, :], o["u"][pr, :], bs[pr, 0:128], ALU.subtract),
                         reads=[o["u"].b, bs.b], writes=[d["vn"].b])
                    b2 = nbank(kb)
                    mm_group(kb, b2, b2[:, 0:128], [(o["kdec"][pr, :], d["vn"][pr, :])], reads=[o["kdec"].b, d["vn"].b])
                    mm_group(kb, b2, b2[pr, 128:256], [(o["aqkT"][pr, pr], d["vn"][pr, :])], reads=[o["aqkT"].b, d["vn"].b])
                    s.op("dve", lambda e: e.scalar_tensor_tensor(S32[h][:], S32[h][:], egl[:, col], b2[:, 0:128], ALU.mult, ALU.add),
                         reads=[S32[h].b, egl.b, b2.b], writes=[S32[h].b])
                    s.op("dve", lambda e: e.tensor_copy(Sbf[h][:], S32[h][:]), reads=[S32[h].b], writes=[Sbf[h].b])
                    s.op("dve", lambda e: e.tensor_copy(d["av"][pr, :], b2[pr, 128:256]), reads=[b2.b], writes=[d["av"].b])
                    s.op("dve", lambda e: e.scalar_tensor_tensor(d["O"][pr, :], bs[pr, 128:256], c8["eG"][pr, col], d["av"][pr, :], ALU.mult, ALU.add),
                         reads=[bs.b, c8["eG"].b, d["av"].b], writes=[d["O"].b])
                    yield
            for h in range(2):
                d = STp[h]
                sg_ = sgate[par][t]
                s.op("act", lambda e: e.activation(d["on"][:], d["O"][:], AF.Square, accum_out=d["ss"][:]), reads=[d["O"].b], writes=[d["on"].b, d["ss"].b])
                s.op("act", lambda e: e.activation(d["rt"][:], d["ss"][:], AF.Sqrt, bias=kb.eps_t[:], scale=1.0 / 128.0), reads=[d["ss"].b], writes=[d["rt"].b])
                s.op("dve", lambda e: e.reciprocal(d["rstd"][:], d["rt"][:]), reads=[d["rt"].b], writes=[d["rstd"].b])
                s.op("dve", lambda e: e.scalar_tensor_tensor(d["on"][:], d["O"][:], d["rstd"][:], onw[:], ALU.mult, ALU.mult),
                     reads=[d["O"].b, d["rstd"].b, onw.b], writes=[d["on"].b])
                s.op("pool", lambda e: e.tensor_tensor(d["ogb"][:], d["on"][:], sg_[:, h * 128:(h + 1) * 128], ALU.mult),
                     reads=[d["on"].b, sg_.b], writes=[d["ogb"].b])
                bo = nbank(kb)
                bov = bo[:, :].bitcast(BF16)
                mm_group(kb, bo, [bov[:, 0:128]], [(d["ogb"][:], identb[:])], reads=[d["ogb"].b, identb.b], transpose=True)
                evc("act", og_[:, h, tc_], bov[:, 0:128], [bo.b], og_.b)
                yield
        if C.get("chunked"):
            b_, c0_ = tok0 // 2048, tok0 % 2048
            s.dma("sp", D["ogt"][b_ * 256:(b_ + 1) * 256, :].rearrange("(h p) n -> p h n", p=128)[:, :, c0_:c0_ + TS], og_[:], reads=[og_.b])
        else:
            s.dma("sp", D["ogt"].rearrange("(h p) n -> p h n", p=128)[:, :, tok0:tok0 + TS], og_[:], reads=[og_.b])

    usebg = bool(kb.bg) and bool(C.get("bg"))
    if usebg:
        kb.bg.attach(kb)
    for _ in pre_gen(0):
        pass
    RATIO = C.get("ratio", 3)
    bgk = 0
    for st in range(NST):
        gs = scan_gen(st)
        gp = pre_gen(st + 1) if st + 1 < NST else iter(())
        alive_s = alive_p = True
        while alive_s or alive_p:
            if alive_s:
                try:
                    next(gs)
                except StopIteration:
                    alive_s = False
            for _ in range(RATIO):
                if alive_p:
                    try:
                        next(gp)
                    except StopIteration:
                        alive_p = False
            bgk += 1
            if usebg and bgk % 2 == 0:
                kb.bg.step()
    if usebg:
        kb.bg.detach()


def kmajor(W):
    K, N = W.shape
    return np.ascontiguousarray(W.reshape(K // 128, 128, N).transpose(1, 0, 2).reshape(128, -1))

def blocks(W, FB=256):
    K, F = W.shape
    NB = F // FB
    return np.ascontiguousarray(W.reshape(8, 128, NB, FB).transpose(1, 2, 0, 3).reshape(128, -1))

def pcol(v):
    return np.ascontiguousarray(v.reshape(-1, 128).T)

def prep_p1(inp, NT, nseg):
    x = inp["x"]
    B, S, _ = x.shape
    f32 = np.float32
    win = kmajor(inp["e_w_in"][0])
    lruc = np.zeros((128, 4, 8), f32)
    cw = inp["e_lru_conv_w"][0]
    for c in range(4):
        sl = slice(c * 128, (c + 1) * 128)
        for j in range(4):
            lruc[:, c, j] = cw[j, sl]
        lruc[:, c, 4] = inp["e_lru_conv_b"][0][sl]
        lruc[:, c, 5] = inp["e_lru_b_a"][0][sl]
        lruc[:, c, 6] = inp["e_lru_b_i"][0][sl]
        lruc[:, c, 7] = inp["e_lru_lambda"][0][sl]
    def bd(w):
        o = np.zeros((128, 4, 128), f32)
        for c in range(4):
            o[0:64, c, 0:64] = w[2 * c]
            o[64:128, c, 64:128] = w[2 * c + 1]
        return o
    prot = np.zeros((64, 64), f32)
    for m in range(32):
        prot[m + 32, m] = -1.0
        prot[m, m + 32] = 1.0
    k_ = np.arange(128)[:, None]; q_ = np.arange(128)[None, :]
    mcur = (k_ <= q_).astype(f32); mprev = (k_ > q_).astype(f32)
    half = 32
    inv_freq = (10000.0 ** (-np.arange(half, dtype=np.float32) / half)).astype(f32)
    common = dict(win32=win, lruc=lruc, wa_bd=bd(inp["e_lru_w_a"][0]), wi_bd=bd(inp["e_lru_w_i"][0]),
                  qkg=np.stack([inp["e_q_norm"][0], inp["e_k_norm"][0]], 1).astype(f32),
                  sinks_b=np.broadcast_to(inp["e_sinks"][0][None, :], (128, 8)).astype(f32).copy(),
                  prot=prot, mask_cur=mcur, mask_prev=mprev, gmix0=pcol(inp["ln_mix"][0]),
                  ident=np.eye(128, dtype=f32))
    outs = []
    for b in range(B):
        for j in range(nseg):
            t0 = j * NT
            d = dict(common)
            d["x"] = np.ascontiguousarray(x[b, t0:t0 + NT])
            d["xhalo"] = np.ascontiguousarray(x[b, t0 - 128:t0]) if j > 0 else np.zeros((128, 1024), f32)
            d["mask_prev0"] = mprev if j > 0 else np.zeros((128, 128), f32)
            pos = np.arange(t0 - 128, t0 + NT).astype(f32)
            ang = pos[None, :] * inv_freq[:, None]
            cs = np.zeros((64, 2, NT + 128), f32)
            cs[0:32, 0] = np.cos(ang); cs[32:64, 0] = np.cos(ang)
            cs[0:32, 1] = np.sin(ang); cs[32:64, 1] = np.sin(ang)
            d["cossin"] = cs
            outs.append(d)
    return outs


def gdn_masks():
    f32 = np.float32
    k = np.arange(128)[:, None]; i = np.arange(128)[None, :]
    same = (k // 64) == (i // 64)
    return dict(maskU=((k <= i) & same).astype(f32), maskL=((k > i) & same).astype(f32), maskB=same.astype(f32),
                maskC0=np.broadcast_to(k < 64, (128, 128)).astype(f32).copy(),
                maskC1=np.broadcast_to(k >= 64, (128, 128)).astype(f32).copy())


def prep_p3(inp, hp):
    f32 = np.float32
    w = inp["o_w_in"][0]
    hs = [2 * hp, 2 * hp + 1]
    cols = [off + h * 128 + np.arange(128) for (off, h) in ((0, hs[0]), (1024, hs[0]), (0, hs[1]), (1024, hs[1]), (2048, hs[0]), (2048, hs[1]))]
    wqkv = w[:, np.concatenate(cols)]
    wgate = w[:, np.concatenate([3072 + h * 128 + np.arange(128) for h in hs])]
    wab = w[:, [4096 + hs[0], 4096 + hs[1], 4104 + hs[0], 4104 + hs[1]]]
    cw = inp["o_conv_w"][0]
    convw = np.zeros((128, 6, 4), f32)
    for b, c in enumerate(cols):
        convw[:, b, :] = cw[:, c].T
    hconst = np.zeros((128, 4), f32)
    hconst[:, 0:2] = inp["o_a_log"][0][hs][None, :]
    hconst[:, 2:4] = inp["o_dt_bias"][0][hs][None, :]
    d = dict(wqkv32=kmajor(wqkv), wgate32=kmajor(wgate), wab32=kmajor(np.ascontiguousarray(wab)), convw=convw, hconst=hconst,
             onw_b=np.broadcast_to(inp["o_out_norm"][0][None, :], (128, 128)).astype(f32).copy(), ident=np.eye(128, dtype=f32))
    d.update(gdn_masks())
    return d


NT_CORE = 4096
NSEG = 4


def _mk_dram(kb, h, outs, scratch):
    D = {}
    for k, v in h.items():
        D[k] = kb.dram(k, list(v.shape), F32 if v.dtype == np.float32 else BF16, "ExternalInput")
    for k, (shape, dt) in scratch.items():
        D[k] = kb.dram(k, shape, dt)
    for k, (shape, dt) in outs.items():
        D[k] = kb.dram(k, shape, dt, "ExternalOutput")
    return D


def _build(h, outs, scratch, casts, phase, C):
    nc = bass.Bass("TRN2", target_bir_lowering=False)
    kb = KB(nc)
    D = _mk_dram(kb, h, outs, scratch)
    pairs = []
    for src, dst, rows in casts:
        for r0 in range(0, rows, 128):
            pairs.append((D[src][r0:r0 + 128, :], D[dst][r0:r0 + 128, :]))
    cast_dram(kb, pairs)
    kb.phase_end()
    phase(kb, C, D)
    kb.phase_end()
    kb.s.finish()
    return nc


def _run(nc, maps):
    res = run_bass_kernel_spmd(nc, maps, core_ids=list(range(len(maps))))
    return res.results


def kernel_unfused(**inputs):
    inp = {k: np.asarray(v) for k, v in inputs.items()}
    f32 = np.float32
    x = inp["x"]
    B, S, _ = x.shape
    NT = NT_CORE
    ncore = B * NSEG
    eye = np.eye(128, dtype=f32)
    hA = prep_p1(inp, NT, NSEG)
    ncA = _build(hA[0], dict(yloc=([512, NT], BF16), pg=([512, NT], BF16), yatt=([512, NT], BF16), seg=([128, 8], F32)),
                 dict(win=([128, 8 * 1792], BF16)), [("win32", "win", 128)], phase1, dict(NTOK=NT))
    rA = _run(ncA, hA)
    hB = []
    cB = dict(ident=eye, gffn0=pcol(inp["ln_ffn"][0]), gmix1=pcol(inp["ln_mix"][1]), wo032=kmajor(inp["e_w_out"][0]),
              fwg32=blocks(inp["e_ffn_w_gate"][0]), fwu32=blocks(inp["e_ffn_w_up"][0]), fwd32=kmajor(inp["e_ffn_w_down"][0]))
    for b in range(B):
        segall = np.concatenate([rA[b * NSEG + j]["seg"] for j in range(NSEG)], 0)
        for j in range(NSEG):
            c = b * NSEG + j
            sel = np.zeros((128, 4), f32)
            sel[:, :j] = 1.0
            d = dict(cB)
            d.update(x=hA[c]["x"], yloc=rA[c]["yloc"], pg=rA[c]["pg"], yatt=rA[c]["yatt"], segall=segall, sel=sel)
            hB.append(d)
    ncB = _build(hB[0], dict(h1=([NT, 1024], F32), hn1T=([1024, NT], BF16)),
                 dict(wo0=([128, 8192], BF16), fwg=([128, 22 * 1024], BF16), fwu=([128, 22 * 1024], BF16), fwd=([128, 22 * 1024], BF16)),
                 [("wo032", "wo0", 128), ("fwg32", "fwg", 128), ("fwu32", "fwu", 128), ("fwd32", "fwd", 128)], phase2, dict(NTOK=NT))
    rB = _run(ncB, hB)
    hC = []
    for b in range(B):
        hng = np.concatenate([rB[b * NSEG + j]["hn1T"] for j in range(NSEG)], 0)
        for hp_ in range(NSEG):
            d = prep_p3(inp, hp_)
            d["hng"] = hng
            hC.append(d)
    ncC = _build(hC[0], dict(ogt=([256, S], BF16)),
                 dict(wqkv=([128, 8 * 768], BF16), wgate=([128, 8 * 256], BF16), wab=([128, 32], BF16)),
                 [("wqkv32", "wqkv", 128), ("wgate32", "wgate", 128), ("wab32", "wab", 128)], phase3, dict(NTF=S, NTK=NT))
    rC = _run(ncC, hC)
    NE = inp["o_moe_w_gate"].shape[1]
    NCm = inp["o_moe_w_gate"].shape[3] // 128
    cD = dict(ident=eye, gffn1=pcol(inp["ln_ffn"][1]), router=kmajor(inp["o_router"][0]), wo1_32=kmajor(inp["o_w_out"][0]),
              mwg_32=np.concatenate([blocks(inp["o_moe_w_gate"][0][e]) for e in range(NE)], 0),
              mwu_32=np.concatenate([blocks(inp["o_moe_w_up"][0][e]) for e in range(NE)], 0),
              mwd_32=np.concatenate([kmajor(inp["o_moe_w_down"][0][e]) for e in range(NE)], 0))
    hD = []
    for b in range(B):
        ogt_full = np.concatenate([rC[b * NSEG + hp_]["ogt"] for hp_ in range(NSEG)], 0)
        for j in range(NSEG):
            c = b * NSEG + j
            d = dict(cD)
            d.update(ogt=np.ascontiguousarray(ogt_full[:, j * NT:(j + 1) * NT]), h1=rB[c]["h1"])
            hD.append(d)
    ncD = _build(hD[0], dict(out=([NT, 1024], F32)),
                 dict(wo1=([128, 8192], BF16), mwg=([NE * 128, NCm * 1024], BF16), mwu=([NE * 128, NCm * 1024], BF16),
                      mwd=([NE * 128, NCm * 1024], BF16)),
                 [("wo1_32", "wo1", 128), ("mwg_32", "mwg", NE * 128), ("mwu_32", "mwu", NE * 128), ("mwd_32", "mwd", NE * 128)],
                 phase4, dict(NTOK=NT, NEXP=NE, NC_MOE=NCm))
    rD = _run(ncD, hD)
    out = np.stack([np.concatenate([rD[b * NSEG + j]["out"] for j in range(NSEG)], 0) for b in range(B)], 0)
    return out.astype(f32)


def p4_consts(NSL, TS=512):
    c = np.zeros((128, 96), np.float32)
    c[:, 0:16] = float(TS) * np.arange(16)[None, :]
    c[:, 16] = np.arange(128)
    c[:, 32:32 + NSL] = float(TS) * np.arange(NSL)[None, :]
    k = np.arange(128)[:, None]
    i = np.arange(128)[None, :]
    return c, (k < i).astype(np.float32)


def sparse_dram(kb, D, NT, NE, NC, TS=512):
    nc = kb.nc
    NB = NC // 2
    NSL = 2 * NT // TS + NE
    D["mwg_b"] = [nc.dram_tensor(f"mwg_b{b}", [NE * 128, 2048], BF16).ap() for b in range(NB)]
    D["mwu_b"] = [nc.dram_tensor(f"mwu_b{b}", [NE * 128, 2048], BF16).ap() for b in range(NB)]
    D["mwd_g"] = [nc.dram_tensor(f"mwd_g{b}", [NE * 128, 7 * 1024], BF16).ap() for b in range(NC // 7)]
    D["h2"] = nc.dram_tensor("h2", [NT, 1024], F32).ap()
    D["hnd"] = nc.dram_tensor("hnd", [NT, 1024], BF16).ap()
    D["xsl"] = nc.dram_tensor("xsl", [NSL * TS, 1028], BF16).ap()
    D["ysl"] = nc.dram_tensor("ysl", [NSL * TS, 1024], F32).ap()
    pairs = []
    for e in range(NE):
        rs = slice(e * 128, (e + 1) * 128)
        for b in range(NB):
            pairs.append((D["mwg_32"][rs, b * 2048:(b + 1) * 2048], D["mwg_b"][b][rs, :]))
            pairs.append((D["mwu_32"][rs, b * 2048:(b + 1) * 2048], D["mwu_b"][b][rs, :]))
        for b in range(NC // 7):
            pairs.append((D["mwd_32"][rs, b * 7168:(b + 1) * 7168], D["mwd_g"][b][rs, :]))
    return pairs


def kernel(**inputs):
    inp = {k: np.asarray(v) for k, v in inputs.items()}
    f32 = np.float32
    x = inp["x"]
    B, S, _ = x.shape
    NT = NT_CORE
    NE = inp["o_moe_w_gate"].shape[1]
    NCm = inp["o_moe_w_gate"].shape[3] // 128
    eye = np.eye(128, dtype=f32)
    hA = prep_p1(inp, NT, NSEG)
    common = dict(ident=eye, gffn0=pcol(inp["ln_ffn"][0]), gmix1=pcol(inp["ln_mix"][1]), wo032=kmajor(inp["e_w_out"][0]),
                  fwg32=blocks(inp["e_ffn_w_gate"][0]), fwu32=blocks(inp["e_ffn_w_up"][0]), fwd32=kmajor(inp["e_ffn_w_down"][0]),
                  gffn1=pcol(inp["ln_ffn"][1]), router=kmajor(inp["o_router"][0]), wo1_32=kmajor(inp["o_w_out"][0]),
                  mwg_32=np.concatenate([blocks(inp["o_moe_w_gate"][0][e]) for e in range(NE)], 0),
                  mwu_32=np.concatenate([blocks(inp["o_moe_w_up"][0][e]) for e in range(NE)], 0),
                  mwd_32=np.concatenate([kmajor(inp["o_moe_w_down"][0][e]) for e in range(NE)], 0))
    p3 = [prep_p3(inp, hp_) for hp_ in range(NSEG)]
    TSZ = 256
    cst4, SU4 = p4_consts(2 * NT // TSZ + NE, TSZ)
    common.update(gffn1_b=np.broadcast_to(inp["ln_ffn"][1][None, :], (128, 1024)).astype(f32).copy(), p4cst=cst4, maskSU=SU4)
    maps = []
    for b in range(B):
        for j in range(NSEG):
            c = b * NSEG + j
            d = dict(common)
            d.update(hA[c])
            d.update(p3[j])
            sel = np.zeros((128, 4), f32)
            sel[:, :j] = 1.0
            sel4 = np.zeros((128, 4), f32)
            sel4[:, j] = 1.0
            d.update(sel=sel, sel4=sel4)
            maps.append(d)
    import os as _os
    KSTOP = int(_os.environ.get("KSTOP", "0"))
    if KSTOP:
        tiny = np.zeros((128, 8), f32)
        for d in maps:
            d.update(mwg_32=tiny, mwu_32=tiny, mwd_32=tiny)
    nc = bass.Bass("TRN2", target_bir_lowering=False)
    kb = KB(nc)
    bf = lambda n: ([128, n], BF16)
    scratch = dict(win=bf(8 * 1792), wo0=bf(8192), fwg=bf(22 * 1024), fwu=bf(22 * 1024), fwd=bf(22 * 1024),
                   wqkv=bf(8 * 768), wgate=bf(8 * 256), wab=bf(32), wo1=bf(8192),
                   yloc=([512, NT], BF16), pg=([512, NT], BF16), yatt=([512, NT], BF16), seg=([128, 8], F32),
                   h1=([NT, 1024], F32), hn1T=([1024, NT], BF16), ogt_own=([(S // 2048) * 256, 2048], BF16))
    D = _mk_dram(kb, maps[0], dict(out=([NT, 1024], F32)), scratch)
    for k, shape, dt in (("segall", [NSEG * 128, 8], F32), ("hng", [8 * NSEG * 128, NT], BF16), ("ogt_all", [(S // 2048) * NSEG * 256, 2048], BF16)):
        D[k] = nc.dram_tensor(k, shape, dt, addr_space="Local", kind="Internal").ap()
    groups = [list(range(b * NSEG, (b + 1) * NSEG)) for b in range(B)]
    casts = [("win32", "win", 128), ("wo032", "wo0", 128), ("fwg32", "fwg", 128), ("fwu32", "fwu", 128), ("fwd32", "fwd", 128),
             ("wqkv32", "wqkv", 128), ("wgate32", "wgate", 128), ("wab32", "wab", 128), ("wo1_32", "wo1", 128)]
    pairs = []
    for src_, dst_, rows in casts:
        for r0 in range(0, rows, 128):
            pairs.append((D[src_][r0:r0 + 128, :], D[dst_][r0:r0 + 128, :]))
    if not KSTOP:
        pairs += sparse_dram(kb, D, NT, NE, NCm, TSZ)
    cast_dram(kb, pairs)
    kb.phase_end()
    phase1(kb, dict(NTOK=NT), D)
    kb.phase_end()
    kb.s.collective("AllGather", D["seg"], D["segall"], groups)
    kb.phase_end()
    if KSTOP == 1:
        kb.s.finish()
        return _run(nc, maps)
    phase2(kb, dict(NTOK=NT), D)
    kb.phase_end()
    if KSTOP == 2:
        kb.s.finish()
        return _run(nc, maps)
    for c_ in range(8):
        kb.s.collective("AllGather", D["hn1T"][c_ * 128:(c_ + 1) * 128, :], D["hng"][c_ * 512:(c_ + 1) * 512, :], groups)
    kb.phase_end()
    if KSTOP == 3:
        kb.s.finish()
        return _run(nc, maps)
    D3 = dict(D)
    D3["ogt"] = D["ogt_own"]
    phase3(kb, dict(NTF=S, NTK=NT, chunked=True), D3)
    kb.phase_end()
    if KSTOP == 4:
        kb.s.finish()
        return _run(nc, maps)
    for b_ in range(S // 2048):
        kb.s.collective("AllGather", D["ogt_own"][b_ * 256:(b_ + 1) * 256, :], D["ogt_all"][b_ * 1024:(b_ + 1) * 1024, :], groups)
    kb.phase_end()
    if KSTOP == 5:
        kb.s.finish()
        return _run(nc, maps)
    D4 = dict(D)
    D4["ogt"] = D["ogt_all"]
    phase4s(kb, dict(NTOK=NT, NEXP=NE, NC_MOE=NCm, fused=True, tsz=TSZ), D4)
    kb.phase_end()
    kb.s.finish()
    res = _run(nc, maps)
    out = np.stack([np.concatenate([res[b * NSEG + j]["out"] for j in range(NSEG)], 0) for b in range(B)], 0)
    return out.astype(f32)


I32 = mybir.dt.int32


def idma(kb, out, in_, idx_ap, scatter, reads=(), writes=()):
    s = kb.s
    E = s.E["pool"]
    deps = s._deps(reads, writes)
    slot = s.dsem["pool"][s.drr["pool"]]
    s.drr["pool"] = (s.drr["pool"] + 1) % NDS
    sem, val = slot
    if val > 0:
        deps.append((sem, val))
    s._wait(E, deps)
    slot[1] = val + 16
    off = bass.IndirectOffsetOnAxis(ap=idx_ap, axis=0)
    if scatter:
        kb.nc.gpsimd.indirect_dma_start(out=out, out_offset=off, in_=in_, in_offset=None).then_inc(sem, 16)
    else:
        kb.nc.gpsimd.indirect_dma_start(out=out, out_offset=None, in_=in_, in_offset=off).then_inc(sem, 16)
    s.nins += 1
    tok = (sem, val + 16)
    k = id(sem)
    for b in reads:
        b.r[k] = tok
    for b in writes:
        b.w = tok
        b.r = {}


def phase4s(kb, C, D):
    s = kb.s
    NT, NE, NCm = C["NTOK"], C["NEXP"], C["NC_MOE"]
    TS = C.get("tsz", 512)
    NTT = NT // 128
    NSL = (2 * NT) // TS + NE
    NTH = NT // TS
    NSUB = TS // 128
    NB = NCm // 2
    kb.D = D
    IDX1 = kb.sbp([128, NTT], I32, "IDX1")
    IDX2 = kb.sbp([128, NTT], I32, "IDX2")
    IDXW = kb.sbp([128, NSL], I32, "IDXW")
    W = norm_scratch(kb)
    ident, identb = W["ident"], W["identb"]
    g = kb.sb([128, 8], F32, "gffn")
    s.dma("sp", g[:], D["gffn1"], writes=[g.b])
    gtm = kb.sb([128, 1024], F32, "gtm")
    s.dma("sp", gtm[:], D["gffn1_b"], writes=[gtm.b])
    wo = kb.sb([128, 8, 1024], BF16, "wo")
    s.dma("sp", wo[:], D["wo1"].rearrange("p (k n) -> p k n", n=1024), writes=[wo.b])
    rtr = kb.sb([128, 8, 8], F32, "rtr")
    s.dma("sp", rtr[:], D["router"].rearrange("p (k n) -> p k n", n=8), writes=[rtr.b])
    SU32 = kb.sb([128, 128], F32, "SU32")
    s.dma("sp", SU32[:], D["maskSU"], writes=[SU32.b])
    SUb = kb.sb([128, 128], BF16, "SUb")
    s.op("dve", lambda e: e.tensor_copy(SUb[:], SU32[:]), reads=[SU32.b], writes=[SUb.b])
    onesb = kb.sb([128, 128], BF16, "onesb")
    s.op("pool", lambda e: e.memset(onesb[:], 1.0), writes=[onesb.b])
    cst = kb.sb([128, 96], F32, "p4cst")
    s.dma("sp", cst[:], D["p4cst"], writes=[cst.b])
    R1 = kb.sb([128, NTT, 8], F32, "R1")
    R2 = kb.sb([128, NTT, 8], F32, "R2")
    W12 = kb.sb([128, NTT, 2], F32, "W12")
    if C.get("fused"):
        ogc = [kb.sb([128, 4, 8, 128], BF16, "ogc") for _ in range(2)]
        sel4 = kb.sb([128, 4], F32, "sel4")
        s.dma("sp", sel4[:], D["sel4"], writes=[sel4.b])
    else:
        ogt_v = D["ogt"].rearrange("(c p) n -> p c n", p=128)
    og = [kb.sb([128, 8, 128], BF16, "og") for _ in range(2)]
    h1 = [kb.sb([128, 1024], F32, "h1") for _ in range(2)]
    acc = [kb.sb([128, 1024], F32, "acc") for _ in range(2)]
    xs2 = [kb.sb([128, 1024], F32, "xs") for _ in range(2)]
    junk2 = [kb.sb([128, 1024], BF16, "junk") for _ in range(2)]
    hn322 = [kb.sb([128, 8, 128], F32, "hn32") for _ in range(2)]
    HN = [kb.sb([128, 1028], BF16, "HN") for _ in range(2)]
    sm2 = [{k: kb.sb([128, 8], F32, k) for k in ("lg", "mx")} for _ in range(2)]
    sc2 = [{k: kb.sb([128, 1], F32, k) for k in ("ss", "rt", "rstd", "dd", "ex", "den")} for _ in range(2)]
    bh2, bhn, bxs, bys = Buf(), Buf(), Buf(), Buf()
    if kb.bg:
        kb.bg.attach(kb)
    for tt in range(NTT):
        if kb.bg:
            for _ in range(3):
                kb.bg.step()
        tok0 = tt * 128
        o_, h_, a_, hn_ = og[tt % 2], h1[tt % 2], acc[tt % 2], HN[tt % 2]
        xs, junk, hn32, sm, sc = xs2[tt % 2], junk2[tt % 2], hn322[tt % 2], sm2[tt % 2], sc2[tt % 2]
        if C.get("fused"):
            cd_ = ogc[tt % 2]
            for r_ in range(4):
                g_ = r_ * NT + tok0
                b_, c0_ = g_ // 2048, g_ % 2048
                s.dma("act", cd_[:, r_], D["ogt"][b_ * 1024:(b_ + 1) * 1024, :].rearrange("(c p) n -> p c n", p=128)[:, :, c0_:c0_ + 128],
                      writes=[cd_.b])
            s.op("dve", lambda e: e.tensor_scalar(o_[:], cd_[:, 0], sel4[:, 0:1], None, ALU.mult), reads=[cd_.b, sel4.b], writes=[o_.b])
            for r_ in range(1, 4):
                s.op("dve", lambda e: e.scalar_tensor_tensor(o_[:], cd_[:, r_], sel4[:, r_:r_ + 1], o_[:], ALU.mult, ALU.add),
                     reads=[cd_.b, sel4.b, o_.b], writes=[o_.b])
        else:
            s.dma("act", o_[:], ogt_v[:, :, tok0:tok0 + 128], writes=[o_.b])
        s.dma("act", h_[:], D["h1"][tok0:tok0 + 128, :], writes=[h_.b])
        for half in range(2):
            bk = nbank(kb)
            mm_group(kb, bk, bk[:, :], [(o_[:, c, :], wo[:, c, half * 512:(half + 1) * 512]) for c in range(8)], reads=[o_.b, wo.b])
            s.op("dve", lambda e: e.tensor_tensor(a_[:, half * 512:(half + 1) * 512], h_[:, half * 512:(half + 1) * 512], bk[:, :], ALU.add),
                 reads=[bk.b, h_.b], writes=[a_.b])
        s.dma("sp", D["h2"][tok0:tok0 + 128, :], a_[:], reads=[a_.b], writes=[bh2])
        rms_stats(kb, a_[:], a_.b, junk, sc["ss"], sc["rt"], sc["rstd"])
        s.op("act", lambda e: e.activation(xs[:], a_[:], AF.Copy, scale=sc["rstd"][:]), reads=[a_.b, sc["rstd"].b], writes=[xs.b])
        s.op("pool", lambda e: e.tensor_tensor(hn_[:, 0:1024], xs[:], gtm[:], ALU.mult), reads=[xs.b, gtm.b], writes=[hn_.b])
        s.dma("sp", D["hnd"][tok0:tok0 + 128, :], hn_[:, 0:1024], reads=[hn_.b], writes=[bhn])
        for hh in range(2):
            bk = nbank(kb)
            mm_group(kb, bk, [bk[:, j * 128:(j + 1) * 128] for j in range(4)],
                     [(xs[:, (hh * 4 + j) * 128:(hh * 4 + j + 1) * 128], ident[:]) for j in range(4)], reads=[xs.b, ident.b], transpose=True)
            s.op("dve", lambda e: e.tensor_tensor(hn32[:, hh * 4:(hh + 1) * 4, :], bk[:, :].rearrange("p (c n) -> p c n", n=128),
                                                   bc(g[:, hh * 4:(hh + 1) * 4].unsqueeze(2), [128, 4, 128]), ALU.mult),
                 reads=[bk.b, g.b], writes=[hn32.b])
        bk = nbank(kb)
        mm_group(kb, bk, bk[:, 0:8], [(hn32[:, c, :], rtr[:, c, :]) for c in range(8)], reads=[hn32.b, rtr.b])
        lg, mx = sm["lg"], sm["mx"]
        s.op("dve", lambda e: e.tensor_copy(lg[:], bk[:, 0:8]), reads=[bk.b], writes=[lg.b])
        s.op("dve", lambda e: e.max(mx[:], lg[:]), reads=[lg.b], writes=[mx.b])
        s.op("dve", lambda e: e.tensor_tensor(sc["dd"][:], mx[:, 1:2], mx[:, 0:1], ALU.subtract), reads=[mx.b], writes=[sc["dd"].b])
        s.op("act", lambda e: e.activation(sc["ex"][:], sc["dd"][:], AF.Exp), reads=[sc["dd"].b], writes=[sc["ex"].b])
        s.op("dve", lambda e: e.tensor_scalar(sc["den"][:], sc["ex"][:], 1.0, None, ALU.add), reads=[sc["ex"].b], writes=[sc["den"].b])
        s.op("dve", lambda e: e.reciprocal(W12[:, tt, 0:1], sc["den"][:]), reads=[sc["den"].b], writes=[W12.b])
        s.op("dve", lambda e: e.tensor_tensor(W12[:, tt, 1:2], sc["ex"][:], W12[:, tt, 0:1], ALU.mult), reads=[sc["ex"].b, W12.b], writes=[W12.b])
        s.op("dve", lambda e: e.tensor_scalar(R1[:, tt, :], lg[:], mx[:, 0:1], None, ALU.is_equal), reads=[lg.b, mx.b], writes=[R1.b])
        s.op("dve", lambda e: e.tensor_scalar(R2[:, tt, :], lg[:], mx[:, 1:2], None, ALU.is_equal), reads=[lg.b, mx.b], writes=[R2.b])
    if kb.bg:
        while kb.bg.loaded or kb.bg.remaining():
            kb.bg.step()
        kb.bg.detach()
    barrier(kb)
    NC8 = NTT * 8
    Rm = kb.sb([128, NC8], BF16, "Rm")
    s.op("dve", lambda e: e.tensor_tensor(Rm[:], R1[:].rearrange("p t e -> p (t e)"), R2[:].rearrange("p t e -> p (t e)"), ALU.add),
         reads=[R1.b, R2.b], writes=[Rm.b])
    bw_ = nbank(kb)
    mm_group(kb, bw_, bw_[:, 0:NC8], [(SUb[:], Rm[:])], reads=[SUb.b, Rm.b])
    bt_ = nbank(kb)
    mm_group(kb, bt_, bt_[:, 0:NC8], [(onesb[:], Rm[:])], reads=[onesb.b, Rm.b])
    totT = kb.sb([128, 8, NTT], F32, "totT")
    s.op("dve", lambda e: e.tensor_copy(totT[:], bt_[:, 0:NC8].rearrange("p (t e) -> p e t", e=8)), reads=[bt_.b], writes=[totT.b])
    ones32 = kb.sb([128, 64], F32, "ones32")
    s.op("pool", lambda e: e.memset(ones32[:], 1.0), writes=[ones32.b])
    cumI = kb.sb([128, 8, NTT], F32, "cumI")
    for e_ in range(8):
        s.op("dve", lambda e: e.tensor_tensor_scan(cumI[:, e_, :], ones32[:, 0:NTT], totT[:, e_, :], 0.0, ALU.mult, ALU.add),
             reads=[ones32.b, totT.b], writes=[cumI.b])
    toff = kb.sb([128, 8, NTT], F32, "toff")
    s.op("dve", lambda e: e.tensor_tensor(toff[:], cumI[:], totT[:], ALU.subtract), reads=[cumI.b, totT.b], writes=[toff.b])
    cnt = kb.sb([128, 8], F32, "cnt")
    s.op("dve", lambda e: e.tensor_copy(cnt[:], cumI[:, :, NTT - 1]), reads=[cumI.b], writes=[cnt.b])
    cmp = kb.sb([128, 8, NTH], F32, "cmp")
    s.op("dve", lambda e: e.tensor_tensor(cmp[:], bc(cnt[:].unsqueeze(2), [128, 8, NTH]), bc(cst[:, 0:NTH].unsqueeze(1), [128, 8, NTH]), ALU.is_gt),
         reads=[cnt.b, cst.b], writes=[cmp.b])
    padded = kb.sb([128, 8], F32, "padded")
    s.op("dve", lambda e: e.tensor_reduce(padded[:], cmp[:], AX.X, ALU.add), reads=[cmp.b], writes=[padded.b])
    s.op("dve", lambda e: e.tensor_scalar(padded[:], padded[:], float(TS), None, ALU.mult), reads=[padded.b], writes=[padded.b])
    ends = kb.sb([128, 8], F32, "ends")
    s.op("dve", lambda e: e.tensor_tensor_scan(ends[:], ones32[:, 0:8], padded[:], 0.0, ALU.mult, ALU.add), reads=[ones32.b, padded.b], writes=[ends.b])
    base = kb.sb([128, 8], F32, "base")
    s.op("dve", lambda e: e.tensor_tensor(base[:], ends[:], padded[:], ALU.subtract), reads=[ends.b, padded.b], writes=[base.b])
    smat = kb.sb([128, NTT, 8], F32, "smat")
    s.op("dve", lambda e: e.tensor_tensor(smat[:], bw_[:, 0:NC8].rearrange("p (t e) -> p t e", e=8), toff[:].rearrange("p e t -> p t e"), ALU.add),
         reads=[bw_.b, toff.b], writes=[smat.b])
    s.op("dve", lambda e: e.tensor_tensor(smat[:], smat[:], bc(base[:].unsqueeze(1), [128, NTT, 8]), ALU.add), reads=[smat.b, base.b], writes=[smat.b])
    tmp3 = kb.sb([128, NTT, 8], F32, "tmp3")
    sl = kb.sb([128, NTT], F32, "sl")
    for Rx, IDX in ((R1, IDX1), (R2, IDX2)):
        s.op("dve", lambda e: e.tensor_tensor(tmp3[:], smat[:], Rx[:], ALU.mult), reads=[smat.b, Rx.b], writes=[tmp3.b])
        s.op("dve", lambda e: e.tensor_reduce(sl[:], tmp3[:], AX.X, ALU.add), reads=[tmp3.b], writes=[sl.b])
        s.op("dve", lambda e: e.tensor_copy(IDX[:], sl[:]), reads=[sl.b], writes=[IDX.b])
    cw = kb.sb([128, NSL, 8], F32, "cw")
    s.op("dve", lambda e: e.tensor_tensor(cw[:], bc(ends[:].unsqueeze(1), [128, NSL, 8]), bc(cst[:, 32:32 + NSL].unsqueeze(2), [128, NSL, 8]), ALU.is_le),
         reads=[ends.b, cst.b], writes=[cw.b])
    ew = kb.sb([128, NSL], F32, "ew")
    s.op("dve", lambda e: e.tensor_reduce(ew[:], cw[:], AX.X, ALU.add), reads=[cw.b], writes=[ew.b])
    s.op("dve", lambda e: e.tensor_scalar(ew[:], ew[:], float(NE - 1), 128.0, ALU.min, ALU.mult), reads=[ew.b], writes=[ew.b])
    s.op("dve", lambda e: e.tensor_scalar(ew[:], ew[:], cst[:, 16:17], None, ALU.add), reads=[ew.b, cst.b], writes=[ew.b])
    s.op("dve", lambda e: e.tensor_copy(IDXW[:], ew[:]), reads=[ew.b], writes=[IDXW.b])
    zt = kb.sb([128, 1028], BF16, "zt")
    s.op("pool", lambda e: e.memset(zt[:], 0.0), writes=[zt.b])
    for r0 in range(0, NSL * TS, 128):
        s.dma("sp" if (r0 // 128) % 2 == 0 else "act", D["xsl"][r0:r0 + 128, :], zt[:], reads=[zt.b], writes=[bxs])
    barrier(kb)
    for tt in range(NTT):
        tok0 = tt * 128
        hn_ = HN[tt % 2]
        s.dma("sp", hn_[:, 0:1024], D["hnd"][tok0:tok0 + 128, :], reads=[bhn], writes=[hn_.b])
        for k_, IDX in ((0, IDX1), (1, IDX2)):
            s.op("dve", lambda e: e.tensor_copy(hn_[:, 1024:1026].bitcast(F32), W12[:, tt, k_:k_ + 1]), reads=[W12.b], writes=[hn_.b])
            idma(kb, D["xsl"][:, :], hn_[:, :], IDX[:, tt:tt + 1], True, reads=[hn_.b, IDX.b], writes=[bxs])
    kb.phase_end()
    W = norm_scratch(kb)
    identb = W["identb"]
    r = ffn_alloc(kb, TS, NCm)
    hnT = [kb.sb([128, 8, TS], BF16, "hnT") for _ in range(2)]
    xt = [kb.sb([128, 1028], BF16, "xslt") for _ in range(3)]
    GW = [kb.sb([128, NSUB], F32, "GW") for _ in range(2)]
    YS = kb.sb([128, NSUB, 1024], F32, "YS")
    ix = 0
    for w in range(NSL):
        hT_ = hnT[w % 2]
        gw_ = GW[w % 2]
        for sub in range(NSUB):
            x_ = xt[ix % 3]
            ix += 1
            r0 = w * TS + sub * 128
            s.dma("sp", x_[:], D["xsl"][r0:r0 + 128, :], reads=[bxs], writes=[x_.b])
            s.op("pool", lambda e: e.tensor_copy(gw_[:, sub:sub + 1], x_[:, 1024:1026].bitcast(F32)), reads=[x_.b], writes=[gw_.b])
            bk = nbank(kb)
            bv = bk[:, :].bitcast(BF16)
            mm_group(kb, bk, [bv[:, j * 128:(j + 1) * 128] for j in range(8)],
                     [(x_[:, j * 128:(j + 1) * 128], identb[:]) for j in range(8)], reads=[x_.b, identb.b], transpose=True)
            s.op("act", lambda e: e.copy(hT_[:, :, sub * 128:(sub + 1) * 128], bv.rearrange("p (c n) -> p c n", n=128)), reads=[bk.b], writes=[hT_.b])
        iw = IDXW[:, w:w + 1]

        def evac(t, half, bk):
            s.op("dve", lambda e: e.tensor_scalar(YS[:, t, half * 512:(half + 1) * 512], bk[:, :], gw_[:, t:t + 1], None, ALU.mult),
                 reads=[bk.b, gw_.b], writes=[YS.b])

        def lw(kind, b, tile):
            if kind == "d":
                idma(kb, tile, D["mwd_g"][b][:, :], iw, False, reads=[IDXW.b], writes=[r.wd.b])
            else:
                idma(kb, tile[:].rearrange("p k n -> p (k n)"), D["mwg_b" if kind == "g" else "mwu_b"][b][:, :], iw, False,
                     reads=[IDXW.b], writes=[tile.b])
        ffn_expert(kb, r, hT_, None, None, None, NCm, evac, loader=lw)
        for t in range(NSUB):
            r0 = w * TS + t * 128
            s.dma("act", D["ysl"][r0:r0 + 128, :], YS[:, t, :], reads=[YS.b], writes=[bys])
    kb.phase_end()
    acc = [kb.sb([128, 1024], F32, "acc") for _ in range(2)]
    y1 = [kb.sb([128, 1024], F32, "y1") for _ in range(2)]
    y2 = [kb.sb([128, 1024], F32, "y2") for _ in range(2)]
    for tt in range(NTT):
        tok0 = tt * 128
        a_, y1_, y2_ = acc[tt % 2], y1[tt % 2], y2[tt % 2]
        s.dma("sp", a_[:], D["h2"][tok0:tok0 + 128, :], reads=[bh2], writes=[a_.b])
        idma(kb, y1_[:, :], D["ysl"][:, :], IDX1[:, tt:tt + 1], False, reads=[bys, IDX1.b], writes=[y1_.b])
        idma(kb, y2_[:, :], D["ysl"][:, :], IDX2[:, tt:tt + 1], False, reads=[bys, IDX2.b], writes=[y2_.b])
        s.op("dve", lambda e: e.tensor_tensor(a_[:], a_[:], y1_[:], ALU.add), reads=[a_.b, y1_.b], writes=[a_.b])
        s.op("dve", lambda e: e.tensor_tensor(a_[:], a_[:], y2_[:], ALU.add), reads=[a_.b, y2_.b], writes=[a_.b])
        s.dma("act", D["out"][tok0:tok0 + 128, :], a_[:], reads=[a_.b])
```

```python
import numpy as np
from contextlib import ExitStack
import concourse.bass as bass
import concourse.mybir as mybir
from concourse.bass_utils import run_bass_kernel_spmd

F32 = mybir.dt.float32
BF16 = mybir.dt.bfloat16
AF = mybir.ActivationFunctionType
ALU = mybir.AluOpType
AX = mybir.AxisListType
EPS = 1e-6
NDS = 20


class Buf:
    __slots__ = ("w", "r", "name", "excl")

    def __init__(self, name="", excl=False):
        self.w = None
        self.r = {}
        self.name = name
        self.excl = excl


class Sched:
    def __init__(self, nc):
        self.nc = nc
        self.E = {}
        for nm, eng in (("pe", nc.tensor), ("dve", nc.vector), ("act", nc.scalar),
                        ("pool", nc.gpsimd), ("sp", nc.sync)):
            self.E[nm] = dict(eng=eng, sem=nc.alloc_semaphore("s_" + nm), cnt=0, known={}, name=nm)
        self.dsem = {q: [[nc.alloc_semaphore(f"d_{q}{i}"), 0] for i in range(NDS)]
                     for q in ("sp", "act", "pool")}
        self.drr = {q: 0 for q in self.dsem}
        self.nins = 0
        self.ccsem = nc.alloc_semaphore("s_cc")
        self.ccval = 0
        self.extra = []

    def collective(self, kind, in_ap, out_ap, groups, deps=None):
        if deps:
            self._wait(self.E["pool"], deps)
        self.ccval += 1
        self.nc.gpsimd.collective_compute(kind, ALU.bypass, replica_groups=groups, ins=[in_ap], outs=[out_ap]).then_inc(self.ccsem)
        self.extra.append((self.ccsem, self.ccval))
        self.nins += 1

    def _deps(self, reads, writes):
        deps = []
        for b in reads:
            if b.w is not None:
                deps.append(b.w)
            if b.excl:
                deps.extend(b.r.values())
        for b in writes:
            if b.w is not None:
                deps.append(b.w)
            deps.extend(b.r.values())
        return deps

    def _wait(self, E, deps):
        for (sem, val) in deps:
            k = id(sem)
            if E["known"].get(k, 0) >= val:
                continue
            E["eng"].wait_ge(sem, val)
            E["known"][k] = val

    def op(self, en, fn, reads=(), writes=(), inc=True):
        E = self.E[en]
        deps = self._deps(reads, writes)
        if en == "pe":
            deps = [d for d in deps if d[0] is not E["sem"]]
        self._wait(E, deps)
        ins = fn(E["eng"])
        self.nins += 1
        if inc:
            E["cnt"] += 1
            ins.then_inc(E["sem"], 1)
            tok = (E["sem"], E["cnt"])
        else:
            tok = (E["sem"], E["cnt"] + 1)
        k = id(E["sem"])
        for b in reads:
            b.r[k] = tok
        for b in writes:
            b.w = tok
            b.r = {}
        return tok

    def dma(self, q, out, in_, reads=(), writes=(), **kw):
        E = self.E[q]
        deps = self._deps(reads, writes)
        slot = self.dsem[q][self.drr[q]]
        self.drr[q] = (self.drr[q] + 1) % NDS
        sem, val = slot
        if val > 0:
            deps.append((sem, val))
        self._wait(E, deps)
        slot[1] = val + 16
        E["eng"].dma_start(out=out, in_=in_, **kw).then_inc(sem, 16)
        self.nins += 1
        tok = (sem, val + 16)
        k = id(sem)
        for b in reads:
            b.r[k] = tok
        for b in writes:
            b.w = tok
            b.r = {}
        return tok

    def finish(self):
        for q, slots in self.dsem.items():
            E = self.E[q]
            self._wait(E, [(s, v) for s, v in slots if v > 0])


class T:
    def __init__(self, t, nbuf=1, name=""):
        self.t = t
        self.b = Buf(name)

    def __getitem__(self, idx):
        return self.t[idx]


class KB:
    def __init__(self, nc):
        self.nc = nc
        self.s = Sched(nc)
        self.n = 0
        self.stack = ExitStack()
        self.banks = [self.psum(f"bank{i}") for i in range(8)]

    def sb(self, shape, dt, name=None):
        self.n += 1
        name = (name or "t") + f"_{self.n}"
        return T(self.stack.enter_context(self.nc.sbuf_tensor(name, list(shape), dt)), name=name)

    def sbp(self, shape, dt, name):
        self.n += 1
        return T(self.nc.alloc_sbuf_tensor(name + f"_{self.n}", list(shape), dt), name=name)

    def phase_end(self):
        barrier(self)
        self.stack.close()
        self.stack = ExitStack()

    def psum(self, name):
        t = T(self.nc.alloc_psum_tensor(name, [128, 512], F32), name=name)
        t.b.excl = True
        return t

    def dram(self, name, shape, dt, kind=None):
        if kind:
            return self.nc.dram_tensor(name, list(shape), dt, kind=kind).ap()
        return self.nc.dram_tensor(name, list(shape), dt).ap()


def bc(ap, shape):
    return ap.to_broadcast(list(shape))


def barrier(kb):
    s = kb.s
    toks = [(E["sem"], E["cnt"]) for E in s.E.values() if E["cnt"] > 0]
    for q, slots in s.dsem.items():
        toks += [(sm, v) for sm, v in slots if v > 0]
    toks += s.extra
    for E in s.E.values():
        s._wait(E, [t for t in toks if t[0] is not E["sem"]])


def mm_group(kb, bank, out_ap, pairs, reads, transpose=False):
    n = len(pairs)
    for i, (l, r) in enumerate(pairs):
        if transpose:
            fn = (lambda e, l=l, r=r, o=out_ap[i]: e.transpose(o, l, r))
        else:
            fn = (lambda e, l=l, r=r, i=i: e.matmul(out_ap, l, r, start=(i == 0), stop=(i == n - 1)))
        kb.s.op("pe", fn, reads=reads, writes=[bank.b], inc=(i == n - 1))


def cast_dram(kb, pairs, CH=4096):
    s = kb.s
    NB_ = 4
    st32 = [kb.sb([128, CH], F32) for _ in range(NB_)]
    st16 = [kb.sb([128, CH], BF16) for _ in range(NB_)]
    work = []
    for src, dst in pairs:
        M = src.shape[1]
        for c0 in range(0, M, CH):
            work.append((src, dst, c0, min(CH, M - c0)))
    engs = ["pool", "act", "dve"]

    def load(i):
        src, dst, c0, w = work[i]
        a = st32[i % NB_]
        s.dma("sp", a[:, 0:w], src[:, c0:c0 + w], writes=[a.b])

    for i in range(min(2, len(work))):
        load(i)
    for i in range(len(work)):
        if i + 2 < len(work):
            load(i + 2)
        src, dst, c0, w = work[i]
        a = st32[i % NB_]
        b = st16[i % NB_]
        en = engs[i % 3]
        if en == "act":
            s.op("act", lambda e, a=a, b=b, w=w: e.copy(b[:, 0:w], a[:, 0:w]), reads=[a.b], writes=[b.b])
        else:
            s.op(en, lambda e, a=a, b=b, w=w: e.tensor_copy(b[:, 0:w], a[:, 0:w]), reads=[a.b], writes=[b.b])
        s.dma("sp", dst[:, c0:c0 + w], b[:, 0:w], reads=[b.b])


def rms_stats(kb, x_ap, xbuf, junk, ss, rt, rstd):
    s = kb.s
    s.op("act", lambda e: e.activation(junk[:], x_ap, AF.Square, accum_out=ss[:]), reads=[xbuf], writes=[junk.b, ss.b])
    s.op("act", lambda e: e.activation(rt[:], ss[:], AF.Sqrt, bias=kb.eps_t[:], scale=1.0 / 1024.0), reads=[ss.b], writes=[rt.b])
    s.op("dve", lambda e: e.reciprocal(rstd[:], rt[:]), reads=[rt.b], writes=[rstd.b])


class FFNRes:
    pass


def ffn_alloc(kb, TS, NC):
    r = FFNRes()
    r.TS = TS
    r.wg = [kb.sb([128, 8, 256], BF16, "wg") for _ in range(3)]
    r.wu = [kb.sb([128, 8, 256], BF16, "wu") for _ in range(3)]
    r.wd = kb.sb([128, NC, 1024], BF16, "wd")
    r.hT = kb.sb([128, NC, TS], BF16, "hT")
    r.sg = [kb.sb([128, TS], F32, "sg") for _ in range(2)]
    r.blk = 0
    r.ch = 0
    r.dn = 0
    return r


def ffn_expert(kb, r, hnT, wg_d, wu_d, wd_d, NC, evac, loader=None):
    s = kb.s
    TS = r.TS
    NB = NC // 2
    for g0 in range(0, NC, 7):
        g1 = min(NC, g0 + 7)
        if loader is not None:
            loader("d", g0 // 7, r.wd[:, g0:g1, :].rearrange("p c n -> p (c n)"))
            continue
        s.dma("act", r.wd[:, g0:g1, :], wd_d[:, g0 * 1024:g1 * 1024].rearrange("p (c n) -> p c n", n=1024),
              writes=[r.wd.b])
    for b in range(NB):
        wg = r.wg[r.blk % 3]
        wu = r.wu[r.blk % 3]
        r.blk += 1
        if loader is not None:
            loader("g", b, wg)
            loader("u", b, wu)
        else:
            s.dma("sp", wg[:], wg_d[:, b * 2048:(b + 1) * 2048].rearrange("p (k n) -> p k n", n=256), writes=[wg.b])
            s.dma("sp", wu[:], wu_d[:, b * 2048:(b + 1) * 2048].rearrange("p (k n) -> p k n", n=256), writes=[wu.b])
        for c in range(2):
            ch = b * 2 + c
            bg = kb.banks[(2 * r.ch) % 4]
            bu = kb.banks[(2 * r.ch + 1) % 4]
            sg = r.sg[r.ch % 2]
            r.ch += 1
            mm_group(kb, bg, bg[:, 0:TS], [(wg[:, k, c * 128:(c + 1) * 128], hnT[:, k, :]) for k in range(8)],
                     reads=[wg.b, hnT.b])
            mm_group(kb, bu, bu[:, 0:TS], [(wu[:, k, c * 128:(c + 1) * 128], hnT[:, k, :]) for k in range(8)],
                     reads=[wu.b, hnT.b])
            s.op("act", lambda e, sg=sg, bg=bg: e.activation(sg[:], bg[:, 0:TS], AF.Silu), reads=[bg.b], writes=[sg.b])
            s.op("dve", lambda e, sg=sg, bu=bu, ch=ch: e.tensor_tensor(r.hT[:, ch, :], sg[:], bu[:, 0:TS], ALU.mult),
                 reads=[sg.b, bu.b], writes=[r.hT.b])
    for t in range(TS // 128):
        for half in range(2):
            bk = kb.banks[4 + (r.dn % 4)]
            r.dn += 1
            mm_group(kb, bk, bk[:, :], [(r.hT[:, ch, t * 128:(t + 1) * 128], r.wd[:, ch, half * 512:(half + 1) * 512])
                                        for ch in range(NC)], reads=[r.hT.b, r.wd.b])
            evac(t, half, bk)


def phase4(kb, C, D):
    s = kb.s
    NT, NE, NCm = C["NTOK"], C["NEXP"], C["NC_MOE"]
    TS = 512
    ident = kb.sb([128, 128], F32, "ident")
    s.dma("sp", ident[:], D["ident"], writes=[ident.b])
    kb.eps_t = kb.sb([128, 1], F32, "eps")
    s.op("pool", lambda e: e.memset(kb.eps_t[:], EPS), writes=[kb.eps_t.b])
    g = kb.sb([128, 8], F32, "gffn")
    s.dma("sp", g[:], D["gffn1"], writes=[g.b])
    wo = kb.sb([128, 8, 1024], BF16, "wo")
    s.dma("sp", wo[:], D["wo1"].rearrange("p (k n) -> p k n", n=1024), writes=[wo.b])
    rtr = kb.sb([128, 8, 8], F32, "rtr")
    s.dma("sp", rtr[:], D["router"].rearrange("p (k n) -> p k n", n=8), writes=[rtr.b])
    r = ffn_alloc(kb, TS, NCm)
    hnT = [kb.sb([128, 8, TS], BF16, "hnT") for _ in range(2)]
    acc = kb.sb([128, TS // 128, 1024], F32, "acc")
    accb = [Buf() for _ in range(TS // 128)]
    gw = kb.sb([128, TS // 128, 8], F32, "gw")
    og = [kb.sb([128, 8, 128], BF16, "og") for _ in range(2)]
    h1 = [kb.sb([128, 1024], F32, "h1") for _ in range(2)]
    xs = kb.sb([128, 1024], F32, "xs")
    junk = kb.sb([128, 1024], BF16, "junk")
    hn32 = kb.sb([128, 8, 128], F32, "hn32")
    sm = {k: kb.sb([128, 8], F32, k) for k in ("lg", "mx", "g1", "g2")}
    sc = {k: kb.sb([128, 1], F32, k) for k in ("ss", "rt", "rstd", "dd", "ex", "den", "w1", "w2")}
    ogt_v = None if C.get("fused") else D["ogt"].rearrange("(c p) n -> p c n", p=128)
    if C.get("fused"):
        ogc = [kb.sb([128, 4, 8, 128], BF16, "ogc") for _ in range(2)]
        sel4 = kb.sb([128, 4], F32, "sel4")
        s.dma("sp", sel4[:], D["sel4"], writes=[sel4.b])
    it = 0
    for st in range(NT // TS):
        hT_ = hnT[st % 2]
        for t in range(TS // 128):
            tok0 = st * TS + t * 128
            o_ = og[it % 2]
            h_ = h1[it % 2]
            it += 1
            if C.get("fused"):
                cd_ = ogc[it % 2]
                for r_ in range(4):
                    g_ = r_ * NT + tok0
                    b_, c0_ = g_ // 2048, g_ % 2048
                    s.dma("act", cd_[:, r_], D["ogt"][b_ * 1024:(b_ + 1) * 1024, :].rearrange("(c p) n -> p c n", p=128)[:, :, c0_:c0_ + 128],
                          writes=[cd_.b])
                s.op("dve", lambda e: e.tensor_scalar(o_[:], cd_[:, 0], sel4[:, 0:1], None, ALU.mult),
                     reads=[cd_.b, sel4.b], writes=[o_.b])
                for r_ in range(1, 4):
                    s.op("dve", lambda e, r_=r_: e.scalar_tensor_tensor(o_[:], cd_[:, r_], sel4[:, r_:r_ + 1], o_[:], ALU.mult, ALU.add),
                         reads=[cd_.b, sel4.b, o_.b], writes=[o_.b])
            else:
                s.dma("act", o_[:], ogt_v[:, :, tok0:tok0 + 128], writes=[o_.b])
            s.dma("act", h_[:], D["h1"][tok0:tok0 + 128, :], writes=[h_.b])
            for half in range(2):
                bk = kb.banks[4 + half]
                mm_group(kb, bk, bk[:, :], [(o_[:, c, :], wo[:, c, half * 512:(half + 1) * 512]) for c in range(8)],
                         reads=[o_.b, wo.b])
                s.op("dve", lambda e, bk=bk, h_=h_, t=t, half=half: e.tensor_tensor(
                    acc[:, t, half * 512:(half + 1) * 512], h_[:, half * 512:(half + 1) * 512], bk[:, :], ALU.add),
                    reads=[bk.b, h_.b], writes=[accb[t]])
            rms_stats(kb, acc[:, t, :], accb[t], junk, sc["ss"], sc["rt"], sc["rstd"])
            s.op("act", lambda e, t=t: e.activation(xs[:], acc[:, t, :], AF.Copy, scale=sc["rstd"][:]),
                 reads=[accb[t], sc["rstd"].b], writes=[xs.b])
            for hh in range(2):
                bk = kb.banks[6 + hh]
                mm_group(kb, bk, [bk[:, j * 128:(j + 1) * 128] for j in range(4)],
                         [(xs[:, (hh * 4 + j) * 128:(hh * 4 + j + 1) * 128], ident[:]) for j in range(4)],
                         reads=[xs.b, ident.b], transpose=True)
                s.op("dve", lambda e, bk=bk, hh=hh: e.tensor_tensor(
                    hn32[:, hh * 4:(hh + 1) * 4, :], bk[:, :].rearrange("p (c n) -> p c n", n=128),
                    bc(g[:, hh * 4:(hh + 1) * 4].unsqueeze(2), [128, 4, 128]), ALU.mult),
                    reads=[bk.b, g.b], writes=[hn32.b])
            s.op("pool", lambda e, t=t, hT_=hT_: e.tensor_copy(hT_[:, :, t * 128:(t + 1) * 128], hn32[:]),
                 reads=[hn32.b], writes=[hT_.b])
            bk = kb.banks[4]
            mm_group(kb, bk, bk[:, 0:8], [(hn32[:, c, :], rtr[:, c, :]) for c in range(8)], reads=[hn32.b, rtr.b])
            lg, mx, g1, g2 = sm["lg"], sm["mx"], sm["g1"], sm["g2"]
            s.op("dve", lambda e, bk=bk: e.tensor_copy(lg[:], bk[:, 0:8]), reads=[bk.b], writes=[lg.b])
            s.op("dve", lambda e: e.max(mx[:], lg[:]), reads=[lg.b], writes=[mx.b])
            s.op("dve", lambda e: e.tensor_tensor(sc["dd"][:], mx[:, 1:2], mx[:, 0:1], ALU.subtract),
                 reads=[mx.b], writes=[sc["dd"].b])
            s.op("act", lambda e: e.activation(sc["ex"][:], sc["dd"][:], AF.Exp), reads=[sc["dd"].b], writes=[sc["ex"].b])
            s.op("dve", lambda e: e.tensor_scalar(sc["den"][:], sc["ex"][:], 1.0, None, ALU.add),
                 reads=[sc["ex"].b], writes=[sc["den"].b])
            s.op("dve", lambda e: e.reciprocal(sc["w1"][:], sc["den"][:]), reads=[sc["den"].b], writes=[sc["w1"].b])
            s.op("dve", lambda e: e.tensor_tensor(sc["w2"][:], sc["ex"][:], sc["w1"][:], ALU.mult),
                 reads=[sc["ex"].b, sc["w1"].b], writes=[sc["w2"].b])
            s.op("dve", lambda e: e.tensor_scalar(g1[:], lg[:], mx[:, 0:1], sc["w1"][:], ALU.is_equal, ALU.mult),
                 reads=[lg.b, mx.b, sc["w1"].b], writes=[g1.b])
            s.op("dve", lambda e: e.tensor_scalar(g2[:], lg[:], mx[:, 1:2], sc["w2"][:], ALU.is_equal, ALU.mult),
                 reads=[lg.b, mx.b, sc["w2"].b], writes=[g2.b])
            s.op("dve", lambda e, t=t: e.tensor_tensor(gw[:, t, :], g1[:], g2[:], ALU.add),
                 reads=[g1.b, g2.b], writes=[gw.b])
        for ex in range(NE):
            def evac(t, half, bk, ex=ex):
                s.op("dve", lambda e: e.scalar_tensor_tensor(
                    acc[:, t, half * 512:(half + 1) * 512], bk[:, :], gw[:, t, ex:ex + 1],
                    acc[:, t, half * 512:(half + 1) * 512], ALU.mult, ALU.add),
                    reads=[bk.b, gw.b, accb[t]], writes=[accb[t]])
            ffn_expert(kb, r, hT_, D["mwg"][ex * 128:(ex + 1) * 128, :], D["mwu"][ex * 128:(ex + 1) * 128, :],
                       D["mwd"][ex * 128:(ex + 1) * 128, :], NCm, evac)
        for t in range(TS // 128):
            tok0 = st * TS + t * 128
            s.dma("act", D["out"][tok0:tok0 + 128, :], acc[:, t, :], reads=[accb[t]])


def nbank(kb):
    kb.bk = (getattr(kb, "bk", -1) + 1) % 8
    return kb.banks[kb.bk]


def norm_T(kb, W, x_ap, xbuf, gcol, dst, dcols, ncol=128):
    s = kb.s
    rms_stats(kb, x_ap, xbuf, W["junk"], W["ss"], W["rt"], W["rstd"])
    xs = W["xsb"]
    s.op("act", lambda e: e.activation(xs[:], x_ap, AF.Copy, scale=W["rstd"][:]), reads=[xbuf, W["rstd"].b], writes=[xs.b])
    bk = nbank(kb)
    bv = bk[:, :].bitcast(BF16)
    mm_group(kb, bk, [bv[:, j * 128:(j + 1) * 128] for j in range(8)],
             [(xs[:, j * 128:(j + 1) * 128], W["identb"][:]) for j in range(8)], reads=[xs.b, W["identb"].b], transpose=True)
    s.op("dve", lambda e: e.tensor_tensor(dst[:, :, dcols], bv.rearrange("p (c n) -> p c n", n=128),
                                           bc(gcol[:, 0:8].unsqueeze(2), [128, 8, 128]), ALU.mult),
         reads=[bk.b, gcol.b], writes=[dst.b])


def norm_scratch(kb):
    W = {}
    W["junk"] = kb.sb([128, 1024], BF16, "junk")
    W["xsb"] = kb.sb([128, 1024], BF16, "xsb")
    for k in ("ss", "rt", "rstd"):
        W[k] = kb.sb([128, 1], F32, k)
    ident = kb.sb([128, 128], F32, "ident")
    kb.s.dma("sp", ident[:], kb.D["ident"], writes=[ident.b])
    W["ident"] = ident
    W["identb"] = kb.sb([128, 128], BF16, "identb")
    kb.s.op("dve", lambda e: e.tensor_copy(W["identb"][:], ident[:]), reads=[ident.b], writes=[W["identb"].b])
    kb.eps_t = kb.sb([128, 1], F32, "eps")
    kb.s.op("pool", lambda e: e.memset(kb.eps_t[:], EPS), writes=[kb.eps_t.b])
    return W


def phase1(kb, C, D):
    s = kb.s
    NT = C["NTOK"]
    TS = 512
    kb.D = D
    W = norm_scratch(kb)

    def ld(name, shape, dt=F32, src=None, q="sp"):
        t = kb.sb(shape, dt, name)
        s.dma(q, t[:], D[name] if src is None else src, writes=[t.b])
        return t

    gmix = ld("gmix0", [128, 8])
    win = kb.sb([128, 8, 1792], BF16, "win")
    s.dma("sp", win[:], D["win"].rearrange("p (k n) -> p k n", n=1792), writes=[win.b])
    lruc = ld("lruc", [128, 4, 8])
    wab32 = ld("wa_bd", [128, 4, 128])
    wib32 = ld("wi_bd", [128, 4, 128])
    wab = kb.sb([128, 4, 128], BF16, "wab")
    wib = kb.sb([128, 4, 128], BF16, "wib")
    s.op("dve", lambda e: e.tensor_copy(wab[:], wab32[:]), reads=[wab32.b], writes=[wab.b])
    s.op("dve", lambda e: e.tensor_copy(wib[:], wib32[:]), reads=[wib32.b], writes=[wib.b])
    qkg = ld("qkg", [64, 2])
    esink = ld("sinks_b", [128, 8])
    s.op("act", lambda e: e.activation(esink[:], esink[:], AF.Exp), reads=[esink.b], writes=[esink.b])
    prot32 = ld("prot", [64, 64])
    prot = kb.sb([64, 64], BF16, "protb")
    s.op("dve", lambda e: e.tensor_copy(prot[:], prot32[:]), reads=[prot32.b], writes=[prot.b])
    ones64 = kb.sb([64, 64], F32, "ones64")
    s.op("pool", lambda e: e.memset(ones64[:], 1.0), writes=[ones64.b])
    mcur = ld("mask_cur", [128, 128])
    mprev = ld("mask_prev", [128, 128])
    mprev0 = ld("mask_prev0", [128, 128])
    cl = kb.sb([128, 4], F32, "cl")
    cl2 = kb.sb([128, 4], F32, "cl2")
    s.op("act", lambda e: e.activation(cl[:], lruc[:, :, 7], AF.Exp, scale=-1.0), reads=[lruc.b], writes=[cl.b])
    s.op("act", lambda e: e.activation(cl[:], cl[:], AF.Ln, bias=1.0), reads=[cl.b], writes=[cl.b])
    s.op("dve", lambda e: e.tensor_scalar(cl2[:], cl[:], -16.0, None, ALU.mult), reads=[cl.b], writes=[cl2.b])
    s.op("dve", lambda e: e.tensor_scalar(cl[:], cl[:], -8.0, None, ALU.mult), reads=[cl.b, cl2.b], writes=[cl.b])
    zeros = kb.sb([128, TS], F32, "zeros")
    s.op("pool", lambda e: e.memset(zeros[:], 0.0), writes=[zeros.b])
    eps64 = kb.eps_t

    hnT = [kb.sb([128, 8, TS], BF16, "hnT") for _ in range(2)]
    xt = [kb.sb([128, 1024], F32, "xt") for _ in range(3)]
    xb = [[kb.sb([128, 3 + TS], F32, "xb") for _ in range(2)] for _ in range(4)]
    hcar = [kb.sb([128, 1], F32, "hcar") for _ in range(4)]
    pcar = [kb.sb([128, 1], F32, "pcar") for _ in range(4)]
    for c in range(4):
        s.op("pool", lambda e, c=c: e.memset(hcar[c][:], 0.0), writes=[hcar[c].b])
        s.op("pool", lambda e, c=c: e.memset(pcar[c][:], 1.0), writes=[pcar[c].b])
    L = {k: kb.sb([128, TS], F32, k) for k in ("xc", "r", "gi", "a", "a2", "u", "h", "P", "gg")}
    xcb = kb.sb([128, TS], BF16, "xcb")
    yo = [kb.sb([128, TS], BF16, "yo") for _ in range(4)]
    QR = kb.sb([64, 8, TS], BF16, "QR")
    KR = kb.sb([64, 2, 128 + TS], BF16, "KR")
    Vg = [kb.sb([128, 2, 65], BF16, "Vg") for _ in range(6)]
    for v in Vg:
        s.op("pool", lambda e, v=v: e.memset(v[:], 1.0), writes=[v.b])
    A = {k: kb.sb([64, TS], F32, k) for k in ("sq", "ln", "qn32", "t1", "t2")}
    qnb = kb.sb([64, TS], BF16, "qnb")
    cs = [kb.sb([64, 2, TS], F32, "cs") for _ in range(2)]
    E_ = [kb.sb([128, 512], F32, "E") for _ in range(2)]
    Pt = [kb.sb([128, 512], BF16, "Pt") for _ in range(4)]
    Y = kb.sb([128, 8, 64], BF16, "Y")
    yT = [kb.sb([128, 4, 128], BF16, "yT") for _ in range(2)]
    den = kb.sb([128, 4], F32, "den")
    cnt = {"x": 0, "v": 0, "e": 0, "p": 0, "y": 0, "yo": 0}

    def load_norm(src_ap, dst, dcols):
        x_ = xt[cnt["x"] % 3]
        cnt["x"] += 1
        s.dma("act", x_[:], src_ap, writes=[x_.b])
        norm_T(kb, W, x_[:], x_.b, gmix, dst, dcols)

    def proj(h_, cols0, ncols, ntok, tcols):
        bk = nbank(kb)
        mm_group(kb, bk, bk[0:ncols, 0:ntok], [(win[:, k, cols0:cols0 + ncols], h_[:, k, tcols]) for k in range(8)],
                 reads=[win.b, h_.b])
        return bk

    def qk_norm_rope(bk, ntok, gidx, cst, ccols, dst_ap, dst_buf):
        n = ntok
        s.op("act", lambda e: e.activation(A["sq"][:, 0:n], bk[0:64, 0:n], AF.Square), reads=[bk.b], writes=[A["sq"].b])
        b2 = nbank(kb)
        mm_group(kb, b2, b2[0:64, 0:n], [(ones64[:], A["sq"][:, 0:n])], reads=[ones64.b, A["sq"].b])
        s.op("act", lambda e: e.activation(A["ln"][:, 0:n], b2[0:64, 0:n], AF.Ln, bias=eps64[0:64, :], scale=1.0 / 64),
             reads=[b2.b], writes=[A["ln"].b])
        s.op("act", lambda e: e.activation(A["ln"][:, 0:n], A["ln"][:, 0:n], AF.Exp, scale=-0.5),
             reads=[A["ln"].b], writes=[A["ln"].b])
        s.op("dve", lambda e: e.scalar_tensor_tensor(A["qn32"][:, 0:n], bk[0:64, 0:n], qkg[:, gidx:gidx + 1],
                                                     A["ln"][:, 0:n], ALU.mult, ALU.mult),
             reads=[bk.b, qkg.b, A["ln"].b], writes=[A["qn32"].b])
        s.op("pool", lambda e: e.tensor_copy(qnb[:, 0:n], A["qn32"][:, 0:n]), reads=[A["qn32"].b], writes=[qnb.b])
        b3 = nbank(kb)
        mm_group(kb, b3, b3[0:64, 0:n], [(prot[:], qnb[:, 0:n])], reads=[prot.b, qnb.b])
        s.op("pool", lambda e: e.tensor_tensor(A["t1"][:, 0:n], A["qn32"][:, 0:n], cst[:, 0, ccols], ALU.mult),
             reads=[A["qn32"].b, cst.b], writes=[A["t1"].b])
        s.op("dve", lambda e: e.tensor_tensor(A["t2"][:, 0:n], b3[0:64, 0:n], cst[:, 1, ccols], ALU.mult),
             reads=[b3.b, cst.b], writes=[A["t2"].b])
        s.op("pool", lambda e: e.tensor_tensor(dst_ap, A["t1"][:, 0:n], A["t2"][:, 0:n], ALU.add),
             reads=[A["t1"].b, A["t2"].b], writes=[dst_buf])

    def v_tile(h_, tcols):
        v = Vg[cnt["v"] % 6]
        cnt["v"] += 1
        bk = nbank(kb)
        mm_group(kb, bk, bk[:, 0:128], [(h_[:, k, tcols], win[:, k, 1664:1792]) for k in range(8)], reads=[win.b, h_.b])
        s.op("act", lambda e: e.copy(v[:, :, 0:64], bk[:, 0:128].rearrange("p (h d) -> p h d", d=64)),
             reads=[bk.b], writes=[v.b])
        return v

    hh_ = hnT[1]
    load_norm(D["xhalo"], hh_, slice(0, 128))
    csh = cs[1]
    s.dma("sp", csh[:, :, 0:128], D["cossin"][:, :, 0:128], writes=[csh.b])
    for c in range(4):
        bk = proj(hh_, c * 128, 128, 128, slice(0, 128))
        s.op("act", lambda e, c=c, bk=bk: e.copy(xb[c][0][:, 0:3], bk[:, 125:128]), reads=[bk.b], writes=[xb[c][0].b])
    for h in range(2):
        bk = proj(hh_, 1536 + h * 64, 64, 128, slice(0, 128))
        qk_norm_rope(bk, 128, 1, csh, slice(0, 128), KR[:, h, 0:128], KR.b)
    vprev = v_tile(hh_, slice(0, 128))

    for st in range(NT // TS):
        h_ = hnT[st % 2]
        cst = cs[st % 2]
        s.dma("sp", cst[:], D["cossin"][:, :, 128 + st * TS:128 + (st + 1) * TS], writes=[cst.b])
        for t in range(4):
            tok0 = st * TS + t * 128
            load_norm(D["x"][tok0:tok0 + 128, :], h_, slice(t * 128, (t + 1) * 128))
        allc = slice(0, TS)
        for c in range(4):
            xb_ = xb[c][st % 2]
            xbn = xb[c][(st + 1) % 2]
            bk = proj(h_, c * 128, 128, TS, allc)
            s.op("act", lambda e, bk=bk, xb_=xb_: e.copy(xb_[:, 3:3 + TS], bk[:, 0:TS]), reads=[bk.b], writes=[xb_.b])
            s.op("pool", lambda e, xb_=xb_, xbn=xbn: e.tensor_copy(xbn[:, 0:3], xb_[:, TS:TS + 3]), reads=[xb_.b], writes=[xbn.b])
            bkg = proj(h_, 512 + c * 128, 128, TS, allc)
            s.op("act", lambda e, bkg=bkg: e.activation(L["gg"][:], bkg[:, 0:TS], AF.Gelu_apprx_tanh), reads=[bkg.b], writes=[L["gg"].b])
            xc = L["xc"]
            s.op("dve", lambda e, xb_=xb_, c=c: e.tensor_scalar(xc[:], xb_[:, 0:TS], lruc[:, c, 0:1], lruc[:, c, 4:5], ALU.mult, ALU.add),
                 reads=[xb_.b, lruc.b], writes=[xc.b])
            for j in range(1, 4):
                s.op("dve", lambda e, xb_=xb_, c=c, j=j: e.scalar_tensor_tensor(xc[:], xb_[:, j:j + TS], lruc[:, c, j:j + 1], xc[:], ALU.mult, ALU.add),
                     reads=[xb_.b, lruc.b, xc.b], writes=[xc.b])
            s.op("pool", lambda e: e.tensor_copy(xcb[:], xc[:]), reads=[xc.b], writes=[xcb.b])
            b1 = nbank(kb)
            mm_group(kb, b1, b1[:, 0:TS], [(wab[:, c, :], xcb[:])], reads=[wab.b, xcb.b])
            b2 = nbank(kb)
            mm_group(kb, b2, b2[:, 0:TS], [(wib[:, c, :], xcb[:])], reads=[wib.b, xcb.b])
            s.op("act", lambda e, b1=b1, c=c: e.activation(L["r"][:], b1[:, 0:TS], AF.Sigmoid, bias=lruc[:, c, 5:6]),
                 reads=[b1.b, lruc.b], writes=[L["r"].b])
            s.op("act", lambda e, b2=b2, c=c: e.activation(L["gi"][:], b2[:, 0:TS], AF.Sigmoid, bias=lruc[:, c, 6:7]),
                 reads=[b2.b, lruc.b], writes=[L["gi"].b])
            s.op("act", lambda e, c=c: e.activation(L["a"][:], L["r"][:], AF.Exp, scale=cl[:, c:c + 1]),
                 reads=[L["r"].b, cl.b], writes=[L["a"].b])
            s.op("act", lambda e, c=c: e.activation(L["a2"][:], L["r"][:], AF.Exp, scale=cl2[:, c:c + 1]),
                 reads=[L["r"].b, cl2.b], writes=[L["a2"].b])
            s.op("dve", lambda e: e.tensor_scalar(L["a2"][:], L["a2"][:], -1.0, 1.0, ALU.mult, ALU.add),
                 reads=[L["a2"].b], writes=[L["a2"].b])
            s.op("act", lambda e: e.activation(L["a2"][:], L["a2"][:], AF.Sqrt), reads=[L["a2"].b], writes=[L["a2"].b])
            s.op("pool", lambda e: e.tensor_tensor(L["u"][:], L["gi"][:], xc[:], ALU.mult), reads=[L["gi"].b, xc.b], writes=[L["u"].b])
            s.op("dve", lambda e: e.tensor_tensor(L["u"][:], L["u"][:], L["a2"][:], ALU.mult), reads=[L["u"].b, L["a2"].b], writes=[L["u"].b])
            s.op("dve", lambda e, c=c: e.tensor_tensor_scan(L["h"][:], L["a"][:], L["u"][:], hcar[c][:], ALU.mult, ALU.add),
                 reads=[L["a"].b, L["u"].b, hcar[c].b], writes=[L["h"].b])
            s.op("dve", lambda e, c=c: e.tensor_tensor_scan(L["P"][:], L["a"][:], zeros[:], pcar[c][:], ALU.mult, ALU.add),
                 reads=[L["a"].b, zeros.b, pcar[c].b], writes=[L["P"].b])
            s.op("act", lambda e, c=c: e.copy(hcar[c][:], L["h"][:, TS - 1:TS]), reads=[L["h"].b], writes=[hcar[c].b])
            s.op("act", lambda e, c=c: e.copy(pcar[c][:], L["P"][:, TS - 1:TS]), reads=[L["P"].b], writes=[pcar[c].b])
            y1 = yo[cnt["yo"] % 4]
            y2 = yo[(cnt["yo"] + 1) % 4]
            cnt["yo"] += 2
            s.op("pool", lambda e, y1=y1: e.tensor_tensor(y1[:], L["h"][:], L["gg"][:], ALU.mult), reads=[L["h"].b, L["gg"].b], writes=[y1.b])
            s.op("pool", lambda e, y2=y2: e.tensor_tensor(y2[:], L["P"][:], L["gg"][:], ALU.mult), reads=[L["P"].b, L["gg"].b], writes=[y2.b])
            s.dma("sp", D["yloc"][c * 128:(c + 1) * 128, st * TS:(st + 1) * TS], y1[:], reads=[y1.b])
            s.dma("sp", D["pg"][c * 128:(c + 1) * 128, st * TS:(st + 1) * TS], y2[:], reads=[y2.b])
        for hq in range(8):
            bk = proj(h_, 1024 + hq * 64, 64, TS, allc)
            qk_norm_rope(bk, TS, 0, cst, allc, QR[:, hq, :], QR.b)
        for h in range(2):
            bk = proj(h_, 1536 + h * 64, 64, TS, allc)
            qk_norm_rope(bk, TS, 1, cst, allc, KR[:, h, 128:128 + TS], KR.b)
        vt = [vprev] + [v_tile(h_, slice(t * 128, (t + 1) * 128)) for t in range(4)]
        for b in range(4):
            mp = mprev0 if (st == 0 and b == 0) else mprev
            for h in range(2):
                pts = []
                for w_, (kc0, mk) in enumerate(((b * 128, mp), (128 + b * 128, mcur))):
                    bk = nbank(kb)
                    mm_group(kb, bk, bk[:, :].rearrange("p (g n) -> p g n", n=128),
                             [(KR[:, h, kc0:kc0 + 128], QR[:, 4 * h:4 * h + 4, b * 128:(b + 1) * 128])], reads=[KR.b, QR.b])
                    e_ = E_[cnt["e"] % 2]
                    cnt["e"] += 1
                    s.op("act", lambda e, e_=e_, bk=bk: e.activation(e_[:], bk[:, :], AF.Exp, scale=0.125), reads=[bk.b], writes=[e_.b])
                    p_ = Pt[cnt["p"] % 4]
                    cnt["p"] += 1
                    s.op("dve" if w_ == 0 else "pool", lambda e, p_=p_, e_=e_, mk=mk: e.tensor_tensor(
                        p_[:].rearrange("p (g n) -> p g n", n=128), e_[:].rearrange("p (g n) -> p g n", n=128),
                        bc(mk[:].unsqueeze(1), [128, 4, 128]), ALU.mult), reads=[e_.b, mk.b], writes=[p_.b])
                    pts.append(p_)
                bo = nbank(kb)
                for g in range(4):
                    mm_group(kb, bo, bo[:, g * 65:(g + 1) * 65],
                             [(pts[0][:, g * 128:(g + 1) * 128], vt[b][:, h, :]), (pts[1][:, g * 128:(g + 1) * 128], vt[b + 1][:, h, :])],
                             reads=[pts[0].b, pts[1].b, vt[b].b, vt[b + 1].b])
                bov = bo[:, 0:260].rearrange("p (g n) -> p g n", n=65)
                s.op("dve", lambda e, bov=bov, h=h: e.tensor_tensor(den[:], bov[:, :, 64], esink[:, 4 * h:4 * h + 4], ALU.add),
                     reads=[bo.b, esink.b], writes=[den.b])
                s.op("dve", lambda e: e.reciprocal(den[:], den[:]), reads=[den.b], writes=[den.b])
                s.op("dve", lambda e, bov=bov, h=h: e.tensor_tensor(Y[:, 4 * h:4 * h + 4, :], bov[:, :, 0:64],
                                                                    bc(den[:].unsqueeze(2), [128, 4, 64]), ALU.mult),
                     reads=[bo.b, den.b], writes=[Y.b])
            bt = nbank(kb)
            btv = bt[:, :].bitcast(BF16)
            Yf = Y[:].rearrange("p h d -> p (h d)")
            mm_group(kb, bt, [btv[:, j * 128:(j + 1) * 128] for j in range(4)],
                     [(Yf[:, j * 128:(j + 1) * 128], W["identb"][:]) for j in range(4)], reads=[Y.b, W["identb"].b], transpose=True)
            yt_ = yT[cnt["y"] % 2]
            cnt["y"] += 1
            s.op("act", lambda e, yt_=yt_, btv=btv: e.copy(yt_[:], btv[:, 0:512].rearrange("p (c n) -> p c n", n=128)),
                 reads=[bt.b], writes=[yt_.b])
            tok0 = st * TS + b * 128
            s.dma("sp", D["yatt"].rearrange("(c p) n -> p c n", p=128)[:, :, tok0:tok0 + 128], yt_[:], reads=[yt_.b])
        vprev = vt[4]
        s.op("pool", lambda e: e.tensor_copy(KR[:, :, 0:128], KR[:, :, TS:TS + 128]), reads=[KR.b], writes=[KR.b])
    seg = kb.sb([128, 8], F32, "seg")
    for c in range(4):
        s.op("act", lambda e, c=c: e.copy(seg[:, c:c + 1], pcar[c][:]), reads=[pcar[c].b], writes=[seg.b])
        s.op("act", lambda e, c=c: e.copy(seg[:, 4 + c:5 + c], hcar[c][:]), reads=[hcar[c].b], writes=[seg.b])
    s.dma("sp", D["seg"], seg[:], reads=[seg.b])


def phase2(kb, C, D):
    s = kb.s
    NT = C["NTOK"]
    TS = 512
    NCf = 22
    kb.D = D
    W = norm_scratch(kb)
    gffn = kb.sb([128, 8], F32, "gffn0")
    s.dma("sp", gffn[:], D["gffn0"], writes=[gffn.b])
    gmix = kb.sb([128, 8], F32, "gmix1")
    s.dma("sp", gmix[:], D["gmix1"], writes=[gmix.b])
    wo = kb.sb([128, 8, 1024], BF16, "wo0")
    s.dma("sp", wo[:], D["wo0"].rearrange("p (k n) -> p k n", n=1024), writes=[wo.b])
    sega = kb.sb([128, 4, 8], F32, "sega")
    s.dma("sp", sega[:], D["segall"].rearrange("(r p) n -> p r n", p=128), writes=[sega.b])
    sel = kb.sb([128, 4], F32, "sel")
    s.dma("sp", sel[:], D["sel"], writes=[sel.b])
    hin = kb.sb([128, 4], F32, "hin")
    tmp = kb.sb([128, 4], F32, "tmpc")
    s.op("pool", lambda e: e.memset(hin[:], 0.0), writes=[hin.b])
    for i in range(3):
        s.op("dve", lambda e, i=i: e.tensor_tensor(tmp[:], sega[:, i, 0:4], hin[:], ALU.mult), reads=[sega.b, hin.b], writes=[tmp.b])
        s.op("dve", lambda e, i=i: e.tensor_tensor(tmp[:], tmp[:], sega[:, i, 4:8], ALU.add), reads=[sega.b, tmp.b], writes=[tmp.b])
        s.op("dve", lambda e: e.tensor_tensor(tmp[:], tmp[:], hin[:], ALU.subtract), reads=[tmp.b, hin.b], writes=[tmp.b])
        s.op("dve", lambda e, i=i: e.scalar_tensor_tensor(hin[:], tmp[:], sel[:, i:i + 1], hin[:], ALU.mult, ALU.add),
             reads=[tmp.b, sel.b, hin.b], writes=[hin.b])
    r = ffn_alloc(kb, TS, NCf)
    hnT = [kb.sb([128, 8, TS], BF16, "hnT") for _ in range(2)]
    acc = kb.sb([128, TS // 128, 1024], F32, "acc")
    accb = [Buf() for _ in range(TS // 128)]
    xt = [kb.sb([128, 1024], F32, "xt") for _ in range(2)]
    yl = kb.sb([128, 4, TS], BF16, "yl")
    pg = kb.sb([128, 4, TS], BF16, "pgt")
    ya = kb.sb([128, 4, TS], BF16, "ya")
    ym = kb.sb([128, 4, TS], BF16, "ym")
    hn1 = [kb.sb([128, 8, 128], BF16, "hn1") for _ in range(2)]
    it = 0
    for st in range(NT // TS):
        cols = slice(st * TS, (st + 1) * TS)
        s.dma("act", yl[:], D["yloc"].rearrange("(c p) n -> p c n", p=128)[:, :, cols], writes=[yl.b])
        s.dma("act", pg[:], D["pg"].rearrange("(c p) n -> p c n", p=128)[:, :, cols], writes=[pg.b])
        s.dma("act", ya[:], D["yatt"].rearrange("(c p) n -> p c n", p=128)[:, :, cols], writes=[ya.b])
        for c in range(4):
            s.op("dve", lambda e, c=c: e.scalar_tensor_tensor(ym[:, c, :], pg[:, c, :], hin[:, c:c + 1], yl[:, c, :], ALU.mult, ALU.add),
                 reads=[pg.b, hin.b, yl.b], writes=[ym.b])
        h_ = hnT[st % 2]
        for t in range(TS // 128):
            tok0 = st * TS + t * 128
            tc_ = slice(t * 128, (t + 1) * 128)
            x_ = xt[it % 2]
            it += 1
            s.dma("act", x_[:], D["x"][tok0:tok0 + 128, :], writes=[x_.b])
            for half in range(2):
                bk = nbank(kb)
                mm_group(kb, bk, bk[:, :], [((ym[:, c, tc_] if c < 4 else ya[:, c - 4, tc_]), wo[:, c, half * 512:(half + 1) * 512])
                                            for c in range(8)], reads=[ym.b, ya.b, wo.b])
                s.op("dve", lambda e, bk=bk, x_=x_, t=t, half=half: e.tensor_tensor(
                    acc[:, t, half * 512:(half + 1) * 512], x_[:, half * 512:(half + 1) * 512], bk[:, :], ALU.add),
                    reads=[bk.b, x_.b], writes=[accb[t]])
            norm_T(kb, W, acc[:, t, :], accb[t], gffn, h_, tc_)

        def evac(t, half, bk):
            s.op("dve", lambda e: e.tensor_tensor(acc[:, t, half * 512:(half + 1) * 512], acc[:, t, half * 512:(half + 1) * 512],
                                                  bk[:, :], ALU.add), reads=[bk.b, accb[t]], writes=[accb[t]])
        ffn_expert(kb, r, h_, D["fwg"], D["fwu"], D["fwd"], NCf, evac)
        agt = []
        for t in range(TS // 128):
            tok0 = st * TS + t * 128
            s.dma("act", D["h1"][tok0:tok0 + 128, :], acc[:, t, :], reads=[accb[t]])
            o_ = hn1[t % 2]
            norm_T(kb, W, acc[:, t, :], accb[t], gmix, o_, slice(0, 128))
            if C.get("ag_hn"):
                tk_ = s.dma("act", D["hn1T"][st * 1024:(st + 1) * 1024, :].rearrange("(c p) n -> p c n", p=128)[:, :, t * 128:(t + 1) * 128],
                            o_[:], reads=[o_.b])
                agt.append(tk_)
            else:
                s.dma("act", D["hn1T"].rearrange("(c p) n -> p c n", p=128)[:, :, tok0:tok0 + 128], o_[:], reads=[o_.b])
        if C.get("ag_hn"):
            s.collective("AllGather", D["hn1T"][st * 1024:(st + 1) * 1024, :], D["hng"][st * 4096:(st + 1) * 4096, :], C["ag_hn"], deps=agt)


def phase3(kb, C, D):
    s = kb.s
    NTF, NTK = C["NTF"], C["NTK"]
    TS = 512
    NST = NTF // TS
    kb.D = D
    W = norm_scratch(kb)
    ident, identb = W["ident"], W["identb"]

    def ld(name, shape, dt=F32, src=None):
        t = kb.sb(shape, dt, name)
        s.dma("sp", t[:], D[name] if src is None else src, writes=[t.b])
        return t

    wqkv = kb.sb([128, 8, 768], BF16, "wqkv")
    s.dma("sp", wqkv[:], D["wqkv"].rearrange("p (k n) -> p k n", n=768), writes=[wqkv.b])
    wgate = kb.sb([128, 8, 260], BF16, "wgab")
    s.dma("sp", wgate[:, :, 0:256], D["wgate"].rearrange("p (k n) -> p k n", n=256), writes=[wgate.b])
    s.dma("sp", wgate[:, :, 256:260], D["wab"].rearrange("p (k n) -> p k n", n=4), writes=[wgate.b])
    abS = kb.sb([128, 4, 4], F32, "abS")
    convw = ld("convw", [128, 6, 4])
    hc = ld("hconst", [128, 4])
    onw = ld("onw_b", [128, 128])
    U = ld("maskU", [128, 128])
    Lo = ld("maskL", [128, 128])
    Bs = ld("maskB", [128, 128])
    C0 = ld("maskC0", [128, 128])
    C1 = ld("maskC1", [128, 128])
    nalog = kb.sb([128, 2], F32, "nalog")
    s.op("act", lambda e: e.activation(nalog[:], hc[:, 0:2], AF.Exp), reads=[hc.b], writes=[nalog.b])
    s.op("dve", lambda e: e.tensor_scalar(nalog[:], nalog[:], -1.0, None, ALU.mult), reads=[nalog.b], writes=[nalog.b])
    onesb = kb.sb([128, 128], BF16, "onesb")
    s.op("pool", lambda e: e.memset(onesb[:], 1.0), writes=[onesb.b])

    hnT = [kb.sb([128, 8, TS], BF16, "hnT") for _ in range(2)]
    cb = [[kb.sb([128, 3 + TS], F32, "cb") for _ in range(2)] for _ in range(6)]
    for b in range(6):
        s.op("pool", lambda e, b=b: e.memset(cb[b][0][:, 0:3], 0.0), writes=[cb[b][0].b])
    cacc = [kb.sb([128, TS], F32, "cacc") for _ in range(2)]
    csl = [kb.sb([128, TS], F32, "csl") for _ in range(2)]
    sqb = [kb.sb([128, TS], BF16, "sqb") for _ in range(2)]
    lnr = [kb.sb([128, TS], F32, "lnr") for _ in range(2)]
    FTa = [kb.sb([128, 6, TS], BF16, "FTa") for _ in range(2)]
    sgate = [[kb.sb([128, 256], F32, "sgate") for _ in range(4)] for _ in range(2)]
    sc = {k: kb.sb([128, 4, 2], F32, k) for k in ("xa", "gtm", "beta")}
    cs8 = [{k: kb.sb([128, 8], F32, k) for k in ("eG", "eGlG", "egl0", "egl1", "bEG", "dGl")} for _ in range(2)]
    S32 = [kb.sb([128, 128], F32, "S32") for _ in range(2)]
    Sbf = [kb.sb([128, 128], BF16, "Sbf") for _ in range(2)]
    for h in range(2):
        s.op("pool", lambda e, h=h: e.memset(S32[h][:], 0.0), writes=[S32[h].b])
        s.op("pool", lambda e, h=h: e.memset(Sbf[h][:], 0.0), writes=[Sbf[h].b])

    def mkset(bf_names, f_names, small=()):
        d = {}
        for k in bf_names:
            d[k] = kb.sb([128, 256] if k == "VK" else [128, 128], BF16, k)
        for k in f_names:
            d[k] = kb.sb([128, 128], F32, k)
        for k in small:
            d[k] = kb.sb([128, 1], F32, k)
        return d
    HO = [[[mkset(("wT", "kdec", "aqkT"), ("u",)) for _ in range(2)] for _ in range(4)] for _ in range(2)]
    PT = [[mkset(("VK", "Lb", "Nb", "P", "Q", "XL", "XN", "P2", "Q2", "XL2", "XN2", "wtok"), ("gL", "E", "ET", "t1", "t2"))
           for _ in range(2)] for _ in range(4)]
    STp = [mkset(("vn", "ogb"), ("av", "O", "on"), ("ss", "rt", "rstd")) for _ in range(2)]
    ogs = [kb.sb([128, 2, TS], BF16, "ogs") for _ in range(2)]

    def evc(en, out, src, reads, wbuf, scale=None):
        if en == "act":
            if scale is None:
                s.op("act", lambda e: e.copy(out, src), reads=reads, writes=[wbuf])
            else:
                s.op("act", lambda e: e.activation(out, src, AF.Copy, scale=scale), reads=reads, writes=[wbuf])
        else:
            if scale is None:
                s.op(en, lambda e: e.tensor_copy(out, src), reads=reads, writes=[wbuf])
            else:
                s.op(en, lambda e: e.tensor_scalar(out, src, scale, None, ALU.mult), reads=reads, writes=[wbuf])

    def pre_gen(st):
        par = st % 2
        h_ = hnT[par]
        F_ = FTa[par]
        c8 = cs8[par]
        tok0 = st * TS
        rk, col0 = tok0 // NTK, tok0 % NTK
        if C.get("chunked") == 2:
            stl_ = col0 // TS
            s.dma("act", h_[:], D["hng"][stl_ * 4096 + rk * 1024:stl_ * 4096 + (rk + 1) * 1024, :].rearrange("(c p) n -> p c n", p=128),
                  writes=[h_.b])
        elif C.get("chunked"):
            s.dma("act", h_[:], D["hng"].rearrange("(c r p) n -> p c r n", r=4, p=128)[:, :, rk, col0:col0 + TS], writes=[h_.b])
        else:
            s.dma("act", h_[:], D["hng"][rk * 1024:(rk + 1) * 1024, :].rearrange("(c p) n -> p c n", p=128)[:, :, col0:col0 + TS],
                  writes=[h_.b])
        for b in range(6):
            c_ = cb[b][par]
            cn = cb[b][(st + 1) % 2]
            ca, cl_, sq_, ln_ = cacc[b % 2], csl[b % 2], sqb[b % 2], lnr[b % 2]
            bk = nbank(kb)
            mm_group(kb, bk, bk[:, 0:TS], [(wqkv[:, k, b * 128:(b + 1) * 128], h_[:, k, :]) for k in range(8)], reads=[wqkv.b, h_.b])
            s.op("act", lambda e: e.copy(c_[:, 3:3 + TS], bk[:, 0:TS]), reads=[bk.b], writes=[c_.b])
            s.op("pool", lambda e: e.tensor_copy(cn[:, 0:3], c_[:, TS:TS + 3]), reads=[c_.b], writes=[cn.b])
            s.op("dve", lambda e: e.tensor_scalar(ca[:], c_[:, 0:TS], convw[:, b, 0:1], None, ALU.mult),
                 reads=[c_.b, convw.b], writes=[ca.b])
            for j in range(1, 4):
                s.op("dve", lambda e: e.scalar_tensor_tensor(ca[:], c_[:, j:j + TS], convw[:, b, j:j + 1], ca[:], ALU.mult, ALU.add),
                     reads=[c_.b, convw.b, ca.b], writes=[ca.b])
            yield
            if b >= 4:
                s.op("act", lambda e: e.activation(F_[:, b, :], ca[:], AF.Silu), reads=[ca.b], writes=[F_.b])
                continue
            s.op("act", lambda e: e.activation(cl_[:], ca[:], AF.Silu), reads=[ca.b], writes=[cl_.b])
            s.op("act", lambda e: e.activation(sq_[:], cl_[:], AF.Square), reads=[cl_.b], writes=[sq_.b])
            b2 = nbank(kb)
            mm_group(kb, b2, b2[:, 0:TS], [(onesb[:], sq_[:])], reads=[onesb.b, sq_.b])
            s.op("act", lambda e: e.activation(ln_[:], b2[:, 0:TS], AF.Ln, bias=kb.eps_t[:]), reads=[b2.b], writes=[ln_.b])
            s.op("act", lambda e: e.activation(ln_[:], ln_[:], AF.Exp, scale=-0.5), reads=[ln_.b], writes=[ln_.b])
            s.op("dve", lambda e: e.scalar_tensor_tensor(F_[:, b, :], cl_[:], (128.0 ** -0.5) if b in (0, 2) else 1.0, ln_[:], ALU.mult, ALU.mult),
                 reads=[cl_.b, ln_.b], writes=[F_.b])
            yield
        for t in range(4):
            tc_ = slice(t * 128, (t + 1) * 128)
            bk = nbank(kb)
            sg_ = sgate[par][t]
            mm_group(kb, bk, bk[:, 0:260], [(h_[:, k, tc_], wgate[:, k, :]) for k in range(8)], reads=[wgate.b, h_.b])
            s.op("act", lambda e: e.activation(sg_[:], bk[:, 0:256], AF.Silu), reads=[bk.b], writes=[sg_.b])
            s.op("act", lambda e: e.copy(abS[:, t, :], bk[:, 256:260]), reads=[bk.b], writes=[abS.b])
            yield
        abv = abS
        s.op("dve", lambda e: e.tensor_tensor(sc["xa"][:], abv[:, :, 0:2], bc(hc[:, 2:4].unsqueeze(1), [128, 4, 2]), ALU.add),
             reads=[abS.b, hc.b], writes=[sc["xa"].b])
        s.op("act", lambda e: e.activation(sc["beta"][:], abv[:, :, 2:4], AF.Sigmoid), reads=[abS.b], writes=[sc["beta"].b])
        s.op("act", lambda e: e.activation(sc["xa"][:], sc["xa"][:], AF.Exp), reads=[sc["xa"].b], writes=[sc["xa"].b])
        s.op("act", lambda e: e.activation(sc["xa"][:], sc["xa"][:], AF.Ln, bias=1.0), reads=[sc["xa"].b], writes=[sc["xa"].b])
        s.op("dve", lambda e: e.tensor_tensor(sc["gtm"][:], sc["xa"][:], bc(nalog[:].unsqueeze(1), [128, 4, 2]), ALU.mult),
             reads=[sc["xa"].b, nalog.b], writes=[sc["gtm"].b])
        g8 = sc["gtm"][:].rearrange("p t n -> p (t n)")
        b8 = sc["beta"][:].rearrange("p t n -> p (t n)")
        bcs = nbank(kb)
        for i, m in enumerate((U, Bs, C0, C1)):
            mm_group(kb, bcs, bcs[:, i * 8:(i + 1) * 8], [(m[:], g8)], reads=[m.b, sc["gtm"].b])
        s.op("act", lambda e: e.activation(c8["eG"][:], bcs[:, 0:8], AF.Exp), reads=[bcs.b], writes=[c8["eG"].b])
        s.op("act", lambda e: e.copy(c8["dGl"][:], bcs[:, 8:16]), reads=[bcs.b], writes=[c8["dGl"].b])
        s.op("act", lambda e: e.activation(c8["egl0"][:], bcs[:, 16:24], AF.Exp), reads=[bcs.b], writes=[c8["egl0"].b])
        s.op("act", lambda e: e.activation(c8["egl1"][:], bcs[:, 24:32], AF.Exp), reads=[bcs.b], writes=[c8["egl1"].b])
        s.op("dve", lambda e: e.tensor_tensor(c8["dGl"][:], c8["dGl"][:], bcs[:, 0:8], ALU.subtract), reads=[bcs.b, c8["dGl"].b], writes=[c8["dGl"].b])
        s.op("act", lambda e: e.activation(c8["eGlG"][:], c8["dGl"][:], AF.Exp), reads=[c8["dGl"].b], writes=[c8["eGlG"].b])
        s.op("dve", lambda e: e.tensor_tensor(c8["bEG"][:], c8["eG"][:], b8, ALU.mult), reads=[c8["eG"].b, sc["beta"].b], writes=[c8["bEG"].b])
        yield
        chains = [(t, h) for t in range(4) for h in range(2)]
        for (t, h) in chains:
            tc_ = slice(t * 128, (t + 1) * 128)
            col = slice(t * 2 + h, t * 2 + h + 1)
            d = PT[t][h]
            o = HO[par][t][h]
            bk = nbank(kb)
            bv = bk[:, :].bitcast(BF16)
            mm_group(kb, bk, [bv[:, 0:128], bv[:, 128:256]], [(F_[:, 2 * h + 1, tc_], identb[:]), (F_[:, 4 + h, tc_], identb[:])],
                     reads=[F_.b, identb.b], transpose=True)
            evc("act", d["VK"][:, 0:128], bv[:, 128:256], [bk.b, sc["beta"].b], d["VK"].b, scale=b8[:, col])
            evc("act", d["VK"][:, 128:256], bv[:, 0:128], [bk.b, c8["bEG"].b], d["VK"].b, scale=c8["bEG"][:, col])
            evc("act", o["kdec"][:], bv[:, 0:128], [bk.b, c8["eGlG"].b], o["kdec"].b, scale=c8["eGlG"][:, col])
            s.op("pool", lambda e: e.tensor_scalar(d["gL"][:], Lo[:], g8[:, col], None, ALU.mult),
                 reads=[Lo.b, sc["gtm"].b], writes=[d["gL"].b])
            bD = nbank(kb)
            mm_group(kb, bD, bD[:, 0:128], [(U[:], d["gL"][:])], reads=[U.b, d["gL"].b])
            mm_group(kb, bD, bD[:, 128:256], [(d["gL"][:], U[:])], reads=[U.b, d["gL"].b])
            s.op("act", lambda e: e.activation(d["E"][:], bD[:, 0:128], AF.Exp), reads=[bD.b], writes=[d["E"].b])
            s.op("act", lambda e: e.activation(d["ET"][:], bD[:, 128:256], AF.Exp), reads=[bD.b], writes=[d["ET"].b])
            bK = nbank(kb)
            mm_group(kb, bK, bK[:, 0:256].rearrange("p (a n) -> p a n", n=128), [(F_[:, 2 * h + 1, tc_], F_[:, 2 * h:2 * h + 2, tc_])], reads=[F_.b])
            s.op("dve", lambda e: e.scalar_tensor_tensor(d["t1"][:], bK[:, 128:256], b8[:, col], d["E"][:], ALU.mult, ALU.mult),
                 reads=[bK.b, sc["beta"].b, d["E"].b], writes=[d["t1"].b])
            s.op("dve", lambda e: e.tensor_tensor(d["t2"][:], bK[:, 0:128], d["ET"][:], ALU.mult),
                 reads=[bK.b, d["ET"].b], writes=[d["t2"].b])
            s.op("pool", lambda e: e.tensor_tensor(d["Lb"][:], d["t1"][:], Lo[:], ALU.mult), reads=[d["t1"].b, Lo.b], writes=[d["Lb"].b])
            s.op("pool", lambda e: e.tensor_tensor(o["aqkT"][:], d["t2"][:], U[:], ALU.mult), reads=[d["t2"].b, U.b], writes=[o["aqkT"].b])
            yield
        for (t, h) in chains:
            d = PT[t][h]
            bN = nbank(kb)
            bNv = bN[:, :].bitcast(BF16)
            mm_group(kb, bN, [bNv[:, 0:128]], [(d["Lb"][:], identb[:])], reads=[d["Lb"].b, identb.b], transpose=True)
            evc("act", d["Nb"][:], bNv[:, 0:128], [bN.b], d["Nb"].b)
            s.op("pool", lambda e: e.tensor_tensor(d["P"][:], identb[:], d["Nb"][:], ALU.subtract), reads=[identb.b, d["Nb"].b], writes=[d["P"].b])
        yield
        cur = {ch: dict(XL=PT[ch[0]][ch[1]]["Lb"], XN=PT[ch[0]][ch[1]]["Nb"], P=PT[ch[0]][ch[1]]["P"]) for ch in chains}
        for lvl in range(5):
            alt = (lvl % 2 == 0)
            last = (lvl == 4)
            for i_, ch in enumerate(chains):
                d = PT[ch[0]][ch[1]]
                c_ = cur[ch]
                nXL, nXN = (d["XL"], d["XN"]) if alt else (d["XL2"], d["XN2"])
                bq = nbank(kb)
                mm_group(kb, bq, bq[:, 0:128], [(c_["XN"][:], c_["XL"][:])], reads=[c_["XN"].b, c_["XL"].b])
                if not last:
                    mm_group(kb, bq, bq[:, 128:256], [(c_["XL"][:], c_["XN"][:])], reads=[c_["XN"].b, c_["XL"].b])
                en = "act" if i_ % 2 == 0 else "dve"
                evc(en, nXL[:], bq[:, 0:128], [bq.b], nXL.b)
                if not last:
                    evc(en, nXN[:], bq[:, 128:256], [bq.b], nXN.b)
                c_["XL"], c_["XN"] = nXL, nXN
                if i_ % 2 == 1:
                    yield
            for i_, ch in enumerate(chains):
                d = PT[ch[0]][ch[1]]
                c_ = cur[ch]
                nP = d["P2"] if alt else d["P"]
                Po = c_["P"]
                bp = nbank(kb)
                mm_group(kb, bp, bp[:, 0:128], [(c_["XL"][:], Po[:])], reads=[Po.b, c_["XL"].b])
                s.op("dve", lambda e: e.tensor_tensor(nP[:], Po[:], bp[:, 0:128], ALU.add), reads=[Po.b, bp.b], writes=[nP.b])
                c_["P"] = nP
                if i_ % 2 == 1:
                    yield
        for ch in chains:
            d = PT[ch[0]][ch[1]]
            o = HO[par][ch[0]][ch[1]]
            TT = cur[ch]["P"]
            bu = nbank(kb)
            mm_group(kb, bu, bu[:, 0:256], [(TT[:], d["VK"][:])], reads=[TT.b, d["VK"].b])
            evc("act", o["u"][:], bu[:, 0:128], [bu.b], o["u"].b)
            evc("act", d["wtok"][:], bu[:, 128:256], [bu.b], d["wtok"].b)
            bw = nbank(kb)
            bwv = bw[:, :].bitcast(BF16)
            mm_group(kb, bw, [bwv[:, 0:128]], [(d["wtok"][:], identb[:])], reads=[d["wtok"].b, identb.b], transpose=True)
            evc("act", o["wT"][:], bwv[:, 0:128], [bw.b], o["wT"].b)
            yield

    def scan_gen(st):
        par = st % 2
        F_ = FTa[par]
        c8 = cs8[par]
        og_ = ogs[par]
        tok0 = st * TS
        for t in range(4):
            tc_ = slice(t * 128, (t + 1) * 128)
            for ci in range(2):
                pr = slice(64 * ci, 64 * ci + 64)
                cc = slice(t * 128 + 64 * ci, t * 128 + 64 * ci + 64)
                egl = c8["egl0"] if ci == 0 else c8["egl1"]
                for h in range(2):
                    col = slice(t * 2 + h, t * 2 + h + 1)
                    o = HO[par][t][h]
                    d = STp[h]
                    bs = nbank(kb)
                    mm_group(kb, bs, bs[pr, 0:128], [(o["wT"][:, pr], Sbf[h][:])], reads=[o["wT"].b, Sbf[h].b])
                    mm_group(kb, bs, bs[pr, 128:256], [(F_[:, 2 * h, cc], Sbf[h][:])], reads=[F_.b, Sbf[h].b])
                    s.op("dve", lambda e: e.tensor_tensor(d["vn"][pr, :], o["u"][pr, :], bs[pr, 0:128], ALU.subtract),
                         reads=[o["u"].b, bs.b], writes=[d["vn"].b])
                    b2 = nbank(kb)
                    mm_group(kb, b2, b2[:, 0:128], [(o["kdec"][pr, :], d["vn"][pr, :])], reads=[o["kdec"].b, d["vn"].b])
                    mm_group(kb, b2, b2[pr, 128:256], [(o["aqkT"][pr, pr], d["vn"][pr, :])], reads=[o["aqkT"].b, d["vn"].b])
                    s.op("dve", lambda e: e.scalar_tensor_tensor(S32[h][:], S32[h][:], egl[:, col], b2[:, 0:128], ALU.mult, ALU.add),
                         reads=[S32[h].b, egl.b, b2.b], writes=[S32[h].b])
                    s.op("dve", lambda e: e.tensor_copy(Sbf[h][:], S32[h][:]), reads=[S32[h].b], writes=[Sbf[h].b])
                    s.op("dve", lambda e: e.tensor_copy(d["av"][pr, :], b2[pr, 128:256]), reads=[b2.b], writes=[d["av"].b])
                    s.op("dve", lambda e: e.scalar_tensor_tensor(d["O"][pr, :], bs[pr, 128:256], c8["eG"][pr, col], d["av"][pr, :], ALU.mult, ALU.add),
                         reads=[bs.b, c8["eG"].b, d["av"].b], writes=[d["O"].b])
                    yield
            for h in range(2):
                d = STp[h]
                sg_ = sgate[par][t]
                s.op("act", lambda e: e.activation(d["on"][:], d["O"][:], AF.Square, accum_out=d["ss"][:]), reads=[d["O"].b], writes=[d["on"].b, d["ss"].b])
                s.op("act", lambda e: e.activation(d["rt"][:], d["ss"][:], AF.Sqrt, bias=kb.eps_t[:], scale=1.0 / 128.0), reads=[d["ss"].b], writes=[d["rt"].b])
                s.op("dve", lambda e: e.reciprocal(d["rstd"][:], d["rt"][:]), reads=[d["rt"].b], writes=[d["rstd"].b])
                s.op("dve", lambda e: e.scalar_tensor_tensor(d["on"][:], d["O"][:], d["rstd"][:], onw[:], ALU.mult, ALU.mult),
                     reads=[d["O"].b, d["rstd"].b, onw.b], writes=[d["on"].b])
                s.op("pool", lambda e: e.tensor_tensor(d["ogb"][:], d["on"][:], sg_[:, h * 128:(h + 1) * 128], ALU.mult),
                     reads=[d["on"].b, sg_.b], writes=[d["ogb"].b])
                bo = nbank(kb)
                bov = bo[:, :].bitcast(BF16)
                mm_group(kb, bo, [bov[:, 0:128]], [(d["ogb"][:], identb[:])], reads=[d["ogb"].b, identb.b], transpose=True)
                evc("act", og_[:, h, tc_], bov[:, 0:128], [bo.b], og_.b)
                yield
        if C.get("chunked"):
            b_, c0_ = tok0 // 2048, tok0 % 2048
            tk_ = s.dma("sp", D["ogt"][b_ * 256:(b_ + 1) * 256, :].rearrange("(h p) n -> p h n", p=128)[:, :, c0_:c0_ + TS], og_[:], reads=[og_.b])
            if C.get("ag_og"):
                ogtoks.append(tk_)
                if c0_ + TS == 2048:
                    s.collective("AllGather", D["ogt"][b_ * 256:(b_ + 1) * 256, :], D["ogt_all"][b_ * 1024:(b_ + 1) * 1024, :], C["ag_og"],
                                 deps=list(ogtoks))
                    del ogtoks[:]
        else:
            s.dma("sp", D["ogt"].rearrange("(h p) n -> p h n", p=128)[:, :, tok0:tok0 + TS], og_[:], reads=[og_.b])

    ogtoks = []
    for _ in pre_gen(0):
        pass
    RATIO = C.get("ratio", 3)
    for st in range(NST):
        gs = scan_gen(st)
        gp = pre_gen(st + 1) if st + 1 < NST else iter(())
        alive_s = alive_p = True
        while alive_s or alive_p:
            if alive_s:
                try:
                    next(gs)
                except StopIteration:
                    alive_s = False
            for _ in range(RATIO):
                if alive_p:
                    try:
                        next(gp)
                    except StopIteration:
                        alive_p = False


def kmajor(W):
    K, N = W.shape
    return np.ascontiguousarray(W.reshape(K // 128, 128, N).transpose(1, 0, 2).reshape(128, -1))

def blocks(W, FB=256):
    K, F = W.shape
    NB = F // FB
    return np.ascontiguousarray(W.reshape(8, 128, NB, FB).transpose(1, 2, 0, 3).reshape(128, -1))

def pcol(v):
    return np.ascontiguousarray(v.reshape(-1, 128).T)

def prep_p1(inp, NT, nseg):
    x = inp["x"]
    B, S, _ = x.shape
    f32 = np.float32
    win = kmajor(inp["e_w_in"][0])
    lruc = np.zeros((128, 4, 8), f32)
    cw = inp["e_lru_conv_w"][0]
    for c in range(4):
        sl = slice(c * 128, (c + 1) * 128)
        for j in range(4):
            lruc[:, c, j] = cw[j, sl]
        lruc[:, c, 4] = inp["e_lru_conv_b"][0][sl]
        lruc[:, c, 5] = inp["e_lru_b_a"][0][sl]
        lruc[:, c, 6] = inp["e_lru_b_i"][0][sl]
        lruc[:, c, 7] = inp["e_lru_lambda"][0][sl]
    def bd(w):
        o = np.zeros((128, 4, 128), f32)
        for c in range(4):
            o[0:64, c, 0:64] = w[2 * c]
            o[64:128, c, 64:128] = w[2 * c + 1]
        return o
    prot = np.zeros((64, 64), f32)
    for m in range(32):
        prot[m + 32, m] = -1.0
        prot[m, m + 32] = 1.0
    k_ = np.arange(128)[:, None]; q_ = np.arange(128)[None, :]
    mcur = (k_ <= q_).astype(f32); mprev = (k_ > q_).astype(f32)
    half = 32
    inv_freq = (10000.0 ** (-np.arange(half, dtype=np.float32) / half)).astype(f32)
    common = dict(win32=win, lruc=lruc, wa_bd=bd(inp["e_lru_w_a"][0]), wi_bd=bd(inp["e_lru_w_i"][0]),
                  qkg=np.stack([inp["e_q_norm"][0], inp["e_k_norm"][0]], 1).astype(f32),
                  sinks_b=np.broadcast_to(inp["e_sinks"][0][None, :], (128, 8)).astype(f32).copy(),
                  prot=prot, mask_cur=mcur, mask_prev=mprev, gmix0=pcol(inp["ln_mix"][0]),
                  ident=np.eye(128, dtype=f32))
    outs = []
    for b in range(B):
        for j in range(nseg):
            t0 = j * NT
            d = dict(common)
            d["x"] = np.ascontiguousarray(x[b, t0:t0 + NT])
            d["xhalo"] = np.ascontiguousarray(x[b, t0 - 128:t0]) if j > 0 else np.zeros((128, 1024), f32)
            d["mask_prev0"] = mprev if j > 0 else np.zeros((128, 128), f32)
            pos = np.arange(t0 - 128, t0 + NT).astype(f32)
            ang = pos[None, :] * inv_freq[:, None]
            cs = np.zeros((64, 2, NT + 128), f32)
            cs[0:32, 0] = np.cos(ang); cs[32:64, 0] = np.cos(ang)
            cs[0:32, 1] = np.sin(ang); cs[32:64, 1] = np.sin(ang)
            d["cossin"] = cs
            outs.append(d)
    return outs


def gdn_masks():
    f32 = np.float32
    k = np.arange(128)[:, None]; i = np.arange(128)[None, :]
    same = (k // 64) == (i // 64)
    return dict(maskU=((k <= i) & same).astype(f32), maskL=((k > i) & same).astype(f32), maskB=same.astype(f32),
                maskC0=np.broadcast_to(k < 64, (128, 128)).astype(f32).copy(),
                maskC1=np.broadcast_to(k >= 64, (128, 128)).astype(f32).copy())


def prep_p3(inp, hp):
    f32 = np.float32
    w = inp["o_w_in"][0]
    hs = [2 * hp, 2 * hp + 1]
    cols = [off + h * 128 + np.arange(128) for (off, h) in ((0, hs[0]), (1024, hs[0]), (0, hs[1]), (1024, hs[1]), (2048, hs[0]), (2048, hs[1]))]
    wqkv = w[:, np.concatenate(cols)]
    wgate = w[:, np.concatenate([3072 + h * 128 + np.arange(128) for h in hs])]
    wab = w[:, [4096 + hs[0], 4096 + hs[1], 4104 + hs[0], 4104 + hs[1]]]
    cw = inp["o_conv_w"][0]
    convw = np.zeros((128, 6, 4), f32)
    for b, c in enumerate(cols):
        convw[:, b, :] = cw[:, c].T
    hconst = np.zeros((128, 4), f32)
    hconst[:, 0:2] = inp["o_a_log"][0][hs][None, :]
    hconst[:, 2:4] = inp["o_dt_bias"][0][hs][None, :]
    d = dict(wqkv32=kmajor(wqkv), wgate32=kmajor(wgate), wab32=kmajor(np.ascontiguousarray(wab)), convw=convw, hconst=hconst,
             onw_b=np.broadcast_to(inp["o_out_norm"][0][None, :], (128, 128)).astype(f32).copy(), ident=np.eye(128, dtype=f32))
    d.update(gdn_masks())
    return d


NT_CORE = 4096
NSEG = 4


def _mk_dram(kb, h, outs, scratch):
    D = {}
    for k, v in h.items():
        D[k] = kb.dram(k, list(v.shape), F32 if v.dtype == np.float32 else BF16, "ExternalInput")
    for k, (shape, dt) in scratch.items():
        D[k] = kb.dram(k, shape, dt)
    for k, (shape, dt) in outs.items():
        D[k] = kb.dram(k, shape, dt, "ExternalOutput")
    return D


def _build(h, outs, scratch, casts, phase, C):
    nc = bass.Bass("TRN2", target_bir_lowering=False)
    kb = KB(nc)
    D = _mk_dram(kb, h, outs, scratch)
    pairs = []
    for src, dst, rows in casts:
        for r0 in range(0, rows, 128):
            pairs.append((D[src][r0:r0 + 128, :], D[dst][r0:r0 + 128, :]))
    cast_dram(kb, pairs)
    kb.phase_end()
    phase(kb, C, D)
    kb.phase_end()
    kb.s.finish()
    return nc


def _run(nc, maps):
    res = run_bass_kernel_spmd(nc, maps, core_ids=list(range(len(maps))))
    return res.results


def kernel_unfused(**inputs):
    inp = {k: np.asarray(v) for k, v in inputs.items()}
    f32 = np.float32
    x = inp["x"]
    B, S, _ = x.shape
    NT = NT_CORE
    ncore = B * NSEG
    eye = np.eye(128, dtype=f32)
    hA = prep_p1(inp, NT, NSEG)
    ncA = _build(hA[0], dict(yloc=([512, NT], BF16), pg=([512, NT], BF16), yatt=([512, NT], BF16), seg=([128, 8], F32)),
                 dict(win=([128, 8 * 1792], BF16)), [("win32", "win", 128)], phase1, dict(NTOK=NT))
    rA = _run(ncA, hA)
    hB = []
    cB = dict(ident=eye, gffn0=pcol(inp["ln_ffn"][0]), gmix1=pcol(inp["ln_mix"][1]), wo032=kmajor(inp["e_w_out"][0]),
              fwg32=blocks(inp["e_ffn_w_gate"][0]), fwu32=blocks(inp["e_ffn_w_up"][0]), fwd32=kmajor(inp["e_ffn_w_down"][0]))
    for b in range(B):
        segall = np.concatenate([rA[b * NSEG + j]["seg"] for j in range(NSEG)], 0)
        for j in range(NSEG):
            c = b * NSEG + j
            sel = np.zeros((128, 4), f32)
            sel[:, :j] = 1.0
            d = dict(cB)
            d.update(x=hA[c]["x"], yloc=rA[c]["yloc"], pg=rA[c]["pg"], yatt=rA[c]["yatt"], segall=segall, sel=sel)
            hB.append(d)
    ncB = _build(hB[0], dict(h1=([NT, 1024], F32), hn1T=([1024, NT], BF16)),
                 dict(wo0=([128, 8192], BF16), fwg=([128, 22 * 1024], BF16), fwu=([128, 22 * 1024], BF16), fwd=([128, 22 * 1024], BF16)),
                 [("wo032", "wo0", 128), ("fwg32", "fwg", 128), ("fwu32", "fwu", 128), ("fwd32", "fwd", 128)], phase2, dict(NTOK=NT))
    rB = _run(ncB, hB)
    hC = []
    for b in range(B):
        hng = np.concatenate([rB[b * NSEG + j]["hn1T"] for j in range(NSEG)], 0)
        for hp_ in range(NSEG):
            d = prep_p3(inp, hp_)
            d["hng"] = hng
            hC.append(d)
    ncC = _build(hC[0], dict(ogt=([256, S], BF16)),
                 dict(wqkv=([128, 8 * 768], BF16), wgate=([128, 8 * 256], BF16), wab=([128, 32], BF16)),
                 [("wqkv32", "wqkv", 128), ("wgate32", "wgate", 128), ("wab32", "wab", 128)], phase3, dict(NTF=S, NTK=NT))
    rC = _run(ncC, hC)
    NE = inp["o_moe_w_gate"].shape[1]
    NCm = inp["o_moe_w_gate"].shape[3] // 128
    cD = dict(ident=eye, gffn1=pcol(inp["ln_ffn"][1]), router=kmajor(inp["o_router"][0]), wo1_32=kmajor(inp["o_w_out"][0]),
              mwg_32=np.concatenate([blocks(inp["o_moe_w_gate"][0][e]) for e in range(NE)], 0),
              mwu_32=np.concatenate([blocks(inp["o_moe_w_up"][0][e]) for e in range(NE)], 0),
              mwd_32=np.concatenate([kmajor(inp["o_moe_w_down"][0][e]) for e in range(NE)], 0))
    hD = []
    for b in range(B):
        ogt_full = np.concatenate([rC[b * NSEG + hp_]["ogt"] for hp_ in range(NSEG)], 0)
        for j in range(NSEG):
            c = b * NSEG + j
            d = dict(cD)
            d.update(ogt=np.ascontiguousarray(ogt_full[:, j * NT:(j + 1) * NT]), h1=rB[c]["h1"])
            hD.append(d)
    ncD = _build(hD[0], dict(out=([NT, 1024], F32)),
                 dict(wo1=([128, 8192], BF16), mwg=([NE * 128, NCm * 1024], BF16), mwu=([NE * 128, NCm * 1024], BF16),
                      mwd=([NE * 128, NCm * 1024], BF16)),
                 [("wo1_32", "wo1", 128), ("mwg_32", "mwg", NE * 128), ("mwu_32", "mwu", NE * 128), ("mwd_32", "mwd", NE * 128)],
                 phase4, dict(NTOK=NT, NEXP=NE, NC_MOE=NCm))
    rD = _run(ncD, hD)
    out = np.stack([np.concatenate([rD[b * NSEG + j]["out"] for j in range(NSEG)], 0) for b in range(B)], 0)
    return out.astype(f32)


def p4_consts(NSL):
    c = np.zeros((128, 64), np.float32)
    c[:, 0:8] = 512.0 * np.arange(8)[None, :]
    c[:, 8] = np.arange(128)
    c[:, 16:16 + NSL] = 512.0 * np.arange(NSL)[None, :]
    k = np.arange(128)[:, None]
    i = np.arange(128)[None, :]
    return c, (k < i).astype(np.float32)


def sparse_dram(kb, D, NT, NE, NC):
    nc = kb.nc
    NB = NC // 2
    NSL = 2 * NT // 512 + NE
    D["mwg_b"] = [nc.dram_tensor(f"mwg_b{b}", [NE * 128, 2048], BF16).ap() for b in range(NB)]
    D["mwu_b"] = [nc.dram_tensor(f"mwu_b{b}", [NE * 128, 2048], BF16).ap() for b in range(NB)]
    D["mwd_g"] = [nc.dram_tensor(f"mwd_g{b}", [NE * 128, 7 * 1024], BF16).ap() for b in range(NC // 7)]
    D["h2"] = nc.dram_tensor("h2", [NT, 1024], F32).ap()
    D["hnd"] = nc.dram_tensor("hnd", [NT, 1024], BF16).ap()
    D["xsl"] = nc.dram_tensor("xsl", [NSL * 512, 1028], BF16).ap()
    D["ysl"] = nc.dram_tensor("ysl", [NSL * 512, 1024], F32).ap()
    pairs = []
    for e in range(NE):
        rs = slice(e * 128, (e + 1) * 128)
        for b in range(NB):
            pairs.append((D["mwg_32"][rs, b * 2048:(b + 1) * 2048], D["mwg_b"][b][rs, :]))
            pairs.append((D["mwu_32"][rs, b * 2048:(b + 1) * 2048], D["mwu_b"][b][rs, :]))
        for b in range(NC // 7):
            pairs.append((D["mwd_32"][rs, b * 7168:(b + 1) * 7168], D["mwd_g"][b][rs, :]))
    return pairs


def kernel(**inputs):
    inp = {k: np.asarray(v) for k, v in inputs.items()}
    f32 = np.float32
    x = inp["x"]
    B, S, _ = x.shape
    NT = NT_CORE
    NE = inp["o_moe_w_gate"].shape[1]
    NCm = inp["o_moe_w_gate"].shape[3] // 128
    eye = np.eye(128, dtype=f32)
    hA = prep_p1(inp, NT, NSEG)
    common = dict(ident=eye, gffn0=pcol(inp["ln_ffn"][0]), gmix1=pcol(inp["ln_mix"][1]), wo032=kmajor(inp["e_w_out"][0]),
                  fwg32=blocks(inp["e_ffn_w_gate"][0]), fwu32=blocks(inp["e_ffn_w_up"][0]), fwd32=kmajor(inp["e_ffn_w_down"][0]),
                  gffn1=pcol(inp["ln_ffn"][1]), router=kmajor(inp["o_router"][0]), wo1_32=kmajor(inp["o_w_out"][0]),
                  mwg_32=np.concatenate([blocks(inp["o_moe_w_gate"][0][e]) for e in range(NE)], 0),
                  mwu_32=np.concatenate([blocks(inp["o_moe_w_up"][0][e]) for e in range(NE)], 0),
                  mwd_32=np.concatenate([kmajor(inp["o_moe_w_down"][0][e]) for e in range(NE)], 0))
    p3 = [prep_p3(inp, hp_) for hp_ in range(NSEG)]
    cst4, SU4 = p4_consts(2 * NT // 512 + NE)
    common.update(gffn1_b=np.broadcast_to(inp["ln_ffn"][1][None, :], (128, 1024)).astype(f32).copy(), p4cst=cst4, maskSU=SU4)
    maps = []
    for b in range(B):
        for j in range(NSEG):
            c = b * NSEG + j
            d = dict(common)
            d.update(hA[c])
            d.update(p3[j])
            sel = np.zeros((128, 4), f32)
            sel[:, :j] = 1.0
            sel4 = np.zeros((128, 4), f32)
            sel4[:, j] = 1.0
            d.update(sel=sel, sel4=sel4)
            maps.append(d)
    import os as _os
    KSTOP = int(_os.environ.get("KSTOP", "0"))
    if KSTOP:
        tiny = np.zeros((128, 8), f32)
        for d in maps:
            d.update(mwg_32=tiny, mwu_32=tiny, mwd_32=tiny)
    nc = bass.Bass("TRN2", target_bir_lowering=False)
    kb = KB(nc)
    bf = lambda n: ([128, n], BF16)
    scratch = dict(win=bf(8 * 1792), wo0=bf(8192), fwg=bf(22 * 1024), fwu=bf(22 * 1024), fwd=bf(22 * 1024),
                   wqkv=bf(8 * 768), wgate=bf(8 * 256), wab=bf(32), wo1=bf(8192),
                   yloc=([512, NT], BF16), pg=([512, NT], BF16), yatt=([512, NT], BF16), seg=([128, 8], F32),
                   h1=([NT, 1024], F32), hn1T=([(NT // 512) * 1024, 512], BF16), ogt_own=([(S // 2048) * 256, 2048], BF16))
    D = _mk_dram(kb, maps[0], dict(out=([NT, 1024], F32)), scratch)
    for k, shape, dt in (("segall", [NSEG * 128, 8], F32), ("hng", [(NT // 512) * NSEG * 1024, 512], BF16), ("ogt_all", [(S // 2048) * NSEG * 256, 2048], BF16)):
        D[k] = nc.dram_tensor(k, shape, dt, addr_space="Local", kind="Internal").ap()
    groups = [list(range(b * NSEG, (b + 1) * NSEG)) for b in range(B)]
    casts = [("win32", "win", 128), ("wo032", "wo0", 128), ("fwg32", "fwg", 128), ("fwu32", "fwu", 128), ("fwd32", "fwd", 128),
             ("wqkv32", "wqkv", 128), ("wgate32", "wgate", 128), ("wab32", "wab", 128), ("wo1_32", "wo1", 128)]
    pairs = []
    for src_, dst_, rows in casts:
        for r0 in range(0, rows, 128):
            pairs.append((D[src_][r0:r0 + 128, :], D[dst_][r0:r0 + 128, :]))
    if not KSTOP:
        pairs += sparse_dram(kb, D, NT, NE, NCm)
    cast_dram(kb, pairs)
    kb.phase_end()
    phase1(kb, dict(NTOK=NT), D)
    kb.phase_end()
    kb.s.collective("AllGather", D["seg"], D["segall"], groups)
    kb.phase_end()
    if KSTOP == 1:
        kb.s.finish()
        return _run(nc, maps)
    phase2(kb, dict(NTOK=NT, ag_hn=groups), D)
    kb.phase_end()
    if KSTOP == 2:
        kb.s.finish()
        return _run(nc, maps)
    if KSTOP == 3:
        kb.s.finish()
        return _run(nc, maps)
    D3 = dict(D)
    D3["ogt"] = D["ogt_own"]
    D3["ogt_all"] = D["ogt_all"]
    phase3(kb, dict(NTF=S, NTK=NT, chunked=2, ag_og=groups), D3)
    kb.phase_end()
    if KSTOP == 5:
        kb.s.finish()
        return _run(nc, maps)
    D4 = dict(D)
    D4["ogt"] = D["ogt_all"]
    phase4s(kb, dict(NTOK=NT, NEXP=NE, NC_MOE=NCm, fused=True), D4)
    kb.phase_end()
    kb.s.finish()
    res = _run(nc, maps)
    out = np.stack([np.concatenate([res[b * NSEG + j]["out"] for j in range(NSEG)], 0) for b in range(B)], 0)
    return out.astype(f32)


I32 = mybir.dt.int32


def idma(kb, out, in_, idx_ap, scatter, reads=(), writes=()):
    s = kb.s
    E = s.E["pool"]
    deps = s._deps(reads, writes)
    slot = s.dsem["pool"][s.drr["pool"]]
    s.drr["pool"] = (s.drr["pool"] + 1) % NDS
    sem, val = slot
    if val > 0:
        deps.append((sem, val))
    s._wait(E, deps)
    slot[1] = val + 16
    off = bass.IndirectOffsetOnAxis(ap=idx_ap, axis=0)
    if scatter:
        kb.nc.gpsimd.indirect_dma_start(out=out, out_offset=off, in_=in_, in_offset=None).then_inc(sem, 16)
    else:
        kb.nc.gpsimd.indirect_dma_start(out=out, out_offset=None, in_=in_, in_offset=off).then_inc(sem, 16)
    s.nins += 1
    tok = (sem, val + 16)
    k = id(sem)
    for b in reads:
        b.r[k] = tok
    for b in writes:
        b.w = tok
        b.r = {}


def phase4s(kb, C, D):
    s = kb.s
    NT, NE, NCm = C["NTOK"], C["NEXP"], C["NC_MOE"]
    TS = 512
    NTT = NT // 128
    NSL = (2 * NT) // TS + NE
    NB = NCm // 2
    kb.D = D
    IDX1 = kb.sbp([128, NTT], I32, "IDX1")
    IDX2 = kb.sbp([128, NTT], I32, "IDX2")
    IDXW = kb.sbp([128, NSL], I32, "IDXW")
    W = norm_scratch(kb)
    ident, identb = W["ident"], W["identb"]
    g = kb.sb([128, 8], F32, "gffn")
    s.dma("sp", g[:], D["gffn1"], writes=[g.b])
    gtm = kb.sb([128, 1024], F32, "gtm")
    s.dma("sp", gtm[:], D["gffn1_b"], writes=[gtm.b])
    wo = kb.sb([128, 8, 1024], BF16, "wo")
    s.dma("sp", wo[:], D["wo1"].rearrange("p (k n) -> p k n", n=1024), writes=[wo.b])
    rtr = kb.sb([128, 8, 8], F32, "rtr")
    s.dma("sp", rtr[:], D["router"].rearrange("p (k n) -> p k n", n=8), writes=[rtr.b])
    SU32 = kb.sb([128, 128], F32, "SU32")
    s.dma("sp", SU32[:], D["maskSU"], writes=[SU32.b])
    SUb = kb.sb([128, 128], BF16, "SUb")
    s.op("dve", lambda e: e.tensor_copy(SUb[:], SU32[:]), reads=[SU32.b], writes=[SUb.b])
    onesb = kb.sb([128, 128], BF16, "onesb")
    s.op("pool", lambda e: e.memset(onesb[:], 1.0), writes=[onesb.b])
    cst = kb.sb([128, 64], F32, "p4cst")
    s.dma("sp", cst[:], D["p4cst"], writes=[cst.b])
    R1 = kb.sb([128, NTT, 8], F32, "R1")
    R2 = kb.sb([128, NTT, 8], F32, "R2")
    W12 = kb.sb([128, NTT, 2], F32, "W12")
    if C.get("fused"):
        ogc = [kb.sb([128, 4, 8, 128], BF16, "ogc") for _ in range(2)]
        sel4 = kb.sb([128, 4], F32, "sel4")
        s.dma("sp", sel4[:], D["sel4"], writes=[sel4.b])
    else:
        ogt_v = D["ogt"].rearrange("(c p) n -> p c n", p=128)
    og = [kb.sb([128, 8, 128], BF16, "og") for _ in range(2)]
    h1 = [kb.sb([128, 1024], F32, "h1") for _ in range(2)]
    acc = [kb.sb([128, 1024], F32, "acc") for _ in range(2)]
    xs2 = [kb.sb([128, 1024], F32, "xs") for _ in range(2)]
    junk2 = [kb.sb([128, 1024], BF16, "junk") for _ in range(2)]
    hn322 = [kb.sb([128, 8, 128], F32, "hn32") for _ in range(2)]
    HN = [kb.sb([128, 1028], BF16, "HN") for _ in range(2)]
    sm2 = [{k: kb.sb([128, 8], F32, k) for k in ("lg", "mx")} for _ in range(2)]
    sc2 = [{k: kb.sb([128, 1], F32, k) for k in ("ss", "rt", "rstd", "dd", "ex", "den")} for _ in range(2)]
    bh2, bhn, bxs, bys = Buf(), Buf(), Buf(), Buf()
    for tt in range(NTT):
        tok0 = tt * 128
        o_, h_, a_, hn_ = og[tt % 2], h1[tt % 2], acc[tt % 2], HN[tt % 2]
        xs, junk, hn32, sm, sc = xs2[tt % 2], junk2[tt % 2], hn322[tt % 2], sm2[tt % 2], sc2[tt % 2]
        if C.get("fused"):
            cd_ = ogc[tt % 2]
            for r_ in range(4):
                g_ = r_ * NT + tok0
                b_, c0_ = g_ // 2048, g_ % 2048
                s.dma("act", cd_[:, r_], D["ogt"][b_ * 1024:(b_ + 1) * 1024, :].rearrange("(c p) n -> p c n", p=128)[:, :, c0_:c0_ + 128],
                      writes=[cd_.b])
            s.op("dve", lambda e: e.tensor_scalar(o_[:], cd_[:, 0], sel4[:, 0:1], None, ALU.mult), reads=[cd_.b, sel4.b], writes=[o_.b])
            for r_ in range(1, 4):
                s.op("dve", lambda e: e.scalar_tensor_tensor(o_[:], cd_[:, r_], sel4[:, r_:r_ + 1], o_[:], ALU.mult, ALU.add),
                     reads=[cd_.b, sel4.b, o_.b], writes=[o_.b])
        else:
            s.dma("act", o_[:], ogt_v[:, :, tok0:tok0 + 128], writes=[o_.b])
        s.dma("act", h_[:], D["h1"][tok0:tok0 + 128, :], writes=[h_.b])
        for half in range(2):
            bk = nbank(kb)
            mm_group(kb, bk, bk[:, :], [(o_[:, c, :], wo[:, c, half * 512:(half + 1) * 512]) for c in range(8)], reads=[o_.b, wo.b])
            s.op("dve", lambda e: e.tensor_tensor(a_[:, half * 512:(half + 1) * 512], h_[:, half * 512:(half + 1) * 512], bk[:, :], ALU.add),
                 reads=[bk.b, h_.b], writes=[a_.b])
        s.dma("sp", D["h2"][tok0:tok0 + 128, :], a_[:], reads=[a_.b], writes=[bh2])
        rms_stats(kb, a_[:], a_.b, junk, sc["ss"], sc["rt"], sc["rstd"])
        s.op("act", lambda e: e.activation(xs[:], a_[:], AF.Copy, scale=sc["rstd"][:]), reads=[a_.b, sc["rstd"].b], writes=[xs.b])
        s.op("pool", lambda e: e.tensor_tensor(hn_[:, 0:1024], xs[:], gtm[:], ALU.mult), reads=[xs.b, gtm.b], writes=[hn_.b])
        s.dma("sp", D["hnd"][tok0:tok0 + 128, :], hn_[:, 0:1024], reads=[hn_.b], writes=[bhn])
        for hh in range(2):
            bk = nbank(kb)
            mm_group(kb, bk, [bk[:, j * 128:(j + 1) * 128] for j in range(4)],
                     [(xs[:, (hh * 4 + j) * 128:(hh * 4 + j + 1) * 128], ident[:]) for j in range(4)], reads=[xs.b, ident.b], transpose=True)
            s.op("dve", lambda e: e.tensor_tensor(hn32[:, hh * 4:(hh + 1) * 4, :], bk[:, :].rearrange("p (c n) -> p c n", n=128),
                                                   bc(g[:, hh * 4:(hh + 1) * 4].unsqueeze(2), [128, 4, 128]), ALU.mult),
                 reads=[bk.b, g.b], writes=[hn32.b])
        bk = nbank(kb)
        mm_group(kb, bk, bk[:, 0:8], [(hn32[:, c, :], rtr[:, c, :]) for c in range(8)], reads=[hn32.b, rtr.b])
        lg, mx = sm["lg"], sm["mx"]
        s.op("dve", lambda e: e.tensor_copy(lg[:], bk[:, 0:8]), reads=[bk.b], writes=[lg.b])
        s.op("dve", lambda e: e.max(mx[:], lg[:]), reads=[lg.b], writes=[mx.b])
        s.op("dve", lambda e: e.tensor_tensor(sc["dd"][:], mx[:, 1:2], mx[:, 0:1], ALU.subtract), reads=[mx.b], writes=[sc["dd"].b])
        s.op("act", lambda e: e.activation(sc["ex"][:], sc["dd"][:], AF.Exp), reads=[sc["dd"].b], writes=[sc["ex"].b])
        s.op("dve", lambda e: e.tensor_scalar(sc["den"][:], sc["ex"][:], 1.0, None, ALU.add), reads=[sc["ex"].b], writes=[sc["den"].b])
        s.op("dve", lambda e: e.reciprocal(W12[:, tt, 0:1], sc["den"][:]), reads=[sc["den"].b], writes=[W12.b])
        s.op("dve", lambda e: e.tensor_tensor(W12[:, tt, 1:2], sc["ex"][:], W12[:, tt, 0:1], ALU.mult), reads=[sc["ex"].b, W12.b], writes=[W12.b])
        s.op("dve", lambda e: e.tensor_scalar(R1[:, tt, :], lg[:], mx[:, 0:1], None, ALU.is_equal), reads=[lg.b, mx.b], writes=[R1.b])
        s.op("dve", lambda e: e.tensor_scalar(R2[:, tt, :], lg[:], mx[:, 1:2], None, ALU.is_equal), reads=[lg.b, mx.b], writes=[R2.b])
    barrier(kb)
    NC8 = NTT * 8
    Rm = kb.sb([128, NC8], BF16, "Rm")
    s.op("dve", lambda e: e.tensor_tensor(Rm[:], R1[:].rearrange("p t e -> p (t e)"), R2[:].rearrange("p t e -> p (t e)"), ALU.add),
         reads=[R1.b, R2.b], writes=[Rm.b])
    bw_ = nbank(kb)
    mm_group(kb, bw_, bw_[:, 0:NC8], [(SUb[:], Rm[:])], reads=[SUb.b, Rm.b])
    bt_ = nbank(kb)
    mm_group(kb, bt_, bt_[:, 0:NC8], [(onesb[:], Rm[:])], reads=[onesb.b, Rm.b])
    totT = kb.sb([128, 8, NTT], F32, "totT")
    s.op("dve", lambda e: e.tensor_copy(totT[:], bt_[:, 0:NC8].rearrange("p (t e) -> p e t", e=8)), reads=[bt_.b], writes=[totT.b])
    ones32 = kb.sb([128, 64], F32, "ones32")
    s.op("pool", lambda e: e.memset(ones32[:], 1.0), writes=[ones32.b])
    cumI = kb.sb([128, 8, NTT], F32, "cumI")
    for e_ in range(8):
        s.op("dve", lambda e: e.tensor_tensor_scan(cumI[:, e_, :], ones32[:, 0:NTT], totT[:, e_, :], 0.0, ALU.mult, ALU.add),
             reads=[ones32.b, totT.b], writes=[cumI.b])
    toff = kb.sb([128, 8, NTT], F32, "toff")
    s.op("dve", lambda e: e.tensor_tensor(toff[:], cumI[:], totT[:], ALU.subtract), reads=[cumI.b, totT.b], writes=[toff.b])
    cnt = kb.sb([128, 8], F32, "cnt")
    s.op("dve", lambda e: e.tensor_copy(cnt[:], cumI[:, :, NTT - 1]), reads=[cumI.b], writes=[cnt.b])
    cmp = kb.sb([128, 8, 8], F32, "cmp")
    s.op("dve", lambda e: e.tensor_tensor(cmp[:], bc(cnt[:].unsqueeze(2), [128, 8, 8]), bc(cst[:, 0:8].unsqueeze(1), [128, 8, 8]), ALU.is_gt),
         reads=[cnt.b, cst.b], writes=[cmp.b])
    padded = kb.sb([128, 8], F32, "padded")
    s.op("dve", lambda e: e.tensor_reduce(padded[:], cmp[:], AX.X, ALU.add), reads=[cmp.b], writes=[padded.b])
    s.op("dve", lambda e: e.tensor_scalar(padded[:], padded[:], float(TS), None, ALU.mult), reads=[padded.b], writes=[padded.b])
    ends = kb.sb([128, 8], F32, "ends")
    s.op("dve", lambda e: e.tensor_tensor_scan(ends[:], ones32[:, 0:8], padded[:], 0.0, ALU.mult, ALU.add), reads=[ones32.b, padded.b], writes=[ends.b])
    base = kb.sb([128, 8], F32, "base")
    s.op("dve", lambda e: e.tensor_tensor(base[:], ends[:], padded[:], ALU.subtract), reads=[ends.b, padded.b], writes=[base.b])
    smat = kb.sb([128, NTT, 8], F32, "smat")
    s.op("dve", lambda e: e.tensor_tensor(smat[:], bw_[:, 0:NC8].rearrange("p (t e) -> p t e", e=8), toff[:].rearrange("p e t -> p t e"), ALU.add),
         reads=[bw_.b, toff.b], writes=[smat.b])
    s.op("dve", lambda e: e.tensor_tensor(smat[:], smat[:], bc(base[:].unsqueeze(1), [128, NTT, 8]), ALU.add), reads=[smat.b, base.b], writes=[smat.b])
    tmp3 = kb.sb([128, NTT, 8], F32, "tmp3")
    sl = kb.sb([128, NTT], F32, "sl")
    for Rx, IDX in ((R1, IDX1), (R2, IDX2)):
        s.op("dve", lambda e: e.tensor_tensor(tmp3[:], smat[:], Rx[:], ALU.mult), reads=[smat.b, Rx.b], writes=[tmp3.b])
        s.op("dve", lambda e: e.tensor_reduce(sl[:], tmp3[:], AX.X, ALU.add), reads=[tmp3.b], writes=[sl.b])
        s.op("dve", lambda e: e.tensor_copy(IDX[:], sl[:]), reads=[sl.b], writes=[IDX.b])
    cw = kb.sb([128, NSL, 8], F32, "cw")
    s.op("dve", lambda e: e.tensor_tensor(cw[:], bc(ends[:].unsqueeze(1), [128, NSL, 8]), bc(cst[:, 16:16 + NSL].unsqueeze(2), [128, NSL, 8]), ALU.is_le),
         reads=[ends.b, cst.b], writes=[cw.b])
    ew = kb.sb([128, NSL], F32, "ew")
    s.op("dve", lambda e: e.tensor_reduce(ew[:], cw[:], AX.X, ALU.add), reads=[cw.b], writes=[ew.b])
    s.op("dve", lambda e: e.tensor_scalar(ew[:], ew[:], float(NE - 1), 128.0, ALU.min, ALU.mult), reads=[ew.b], writes=[ew.b])
    s.op("dve", lambda e: e.tensor_scalar(ew[:], ew[:], cst[:, 8:9], None, ALU.add), reads=[ew.b, cst.b], writes=[ew.b])
    s.op("dve", lambda e: e.tensor_copy(IDXW[:], ew[:]), reads=[ew.b], writes=[IDXW.b])
    zt = kb.sb([128, 1028], BF16, "zt")
    s.op("pool", lambda e: e.memset(zt[:], 0.0), writes=[zt.b])
    for r0 in range(0, NSL * TS, 128):
        s.dma("sp" if (r0 // 128) % 2 == 0 else "act", D["xsl"][r0:r0 + 128, :], zt[:], reads=[zt.b], writes=[bxs])
    barrier(kb)
    for tt in range(NTT):
        tok0 = tt * 128
        hn_ = HN[tt % 2]
        s.dma("sp", hn_[:, 0:1024], D["hnd"][tok0:tok0 + 128, :], reads=[bhn], writes=[hn_.b])
        for k_, IDX in ((0, IDX1), (1, IDX2)):
            s.op("dve", lambda e: e.tensor_copy(hn_[:, 1024:1026].bitcast(F32), W12[:, tt, k_:k_ + 1]), reads=[W12.b], writes=[hn_.b])
            idma(kb, D["xsl"][:, :], hn_[:, :], IDX[:, tt:tt + 1], True, reads=[hn_.b, IDX.b], writes=[bxs])
    kb.phase_end()
    W = norm_scratch(kb)
    identb = W["identb"]
    r = ffn_alloc(kb, TS, NCm)
    hnT = [kb.sb([128, 8, TS], BF16, "hnT") for _ in range(2)]
    xt = [kb.sb([128, 1028], BF16, "xslt") for _ in range(3)]
    GW = [kb.sb([128, 4], F32, "GW") for _ in range(2)]
    YS = kb.sb([128, 4, 1024], F32, "YS")
    ix = 0
    for w in range(NSL):
        hT_ = hnT[w % 2]
        gw_ = GW[w % 2]
        for sub in range(4):
            x_ = xt[ix % 3]
            ix += 1
            r0 = w * TS + sub * 128
            s.dma("sp", x_[:], D["xsl"][r0:r0 + 128, :], reads=[bxs], writes=[x_.b])
            s.op("pool", lambda e: e.tensor_copy(gw_[:, sub:sub + 1], x_[:, 1024:1026].bitcast(F32)), reads=[x_.b], writes=[gw_.b])
            bk = nbank(kb)
            bv = bk[:, :].bitcast(BF16)
            mm_group(kb, bk, [bv[:, j * 128:(j + 1) * 128] for j in range(8)],
                     [(x_[:, j * 128:(j + 1) * 128], identb[:]) for j in range(8)], reads=[x_.b, identb.b], transpose=True)
            s.op("act", lambda e: e.copy(hT_[:, :, sub * 128:(sub + 1) * 128], bv.rearrange("p (c n) -> p c n", n=128)), reads=[bk.b], writes=[hT_.b])
        iw = IDXW[:, w:w + 1]

        def evac(t, half, bk):
            s.op("dve", lambda e: e.tensor_scalar(YS[:, t, half * 512:(half + 1) * 512], bk[:, :], gw_[:, t:t + 1], None, ALU.mult),
                 reads=[bk.b, gw_.b], writes=[YS.b])

        def lw(kind, b, tile):
            if kind == "d":
                idma(kb, tile, D["mwd_g"][b][:, :], iw, False, reads=[IDXW.b], writes=[r.wd.b])
            else:
                idma(kb, tile[:].rearrange("p k n -> p (k n)"), D["mwg_b" if kind == "g" else "mwu_b"][b][:, :], iw, False,
                     reads=[IDXW.b], writes=[tile.b])
        ffn_expert(kb, r, hT_, None, None, None, NCm, evac, loader=lw)
        for t in range(4):
            r0 = w * TS + t * 128
            s.dma("act", D["ysl"][r0:r0 + 128, :], YS[:, t, :], reads=[YS.b], writes=[bys])
    kb.phase_end()
    acc = [kb.sb([128, 1024], F32, "acc") for _ in range(2)]
    y1 = [kb.sb([128, 1024], F32, "y1") for _ in range(2)]
    y2 = [kb.sb([128, 1024], F32, "y2") for _ in range(2)]
    for tt in range(NTT):
        tok0 = tt * 128
        a_, y1_, y2_ = acc[tt % 2], y1[tt % 2], y2[tt % 2]
        s.dma("sp", a_[:], D["h2"][tok0:tok0 + 128, :], reads=[bh2], writes=[a_.b])
        idma(kb, y1_[:, :], D["ysl"][:, :], IDX1[:, tt:tt + 1], False, reads=[bys, IDX1.b], writes=[y1_.b])
        idma(kb, y2_[:, :], D["ysl"][:, :], IDX2[:, tt:tt + 1], False, reads=[bys, IDX2.b], writes=[y2_.b])
        s.op("dve", lambda e: e.tensor_tensor(a_[:], a_[:], y1_[:], ALU.add), reads=[a_.b, y1_.b], writes=[a_.b])
        s.op("dve", lambda e: e.tensor_tensor(a_[:], a_[:], y2_[:], ALU.add), reads=[a_.b, y2_.b], writes=[a_.b])
        s.dma("act", D["out"][tok0:tok0 + 128, :], a_[:], reads=[a_.b])
```

```python
import numpy as np
from contextlib import ExitStack
import concourse.bass as bass
import concourse.mybir as mybir
from concourse.bass_utils import run_bass_kernel_spmd

F32 = mybir.dt.float32
BF16 = mybir.dt.bfloat16
AF = mybir.ActivationFunctionType
ALU = mybir.AluOpType
AX = mybir.AxisListType
EPS = 1e-6
NDS = 20


class Buf:
    __slots__ = ("w", "r", "name", "excl")

    def __init__(self, name="", excl=False):
        self.w = None
        self.r = {}
        self.name = name
        self.excl = excl


class Sched:
    def __init__(self, nc):
        self.nc = nc
        self.E = {}
        for nm, eng in (("pe", nc.tensor), ("dve", nc.vector), ("act", nc.scalar),
                        ("pool", nc.gpsimd), ("sp", nc.sync)):
            self.E[nm] = dict(eng=eng, sem=nc.alloc_semaphore("s_" + nm), cnt=0, known={}, name=nm)
        self.dsem = {q: [[nc.alloc_semaphore(f"d_{q}{i}"), 0] for i in range(NDS)]
                     for q in ("sp", "act", "pool")}
        self.drr = {q: 0 for q in self.dsem}
        self.nins = 0
        self.ccsem = nc.alloc_semaphore("s_cc")
        self.ccval = 0
        self.extra = []

    def collective(self, kind, in_ap, out_ap, groups, deps=None):
        if deps:
            self._wait(self.E["pool"], deps)
        self.ccval += 1
        self.nc.gpsimd.collective_compute(kind, ALU.bypass, replica_groups=groups, ins=[in_ap], outs=[out_ap]).then_inc(self.ccsem)
        self.extra.append((self.ccsem, self.ccval))
        self.nins += 1

    def _deps(self, reads, writes):
        deps = []
        for b in reads:
            if b.w is not None:
                deps.append(b.w)
            if b.excl:
                deps.extend(b.r.values())
        for b in writes:
            if b.w is not None:
                deps.append(b.w)
            deps.extend(b.r.values())
        return deps

    def _wait(self, E, deps):
        for (sem, val) in deps:
            k = id(sem)
            if E["known"].get(k, 0) >= val:
                continue
            E["eng"].wait_ge(sem, val)
            E["known"][k] = val

    def op(self, en, fn, reads=(), writes=(), inc=True):
        E = self.E[en]
        deps = self._deps(reads, writes)
        if en == "pe":
            deps = [d for d in deps if d[0] is not E["sem"]]
        self._wait(E, deps)
        ins = fn(E["eng"])
        self.nins += 1
        if inc:
            E["cnt"] += 1
            ins.then_inc(E["sem"], 1)
            tok = (E["sem"], E["cnt"])
        else:
            tok = (E["sem"], E["cnt"] + 1)
        k = id(E["sem"])
        for b in reads:
            b.r[k] = tok
        for b in writes:
            b.w = tok
            b.r = {}
        return tok

    def dma(self, q, out, in_, reads=(), writes=(), **kw):
        E = self.E[q]
        deps = self._deps(reads, writes)
        slot = self.dsem[q][self.drr[q]]
        self.drr[q] = (self.drr[q] + 1) % NDS
        sem, val = slot
        if val > 0:
            deps.append((sem, val))
        self._wait(E, deps)
        slot[1] = val + 16
        E["eng"].dma_start(out=out, in_=in_, **kw).then_inc(sem, 16)
        self.nins += 1
        tok = (sem, val + 16)
        k = id(sem)
        for b in reads:
            b.r[k] = tok
        for b in writes:
            b.w = tok
            b.r = {}
        return tok

    def finish(self):
        for q, slots in self.dsem.items():
            E = self.E[q]
            self._wait(E, [(s, v) for s, v in slots if v > 0])


class T:
    def __init__(self, t, nbuf=1, name=""):
        self.t = t
        self.b = Buf(name)

    def __getitem__(self, idx):
        return self.t[idx]


class KB:
    def __init__(self, nc):
        self.nc = nc
        self.s = Sched(nc)
        self.n = 0
        self.stack = ExitStack()
        self.banks = [self.psum(f"bank{i}") for i in range(8)]

    def sb(self, shape, dt, name=None):
        self.n += 1
        name = (name or "t") + f"_{self.n}"
        return T(self.stack.enter_context(self.nc.sbuf_tensor(name, list(shape), dt)), name=name)

    def sbp(self, shape, dt, name):
        self.n += 1
        return T(self.nc.alloc_sbuf_tensor(name + f"_{self.n}", list(shape), dt), name=name)

    def phase_end(self):
        barrier(self)
        self.stack.close()
        self.stack = ExitStack()

    def psum(self, name):
        t = T(self.nc.alloc_psum_tensor(name, [128, 512], F32), name=name)
        t.b.excl = True
        return t

    def dram(self, name, shape, dt, kind=None):
        if kind:
            return self.nc.dram_tensor(name, list(shape), dt, kind=kind).ap()
        return self.nc.dram_tensor(name, list(shape), dt).ap()


def bc(ap, shape):
    return ap.to_broadcast(list(shape))


def barrier(kb):
    s = kb.s
    toks = [(E["sem"], E["cnt"]) for E in s.E.values() if E["cnt"] > 0]
    for q, slots in s.dsem.items():
        toks += [(sm, v) for sm, v in slots if v > 0]
    toks += s.extra
    for E in s.E.values():
        s._wait(E, [t for t in toks if t[0] is not E["sem"]])


def mm_group(kb, bank, out_ap, pairs, reads, transpose=False):
    n = len(pairs)
    for i, (l, r) in enumerate(pairs):
        if transpose:
            fn = (lambda e, l=l, r=r, o=out_ap[i]: e.transpose(o, l, r))
        else:
            fn = (lambda e, l=l, r=r, i=i: e.matmul(out_ap, l, r, start=(i == 0), stop=(i == n - 1)))
        kb.s.op("pe", fn, reads=reads, writes=[bank.b], inc=(i == n - 1))


def cast_dram(kb, pairs, CH=4096):
    s = kb.s
    NB_ = 4
    st32 = [kb.sb([128, CH], F32) for _ in range(NB_)]
    st16 = [kb.sb([128, CH], BF16) for _ in range(NB_)]
    work = []
    for src, dst in pairs:
        M = src.shape[1]
        for c0 in range(0, M, CH):
            work.append((src, dst, c0, min(CH, M - c0)))
    engs = ["pool", "act", "dve"]

    def load(i):
        src, dst, c0, w = work[i]
        a = st32[i % NB_]
        s.dma("sp", a[:, 0:w], src[:, c0:c0 + w], writes=[a.b])

    for i in range(min(2, len(work))):
        load(i)
    for i in range(len(work)):
        if i + 2 < len(work):
            load(i + 2)
        src, dst, c0, w = work[i]
        a = st32[i % NB_]
        b = st16[i % NB_]
        en = engs[i % 3]
        if en == "act":
            s.op("act", lambda e, a=a, b=b, w=w: e.copy(b[:, 0:w], a[:, 0:w]), reads=[a.b], writes=[b.b])
        else:
            s.op(en, lambda e, a=a, b=b, w=w: e.tensor_copy(b[:, 0:w], a[:, 0:w]), reads=[a.b], writes=[b.b])
        s.dma("sp", dst[:, c0:c0 + w], b[:, 0:w], reads=[b.b])


def rms_stats(kb, x_ap, xbuf, junk, ss, rt, rstd):
    s = kb.s
    s.op("act", lambda e: e.activation(junk[:], x_ap, AF.Square, accum_out=ss[:]), reads=[xbuf], writes=[junk.b, ss.b])
    s.op("act", lambda e: e.activation(rt[:], ss[:], AF.Sqrt, bias=kb.eps_t[:], scale=1.0 / 1024.0), reads=[ss.b], writes=[rt.b])
    s.op("dve", lambda e: e.reciprocal(rstd[:], rt[:]), reads=[rt.b], writes=[rstd.b])


class FFNRes:
    pass


def ffn_alloc(kb, TS, NC):
    r = FFNRes()
    r.TS = TS
    r.wg = [kb.sb([128, 8, 256], BF16, "wg") for _ in range(3)]
    r.wu = [kb.sb([128, 8, 256], BF16, "wu") for _ in range(3)]
    r.wd = kb.sb([128, NC, 1024], BF16, "wd")
    r.hT = kb.sb([128, NC, TS], BF16, "hT")
    r.sg = [kb.sb([128, TS], F32, "sg") for _ in range(2)]
    r.blk = 0
    r.ch = 0
    r.dn = 0
    return r


def ffn_expert(kb, r, hnT, wg_d, wu_d, wd_d, NC, evac, loader=None):
    s = kb.s
    TS = r.TS
    NB = NC // 2
    for g0 in range(0, NC, 7):
        g1 = min(NC, g0 + 7)
        if loader is not None:
            loader("d", g0 // 7, r.wd[:, g0:g1, :].rearrange("p c n -> p (c n)"))
            continue
        s.dma("act", r.wd[:, g0:g1, :], wd_d[:, g0 * 1024:g1 * 1024].rearrange("p (c n) -> p c n", n=1024),
              writes=[r.wd.b])
    for b in range(NB):
        wg = r.wg[r.blk % 3]
        wu = r.wu[r.blk % 3]
        r.blk += 1
        if loader is not None:
            loader("g", b, wg)
            loader("u", b, wu)
        else:
            s.dma("sp", wg[:], wg_d[:, b * 2048:(b + 1) * 2048].rearrange("p (k n) -> p k n", n=256), writes=[wg.b])
            s.dma("sp", wu[:], wu_d[:, b * 2048:(b + 1) * 2048].rearrange("p (k n) -> p k n", n=256), writes=[wu.b])
        for c in range(2):
            ch = b * 2 + c
            bg = kb.banks[(2 * r.ch) % 4]
            bu = kb.banks[(2 * r.ch + 1) % 4]
            sg = r.sg[r.ch % 2]
            r.ch += 1
            mm_group(kb, bg, bg[:, 0:TS], [(wg[:, k, c * 128:(c + 1) * 128], hnT[:, k, :]) for k in range(8)],
                     reads=[wg.b, hnT.b])
            mm_group(kb, bu, bu[:, 0:TS], [(wu[:, k, c * 128:(c + 1) * 128], hnT[:, k, :]) for k in range(8)],
                     reads=[wu.b, hnT.b])
            s.op("act", lambda e, sg=sg, bg=bg: e.activation(sg[:], bg[:, 0:TS], AF.Silu), reads=[bg.b], writes=[sg.b])
            s.op("dve", lambda e, sg=sg, bu=bu, ch=ch: e.tensor_tensor(r.hT[:, ch, :], sg[:], bu[:, 0:TS], ALU.mult),
                 reads=[sg.b, bu.b], writes=[r.hT.b])
    for t in range(TS // 128):
        for half in range(2):
            bk = kb.banks[4 + (r.dn % 4)]
            r.dn += 1
            mm_group(kb, bk, bk[:, :], [(r.hT[:, ch, t * 128:(t + 1) * 128], r.wd[:, ch, half * 512:(half + 1) * 512])
                                        for ch in range(NC)], reads=[r.hT.b, r.wd.b])
            evac(t, half, bk)


def phase4(kb, C, D):
    s = kb.s
    NT, NE, NCm = C["NTOK"], C["NEXP"], C["NC_MOE"]
    TS = 512
    ident = kb.sb([128, 128], F32, "ident")
    s.dma("sp", ident[:], D["ident"], writes=[ident.b])
    kb.eps_t = kb.sb([128, 1], F32, "eps")
    s.op("pool", lambda e: e.memset(kb.eps_t[:], EPS), writes=[kb.eps_t.b])
    g = kb.sb([128, 8], F32, "gffn")
    s.dma("sp", g[:], D["gffn1"], writes=[g.b])
    wo = kb.sb([128, 8, 1024], BF16, "wo")
    s.dma("sp", wo[:], D["wo1"].rearrange("p (k n) -> p k n", n=1024), writes=[wo.b])
    rtr = kb.sb([128, 8, 8], F32, "rtr")
    s.dma("sp", rtr[:], D["router"].rearrange("p (k n) -> p k n", n=8), writes=[rtr.b])
    r = ffn_alloc(kb, TS, NCm)
    hnT = [kb.sb([128, 8, TS], BF16, "hnT") for _ in range(2)]
    acc = kb.sb([128, TS // 128, 1024], F32, "acc")
    accb = [Buf() for _ in range(TS // 128)]
    gw = kb.sb([128, TS // 128, 8], F32, "gw")
    og = [kb.sb([128, 8, 128], BF16, "og") for _ in range(2)]
    h1 = [kb.sb([128, 1024], F32, "h1") for _ in range(2)]
    xs = kb.sb([128, 1024], F32, "xs")
    junk = kb.sb([128, 1024], BF16, "junk")
    hn32 = kb.sb([128, 8, 128], F32, "hn32")
    sm = {k: kb.sb([128, 8], F32, k) for k in ("lg", "mx", "g1", "g2")}
    sc = {k: kb.sb([128, 1], F32, k) for k in ("ss", "rt", "rstd", "dd", "ex", "den", "w1", "w2")}
    ogt_v = None if C.get("fused") else D["ogt"].rearrange("(c p) n -> p c n", p=128)
    if C.get("fused"):
        ogc = [kb.sb([128, 4, 8, 128], BF16, "ogc") for _ in range(2)]
        sel4 = kb.sb([128, 4], F32, "sel4")
        s.dma("sp", sel4[:], D["sel4"], writes=[sel4.b])
    it = 0
    for st in range(NT // TS):
        hT_ = hnT[st % 2]
        for t in range(TS // 128):
            tok0 = st * TS + t * 128
            o_ = og[it % 2]
            h_ = h1[it % 2]
            it += 1
            if C.get("fused"):
                cd_ = ogc[it % 2]
                for r_ in range(4):
                    g_ = r_ * NT + tok0
                    b_, c0_ = g_ // 2048, g_ % 2048
                    s.dma("act", cd_[:, r_], D["ogt"][b_ * 1024:(b_ + 1) * 1024, :].rearrange("(c p) n -> p c n", p=128)[:, :, c0_:c0_ + 128],
                          writes=[cd_.b])
                s.op("dve", lambda e: e.tensor_scalar(o_[:], cd_[:, 0], sel4[:, 0:1], None, ALU.mult),
                     reads=[cd_.b, sel4.b], writes=[o_.b])
                for r_ in range(1, 4):
                    s.op("dve", lambda e, r_=r_: e.scalar_tensor_tensor(o_[:], cd_[:, r_], sel4[:, r_:r_ + 1], o_[:], ALU.mult, ALU.add),
                         reads=[cd_.b, sel4.b, o_.b], writes=[o_.b])
            else:
                s.dma("act", o_[:], ogt_v[:, :, tok0:tok0 + 128], writes=[o_.b])
            s.dma("act", h_[:], D["h1"][tok0:tok0 + 128, :], writes=[h_.b])
            for half in range(2):
                bk = kb.banks[4 + half]
                mm_group(kb, bk, bk[:, :], [(o_[:, c, :], wo[:, c, half * 512:(half + 1) * 512]) for c in range(8)],
                         reads=[o_.b, wo.b])
                s.op("dve", lambda e, bk=bk, h_=h_, t=t, half=half: e.tensor_tensor(
                    acc[:, t, half * 512:(half + 1) * 512], h_[:, half * 512:(half + 1) * 512], bk[:, :], ALU.add),
                    reads=[bk.b, h_.b], writes=[accb[t]])
            rms_stats(kb, acc[:, t, :], accb[t], junk, sc["ss"], sc["rt"], sc["rstd"])
            s.op("act", lambda e, t=t: e.activation(xs[:], acc[:, t, :], AF.Copy, scale=sc["rstd"][:]),
                 reads=[accb[t], sc["rstd"].b], writes=[xs.b])
            for hh in range(2):
                bk = kb.banks[6 + hh]
                mm_group(kb, bk, [bk[:, j * 128:(j + 1) * 128] for j in range(4)],
                         [(xs[:, (hh * 4 + j) * 128:(hh * 4 + j + 1) * 128], ident[:]) for j in range(4)],
                         reads=[xs.b, ident.b], transpose=True)
                s.op("dve", lambda e, bk=bk, hh=hh: e.tensor_tensor(
                    hn32[:, hh * 4:(hh + 1) * 4, :], bk[:, :].rearrange("p (c n) -> p c n", n=128),
                    bc(g[:, hh * 4:(hh + 1) * 4].unsqueeze(2), [128, 4, 128]), ALU.mult),
                    reads=[bk.b, g.b], writes=[hn32.b])
            s.op("pool", lambda e, t=t, hT_=hT_: e.tensor_copy(hT_[:, :, t * 128:(t + 1) * 128], hn32[:]),
                 reads=[hn32.b], writes=[hT_.b])
            bk = kb.banks[4]
            mm_group(kb, bk, bk[:, 0:8], [(hn32[:, c, :], rtr[:, c, :]) for c in range(8)], reads=[hn32.b, rtr.b])
            lg, mx, g1, g2 = sm["lg"], sm["mx"], sm["g1"], sm["g2"]
            s.op("dve", lambda e, bk=bk: e.tensor_copy(lg[:], bk[:, 0:8]), reads=[bk.b], writes=[lg.b])
            s.op("dve", lambda e: e.max(mx[:], lg[:]), reads=[lg.b], writes=[mx.b])
            s.op("dve", lambda e: e.tensor_tensor(sc["dd"][:], mx[:, 1:2], mx[:, 0:1], ALU.subtract),
                 reads=[mx.b], writes=[sc["dd"].b])
            s.op("act", lambda e: e.activation(sc["ex"][:], sc["dd"][:], AF.Exp), reads=[sc["dd"].b], writes=[sc["ex"].b])
            s.op("dve", lambda e: e.tensor_scalar(sc["den"][:], sc["ex"][:], 1.0, None, ALU.add),
                 reads=[sc["ex"].b], writes=[sc["den"].b])
            s.op("dve", lambda e: e.reciprocal(sc["w1"][:], sc["den"][:]), reads=[sc["den"].b], writes=[sc["w1"].b])
            s.op("dve", lambda e: e.tensor_tensor(sc["w2"][:], sc["ex"][:], sc["w1"][:], ALU.mult),
                 reads=[sc["ex"].b, sc["w1"].b], writes=[sc["w2"].b])
            s.op("dve", lambda e: e.tensor_scalar(g1[:], lg[:], mx[:, 0:1], sc["w1"][:], ALU.is_equal, ALU.mult),
                 reads=[lg.b, mx.b, sc["w1"].b], writes=[g1.b])
            s.op("dve", lambda e: e.tensor_scalar(g2[:], lg[:], mx[:, 1:2], sc["w2"][:], ALU.is_equal, ALU.mult),
                 reads=[lg.b, mx.b, sc["w2"].b], writes=[g2.b])
            s.op("dve", lambda e, t=t: e.tensor_tensor(gw[:, t, :], g1[:], g2[:], ALU.add),
                 reads=[g1.b, g2.b], writes=[gw.b])
        for ex in range(NE):
            def evac(t, half, bk, ex=ex):
                s.op("dve", lambda e: e.scalar_tensor_tensor(
                    acc[:, t, half * 512:(half + 1) * 512], bk[:, :], gw[:, t, ex:ex + 1],
                    acc[:, t, half * 512:(half + 1) * 512], ALU.mult, ALU.add),
                    reads=[bk.b, gw.b, accb[t]], writes=[accb[t]])
            ffn_expert(kb, r, hT_, D["mwg"][ex * 128:(ex + 1) * 128, :], D["mwu"][ex * 128:(ex + 1) * 128, :],
                       D["mwd"][ex * 128:(ex + 1) * 128, :], NCm, evac)
        for t in range(TS // 128):
            tok0 = st * TS + t * 128
            s.dma("act", D["out"][tok0:tok0 + 128, :], acc[:, t, :], reads=[accb[t]])


def nbank(kb):
    kb.bk = (getattr(kb, "bk", -1) + 1) % 8
    return kb.banks[kb.bk]


def norm_T(kb, W, x_ap, xbuf, gcol, dst, dcols, ncol=128):
    s = kb.s
    rms_stats(kb, x_ap, xbuf, W["junk"], W["ss"], W["rt"], W["rstd"])
    xs = W["xsb"]
    s.op("act", lambda e: e.activation(xs[:], x_ap, AF.Copy, scale=W["rstd"][:]), reads=[xbuf, W["rstd"].b], writes=[xs.b])
    bk = nbank(kb)
    bv = bk[:, :].bitcast(BF16)
    mm_group(kb, bk, [bv[:, j * 128:(j + 1) * 128] for j in range(8)],
             [(xs[:, j * 128:(j + 1) * 128], W["identb"][:]) for j in range(8)], reads=[xs.b, W["identb"].b], transpose=True)
    s.op("dve", lambda e: e.tensor_tensor(dst[:, :, dcols], bv.rearrange("p (c n) -> p c n", n=128),
                                           bc(gcol[:, 0:8].unsqueeze(2), [128, 8, 128]), ALU.mult),
         reads=[bk.b, gcol.b], writes=[dst.b])


def norm_scratch(kb):
    W = {}
    W["junk"] = kb.sb([128, 1024], BF16, "junk")
    W["xsb"] = kb.sb([128, 1024], BF16, "xsb")
    for k in ("ss", "rt", "rstd"):
        W[k] = kb.sb([128, 1], F32, k)
    ident = kb.sb([128, 128], F32, "ident")
    kb.s.dma("sp", ident[:], kb.D["ident"], writes=[ident.b])
    W["ident"] = ident
    W["identb"] = kb.sb([128, 128], BF16, "identb")
    kb.s.op("dve", lambda e: e.tensor_copy(W["identb"][:], ident[:]), reads=[ident.b], writes=[W["identb"].b])
    kb.eps_t = kb.sb([128, 1], F32, "eps")
    kb.s.op("pool", lambda e: e.memset(kb.eps_t[:], EPS), writes=[kb.eps_t.b])
    return W


def phase1(kb, C, D):
    s = kb.s
    NT = C["NTOK"]
    TS = 512
    kb.D = D
    W = norm_scratch(kb)

    def ld(name, shape, dt=F32, src=None, q="sp"):
        t = kb.sb(shape, dt, name)
        s.dma(q, t[:], D[name] if src is None else src, writes=[t.b])
        return t

    gmix = ld("gmix0", [128, 8])
    win = kb.sb([128, 8, 1792], BF16, "win")
    s.dma("sp", win[:], D["win"].rearrange("p (k n) -> p k n", n=1792), writes=[win.b])
    lruc = ld("lruc", [128, 4, 8])
    wab32 = ld("wa_bd", [128, 4, 128])
    wib32 = ld("wi_bd", [128, 4, 128])
    wab = kb.sb([128, 4, 128], BF16, "wab")
    wib = kb.sb([128, 4, 128], BF16, "wib")
    s.op("dve", lambda e: e.tensor_copy(wab[:], wab32[:]), reads=[wab32.b], writes=[wab.b])
    s.op("dve", lambda e: e.tensor_copy(wib[:], wib32[:]), reads=[wib32.b], writes=[wib.b])
    qkg = ld("qkg", [64, 2])
    esink = ld("sinks_b", [128, 8])
    s.op("act", lambda e: e.activation(esink[:], esink[:], AF.Exp), reads=[esink.b], writes=[esink.b])
    prot32 = ld("prot", [64, 64])
    prot = kb.sb([64, 64], BF16, "protb")
    s.op("dve", lambda e: e.tensor_copy(prot[:], prot32[:]), reads=[prot32.b], writes=[prot.b])
    ones64 = kb.sb([64, 64], F32, "ones64")
    s.op("pool", lambda e: e.memset(ones64[:], 1.0), writes=[ones64.b])
    mcur = ld("mask_cur", [128, 128])
    mprev = ld("mask_prev", [128, 128])
    mprev0 = ld("mask_prev0", [128, 128])
    cl = kb.sb([128, 4], F32, "cl")
    cl2 = kb.sb([128, 4], F32, "cl2")
    s.op("act", lambda e: e.activation(cl[:], lruc[:, :, 7], AF.Exp, scale=-1.0), reads=[lruc.b], writes=[cl.b])
    s.op("act", lambda e: e.activation(cl[:], cl[:], AF.Ln, bias=1.0), reads=[cl.b], writes=[cl.b])
    s.op("dve", lambda e: e.tensor_scalar(cl2[:], cl[:], -16.0, None, ALU.mult), reads=[cl.b], writes=[cl2.b])
    s.op("dve", lambda e: e.tensor_scalar(cl[:], cl[:], -8.0, None, ALU.mult), reads=[cl.b, cl2.b], writes=[cl.b])
    zeros = kb.sb([128, TS], F32, "zeros")
    s.op("pool", lambda e: e.memset(zeros[:], 0.0), writes=[zeros.b])
    eps64 = kb.eps_t

    hnT = [kb.sb([128, 8, TS], BF16, "hnT") for _ in range(2)]
    xt = [kb.sb([128, 1024], F32, "xt") for _ in range(3)]
    xb = [[kb.sb([128, 3 + TS], F32, "xb") for _ in range(2)] for _ in range(4)]
    hcar = [kb.sb([128, 1], F32, "hcar") for _ in range(4)]
    pcar = [kb.sb([128, 1], F32, "pcar") for _ in range(4)]
    for c in range(4):
        s.op("pool", lambda e, c=c: e.memset(hcar[c][:], 0.0), writes=[hcar[c].b])
        s.op("pool", lambda e, c=c: e.memset(pcar[c][:], 1.0), writes=[pcar[c].b])
    L = {k: kb.sb([128, TS], F32, k) for k in ("xc", "r", "gi", "a", "a2", "u", "h", "P", "gg")}
    xcb = kb.sb([128, TS], BF16, "xcb")
    yo = [kb.sb([128, TS], BF16, "yo") for _ in range(4)]
    QR = kb.sb([64, 8, TS], BF16, "QR")
    KR = kb.sb([64, 2, 128 + TS], BF16, "KR")
    Vg = [kb.sb([128, 2, 65], BF16, "Vg") for _ in range(6)]
    for v in Vg:
        s.op("pool", lambda e, v=v: e.memset(v[:], 1.0), writes=[v.b])
    A = {k: kb.sb([64, TS], F32, k) for k in ("sq", "ln", "qn32", "t1", "t2")}
    qnb = kb.sb([64, TS], BF16, "qnb")
    cs = [kb.sb([64, 2, TS], F32, "cs") for _ in range(2)]
    E_ = [kb.sb([128, 512], F32, "E") for _ in range(2)]
    Pt = [kb.sb([128, 512], BF16, "Pt") for _ in range(4)]
    Y = kb.sb([128, 8, 64], BF16, "Y")
    yT = [kb.sb([128, 4, 128], BF16, "yT") for _ in range(2)]
    den = kb.sb([128, 4], F32, "den")
    cnt = {"x": 0, "v": 0, "e": 0, "p": 0, "y": 0, "yo": 0}

    def load_norm(src_ap, dst, dcols):
        x_ = xt[cnt["x"] % 3]
        cnt["x"] += 1
        s.dma("act", x_[:], src_ap, writes=[x_.b])
        norm_T(kb, W, x_[:], x_.b, gmix, dst, dcols)

    def proj(h_, cols0, ncols, ntok, tcols):
        bk = nbank(kb)
        mm_group(kb, bk, bk[0:ncols, 0:ntok], [(win[:, k, cols0:cols0 + ncols], h_[:, k, tcols]) for k in range(8)],
                 reads=[win.b, h_.b])
        return bk

    def qk_norm_rope(bk, ntok, gidx, cst, ccols, dst_ap, dst_buf):
        n = ntok
        s.op("act", lambda e: e.activation(A["sq"][:, 0:n], bk[0:64, 0:n], AF.Square), reads=[bk.b], writes=[A["sq"].b])
        b2 = nbank(kb)
        mm_group(kb, b2, b2[0:64, 0:n], [(ones64[:], A["sq"][:, 0:n])], reads=[ones64.b, A["sq"].b])
        s.op("act", lambda e: e.activation(A["ln"][:, 0:n], b2[0:64, 0:n], AF.Ln, bias=eps64[0:64, :], scale=1.0 / 64),
             reads=[b2.b], writes=[A["ln"].b])
        s.op("act", lambda e: e.activation(A["ln"][:, 0:n], A["ln"][:, 0:n], AF.Exp, scale=-0.5),
             reads=[A["ln"].b], writes=[A["ln"].b])
        s.op("dve", lambda e: e.scalar_tensor_tensor(A["qn32"][:, 0:n], bk[0:64, 0:n], qkg[:, gidx:gidx + 1],
                                                     A["ln"][:, 0:n], ALU.mult, ALU.mult),
             reads=[bk.b, qkg.b, A["ln"].b], writes=[A["qn32"].b])
        s.op("pool", lambda e: e.tensor_copy(qnb[:, 0:n], A["qn32"][:, 0:n]), reads=[A["qn32"].b], writes=[qnb.b])
        b3 = nbank(kb)
        mm_group(kb, b3, b3[0:64, 0:n], [(prot[:], qnb[:, 0:n])], reads=[prot.b, qnb.b])
        s.op("pool", lambda e: e.tensor_tensor(A["t1"][:, 0:n], A["qn32"][:, 0:n], cst[:, 0, ccols], ALU.mult),
             reads=[A["qn32"].b, cst.b], writes=[A["t1"].b])
        s.op("dve", lambda e: e.tensor_tensor(A["t2"][:, 0:n], b3[0:64, 0:n], cst[:, 1, ccols], ALU.mult),
             reads=[b3.b, cst.b], writes=[A["t2"].b])
        s.op("pool", lambda e: e.tensor_tensor(dst_ap, A["t1"][:, 0:n], A["t2"][:, 0:n], ALU.add),
             reads=[A["t1"].b, A["t2"].b], writes=[dst_buf])

    def v_tile(h_, tcols):
        v = Vg[cnt["v"] % 6]
        cnt["v"] += 1
        bk = nbank(kb)
        mm_group(kb, bk, bk[:, 0:128], [(h_[:, k, tcols], win[:, k, 1664:1792]) for k in range(8)], reads=[win.b, h_.b])
        s.op("act", lambda e: e.copy(v[:, :, 0:64], bk[:, 0:128].rearrange("p (h d) -> p h d", d=64)),
             reads=[bk.b], writes=[v.b])
        return v

    hh_ = hnT[1]
    load_norm(D["xhalo"], hh_, slice(0, 128))
    csh = cs[1]
    s.dma("sp", csh[:, :, 0:128], D["cossin"][:, :, 0:128], writes=[csh.b])
    for c in range(4):
        bk = proj(hh_, c * 128, 128, 128, slice(0, 128))
        s.op("act", lambda e, c=c, bk=bk: e.copy(xb[c][0][:, 0:3], bk[:, 125:128]), reads=[bk.b], writes=[xb[c][0].b])
    for h in range(2):
        bk = proj(hh_, 1536 + h * 64, 64, 128, slice(0, 128))
        qk_norm_rope(bk, 128, 1, csh, slice(0, 128), KR[:, h, 0:128], KR.b)
    vprev = v_tile(hh_, slice(0, 128))

    for st in range(NT // TS):
        h_ = hnT[st % 2]
        cst = cs[st % 2]
        s.dma("sp", cst[:], D["cossin"][:, :, 128 + st * TS:128 + (st + 1) * TS], writes=[cst.b])
        for t in range(4):
            tok0 = st * TS + t * 128
            load_norm(D["x"][tok0:tok0 + 128, :], h_, slice(t * 128, (t + 1) * 128))
        allc = slice(0, TS)
        for c in range(4):
            xb_ = xb[c][st % 2]
            xbn = xb[c][(st + 1) % 2]
            bk = proj(h_, c * 128, 128, TS, allc)
            s.op("act", lambda e, bk=bk, xb_=xb_: e.copy(xb_[:, 3:3 + TS], bk[:, 0:TS]), reads=[bk.b], writes=[xb_.b])
            s.op("pool", lambda e, xb_=xb_, xbn=xbn: e.tensor_copy(xbn[:, 0:3], xb_[:, TS:TS + 3]), reads=[xb_.b], writes=[xbn.b])
            bkg = proj(h_, 512 + c * 128, 128, TS, allc)
            s.op("act", lambda e, bkg=bkg: e.activation(L["gg"][:], bkg[:, 0:TS], AF.Gelu_apprx_tanh), reads=[bkg.b], writes=[L["gg"].b])
            xc = L["xc"]
            s.op("dve", lambda e, xb_=xb_, c=c: e.tensor_scalar(xc[:], xb_[:, 0:TS], lruc[:, c, 0:1], lruc[:, c, 4:5], ALU.mult, ALU.add),
                 reads=[xb_.b, lruc.b], writes=[xc.b])
            for j in range(1, 4):
                s.op("dve", lambda e, xb_=xb_, c=c, j=j: e.scalar_tensor_tensor(xc[:], xb_[:, j:j + TS], lruc[:, c, j:j + 1], xc[:], ALU.mult, ALU.add),
                     reads=[xb_.b, lruc.b, xc.b], writes=[xc.b])
            s.op("pool", lambda e: e.tensor_copy(xcb[:], xc[:]), reads=[xc.b], writes=[xcb.b])
            b1 = nbank(kb)
            mm_group(kb, b1, b1[:, 0:TS], [(wab[:, c, :], xcb[:])], reads=[wab.b, xcb.b])
            b2 = nbank(kb)
            mm_group(kb, b2, b2[:, 0:TS], [(wib[:, c, :], xcb[:])], reads=[wib.b, xcb.b])
            s.op("act", lambda e, b1=b1, c=c: e.activation(L["r"][:], b1[:, 0:TS], AF.Sigmoid, bias=lruc[:, c, 5:6]),
                 reads=[b1.b, lruc.b], writes=[L["r"].b])
            s.op("act", lambda e, b2=b2, c=c: e.activation(L["gi"][:], b2[:, 0:TS], AF.Sigmoid, bias=lruc[:, c, 6:7]),
                 reads=[b2.b, lruc.b], writes=[L["gi"].b])
            s.op("act", lambda e, c=c: e.activation(L["a"][:], L["r"][:], AF.Exp, scale=cl[:, c:c + 1]),
                 reads=[L["r"].b, cl.b], writes=[L["a"].b])
            s.op("act", lambda e, c=c: e.activation(L["a2"][:], L["r"][:], AF.Exp, scale=cl2[:, c:c + 1]),
                 reads=[L["r"].b, cl2.b], writes=[L["a2"].b])
            s.op("dve", lambda e: e.tensor_scalar(L["a2"][:], L["a2"][:], -1.0, 1.0, ALU.mult, ALU.add),
                 reads=[L["a2"].b], writes=[L["a2"].b])
            s.op("act", lambda e: e.activation(L["a2"][:], L["a2"][:], AF.Sqrt), reads=[L["a2"].b], writes=[L["a2"].b])
            s.op("pool", lambda e: e.tensor_tensor(L["u"][:], L["gi"][:], xc[:], ALU.mult), reads=[L["gi"].b, xc.b], writes=[L["u"].b])
            s.op("dve", lambda e: e.tensor_tensor(L["u"][:], L["u"][:], L["a2"][:], ALU.mult), reads=[L["u"].b, L["a2"].b], writes=[L["u"].b])
            s.op("dve", lambda e, c=c: e.tensor_tensor_scan(L["h"][:], L["a"][:], L["u"][:], hcar[c][:], ALU.mult, ALU.add),
                 reads=[L["a"].b, L["u"].b, hcar[c].b], writes=[L["h"].b])
            s.op("dve", lambda e, c=c: e.tensor_tensor_scan(L["P"][:], L["a"][:], zeros[:], pcar[c][:], ALU.mult, ALU.add),
                 reads=[L["a"].b, zeros.b, pcar[c].b], writes=[L["P"].b])
            s.op("act", lambda e, c=c: e.copy(hcar[c][:], L["h"][:, TS - 1:TS]), reads=[L["h"].b], writes=[hcar[c].b])
            s.op("act", lambda e, c=c: e.copy(pcar[c][:], L["P"][:, TS - 1:TS]), reads=[L["P"].b], writes=[pcar[c].b])
            y1 = yo[cnt["yo"] % 4]
            y2 = yo[(cnt["yo"] + 1) % 4]
            cnt["yo"] += 2
            s.op("pool", lambda e, y1=y1: e.tensor_tensor(y1[:], L["h"][:], L["gg"][:], ALU.mult), reads=[L["h"].b, L["gg"].b], writes=[y1.b])
            s.op("pool", lambda e, y2=y2: e.tensor_tensor(y2[:], L["P"][:], L["gg"][:], ALU.mult), reads=[L["P"].b, L["gg"].b], writes=[y2.b])
            s.dma("sp", D["yloc"][c * 128:(c + 1) * 128, st * TS:(st + 1) * TS], y1[:], reads=[y1.b])
            s.dma("sp", D["pg"][c * 128:(c + 1) * 128, st * TS:(st + 1) * TS], y2[:], reads=[y2.b])
        for hq in range(8):
            bk = proj(h_, 1024 + hq * 64, 64, TS, allc)
            qk_norm_rope(bk, TS, 0, cst, allc, QR[:, hq, :], QR.b)
        for h in range(2):
            bk = proj(h_, 1536 + h * 64, 64, TS, allc)
            qk_norm_rope(bk, TS, 1, cst, allc, KR[:, h, 128:128 + TS], KR.b)
        vt = [vprev] + [v_tile(h_, slice(t * 128, (t + 1) * 128)) for t in range(4)]
        for b in range(4):
            mp = mprev0 if (st == 0 and b == 0) else mprev
            for h in range(2):
                pts = []
                for w_, (kc0, mk) in enumerate(((b * 128, mp), (128 + b * 128, mcur))):
                    bk = nbank(kb)
                    mm_group(kb, bk, bk[:, :].rearrange("p (g n) -> p g n", n=128),
                             [(KR[:, h, kc0:kc0 + 128], QR[:, 4 * h:4 * h + 4, b * 128:(b + 1) * 128])], reads=[KR.b, QR.b])
                    e_ = E_[cnt["e"] % 2]
                    cnt["e"] += 1
                    s.op("act", lambda e, e_=e_, bk=bk: e.activation(e_[:], bk[:, :], AF.Exp, scale=0.125), reads=[bk.b], writes=[e_.b])
                    p_ = Pt[cnt["p"] % 4]
                    cnt["p"] += 1
                    s.op("dve" if w_ == 0 else "pool", lambda e, p_=p_, e_=e_, mk=mk: e.tensor_tensor(
                        p_[:].rearrange("p (g n) -> p g n", n=128), e_[:].rearrange("p (g n) -> p g n", n=128),
                        bc(mk[:].unsqueeze(1), [128, 4, 128]), ALU.mult), reads=[e_.b, mk.b], writes=[p_.b])
                    pts.append(p_)
                bo = nbank(kb)
                for g in range(4):
                    mm_group(kb, bo, bo[:, g * 65:(g + 1) * 65],
                             [(pts[0][:, g * 128:(g + 1) * 128], vt[b][:, h, :]), (pts[1][:, g * 128:(g + 1) * 128], vt[b + 1][:, h, :])],
                             reads=[pts[0].b, pts[1].b, vt[b].b, vt[b + 1].b])
                bov = bo[:, 0:260].rearrange("p (g n) -> p g n", n=65)
                s.op("dve", lambda e, bov=bov, h=h: e.tensor_tensor(den[:], bov[:, :, 64], esink[:, 4 * h:4 * h + 4], ALU.add),
                     reads=[bo.b, esink.b], writes=[den.b])
                s.op("dve", lambda e: e.reciprocal(den[:], den[:]), reads=[den.b], writes=[den.b])
                s.op("dve", lambda e, bov=bov, h=h: e.tensor_tensor(Y[:, 4 * h:4 * h + 4, :], bov[:, :, 0:64],
                                                                    bc(den[:].unsqueeze(2), [128, 4, 64]), ALU.mult),
                     reads=[bo.b, den.b], writes=[Y.b])
            bt = nbank(kb)
            btv = bt[:, :].bitcast(BF16)
            Yf = Y[:].rearrange("p h d -> p (h d)")
            mm_group(kb, bt, [btv[:, j * 128:(j + 1) * 128] for j in range(4)],
                     [(Yf[:, j * 128:(j + 1) * 128], W["identb"][:]) for j in range(4)], reads=[Y.b, W["identb"].b], transpose=True)
            yt_ = yT[cnt["y"] % 2]
            cnt["y"] += 1
            s.op("act", lambda e, yt_=yt_, btv=btv: e.copy(yt_[:], btv[:, 0:512].rearrange("p (c n) -> p c n", n=128)),
                 reads=[bt.b], writes=[yt_.b])
            tok0 = st * TS + b * 128
            s.dma("sp", D["yatt"].rearrange("(c p) n -> p c n", p=128)[:, :, tok0:tok0 + 128], yt_[:], reads=[yt_.b])
        vprev = vt[4]
        s.op("pool", lambda e: e.tensor_copy(KR[:, :, 0:128], KR[:, :, TS:TS + 128]), reads=[KR.b], writes=[KR.b])
    seg = kb.sb([128, 8], F32, "seg")
    for c in range(4):
        s.op("act", lambda e, c=c: e.copy(seg[:, c:c + 1], pcar[c][:]), reads=[pcar[c].b], writes=[seg.b])
        s.op("act", lambda e, c=c: e.copy(seg[:, 4 + c:5 + c], hcar[c][:]), reads=[hcar[c].b], writes=[seg.b])
    s.dma("sp", D["seg"], seg[:], reads=[seg.b])


def phase2(kb, C, D):
    s = kb.s
    NT = C["NTOK"]
    TS = 512
    NCf = 22
    kb.D = D
    W = norm_scratch(kb)
    gffn = kb.sb([128, 8], F32, "gffn0")
    s.dma("sp", gffn[:], D["gffn0"], writes=[gffn.b])
    gmix = kb.sb([128, 8], F32, "gmix1")
    s.dma("sp", gmix[:], D["gmix1"], writes=[gmix.b])
    wo = kb.sb([128, 8, 1024], BF16, "wo0")
    s.dma("sp", wo[:], D["wo0"].rearrange("p (k n) -> p k n", n=1024), writes=[wo.b])
    sega = kb.sb([128, 4, 8], F32, "sega")
    s.dma("sp", sega[:], D["segall"].rearrange("(r p) n -> p r n", p=128), writes=[sega.b])
    sel = kb.sb([128, 4], F32, "sel")
    s.dma("sp", sel[:], D["sel"], writes=[sel.b])
    hin = kb.sb([128, 4], F32, "hin")
    tmp = kb.sb([128, 4], F32, "tmpc")
    s.op("pool", lambda e: e.memset(hin[:], 0.0), writes=[hin.b])
    for i in range(3):
        s.op("dve", lambda e, i=i: e.tensor_tensor(tmp[:], sega[:, i, 0:4], hin[:], ALU.mult), reads=[sega.b, hin.b], writes=[tmp.b])
        s.op("dve", lambda e, i=i: e.tensor_tensor(tmp[:], tmp[:], sega[:, i, 4:8], ALU.add), reads=[sega.b, tmp.b], writes=[tmp.b])
        s.op("dve", lambda e: e.tensor_tensor(tmp[:], tmp[:], hin[:], ALU.subtract), reads=[tmp.b, hin.b], writes=[tmp.b])
        s.op("dve", lambda e, i=i: e.scalar_tensor_tensor(hin[:], tmp[:], sel[:, i:i + 1], hin[:], ALU.mult, ALU.add),
             reads=[tmp.b, sel.b, hin.b], writes=[hin.b])
    r = ffn_alloc(kb, TS, NCf)
    hnT = [kb.sb([128, 8, TS], BF16, "hnT") for _ in range(2)]
    acc = kb.sb([128, TS // 128, 1024], F32, "acc")
    accb = [Buf() for _ in range(TS // 128)]
    xt = [kb.sb([128, 1024], F32, "xt") for _ in range(2)]
    yl = kb.sb([128, 4, TS], BF16, "yl")
    pg = kb.sb([128, 4, TS], BF16, "pgt")
    ya = kb.sb([128, 4, TS], BF16, "ya")
    ym = kb.sb([128, 4, TS], BF16, "ym")
    hn1 = [kb.sb([128, 8, 128], BF16, "hn1") for _ in range(2)]
    it = 0
    for st in range(NT // TS):
        cols = slice(st * TS, (st + 1) * TS)
        s.dma("act", yl[:], D["yloc"].rearrange("(c p) n -> p c n", p=128)[:, :, cols], writes=[yl.b])
        s.dma("act", pg[:], D["pg"].rearrange("(c p) n -> p c n", p=128)[:, :, cols], writes=[pg.b])
        s.dma("act", ya[:], D["yatt"].rearrange("(c p) n -> p c n", p=128)[:, :, cols], writes=[ya.b])
        for c in range(4):
            s.op("dve", lambda e, c=c: e.scalar_tensor_tensor(ym[:, c, :], pg[:, c, :], hin[:, c:c + 1], yl[:, c, :], ALU.mult, ALU.add),
                 reads=[pg.b, hin.b, yl.b], writes=[ym.b])
        h_ = hnT[st % 2]
        for t in range(TS // 128):
            tok0 = st * TS + t * 128
            tc_ = slice(t * 128, (t + 1) * 128)
            x_ = xt[it % 2]
            it += 1
            s.dma("act", x_[:], D["x"][tok0:tok0 + 128, :], writes=[x_.b])
            for half in range(2):
                bk = nbank(kb)
                mm_group(kb, bk, bk[:, :], [((ym[:, c, tc_] if c < 4 else ya[:, c - 4, tc_]), wo[:, c, half * 512:(half + 1) * 512])
                                            for c in range(8)], reads=[ym.b, ya.b, wo.b])
                s.op("dve", lambda e, bk=bk, x_=x_, t=t, half=half: e.tensor_tensor(
                    acc[:, t, half * 512:(half + 1) * 512], x_[:, half * 512:(half + 1) * 512], bk[:, :], ALU.add),
                    reads=[bk.b, x_.b], writes=[accb[t]])
            norm_T(kb, W, acc[:, t, :], accb[t], gffn, h_, tc_)

        def evac(t, half, bk):
            s.op("dve", lambda e: e.tensor_tensor(acc[:, t, half * 512:(half + 1) * 512], acc[:, t, half * 512:(half + 1) * 512],
                                                  bk[:, :], ALU.add), reads=[bk.b, accb[t]], writes=[accb[t]])
        ffn_expert(kb, r, h_, D["fwg"], D["fwu"], D["fwd"], NCf, evac)
        agt = []
        for t in range(TS // 128):
            tok0 = st * TS + t * 128
            s.dma("act", D["h1"][tok0:tok0 + 128, :], acc[:, t, :], reads=[accb[t]])
            o_ = hn1[t % 2]
            norm_T(kb, W, acc[:, t, :], accb[t], gmix, o_, slice(0, 128))
            if C.get("ag_hn"):
                tk_ = s.dma("act", D["hn1T"][st * 1024:(st + 1) * 1024, :].rearrange("(c p) n -> p c n", p=128)[:, :, t * 128:(t + 1) * 128],
                            o_[:], reads=[o_.b])
                agt.append(tk_)
            else:
                s.dma("act", D["hn1T"].rearrange("(c p) n -> p c n", p=128)[:, :, tok0:tok0 + 128], o_[:], reads=[o_.b])
        if C.get("ag_hn"):
            s.collective("AllGather", D["hn1T"][st * 1024:(st + 1) * 1024, :], D["hng"][st * 4096:(st + 1) * 4096, :], C["ag_hn"], deps=agt)


def phase3(kb, C, D):
    s = kb.s
    NTF, NTK = C["NTF"], C["NTK"]
    TS = 512
    NST = NTF // TS
    kb.D = D
    W = norm_scratch(kb)
    ident, identb = W["ident"], W["identb"]

    def ld(name, shape, dt=F32, src=None):
        t = kb.sb(shape, dt, name)
        s.dma("sp", t[:], D[name] if src is None else src, writes=[t.b])
        return t

    wqkv = kb.sb([128, 8, 768], BF16, "wqkv")
    s.dma("sp", wqkv[:], D["wqkv"].rearrange("p (k n) -> p k n", n=768), writes=[wqkv.b])
    wgate = kb.sb([128, 8, 260], BF16, "wgab")
    s.dma("sp", wgate[:, :, 0:256], D["wgate"].rearrange("p (k n) -> p k n", n=256), writes=[wgate.b])
    s.dma("sp", wgate[:, :, 256:260], D["wab"].rearrange("p (k n) -> p k n", n=4), writes=[wgate.b])
    abS = kb.sb([128, 4, 4], F32, "abS")
    convw = ld("convw", [128, 6, 4])
    hc = ld("hconst", [128, 4])
    onw = ld("onw_b", [128, 128])
    U = ld("maskU", [128, 128])
    Lo = ld("maskL", [128, 128])
    Bs = ld("maskB", [128, 128])
    C0 = ld("maskC0", [128, 128])
    C1 = ld("maskC1", [128, 128])
    nalog = kb.sb([128, 2], F32, "nalog")
    s.op("act", lambda e: e.activation(nalog[:], hc[:, 0:2], AF.Exp), reads=[hc.b], writes=[nalog.b])
    s.op("dve", lambda e: e.tensor_scalar(nalog[:], nalog[:], -1.0, None, ALU.mult), reads=[nalog.b], writes=[nalog.b])
    onesb = kb.sb([128, 128], BF16, "onesb")
    s.op("pool", lambda e: e.memset(onesb[:], 1.0), writes=[onesb.b])

    hnT = [kb.sb([128, 8, TS], BF16, "hnT") for _ in range(2)]
    cb = [[kb.sb([128, 3 + TS], F32, "cb") for _ in range(2)] for _ in range(6)]
    for b in range(6):
        s.op("pool", lambda e, b=b: e.memset(cb[b][0][:, 0:3], 0.0), writes=[cb[b][0].b])
    cacc = [kb.sb([128, TS], F32, "cacc") for _ in range(2)]
    csl = [kb.sb([128, TS], F32, "csl") for _ in range(2)]
    sqb = [kb.sb([128, TS], BF16, "sqb") for _ in range(2)]
    lnr = [kb.sb([128, TS], F32, "lnr") for _ in range(2)]
    FTa = [kb.sb([128, 6, TS], BF16, "FTa") for _ in range(2)]
    sgate = [[kb.sb([128, 256], F32, "sgate") for _ in range(4)] for _ in range(2)]
    sc = {k: kb.sb([128, 4, 2], F32, k) for k in ("xa", "gtm", "beta")}
    cs8 = [{k: kb.sb([128, 8], F32, k) for k in ("eG", "eGlG", "egl0", "egl1", "bEG", "dGl")} for _ in range(2)]
    S32 = [kb.sb([128, 128], F32, "S32") for _ in range(2)]
    Sbf = [kb.sb([128, 128], BF16, "Sbf") for _ in range(2)]
    for h in range(2):
        s.op("pool", lambda e, h=h: e.memset(S32[h][:], 0.0), writes=[S32[h].b])
        s.op("pool", lambda e, h=h: e.memset(Sbf[h][:], 0.0), writes=[Sbf[h].b])

    def mkset(bf_names, f_names, small=()):
        d = {}
        for k in bf_names:
            d[k] = kb.sb([128, 256] if k == "VK" else [128, 128], BF16, k)
        for k in f_names:
            d[k] = kb.sb([128, 128], F32, k)
        for k in small:
            d[k] = kb.sb([128, 1], F32, k)
        return d
    HO = [[[mkset(("wT", "kdec", "aqkT"), ("u",)) for _ in range(2)] for _ in range(4)] for _ in range(2)]
    PT = [[mkset(("VK", "Lb", "Nb", "P", "Q", "XL", "XN", "P2", "Q2", "XL2", "XN2", "wtok"), ("gL", "E", "ET", "t1", "t2"))
           for _ in range(2)] for _ in range(4)]
    STp = [mkset(("vn", "ogb"), ("av", "O", "on"), ("ss", "rt", "rstd")) for _ in range(2)]
    ogs = [kb.sb([128, 2, TS], BF16, "ogs") for _ in range(2)]

    def evc(en, out, src, reads, wbuf, scale=None):
        if en == "act":
            if scale is None:
                s.op("act", lambda e: e.copy(out, src), reads=reads, writes=[wbuf])
            else:
                s.op("act", lambda e: e.activation(out, src, AF.Copy, scale=scale), reads=reads, writes=[wbuf])
        else:
            if scale is None:
                s.op(en, lambda e: e.tensor_copy(out, src), reads=reads, writes=[wbuf])
            else:
                s.op(en, lambda e: e.tensor_scalar(out, src, scale, None, ALU.mult), reads=reads, writes=[wbuf])

    def pre_gen(st):
        par = st % 2
        h_ = hnT[par]
        F_ = FTa[par]
        c8 = cs8[par]
        tok0 = st * TS
        rk, col0 = tok0 // NTK, tok0 % NTK
        if C.get("chunked") == 2:
            stl_ = col0 // TS
            s.dma("act", h_[:], D["hng"][stl_ * 4096 + rk * 1024:stl_ * 4096 + (rk + 1) * 1024, :].rearrange("(c p) n -> p c n", p=128),
                  writes=[h_.b])
        elif C.get("chunked"):
            s.dma("act", h_[:], D["hng"].rearrange("(c r p) n -> p c r n", r=4, p=128)[:, :, rk, col0:col0 + TS], writes=[h_.b])
        else:
            s.dma("act", h_[:], D["hng"][rk * 1024:(rk + 1) * 1024, :].rearrange("(c p) n -> p c n", p=128)[:, :, col0:col0 + TS],
                  writes=[h_.b])
        for b in range(6):
            c_ = cb[b][par]
            cn = cb[b][(st + 1) % 2]
            ca, cl_, sq_, ln_ = cacc[b % 2], csl[b % 2], sqb[b % 2], lnr[b % 2]
            bk = nbank(kb)
            mm_group(kb, bk, bk[:, 0:TS], [(wqkv[:, k, b * 128:(b + 1) * 128], h_[:, k, :]) for k in range(8)], reads=[wqkv.b, h_.b])
            s.op("act", lambda e: e.copy(c_[:, 3:3 + TS], bk[:, 0:TS]), reads=[bk.b], writes=[c_.b])
            s.op("pool", lambda e: e.tensor_copy(cn[:, 0:3], c_[:, TS:TS + 3]), reads=[c_.b], writes=[cn.b])
            s.op("dve", lambda e: e.tensor_scalar(ca[:], c_[:, 0:TS], convw[:, b, 0:1], None, ALU.mult),
                 reads=[c_.b, convw.b], writes=[ca.b])
            for j in range(1, 4):
                s.op("dve", lambda e: e.scalar_tensor_tensor(ca[:], c_[:, j:j + TS], convw[:, b, j:j + 1], ca[:], ALU.mult, ALU.add),
                     reads=[c_.b, convw.b, ca.b], writes=[ca.b])
            yield
            if b >= 4:
                s.op("act", lambda e: e.activation(F_[:, b, :], ca[:], AF.Silu), reads=[ca.b], writes=[F_.b])
                continue
            s.op("act", lambda e: e.activation(cl_[:], ca[:], AF.Silu), reads=[ca.b], writes=[cl_.b])
            s.op("act", lambda e: e.activation(sq_[:], cl_[:], AF.Square), reads=[cl_.b], writes=[sq_.b])
            b2 = nbank(kb)
            mm_group(kb, b2, b2[:, 0:TS], [(onesb[:], sq_[:])], reads=[onesb.b, sq_.b])
            s.op("act", lambda e: e.activation(ln_[:], b2[:, 0:TS], AF.Ln, bias=kb.eps_t[:]), reads=[b2.b], writes=[ln_.b])
            s.op("act", lambda e: e.activation(ln_[:], ln_[:], AF.Exp, scale=-0.5), reads=[ln_.b], writes=[ln_.b])
            s.op("dve", lambda e: e.scalar_tensor_tensor(F_[:, b, :], cl_[:], (128.0 ** -0.5) if b in (0, 2) else 1.0, ln_[:], ALU.mult, ALU.mult),
                 reads=[cl_.b, ln_.b], writes=[F_.b])
            yield
        for t in range(4):
            tc_ = slice(t * 128, (t + 1) * 128)
            bk = nbank(kb)
            sg_ = sgate[par][t]
            mm_group(kb, bk, bk[:, 0:260], [(h_[:, k, tc_], wgate[:, k, :]) for k in range(8)], reads=[wgate.b, h_.b])
            s.op("act", lambda e: e.activation(sg_[:], bk[:, 0:256], AF.Silu), reads=[bk.b], writes=[sg_.b])
            s.op("act", lambda e: e.copy(abS[:, t, :], bk[:, 256:260]), reads=[bk.b], writes=[abS.b])
            yield
        abv = abS
        s.op("dve", lambda e: e.tensor_tensor(sc["xa"][:], abv[:, :, 0:2], bc(hc[:, 2:4].unsqueeze(1), [128, 4, 2]), ALU.add),
             reads=[abS.b, hc.b], writes=[sc["xa"].b])
        s.op("act", lambda e: e.activation(sc["beta"][:], abv[:, :, 2:4], AF.Sigmoid), reads=[abS.b], writes=[sc["beta"].b])
        s.op("act", lambda e: e.activation(sc["xa"][:], sc["xa"][:], AF.Exp), reads=[sc["xa"].b], writes=[sc["xa"].b])
        s.op("act", lambda e: e.activation(sc["xa"][:], sc["xa"][:], AF.Ln, bias=1.0), reads=[sc["xa"].b], writes=[sc["xa"].b])
        s.op("dve", lambda e: e.tensor_tensor(sc["gtm"][:], sc["xa"][:], bc(nalog[:].unsqueeze(1), [128, 4, 2]), ALU.mult),
             reads=[sc["xa"].b, nalog.b], writes=[sc["gtm"].b])
        g8 = sc["gtm"][:].rearrange("p t n -> p (t n)")
        b8 = sc["beta"][:].rearrange("p t n -> p (t n)")
        bcs = nbank(kb)
        for i, m in enumerate((U, Bs, C0, C1)):
            mm_group(kb, bcs, bcs[:, i * 8:(i + 1) * 8], [(m[:], g8)], reads=[m.b, sc["gtm"].b])
        s.op("act", lambda e: e.activation(c8["eG"][:], bcs[:, 0:8], AF.Exp), reads=[bcs.b], writes=[c8["eG"].b])
        s.op("act", lambda e: e.copy(c8["dGl"][:], bcs[:, 8:16]), reads=[bcs.b], writes=[c8["dGl"].b])
        s.op("act", lambda e: e.activation(c8["egl0"][:], bcs[:, 16:24], AF.Exp), reads=[bcs.b], writes=[c8["egl0"].b])
        s.op("act", lambda e: e.activation(c8["egl1"][:], bcs[:, 24:32], AF.Exp), reads=[bcs.b], writes=[c8["egl1"].b])
        s.op("dve", lambda e: e.tensor_tensor(c8["dGl"][:], c8["dGl"][:], bcs[:, 0:8], ALU.subtract), reads=[bcs.b, c8["dGl"].b], writes=[c8["dGl"].b])
        s.op("act", lambda e: e.activation(c8["eGlG"][:], c8["dGl"][:], AF.Exp), reads=[c8["dGl"].b], writes=[c8["eGlG"].b])
        s.op("dve", lambda e: e.tensor_tensor(c8["bEG"][:], c8["eG"][:], b8, ALU.mult), reads=[c8["eG"].b, sc["beta"].b], writes=[c8["bEG"].b])
        yield
        chains = [(t, h) for t in range(4) for h in range(2)]
        for (t, h) in chains:
            tc_ = slice(t * 128, (t + 1) * 128)
            col = slice(t * 2 + h, t * 2 + h + 1)
            d = PT[t][h]
            o = HO[par][t][h]
            bk = nbank(kb)
            bv = bk[:, :].bitcast(BF16)
            mm_group(kb, bk, [bv[:, 0:128], bv[:, 128:256]], [(F_[:, 2 * h + 1, tc_], identb[:]), (F_[:, 4 + h, tc_], identb[:])],
                     reads=[F_.b, identb.b], transpose=True)
            evc("act", d["VK"][:, 0:128], bv[:, 128:256], [bk.b, sc["beta"].b], d["VK"].b, scale=b8[:, col])
            evc("act", d["VK"][:, 128:256], bv[:, 0:128], [bk.b, c8["bEG"].b], d["VK"].b, scale=c8["bEG"][:, col])
            evc("act", o["kdec"][:], bv[:, 0:128], [bk.b, c8["eGlG"].b], o["kdec"].b, scale=c8["eGlG"][:, col])
            s.op("pool", lambda e: e.tensor_scalar(d["gL"][:], Lo[:], g8[:, col], None, ALU.mult),
                 reads=[Lo.b, sc["gtm"].b], writes=[d["gL"].b])
            bD = nbank(kb)
            mm_group(kb, bD, bD[:, 0:128], [(U[:], d["gL"][:])], reads=[U.b, d["gL"].b])
            mm_group(kb, bD, bD[:, 128:256], [(d["gL"][:], U[:])], reads=[U.b, d["gL"].b])
            s.op("act", lambda e: e.activation(d["E"][:], bD[:, 0:128], AF.Exp), reads=[bD.b], writes=[d["E"].b])
            s.op("act", lambda e: e.activation(d["ET"][:], bD[:, 128:256], AF.Exp), reads=[bD.b], writes=[d["ET"].b])
            bK = nbank(kb)
            mm_group(kb, bK, bK[:, 0:256].rearrange("p (a n) -> p a n", n=128), [(F_[:, 2 * h + 1, tc_], F_[:, 2 * h:2 * h + 2, tc_])], reads=[F_.b])
            s.op("dve", lambda e: e.scalar_tensor_tensor(d["t1"][:], bK[:, 128:256], b8[:, col], d["E"][:], ALU.mult, ALU.mult),
                 reads=[bK.b, sc["beta"].b, d["E"].b], writes=[d["t1"].b])
            s.op("dve", lambda e: e.tensor_tensor(d["t2"][:], bK[:, 0:128], d["ET"][:], ALU.mult),
                 reads=[bK.b, d["ET"].b], writes=[d["t2"].b])
            s.op("pool", lambda e: e.tensor_tensor(d["Lb"][:], d["t1"][:], Lo[:], ALU.mult), reads=[d["t1"].b, Lo.b], writes=[d["Lb"].b])
            s.op("pool", lambda e: e.tensor_tensor(o["aqkT"][:], d["t2"][:], U[:], ALU.mult), reads=[d["t2"].b, U.b], writes=[o["aqkT"].b])
            yield
        for (t, h) in chains:
            d = PT[t][h]
            bN = nbank(kb)
            bNv = bN[:, :].bitcast(BF16)
            mm_group(kb, bN, [bNv[:, 0:128]], [(d["Lb"][:], identb[:])], reads=[d["Lb"].b, identb.b], transpose=True)
            evc("act", d["Nb"][:], bNv[:, 0:128], [bN.b], d["Nb"].b)
            s.op("pool", lambda e: e.tensor_tensor(d["P"][:], identb[:], d["Nb"][:], ALU.subtract), reads=[identb.b, d["Nb"].b], writes=[d["P"].b])
        yield
        cur = {ch: dict(XL=PT[ch[0]][ch[1]]["Lb"], XN=PT[ch[0]][ch[1]]["Nb"], P=PT[ch[0]][ch[1]]["P"]) for ch in chains}
        for lvl in range(5):
            alt = (lvl % 2 == 0)
            last = (lvl == 4)
            for i_, ch in enumerate(chains):
                d = PT[ch[0]][ch[1]]
                c_ = cur[ch]
                nXL, nXN = (d["XL"], d["XN"]) if alt else (d["XL2"], d["XN2"])
                bq = nbank(kb)
                mm_group(kb, bq, bq[:, 0:128], [(c_["XN"][:], c_["XL"][:])], reads=[c_["XN"].b, c_["XL"].b])
                if not last:
                    mm_group(kb, bq, bq[:, 128:256], [(c_["XL"][:], c_["XN"][:])], reads=[c_["XN"].b, c_["XL"].b])
                en = "act" if i_ % 2 == 0 else "dve"
                evc(en, nXL[:], bq[:, 0:128], [bq.b], nXL.b)
                if not last:
                    evc(en, nXN[:], bq[:, 128:256], [bq.b], nXN.b)
                c_["XL"], c_["XN"] = nXL, nXN
                if i_ % 2 == 1:
                    yield
            for i_, ch in enumerate(chains):
                d = PT[ch[0]][ch[1]]
                c_ = cur[ch]
                nP = d["P2"] if alt else d["P"]
                Po = c_["P"]
                bp = nbank(kb)
                mm_group(kb, bp, bp[:, 0:128], [(c_["XL"][:], Po[:])], reads=[Po.b, c_["XL"].b])
                s.op("dve", lambda e: e.tensor_tensor(nP[:], Po[:], bp[:, 0:128], ALU.add), reads=[Po.b, bp.b], writes=[nP.b])
                c_["P"] = nP
                if i_ % 2 == 1:
                    yield
        for ch in chains:
            d = PT[ch[0]][ch[1]]
            o = HO[par][ch[0]][ch[1]]
            TT = cur[ch]["P"]
            bu = nbank(kb)
            mm_group(kb, bu, bu[:, 0:256], [(TT[:], d["VK"][:])], reads=[TT.b, d["VK"].b])
            evc("act", o["u"][:], bu[:, 0:128], [bu.b], o["u"].b)
            evc("act", d["wtok"][:], bu[:, 128:256], [bu.b], d["wtok"].b)
            bw = nbank(kb)
            bwv = bw[:, :].bitcast(BF16)
            mm_group(kb, bw, [bwv[:, 0:128]], [(d["wtok"][:], identb[:])], reads=[d["wtok"].b, identb.b], transpose=True)
            evc("act", o["wT"][:], bwv[:, 0:128], [bw.b], o["wT"].b)
            yield

    def scan_gen(st):
        par = st % 2
        F_ = FTa[par]
        c8 = cs8[par]
        og_ = ogs[par]
        tok0 = st * TS
        for t in range(4):
            tc_ = slice(t * 128, (t + 1) * 128)
            for ci in range(2):
                pr = slice(64 * ci, 64 * ci + 64)
                cc = slice(t * 128 + 64 * ci, t * 128 + 64 * ci + 64)
                egl = c8["egl0"] if ci == 0 else c8["egl1"]
                for h in range(2):
                    col = slice(t * 2 + h, t * 2 + h + 1)
                    o = HO[par][t][h]
                    d = STp[h]
                    bs = nbank(kb)
                    mm_group(kb, bs, bs[pr, 0:128], [(o["wT"][:, pr], Sbf[h][:])], reads=[o["wT"].b, Sbf[h].b])
                    mm_group(kb, bs, bs[pr, 128:256], [(F_[:, 2 * h, cc], Sbf[h][:])], reads=[F_.b, Sbf[h].b])
                    s.op("dve", lambda e: e.tensor_tensor(d["vn"][pr, :], o["u"][pr, :], bs[pr, 0:128], ALU.subtract),
                         reads=[o["u"].b, bs.b], writes=[d["vn"].b])
                    b2 = nbank(kb)
                    mm_group(kb, b2, b2[:, 0:128], [(o["kdec"][pr, :], d["vn"][pr, :])], reads=[o["kdec"].b, d["vn"].b])
                    mm_group(kb, b2, b2[pr, 128:256], [(o["aqkT"][pr, pr], d["vn"][pr, :])], reads=[o["aqkT"].b, d["vn"].b])
                    s.op("dve", lambda e: e.scalar_tensor_tensor(S32[h][:], S32[h][:], egl[:, col], b2[:, 0:128], ALU.mult, ALU.add),
                         reads=[S32[h].b, egl.b, b2.b], writes=[S32[h].b])
                    s.op("dve", lambda e: e.tensor_copy(Sbf[h][:], S32[h][:]), reads=[S32[h].b], writes=[Sbf[h].b])
                    s.op("dve", lambda e: e.tensor_copy(d["av"][pr, :], b2[pr, 128:256]), reads=[b2.b], writes=[d["av"].b])
                    s.op("dve", lambda e: e.scalar_tensor_tensor(d["O"][pr, :], bs[pr, 128:256], c8["eG"][pr, col], d["av"][pr, :], ALU.mult, ALU.add),
                         reads=[bs.b, c8["eG"].b, d["av"].b], writes=[d["O"].b])
                    yield
            for h in range(2):
                d = STp[h]
                sg_ = sgate[par][t]
                s.op("act", lambda e: e.activation(d["on"][:], d["O"][:], AF.Square, accum_out=d["ss"][:]), reads=[d["O"].b], writes=[d["on"].b, d["ss"].b])
                s.op("act", lambda e: e.activation(d["rt"][:], d["ss"][:], AF.Sqrt, bias=kb.eps_t[:], scale=1.0 / 128.0), reads=[d["ss"].b], writes=[d["rt"].b])
                s.op("dve", lambda e: e.reciprocal(d["rstd"][:], d["rt"][:]), reads=[d["rt"].b], writes=[d["rstd"].b])
                s.op("dve", lambda e: e.scalar_tensor_tensor(d["on"][:], d["O"][:], d["rstd"][:], onw[:], ALU.mult, ALU.mult),
                     reads=[d["O"].b, d["rstd"].b, onw.b], writes=[d["on"].b])
                s.op("pool", lambda e: e.tensor_tensor(d["ogb"][:], d["on"][:], sg_[:, h * 128:(h + 1) * 128], ALU.mult),
                     reads=[d["on"].b, sg_.b], writes=[d["ogb"].b])
                bo = nbank(kb)
                bov = bo[:, :].bitcast(BF16)
                mm_group(kb, bo, [bov[:, 0:128]], [(d["ogb"][:], identb[:])], reads=[d["ogb"].b, identb.b], transpose=True)
                evc("act", og_[:, h, tc_], bov[:, 0:128], [bo.b], og_.b)
                yield
        if C.get("chunked"):
            b_, c0_ = tok0 // 2048, tok0 % 2048
            tk_ = s.dma("sp", D["ogt"][b_ * 256:(b_ + 1) * 256, :].rearrange("(h p) n -> p h n", p=128)[:, :, c0_:c0_ + TS], og_[:], reads=[og_.b])
            if C.get("ag_og"):
                ogtoks.append(tk_)
                if c0_ + TS == 2048:
                    s.collective("AllGather", D["ogt"][b_ * 256:(b_ + 1) * 256, :], D["ogt_all"][b_ * 1024:(b_ + 1) * 1024, :], C["ag_og"],
                                 deps=list(ogtoks))
                    del ogtoks[:]
        else:
            s.dma("sp", D["ogt"].rearrange("(h p) n -> p h n", p=128)[:, :, tok0:tok0 + TS], og_[:], reads=[og_.b])

    ogtoks = []
    for _ in pre_gen(0):
        pass
    RATIO = C.get("ratio", 3)
    for st in range(NST):
        gs = scan_gen(st)
        gp = pre_gen(st + 1) if st + 1 < NST else iter(())
        alive_s = alive_p = True
        while alive_s or alive_p:
            if alive_s:
                try:
                    next(gs)
                except StopIteration:
                    alive_s = False
            for _ in range(RATIO):
                if alive_p:
                    try:
                        next(gp)
                    except StopIteration:
                        alive_p = False


def kmajor(W):
    K, N = W.shape
    return np.ascontiguousarray(W.reshape(K // 128, 128, N).transpose(1, 0, 2).reshape(128, -1))

def blocks(W, FB=256):
    K, F = W.shape
    NB = F // FB
    return np.ascontiguousarray(W.reshape(8, 128, NB, FB).transpose(1, 2, 0, 3).reshape(128, -1))

def pcol(v):
    return np.ascontiguousarray(v.reshape(-1, 128).T)

def prep_p1(inp, NT, nseg):
    x = inp["x"]
    B, S, _ = x.shape
    f32 = np.float32
    win = kmajor(inp["e_w_in"][0])
    lruc = np.zeros((128, 4, 8), f32)
    cw = inp["e_lru_conv_w"][0]
    for c in range(4):
        sl = slice(c * 128, (c + 1) * 128)
        for j in range(4):
            lruc[:, c, j] = cw[j, sl]
        lruc[:, c, 4] = inp["e_lru_conv_b"][0][sl]
        lruc[:, c, 5] = inp["e_lru_b_a"][0][sl]
        lruc[:, c, 6] = inp["e_lru_b_i"][0][sl]
        lruc[:, c, 7] = inp["e_lru_lambda"][0][sl]
    def bd(w):
        o = np.zeros((128, 4, 128), f32)
        for c in range(4):
            o[0:64, c, 0:64] = w[2 * c]
            o[64:128, c, 64:128] = w[2 * c + 1]
        return o
    prot = np.zeros((64, 64), f32)
    for m in range(32):
        prot[m + 32, m] = -1.0
        prot[m, m + 32] = 1.0
    k_ = np.arange(128)[:, None]; q_ = np.arange(128)[None, :]
    mcur = (k_ <= q_).astype(f32); mprev = (k_ > q_).astype(f32)
    half = 32
    inv_freq = (10000.0 ** (-np.arange(half, dtype=np.float32) / half)).astype(f32)
    common = dict(win32=win, lruc=lruc, wa_bd=bd(inp["e_lru_w_a"][0]), wi_bd=bd(inp["e_lru_w_i"][0]),
                  qkg=np.stack([inp["e_q_norm"][0], inp["e_k_norm"][0]], 1).astype(f32),
                  sinks_b=np.broadcast_to(inp["e_sinks"][0][None, :], (128, 8)).astype(f32).copy(),
                  prot=prot, mask_cur=mcur, mask_prev=mprev, gmix0=pcol(inp["ln_mix"][0]),
                  ident=np.eye(128, dtype=f32))
    outs = []
    for b in range(B):
        for j in range(nseg):
            t0 = j * NT
            d = dict(common)
            d["x"] = np.ascontiguousarray(x[b, t0:t0 + NT])
            d["xhalo"] = np.ascontiguousarray(x[b, t0 - 128:t0]) if j > 0 else np.zeros((128, 1024), f32)
            d["mask_prev0"] = mprev if j > 0 else np.zeros((128, 128), f32)
            pos = np.arange(t0 - 128, t0 + NT).astype(f32)
            ang = pos[None, :] * inv_freq[:, None]
            cs = np.zeros((64, 2, NT + 128), f32)
            cs[0:32, 0] = np.cos(ang); cs[32:64, 0] = np.cos(ang)
            cs[0:32, 1] = np.sin(ang); cs[32:64, 1] = np.sin(ang)
            d["cossin"] = cs
            outs.append(d)
    return outs


def gdn_masks():
    f32 = np.float32
    k = np.arange(128)[:, None]; i = np.arange(128)[None, :]
    same = (k // 64) == (i // 64)
    return dict(maskU=((k <= i) & same).astype(f32), maskL=((k > i) & same).astype(f32), maskB=same.astype(f32),
                maskC0=np.broadcast_to(k < 64, (128, 128)).astype(f32).copy(),
                maskC1=np.broadcast_to(k >= 64, (128, 128)).astype(f32).copy())


def prep_p3(inp, hp):
    f32 = np.float32
    w = inp["o_w_in"][0]
    hs = [2 * hp, 2 * hp + 1]
    cols = [off + h * 128 + np.arange(128) for (off, h) in ((0, hs[0]), (1024, hs[0]), (0, hs[1]), (1024, hs[1]), (2048, hs[0]), (2048, hs[1]))]
    wqkv = w[:, np.concatenate(cols)]
    wgate = w[:, np.concatenate([3072 + h * 128 + np.arange(128) for h in hs])]
    wab = w[:, [4096 + hs[0], 4096 + hs[1], 4104 + hs[0], 4104 + hs[1]]]
    cw = inp["o_conv_w"][0]
    convw = np.zeros((128, 6, 4), f32)
    for b, c in enumerate(cols):
        convw[:, b, :] = cw[:, c].T
    hconst = np.zeros((128, 4), f32)
    hconst[:, 0:2] = inp["o_a_log"][0][hs][None, :]
    hconst[:, 2:4] = inp["o_dt_bias"][0][hs][None, :]
    d = dict(wqkv32=kmajor(wqkv), wgate32=kmajor(wgate), wab32=kmajor(np.ascontiguousarray(wab)), convw=convw, hconst=hconst,
             onw_b=np.broadcast_to(inp["o_out_norm"][0][None, :], (128, 128)).astype(f32).copy(), ident=np.eye(128, dtype=f32))
    d.update(gdn_masks())
    return d


NT_CORE = 4096
NSEG = 4


def _mk_dram(kb, h, outs, scratch):
    D = {}
    for k, v in h.items():
        D[k] = kb.dram(k, list(v.shape), F32 if v.dtype == np.float32 else BF16, "ExternalInput")
    for k, (shape, dt) in scratch.items():
        D[k] = kb.dram(k, shape, dt)
    for k, (shape, dt) in outs.items():
        D[k] = kb.dram(k, shape, dt, "ExternalOutput")
    return D


def _build(h, outs, scratch, casts, phase, C):
    nc = bass.Bass("TRN2", target_bir_lowering=False)
    kb = KB(nc)
    D = _mk_dram(kb, h, outs, scratch)
    pairs = []
    for src, dst, rows in casts:
        for r0 in range(0, rows, 128):
            pairs.append((D[src][r0:r0 + 128, :], D[dst][r0:r0 + 128, :]))
    cast_dram(kb, pairs)
    kb.phase_end()
    phase(kb, C, D)
    kb.phase_end()
    kb.s.finish()
    return nc


def _run(nc, maps):
    res = run_bass_kernel_spmd(nc, maps, core_ids=list(range(len(maps))))
    return res.results


def kernel_unfused(**inputs):
    inp = {k: np.asarray(v) for k, v in inputs.items()}
    f32 = np.float32
    x = inp["x"]
    B, S, _ = x.shape
    NT = NT_CORE
    ncore = B * NSEG
    eye = np.eye(128, dtype=f32)
    hA = prep_p1(inp, NT, NSEG)
    ncA = _build(hA[0], dict(yloc=([512, NT], BF16), pg=([512, NT], BF16), yatt=([512, NT], BF16), seg=([128, 8], F32)),
                 dict(win=([128, 8 * 1792], BF16)), [("win32", "win", 128)], phase1, dict(NTOK=NT))
    rA = _run(ncA, hA)
    hB = []
    cB = dict(ident=eye, gffn0=pcol(inp["ln_ffn"][0]), gmix1=pcol(inp["ln_mix"][1]), wo032=kmajor(inp["e_w_out"][0]),
              fwg32=blocks(inp["e_ffn_w_gate"][0]), fwu32=blocks(inp["e_ffn_w_up"][0]), fwd32=kmajor(inp["e_ffn_w_down"][0]))
    for b in range(B):
        segall = np.concatenate([rA[b * NSEG + j]["seg"] for j in range(NSEG)], 0)
        for j in range(NSEG):
            c = b * NSEG + j
            sel = np.zeros((128, 4), f32)
            sel[:, :j] = 1.0
            d = dict(cB)
            d.update(x=hA[c]["x"], yloc=rA[c]["yloc"], pg=rA[c]["pg"], yatt=rA[c]["yatt"], segall=segall, sel=sel)
            hB.append(d)
    ncB = _build(hB[0], dict(h1=([NT, 1024], F32), hn1T=([1024, NT], BF16)),
                 dict(wo0=([128, 8192], BF16), fwg=([128, 22 * 1024], BF16), fwu=([128, 22 * 1024], BF16), fwd=([128, 22 * 1024], BF16)),
                 [("wo032", "wo0", 128), ("fwg32", "fwg", 128), ("fwu32", "fwu", 128), ("fwd32", "fwd", 128)], phase2, dict(NTOK=NT))
    rB = _run(ncB, hB)
    hC = []
    for b in range(B):
        hng = np.concatenate([rB[b * NSEG + j]["hn1T"] for j in range(NSEG)], 0)
        for hp_ in range(NSEG):
            d = prep_p3(inp, hp_)
            d["hng"] = hng
            hC.append(d)
    ncC = _build(hC[0], dict(ogt=([256, S], BF16)),
                 dict(wqkv=([128, 8 * 768], BF16), wgate=([128, 8 * 256], BF16), wab=([128, 32], BF16)),
                 [("wqkv32", "wqkv", 128), ("wgate32", "wgate", 128), ("wab32", "wab", 128)], phase3, dict(NTF=S, NTK=NT))
    rC = _run(ncC, hC)
    NE = inp["o_moe_w_gate"].shape[1]
    NCm = inp["o_moe_w_gate"].shape[3] // 128
    cD = dict(ident=eye, gffn1=pcol(inp["ln_ffn"][1]), router=kmajor(inp["o_router"][0]), wo1_32=kmajor(inp["o_w_out"][0]),
              mwg_32=np.concatenate([blocks(inp["o_moe_w_gate"][0][e]) for e in range(NE)], 0),
              mwu_32=np.concatenate([blocks(inp["o_moe_w_up"][0][e]) for e in range(NE)], 0),
              mwd_32=np.concatenate([kmajor(inp["o_moe_w_down"][0][e]) for e in range(NE)], 0))
    hD = []
    for b in range(B):
        ogt_full = np.concatenate([rC[b * NSEG + hp_]["ogt"] for hp_ in range(NSEG)], 0)
        for j in range(NSEG):
            c = b * NSEG + j
            d = dict(cD)
            d.update(ogt=np.ascontiguousarray(ogt_full[:, j * NT:(j + 1) * NT]), h1=rB[c]["h1"])
            hD.append(d)
    ncD = _build(hD[0], dict(out=([NT, 1024], F32)),
                 dict(wo1=([128, 8192], BF16), mwg=([NE * 128, NCm * 1024], BF16), mwu=([NE * 128, NCm * 1024], BF16),
                      mwd=([NE * 128, NCm * 1024], BF16)),
                 [("wo1_32", "wo1", 128), ("mwg_32", "mwg", NE * 128), ("mwu_32", "mwu", NE * 128), ("mwd_32", "mwd", NE * 128)],
                 phase4, dict(NTOK=NT, NEXP=NE, NC_MOE=NCm))
    rD = _run(ncD, hD)
    out = np.stack([np.concatenate([rD[b * NSEG + j]["out"] for j in range(NSEG)], 0) for b in range(B)], 0)
    return out.astype(f32)


def p4_consts(NSL):
    c = np.zeros((128, 64), np.float32)
    c[:, 0:8] = 512.0 * np.arange(8)[None, :]
    c[:, 8] = np.arange(128)
    c[:, 16:16 + NSL] = 512.0 * np.arange(NSL)[None, :]
    k = np.arange(128)[:, None]
    i = np.arange(128)[None, :]
    return c, (k < i).astype(np.float32)


def sparse_dram(kb, D, NT, NE, NC):
    nc = kb.nc
    NB = NC // 2
    NSL = (2 * NT + NE * 511) // 512
    D["mwg_b"] = [nc.dram_tensor(f"mwg_b{b}", [NE * 128, 2048], BF16).ap() for b in range(NB)]
    D["mwu_b"] = [nc.dram_tensor(f"mwu_b{b}", [NE * 128, 2048], BF16).ap() for b in range(NB)]
    D["mwd_g"] = [nc.dram_tensor(f"mwd_g{b}", [NE * 128, 7 * 1024], BF16).ap() for b in range(NC // 7)]
    D["h2"] = nc.dram_tensor("h2", [NT, 1024], F32).ap()
    D["hnd"] = nc.dram_tensor("hnd", [NT, 1024], BF16).ap()
    D["xsl"] = nc.dram_tensor("xsl", [NSL * 512, 1028], BF16).ap()
    D["ysl"] = nc.dram_tensor("ysl", [NSL * 512, 1024], F32).ap()
    pairs = []
    for e in range(NE):
        rs = slice(e * 128, (e + 1) * 128)
        for b in range(NB):
            pairs.append((D["mwg_32"][rs, b * 2048:(b + 1) * 2048], D["mwg_b"][b][rs, :]))
            pairs.append((D["mwu_32"][rs, b * 2048:(b + 1) * 2048], D["mwu_b"][b][rs, :]))
        for b in range(NC // 7):
            pairs.append((D["mwd_32"][rs, b * 7168:(b + 1) * 7168], D["mwd_g"][b][rs, :]))
    return pairs


def kernel(**inputs):
    inp = {k: np.asarray(v) for k, v in inputs.items()}
    f32 = np.float32
    x = inp["x"]
    B, S, _ = x.shape
    NT = NT_CORE
    NE = inp["o_moe_w_gate"].shape[1]
    NCm = inp["o_moe_w_gate"].shape[3] // 128
    eye = np.eye(128, dtype=f32)
    hA = prep_p1(inp, NT, NSEG)
    common = dict(ident=eye, gffn0=pcol(inp["ln_ffn"][0]), gmix1=pcol(inp["ln_mix"][1]), wo032=kmajor(inp["e_w_out"][0]),
                  fwg32=blocks(inp["e_ffn_w_gate"][0]), fwu32=blocks(inp["e_ffn_w_up"][0]), fwd32=kmajor(inp["e_ffn_w_down"][0]),
                  gffn1=pcol(inp["ln_ffn"][1]), router=kmajor(inp["o_router"][0]), wo1_32=kmajor(inp["o_w_out"][0]),
                  mwg_32=np.concatenate([blocks(inp["o_moe_w_gate"][0][e]) for e in range(NE)], 0),
                  mwu_32=np.concatenate([blocks(inp["o_moe_w_up"][0][e]) for e in range(NE)], 0),
                  mwd_32=np.concatenate([kmajor(inp["o_moe_w_down"][0][e]) for e in range(NE)], 0))
    p3 = [prep_p3(inp, hp_) for hp_ in range(NSEG)]
    cst4, SU4 = p4_consts((2 * NT + NE * 511) // 512)
    common.update(gffn1_b=np.broadcast_to(inp["ln_ffn"][1][None, :], (128, 1024)).astype(f32).copy(), p4cst=cst4, maskSU=SU4)
    maps = []
    for b in range(B):
        for j in range(NSEG):
            c = b * NSEG + j
            d = dict(common)
            d.update(hA[c])
            d.update(p3[j])
            sel = np.zeros((128, 4), f32)
            sel[:, :j] = 1.0
            sel4 = np.zeros((128, 4), f32)
            sel4[:, j] = 1.0
            d.update(sel=sel, sel4=sel4)
            maps.append(d)
    import os as _os
    KSTOP = int(_os.environ.get("KSTOP", "0"))
    if KSTOP:
        tiny = np.zeros((128, 8), f32)
        for d in maps:
            d.update(mwg_32=tiny, mwu_32=tiny, mwd_32=tiny)
    nc = bass.Bass("TRN2", target_bir_lowering=False)
    kb = KB(nc)
    bf = lambda n: ([128, n], BF16)
    scratch = dict(win=bf(8 * 1792), wo0=bf(8192), fwg=bf(22 * 1024), fwu=bf(22 * 1024), fwd=bf(22 * 1024),
                   wqkv=bf(8 * 768), wgate=bf(8 * 256), wab=bf(32), wo1=bf(8192),
                   yloc=([512, NT], BF16), pg=([512, NT], BF16), yatt=([512, NT], BF16), seg=([128, 8], F32),
                   h1=([NT, 1024], F32), hn1T=([(NT // 512) * 1024, 512], BF16), ogt_own=([(S // 2048) * 256, 2048], BF16))
    D = _mk_dram(kb, maps[0], dict(out=([NT, 1024], F32)), scratch)
    for k, shape, dt in (("segall", [NSEG * 128, 8], F32), ("hng", [(NT // 512) * NSEG * 1024, 512], BF16), ("ogt_all", [(S // 2048) * NSEG * 256, 2048], BF16)):
        D[k] = nc.dram_tensor(k, shape, dt, addr_space="Local", kind="Internal").ap()
    groups = [list(range(b * NSEG, (b + 1) * NSEG)) for b in range(B)]
    casts = [("win32", "win", 128), ("wo032", "wo0", 128), ("fwg32", "fwg", 128), ("fwu32", "fwu", 128), ("fwd32", "fwd", 128),
             ("wqkv32", "wqkv", 128), ("wgate32", "wgate", 128), ("wab32", "wab", 128), ("wo1_32", "wo1", 128)]
    pairs = []
    for src_, dst_, rows in casts:
        for r0 in range(0, rows, 128):
            pairs.append((D[src_][r0:r0 + 128, :], D[dst_][r0:r0 + 128, :]))
    if not KSTOP:
        pairs += sparse_dram(kb, D, NT, NE, NCm)
    cast_dram(kb, pairs)
    kb.phase_end()
    phase1(kb, dict(NTOK=NT), D)
    kb.phase_end()
    kb.s.collective("AllGather", D["seg"], D["segall"], groups)
    kb.phase_end()
    if KSTOP == 1:
        kb.s.finish()
        return _run(nc, maps)
    phase2(kb, dict(NTOK=NT, ag_hn=groups), D)
    kb.phase_end()
    if KSTOP == 2:
        kb.s.finish()
        return _run(nc, maps)
    if KSTOP == 3:
        kb.s.finish()
        return _run(nc, maps)
    D3 = dict(D)
    D3["ogt"] = D["ogt_own"]
    D3["ogt_all"] = D["ogt_all"]
    phase3(kb, dict(NTF=S, NTK=NT, chunked=2, ag_og=groups), D3)
    kb.phase_end()
    if KSTOP == 5:
        kb.s.finish()
        return _run(nc, maps)
    D4 = dict(D)
    D4["ogt"] = D["ogt_all"]
    phase4s(kb, dict(NTOK=NT, NEXP=NE, NC_MOE=NCm, fused=True), D4)
    kb.phase_end()
    kb.s.finish()
    res = _run(nc, maps)
    out = np.stack([np.concatenate([res[b * NSEG + j]["out"] for j in range(NSEG)], 0) for b in range(B)], 0)
    return out.astype(f32)


I32 = mybir.dt.int32


def idma(kb, out, in_, idx_ap, scatter, reads=(), writes=()):
    s = kb.s
    E = s.E["pool"]
    deps = s._deps(reads, writes)
    slot = s.dsem["pool"][s.drr["pool"]]
    s.drr["pool"] = (s.drr["pool"] + 1) % NDS
    sem, val = slot
    if val > 0:
        deps.append((sem, val))
    s._wait(E, deps)
    slot[1] = val + 16
    off = bass.IndirectOffsetOnAxis(ap=idx_ap, axis=0)
    if scatter:
        kb.nc.gpsimd.indirect_dma_start(out=out, out_offset=off, in_=in_, in_offset=None).then_inc(sem, 16)
    else:
        kb.nc.gpsimd.indirect_dma_start(out=out, out_offset=None, in_=in_, in_offset=off).then_inc(sem, 16)
    s.nins += 1
    tok = (sem, val + 16)
    k = id(sem)
    for b in reads:
        b.r[k] = tok
    for b in writes:
        b.w = tok
        b.r = {}


def phase4s(kb, C, D):
    s = kb.s
    NT, NE, NCm = C["NTOK"], C["NEXP"], C["NC_MOE"]
    TS = 512
    NTT = NT // 128
    NSL = (2 * NT + NE * (TS - 1)) // TS
    NB = NCm // 2
    kb.D = D
    IDX1 = kb.sbp([128, NTT], I32, "IDX1")
    IDX2 = kb.sbp([128, NTT], I32, "IDX2")
    IDXW = kb.sbp([128, NSL], I32, "IDXW")
    W = norm_scratch(kb)
    ident, identb = W["ident"], W["identb"]
    g = kb.sb([128, 8], F32, "gffn")
    s.dma("sp", g[:], D["gffn1"], writes=[g.b])
    gtm = kb.sb([128, 1024], F32, "gtm")
    s.dma("sp", gtm[:], D["gffn1_b"], writes=[gtm.b])
    wo = kb.sb([128, 8, 1024], BF16, "wo")
    s.dma("sp", wo[:], D["wo1"].rearrange("p (k n) -> p k n", n=1024), writes=[wo.b])
    rtr = kb.sb([128, 8, 8], F32, "rtr")
    s.dma("sp", rtr[:], D["router"].rearrange("p (k n) -> p k n", n=8), writes=[rtr.b])
    SU32 = kb.sb([128, 128], F32, "SU32")
    s.dma("sp", SU32[:], D["maskSU"], writes=[SU32.b])
    SUb = kb.sb([128, 128], BF16, "SUb")
    s.op("dve", lambda e: e.tensor_copy(SUb[:], SU32[:]), reads=[SU32.b], writes=[SUb.b])
    onesb = kb.sb([128, 128], BF16, "onesb")
    s.op("pool", lambda e: e.memset(onesb[:], 1.0), writes=[onesb.b])
    cst = kb.sb([128, 64], F32, "p4cst")
    s.dma("sp", cst[:], D["p4cst"], writes=[cst.b])
    R1 = kb.sb([128, NTT, 8], F32, "R1")
    R2 = kb.sb([128, NTT, 8], F32, "R2")
    W12 = kb.sb([128, NTT, 2], F32, "W12")
    if C.get("fused"):
        ogc = [kb.sb([128, 4, 8, 128], BF16, "ogc") for _ in range(2)]
        sel4 = kb.sb([128, 4], F32, "sel4")
        s.dma("sp", sel4[:], D["sel4"], writes=[sel4.b])
    else:
        ogt_v = D["ogt"].rearrange("(c p) n -> p c n", p=128)
    og = [kb.sb([128, 8, 128], BF16, "og") for _ in range(2)]
    h1 = [kb.sb([128, 1024], F32, "h1") for _ in range(2)]
    acc = [kb.sb([128, 1024], F32, "acc") for _ in range(2)]
    xs2 = [kb.sb([128, 1024], F32, "xs") for _ in range(2)]
    junk2 = [kb.sb([128, 1024], BF16, "junk") for _ in range(2)]
    hn322 = [kb.sb([128, 8, 128], F32, "hn32") for _ in range(2)]
    HN = [kb.sb([128, 1028], BF16, "HN") for _ in range(2)]
    sm2 = [{k: kb.sb([128, 8], F32, k) for k in ("lg", "mx")} for _ in range(2)]
    sc2 = [{k: kb.sb([128, 1], F32, k) for k in ("ss", "rt", "rstd", "dd", "ex", "den")} for _ in range(2)]
    bh2, bhn, bxs, bys = Buf(), Buf(), Buf(), Buf()
    for tt in range(NTT):
        tok0 = tt * 128
        o_, h_, a_, hn_ = og[tt % 2], h1[tt % 2], acc[tt % 2], HN[tt % 2]
        xs, junk, hn32, sm, sc = xs2[tt % 2], junk2[tt % 2], hn322[tt % 2], sm2[tt % 2], sc2[tt % 2]
        if C.get("fused"):
            cd_ = ogc[tt % 2]
            for r_ in range(4):
                g_ = r_ * NT + tok0
                b_, c0_ = g_ // 2048, g_ % 2048
                s.dma("act", cd_[:, r_], D["ogt"][b_ * 1024:(b_ + 1) * 1024, :].rearrange("(c p) n -> p c n", p=128)[:, :, c0_:c0_ + 128],
                      writes=[cd_.b])
            s.op("dve", lambda e: e.tensor_scalar(o_[:], cd_[:, 0], sel4[:, 0:1], None, ALU.mult), reads=[cd_.b, sel4.b], writes=[o_.b])
            for r_ in range(1, 4):
                s.op("dve", lambda e: e.scalar_tensor_tensor(o_[:], cd_[:, r_], sel4[:, r_:r_ + 1], o_[:], ALU.mult, ALU.add),
                     reads=[cd_.b, sel4.b, o_.b], writes=[o_.b])
        else:
            s.dma("act", o_[:], ogt_v[:, :, tok0:tok0 + 128], writes=[o_.b])
        s.dma("act", h_[:], D["h1"][tok0:tok0 + 128, :], writes=[h_.b])
        for half in range(2):
            bk = nbank(kb)
            mm_group(kb, bk, bk[:, :], [(o_[:, c, :], wo[:, c, half * 512:(half + 1) * 512]) for c in range(8)], reads=[o_.b, wo.b])
            s.op("dve", lambda e: e.tensor_tensor(a_[:, half * 512:(half + 1) * 512], h_[:, half * 512:(half + 1) * 512], bk[:, :], ALU.add),
                 reads=[bk.b, h_.b], writes=[a_.b])
        s.dma("sp", D["h2"][tok0:tok0 + 128, :], a_[:], reads=[a_.b], writes=[bh2])
        rms_stats(kb, a_[:], a_.b, junk, sc["ss"], sc["rt"], sc["rstd"])
        s.op("act", lambda e: e.activation(xs[:], a_[:], AF.Copy, scale=sc["rstd"][:]), reads=[a_.b, sc["rstd"].b], writes=[xs.b])
        s.op("pool", lambda e: e.tensor_tensor(hn_[:, 0:1024], xs[:], gtm[:], ALU.mult), reads=[xs.b, gtm.b], writes=[hn_.b])
        s.dma("sp", D["hnd"][tok0:tok0 + 128, :], hn_[:, 0:1024], reads=[hn_.b], writes=[bhn])
        for hh in range(2):
            bk = nbank(kb)
            mm_group(kb, bk, [bk[:, j * 128:(j + 1) * 128] for j in range(4)],
                     [(xs[:, (hh * 4 + j) * 128:(hh * 4 + j + 1) * 128], ident[:]) for j in range(4)], reads=[xs.b, ident.b], transpose=True)
            s.op("dve", lambda e: e.tensor_tensor(hn32[:, hh * 4:(hh + 1) * 4, :], bk[:, :].rearrange("p (c n) -> p c n", n=128),
                                                   bc(g[:, hh * 4:(hh + 1) * 4].unsqueeze(2), [128, 4, 128]), ALU.mult),
                 reads=[bk.b, g.b], writes=[hn32.b])
        bk = nbank(kb)
        mm_group(kb, bk, bk[:, 0:8], [(hn32[:, c, :], rtr[:, c, :]) for c in range(8)], reads=[hn32.b, rtr.b])
        lg, mx = sm["lg"], sm["mx"]
        s.op("dve", lambda e: e.tensor_copy(lg[:], bk[:, 0:8]), reads=[bk.b], writes=[lg.b])
        s.op("dve", lambda e: e.max(mx[:], lg[:]), reads=[lg.b], writes=[mx.b])
        s.op("dve", lambda e: e.tensor_tensor(sc["dd"][:], mx[:, 1:2], mx[:, 0:1], ALU.subtract), reads=[mx.b], writes=[sc["dd"].b])
        s.op("act", lambda e: e.activation(sc["ex"][:], sc["dd"][:], AF.Exp), reads=[sc["dd"].b], writes=[sc["ex"].b])
        s.op("dve", lambda e: e.tensor_scalar(sc["den"][:], sc["ex"][:], 1.0, None, ALU.add), reads=[sc["ex"].b], writes=[sc["den"].b])
        s.op("dve", lambda e: e.reciprocal(W12[:, tt, 0:1], sc["den"][:]), reads=[sc["den"].b], writes=[W12.b])
        s.op("dve", lambda e: e.tensor_tensor(W12[:, tt, 1:2], sc["ex"][:], W12[:, tt, 0:1], ALU.mult), reads=[sc["ex"].b, W12.b], writes=[W12.b])
        s.op("dve", lambda e: e.tensor_scalar(R1[:, tt, :], lg[:], mx[:, 0:1], None, ALU.is_equal), reads=[lg.b, mx.b], writes=[R1.b])
        s.op("dve", lambda e: e.tensor_scalar(R2[:, tt, :], lg[:], mx[:, 1:2], None, ALU.is_equal), reads=[lg.b, mx.b], writes=[R2.b])
    barrier(kb)
    NC8 = NTT * 8
    Rm = kb.sb([128, NC8], BF16, "Rm")
    s.op("dve", lambda e: e.tensor_tensor(Rm[:], R1[:].rearrange("p t e -> p (t e)"), R2[:].rearrange("p t e -> p (t e)"), ALU.add),
         reads=[R1.b, R2.b], writes=[Rm.b])
    bw_ = nbank(kb)
    mm_group(kb, bw_, bw_[:, 0:NC8], [(SUb[:], Rm[:])], reads=[SUb.b, Rm.b])
    bt_ = nbank(kb)
    mm_group(kb, bt_, bt_[:, 0:NC8], [(onesb[:], Rm[:])], reads=[onesb.b, Rm.b])
    totT = kb.sb([128, 8, NTT], F32, "totT")
    s.op("dve", lambda e: e.tensor_copy(totT[:], bt_[:, 0:NC8].rearrange("p (t e) -> p e t", e=8)), reads=[bt_.b], writes=[totT.b])
    ones32 = kb.sb([128, 64], F32, "ones32")
    s.op("pool", lambda e: e.memset(ones32[:], 1.0), writes=[ones32.b])
    cumI = kb.sb([128, 8, NTT], F32, "cumI")
    for e_ in range(8):
        s.op("dve", lambda e: e.tensor_tensor_scan(cumI[:, e_, :], ones32[:, 0:NTT], totT[:, e_, :], 0.0, ALU.mult, ALU.add),
             reads=[ones32.b, totT.b], writes=[cumI.b])
    toff = kb.sb([128, 8, NTT], F32, "toff")
    s.op("dve", lambda e: e.tensor_tensor(toff[:], cumI[:], totT[:], ALU.subtract), reads=[cumI.b, totT.b], writes=[toff.b])
    cnt = kb.sb([128, 8], F32, "cnt")
    s.op("dve", lambda e: e.tensor_copy(cnt[:], cumI[:, :, NTT - 1]), reads=[cumI.b], writes=[cnt.b])
    cmp = kb.sb([128, 8, 8], F32, "cmp")
    s.op("dve", lambda e: e.tensor_tensor(cmp[:], bc(cnt[:].unsqueeze(2), [128, 8, 8]), bc(cst[:, 0:8].unsqueeze(1), [128, 8, 8]), ALU.is_gt),
         reads=[cnt.b, cst.b], writes=[cmp.b])
    padded = kb.sb([128, 8], F32, "padded")
    s.op("dve", lambda e: e.tensor_reduce(padded[:], cmp[:], AX.X, ALU.add), reads=[cmp.b], writes=[padded.b])
    s.op("dve", lambda e: e.tensor_scalar(padded[:], padded[:], float(TS), None, ALU.mult), reads=[padded.b], writes=[padded.b])
    ends = kb.sb([128, 8], F32, "ends")
    s.op("dve", lambda e: e.tensor_tensor_scan(ends[:], ones32[:, 0:8], padded[:], 0.0, ALU.mult, ALU.add), reads=[ones32.b, padded.b], writes=[ends.b])
    base = kb.sb([128, 8], F32, "base")
    s.op("dve", lambda e: e.tensor_tensor(base[:], ends[:], padded[:], ALU.subtract), reads=[ends.b, padded.b], writes=[base.b])
    smat = kb.sb([128, NTT, 8], F32, "smat")
    s.op("dve", lambda e: e.tensor_tensor(smat[:], bw_[:, 0:NC8].rearrange("p (t e) -> p t e", e=8), toff[:].rearrange("p e t -> p t e"), ALU.add),
         reads=[bw_.b, toff.b], writes=[smat.b])
    s.op("dve", lambda e: e.tensor_tensor(smat[:], smat[:], bc(base[:].unsqueeze(1), [128, NTT, 8]), ALU.add), reads=[smat.b, base.b], writes=[smat.b])
    tmp3 = kb.sb([128, NTT, 8], F32, "tmp3")
    sl = kb.sb([128, NTT], F32, "sl")
    for Rx, IDX in ((R1, IDX1), (R2, IDX2)):
        s.op("dve", lambda e: e.tensor_tensor(tmp3[:], smat[:], Rx[:], ALU.mult), reads=[smat.b, Rx.b], writes=[tmp3.b])
        s.op("dve", lambda e: e.tensor_reduce(sl[:], tmp3[:], AX.X, ALU.add), reads=[tmp3.b], writes=[sl.b])
        s.op("dve", lambda e: e.tensor_copy(IDX[:], sl[:]), reads=[sl.b], writes=[IDX.b])
    cw = kb.sb([128, NSL, 8], F32, "cw")
    s.op("dve", lambda e: e.tensor_tensor(cw[:], bc(ends[:].unsqueeze(1), [128, NSL, 8]), bc(cst[:, 16:16 + NSL].unsqueeze(2), [128, NSL, 8]), ALU.is_le),
         reads=[ends.b, cst.b], writes=[cw.b])
    ew = kb.sb([128, NSL], F32, "ew")
    s.op("dve", lambda e: e.tensor_reduce(ew[:], cw[:], AX.X, ALU.add), reads=[cw.b], writes=[ew.b])
    s.op("dve", lambda e: e.tensor_scalar(ew[:], ew[:], float(NE - 1), 128.0, ALU.min, ALU.mult), reads=[ew.b], writes=[ew.b])
    s.op("dve", lambda e: e.tensor_scalar(ew[:], ew[:], cst[:, 8:9], None, ALU.add), reads=[ew.b, cst.b], writes=[ew.b])
    s.op("dve", lambda e: e.tensor_copy(IDXW[:], ew[:]), reads=[ew.b], writes=[IDXW.b])
    zt = kb.sb([128, 1028], BF16, "zt")
    s.op("pool", lambda e: e.memset(zt[:], 0.0), writes=[zt.b])
    for r0 in range(0, NSL * TS, 128):
        s.dma("sp" if (r0 // 128) % 2 == 0 else "act", D["xsl"][r0:r0 + 128, :], zt[:], reads=[zt.b], writes=[bxs])
    barrier(kb)
    for tt in range(NTT):
        tok0 = tt * 128
        hn_ = HN[tt % 2]
        s.dma("sp", hn_[:, 0:1024], D["hnd"][tok0:tok0 + 128, :], reads=[bhn], writes=[hn_.b])
        for k_, IDX in ((0, IDX1), (1, IDX2)):
            s.op("dve", lambda e: e.tensor_copy(hn_[:, 1024:1026].bitcast(F32), W12[:, tt, k_:k_ + 1]), reads=[W12.b], writes=[hn_.b])
            idma(kb, D["xsl"][:, :], hn_[:, :], IDX[:, tt:tt + 1], True, reads=[hn_.b, IDX.b], writes=[bxs])
    kb.phase_end()
    W = norm_scratch(kb)
    identb = W["identb"]
    r = ffn_alloc(kb, TS, NCm)
    hnT = [kb.sb([128, 8, TS], BF16, "hnT") for _ in range(2)]
    xt = [kb.sb([128, 1028], BF16, "xslt") for _ in range(3)]
    GW = [kb.sb([128, 4], F32, "GW") for _ in range(2)]
    YS = kb.sb([128, 4, 1024], F32, "YS")
    ix = 0
    for w in range(NSL):
        hT_ = hnT[w % 2]
        gw_ = GW[w % 2]
        for sub in range(4):
            x_ = xt[ix % 3]
            ix += 1
            r0 = w * TS + sub * 128
            s.dma("sp", x_[:], D["xsl"][r0:r0 + 128, :], reads=[bxs], writes=[x_.b])
            s.op("pool", lambda e: e.tensor_copy(gw_[:, sub:sub + 1], x_[:, 1024:1026].bitcast(F32)), reads=[x_.b], writes=[gw_.b])
            bk = nbank(kb)
            bv = bk[:, :].bitcast(BF16)
            mm_group(kb, bk, [bv[:, j * 128:(j + 1) * 128] for j in range(8)],
                     [(x_[:, j * 128:(j + 1) * 128], identb[:]) for j in range(8)], reads=[x_.b, identb.b], transpose=True)
            s.op("act", lambda e: e.copy(hT_[:, :, sub * 128:(sub + 1) * 128], bv.rearrange("p (c n) -> p c n", n=128)), reads=[bk.b], writes=[hT_.b])
        iw = IDXW[:, w:w + 1]

        def evac(t, half, bk):
            s.op("dve", lambda e: e.tensor_scalar(YS[:, t, half * 512:(half + 1) * 512], bk[:, :], gw_[:, t:t + 1], None, ALU.mult),
                 reads=[bk.b, gw_.b], writes=[YS.b])

        def lw(kind, b, tile):
            if kind == "d":
                idma(kb, tile, D["mwd_g"][b][:, :], iw, False, reads=[IDXW.b], writes=[r.wd.b])
            else:
                idma(kb, tile[:].rearrange("p k n -> p (k n)"), D["mwg_b" if kind == "g" else "mwu_b"][b][:, :], iw, False,
                     reads=[IDXW.b], writes=[tile.b])
        ffn_expert(kb, r, hT_, None, None, None, NCm, evac, loader=lw)
        for t in range(4):
            r0 = w * TS + t * 128
            s.dma("act", D["ysl"][r0:r0 + 128, :], YS[:, t, :], reads=[YS.b], writes=[bys])
    kb.phase_end()
    acc = [kb.sb([128, 1024], F32, "acc") for _ in range(2)]
    y1 = [kb.sb([128, 1024], F32, "y1") for _ in range(2)]
    y2 = [kb.sb([128, 1024], F32, "y2") for _ in range(2)]
    for tt in range(NTT):
        tok0 = tt * 128
        a_, y1_, y2_ = acc[tt % 2], y1[tt % 2], y2[tt % 2]
        s.dma("sp", a_[:], D["h2"][tok0:tok0 + 128, :], reads=[bh2], writes=[a_.b])
        idma(kb, y1_[:, :], D["ysl"][:, :], IDX1[:, tt:tt + 1], False, reads=[bys, IDX1.b], writes=[y1_.b])
        idma(kb, y2_[:, :], D["ysl"][:, :], IDX2[:, tt:tt + 1], False, reads=[bys, IDX2.b], writes=[y2_.b])
        s.op("dve", lambda e: e.tensor_tensor(a_[:], a_[:], y1_[:], ALU.add), reads=[a_.b, y1_.b], writes=[a_.b])
        s.op("dve", lambda e: e.tensor_tensor(a_[:], a_[:], y2_[:], ALU.add), reads=[a_.b, y2_.b], writes=[a_.b])
        s.dma("act", D["out"][tok0:tok0 + 128, :], a_[:], reads=[a_.b])
```
